# Optimizing a Trainium2 kernel written in Bass

```python
import jax
import jax.numpy as jnp
from jax import lax
import numpy as np

D_MODEL = 1024
BATCH = 8
SEQ = 4096
DEPTH = 1

N_META = 16
RWKV_WIDTH = D_MODEL // 2
RWKV_HEAD = 64
RWKV_HEADS = RWKV_WIDTH // RWKV_HEAD
W_LORA = max(32, int(round(1.8 * D_MODEL ** 0.5 / 32)) * 32)
A_LORA = max(32, int(round(1.8 * D_MODEL ** 0.5 / 32)) * 32)
G_LORA = max(32, int(round(0.6 * D_MODEL ** 0.8 / 32)) * 32)
LNX_EPS = 64e-5
MLSTM_WIDTH = D_MODEL - RWKV_WIDTH
MLSTM_HEADS = 4
MLSTM_DV = MLSTM_WIDTH // MLSTM_HEADS
MLSTM_DQK = MLSTM_DV // 2
MLSTM_CHUNK = 64
CONV_WIDTH = 4
GATE_CAP = 15.0
PAD_LOG_GATE = -1.0e4
N_GROUPS = 4
EXPERTS_PER_GROUP = 8
N_EXPERTS = N_GROUPS * EXPERTS_PER_GROUP
TOP_K = 2
D_EXPERT = D_MODEL // 2
EXPERT_BLOCK = 512
RMS_EPS = 1e-6
PROJ_WIDTHS = (RWKV_WIDTH, RWKV_WIDTH, RWKV_WIDTH, MLSTM_HEADS * MLSTM_DQK, MLSTM_HEADS * MLSTM_DQK, MLSTM_WIDTH, MLSTM_WIDTH, 2 * MLSTM_HEADS)
D_IN = sum(PROJ_WIDTHS)

kernel_name = 'hybrid_rwkv7_mlstm_hmoe'


def _split_points():
    pts, acc = [], 0
    for w in PROJ_WIDTHS[:-1]:
        acc += w
        pts.append(acc)
    return pts


def _rmsnorm(x, w):
    xf = x.astype(jnp.float32)
    y = xf * lax.rsqrt(jnp.mean(xf * xf, axis=-1, keepdims=True) + RMS_EPS)
    return (y * w.astype(jnp.float32)).astype(x.dtype)


def _shift(x):
    return jnp.pad(x[:, :-1], ((0, 0), (1, 0), (0, 0)))


def _causal_conv(x, w, b):
    K, L = w.shape[0], x.shape[1]
    xp = jnp.pad(x, ((0, 0), (K - 1, 0), (0, 0)))
    y = b
    for i in range(K):
        y = y + xp[:, i:i + L] * w[i]
    return y


def _rwkv7_time_mix(xn, r, k, v, mu_rkv, mu_wag, w0, w_la, w_lb, a0, a_la, a_lb,
                    g_la, g_lb, k_k, k_a, r_k, lnx_w, lnx_b):
    B, L, _ = xn.shape
    H, N = RWKV_HEADS, RWKV_HEAD
    f32 = jnp.float32
    r = r + (_shift(r) - r) * mu_rkv[0]
    k = k + (_shift(k) - k) * mu_rkv[1]
    v = v + (_shift(v) - v) * mu_rkv[2]
    xx = _shift(xn) - xn
    xw = xn + xx * mu_wag[0]
    xa = xn + xx * mu_wag[1]
    xg = xn + xx * mu_wag[2]
    w_log = -jax.nn.softplus(-(w0 + jnp.tanh(xw @ w_la) @ w_lb)) - 0.5
    a = jax.nn.sigmoid(a0 + (xa @ a_la) @ a_lb)
    g = jax.nn.sigmoid(xg @ g_la) @ g_lb
    kk = (k * k_k).reshape(B, L, H, N).astype(f32)
    kk = kk / jnp.maximum(jnp.sqrt(jnp.sum(kk * kk, axis=-1, keepdims=True)), 1e-12)
    k = k * (1.0 + (a - 1.0) * k_a)

    def heads(t):
        return t.reshape(B, L, H, N).astype(f32)

    rh, kh, vh, ah = heads(r), heads(k), heads(v), heads(a)
    decay = jnp.exp(-jnp.exp(heads(w_log)))
    a_vec = -kk
    b_vec = kk * ah

    def tm(t):
        return jnp.swapaxes(t, 0, 1)

    def step(S, inp):
        r_t, d_t, k_t, v_t, a_t, b_t = inp
        Sa = jnp.einsum('bhvk,bhk->bhv', S, a_t)
        S = S * d_t[:, :, None, :] + Sa[..., None] * b_t[:, :, None, :] + v_t[..., None] * k_t[:, :, None, :]
        return S, jnp.einsum('bhvk,bhk->bhv', S, r_t)

    S0 = jnp.zeros((B, H, N, N), f32)
    _, y = lax.scan(step, S0, (tm(rh), tm(decay), tm(kh), tm(vh), tm(a_vec), tm(b_vec)))
    y = tm(y)
    mu = jnp.mean(y, axis=-1, keepdims=True)
    var = jnp.mean(jnp.square(y - mu), axis=-1, keepdims=True)
    y = ((y - mu) * lax.rsqrt(var + LNX_EPS)).reshape(B, L, RWKV_WIDTH)
    y = y * lnx_w.astype(f32) + lnx_b.astype(f32)
    bonus = jnp.sum(rh * kh * r_k.astype(f32).reshape(H, N), axis=-1, keepdims=True) * vh
    y = (y + bonus.reshape(B, L, RWKV_WIDTH)) * g.astype(f32)
    return y.astype(xn.dtype)


def _mlstm_mix(q, k, v, o, gates, conv_w, conv_b, gate_b, norm_w):
    B, L, _ = q.shape
    H, DK, DV, CL = MLSTM_HEADS, MLSTM_DQK, MLSTM_DV, MLSTM_CHUNK
    f32 = jnp.float32
    out_dtype = v.dtype
    qk = jax.nn.silu(_causal_conv(jnp.concatenate([q, k], axis=-1), conv_w, conv_b)).astype(f32)
    q, k = jnp.split(qk, 2, axis=-1)
    q = q.reshape(B, L, H, DK)
    k = k.reshape(B, L, H, DK) * (DK ** -0.5)
    v = v.astype(f32).reshape(B, L, H, DV)
    gts = gates.astype(f32) + gate_b.astype(f32)
    gts = GATE_CAP * jnp.tanh(gts / GATE_CAP)
    log_i, f_pre = jnp.split(gts, 2, axis=-1)
    log_f = jax.nn.log_sigmoid(f_pre)
    pad = CL - N_META

    def padt(t, c=0.0):
        return jnp.pad(t, ((0, 0), (pad, 0)) + ((0, 0),) * (t.ndim - 2), constant_values=c)

    q, k, v, log_f = padt(q), padt(k), padt(v), padt(log_f)
    log_i = padt(log_i, PAD_LOG_GATE)
    NC = (L + pad) // CL

    def chunks4(t):
        return t.reshape(B, NC, CL, H, t.shape[-1]).transpose(1, 0, 3, 2, 4)

    def chunks3(t):
        return t.reshape(B, NC, CL, H).transpose(1, 0, 3, 2)

    causal = jnp.tril(jnp.ones((CL, CL), dtype=bool))

    def chunk_step(carry, inp):
        C, n, m = carry
        qc, kc, vc, li, lf = inp
        bcum = jnp.cumsum(lf, axis=-1)
        gtot = bcum[..., -1]
        Dm = jnp.where(causal, bcum[..., :, None] - bcum[..., None, :] + li[..., None, :], -jnp.inf)
        inter = bcum + m[..., None]
        m_t = jnp.maximum(inter, jnp.max(Dm, axis=-1))
        s = jnp.einsum('bhtk,bhsk->bhts', qc, kc) * jnp.exp(Dm - m_t[..., None])
        scale = jnp.exp(inter - m_t)[..., None]
        num = jnp.einsum('bhts,bhsv->bhtv', s, vc) + scale * jnp.einsum('bhvk,bhtk->bhtv', C, qc)
        den = jnp.sum(s, axis=-1, keepdims=True) + scale * jnp.einsum('bhk,bhtk->bht', n, qc)[..., None]
        h = num / jnp.maximum(jnp.abs(den), jnp.exp(-m_t)[..., None])
        a_log = gtot[..., None] - bcum + li
        m_new = jnp.maximum(gtot + m, jnp.max(a_log, axis=-1))
        wj = jnp.exp(a_log - m_new[..., None])
        dec = jnp.exp(gtot + m - m_new)
        C = dec[..., None, None] * C + jnp.einsum('bhs,bhsv,bhsk->bhvk', wj, vc, kc)
        n = dec[..., None] * n + jnp.einsum('bhs,bhsk->bhk', wj, kc)
        return (C, n, m_new), h

    carry0 = (jnp.zeros((B, H, DV, DK), f32), jnp.zeros((B, H, DK), f32), jnp.zeros((B, H), f32))
    _, h = lax.scan(chunk_step, carry0,
                    (chunks4(q), chunks4(k), chunks4(v), chunks3(log_i), chunks3(log_f)))
    h = h.transpose(1, 0, 3, 2, 4).reshape(B, NC * CL, H, DV)[:, pad:]
    h = h * lax.rsqrt(jnp.mean(h * h, axis=-1, keepdims=True) + RMS_EPS)
    h = h.reshape(B, L, MLSTM_WIDTH) * norm_w.astype(f32) * jax.nn.sigmoid(o.astype(f32))
    return h.astype(out_dtype)


def _hmoe(xn, wg, bg, we, be, w_gu, w_dn):
    B, L, D = xn.shape
    T = B * L
    A = T * TOP_K
    NB = -(-A // EXPERT_BLOCK) + N_EXPERTS
    f32 = jnp.float32
    xf = xn.reshape(T, D)
    xr = xf.astype(f32)
    pg = jax.nn.softmax(xr @ wg.astype(f32) + bg.astype(f32), axis=-1)
    pg_top, g_idx = lax.top_k(pg, 1)
    el = (xr @ we.astype(f32) + be.astype(f32)).reshape(T, N_GROUPS, EXPERTS_PER_GROUP)
    el = jnp.take_along_axis(el, g_idx[:, :, None], axis=1)[:, 0]
    pe_top, e_loc = lax.top_k(jax.nn.softmax(el, axis=-1), TOP_K)
    gate = pg_top * pe_top / jnp.sum(pe_top, axis=-1, keepdims=True)
    e_flat = (g_idx * EXPERTS_PER_GROUP + e_loc).reshape(A)
    tok_flat = jnp.arange(A, dtype=jnp.int32) // TOP_K
    order = jnp.argsort(e_flat)
    e_s = e_flat[order]
    counts = jnp.bincount(e_flat, length=N_EXPERTS)
    starts = jnp.cumsum(counts) - counts
    pcounts = (counts + EXPERT_BLOCK - 1) // EXPERT_BLOCK * EXPERT_BLOCK
    pend = jnp.cumsum(pcounts)
    pstarts = pend - pcounts
    dest = pstarts[e_s] + jnp.arange(A, dtype=jnp.int32) - starts[e_s]
    buf_tok = jnp.zeros((NB * EXPERT_BLOCK,), jnp.int32).at[dest].set(tok_flat[order])
    buf_gate = jnp.zeros((NB * EXPERT_BLOCK,), f32).at[dest].set(gate.reshape(A)[order])
    blk_e = jnp.minimum(jnp.searchsorted(pend, jnp.arange(NB, dtype=jnp.int32) * EXPERT_BLOCK, side='right'),
                        N_EXPERTS - 1)

    def expert_block(args):
        tok, e = args
        xb = xf[tok]
        gt, up = jnp.split(xb @ w_gu[e], 2, axis=-1)
        return (jax.nn.silu(gt) * up) @ w_dn[e]

    yb = lax.map(expert_block, (buf_tok.reshape(NB, EXPERT_BLOCK), blk_e))
    y = jnp.zeros((T, D), f32).at[buf_tok].add(yb.reshape(NB * EXPERT_BLOCK, D).astype(f32) * buf_gate[:, None])
    return y.reshape(B, L, D).astype(xn.dtype)


def setup_inputs(seed: int = 0) -> dict:
    key = jax.random.key(seed)
    ks = jax.random.split(key, 40)

    def nrm(i, shape, scale):
        return jax.random.normal(ks[i], shape, jnp.float32) * scale

    def uni(i, shape):
        return jax.random.uniform(ks[i], shape, jnp.float32)

    Lr, D, RW = DEPTH, D_MODEL, RWKV_WIDTH
    QK = 2 * MLSTM_HEADS * MLSTM_DQK
    ratio = jnp.arange(RW, dtype=jnp.float32) / (RW - 1)
    w0 = -6.5 + 5.0 * ratio ** 0.85
    gate_bias = jnp.concatenate([-jnp.ones((MLSTM_HEADS,), jnp.float32),
                                 jnp.linspace(3.0, 6.0, MLSTM_HEADS, dtype=jnp.float32)])
    return {
        'x': nrm(0, (BATCH, SEQ, D), 1.0),
        'meta_tokens': nrm(1, (N_META, D), 1.0),
        'norm_mix_w': 1.0 + nrm(2, (Lr, D), 0.02),
        'norm_ffn_w': 1.0 + nrm(3, (Lr, D), 0.02),
        'norm_final_w': 1.0 + nrm(4, (D,), 0.02),
        'w_in': nrm(5, (Lr, D, D_IN), D ** -0.5),
        'w_out': nrm(6, (Lr, D, D), D ** -0.5),
        'rwkv_mu_rkv': uni(7, (Lr, 3, RW)),
        'rwkv_mu_wag': uni(8, (Lr, 3, D)),
        'rwkv_w0': w0 + nrm(9, (Lr, RW), 0.1),
        'rwkv_w_lora_a': nrm(10, (Lr, D, W_LORA), D ** -0.5),
        'rwkv_w_lora_b': nrm(11, (Lr, W_LORA, RW), 0.5 * W_LORA ** -0.5),
        'rwkv_a0': nrm(12, (Lr, RW), 0.1),
        'rwkv_a_lora_a': nrm(13, (Lr, D, A_LORA), D ** -0.5),
        'rwkv_a_lora_b': nrm(14, (Lr, A_LORA, RW), A_LORA ** -0.5),
        'rwkv_g_lora_a': nrm(15, (Lr, D, G_LORA), D ** -0.5),
        'rwkv_g_lora_b': nrm(16, (Lr, G_LORA, RW), G_LORA ** -0.5),
        'rwkv_k_k': 0.85 + nrm(17, (Lr, RW), 0.05),
        'rwkv_k_a': 1.0 + nrm(18, (Lr, RW), 0.05),
        'rwkv_r_k': -0.04 + nrm(19, (Lr, RW), 0.1),
        'rwkv_lnx_w': 1.0 + nrm(20, (Lr, RW), 0.02),
        'rwkv_lnx_b': nrm(21, (Lr, RW), 0.02),
        'mlstm_conv_w': nrm(22, (Lr, CONV_WIDTH, QK), CONV_WIDTH ** -0.5),
        'mlstm_conv_b': nrm(23, (Lr, QK), 0.02),
        'mlstm_gate_b': gate_bias + nrm(24, (Lr, 2 * MLSTM_HEADS), 0.1),
        'mlstm_norm_w': 1.0 + nrm(25, (Lr, MLSTM_WIDTH), 0.02),
        'router_group_w': nrm(26, (Lr, D, N_GROUPS), D ** -0.5),
        'router_group_b': nrm(27, (Lr, N_GROUPS), 0.01),
        'router_expert_w': nrm(28, (Lr, D, N_EXPERTS), D ** -0.5),
        'router_expert_b': nrm(29, (Lr, N_EXPERTS), 0.01),
        'expert_w_gate_up': nrm(30, (Lr, N_EXPERTS, D, 2 * D_EXPERT), D ** -0.5),
        'expert_w_down': nrm(31, (Lr, N_EXPERTS, D_EXPERT, D), D_EXPERT ** -0.5),
    }


def reference(x, meta_tokens, norm_mix_w, norm_ffn_w, norm_final_w, w_in, w_out,
              rwkv_mu_rkv, rwkv_mu_wag, rwkv_w0, rwkv_w_lora_a, rwkv_w_lora_b,
              rwkv_a0, rwkv_a_lora_a, rwkv_a_lora_b, rwkv_g_lora_a, rwkv_g_lora_b,
              rwkv_k_k, rwkv_k_a, rwkv_r_k, rwkv_lnx_w, rwkv_lnx_b,
              mlstm_conv_w, mlstm_conv_b, mlstm_gate_b, mlstm_norm_w,
              router_group_w, router_group_b, router_expert_w, router_expert_b,
              expert_w_gate_up, expert_w_down):
    B = x.shape[0]
    meta = jnp.broadcast_to(meta_tokens[None].astype(x.dtype), (B, N_META, D_MODEL))
    h = jnp.concatenate([meta, x], axis=1)
    for l in range(DEPTH):
        xn = _rmsnorm(h, norm_mix_w[l])
        proj = xn @ w_in[l]
        r, k, v, qm, km, vm, om, gm = jnp.split(proj, _split_points(), axis=-1)
        y_a = _rwkv7_time_mix(xn, r, k, v, rwkv_mu_rkv[l], rwkv_mu_wag[l], rwkv_w0[l],
                              rwkv_w_lora_a[l], rwkv_w_lora_b[l], rwkv_a0[l], rwkv_a_lora_a[l],
                              rwkv_a_lora_b[l], rwkv_g_lora_a[l], rwkv_g_lora_b[l], rwkv_k_k[l],
                              rwkv_k_a[l], rwkv_r_k[l], rwkv_lnx_w[l], rwkv_lnx_b[l])
        y_b = _mlstm_mix(qm, km, vm, om, gm, mlstm_conv_w[l], mlstm_conv_b[l],
                         mlstm_gate_b[l], mlstm_norm_w[l])
        h = h + jnp.concatenate([y_a, y_b], axis=-1) @ w_out[l]
        xn = _rmsnorm(h, norm_ffn_w[l])
        h = h + _hmoe(xn, router_group_w[l], router_group_b[l], router_expert_w[l],
                      router_expert_b[l], expert_w_gate_up[l], expert_w_down[l])
    return _rmsnorm(h, norm_final_w)[:, N_META:]
```

```python
import math
import numpy as np
from contextlib import ExitStack
import concourse.bass as bass
import concourse.mybir as mybir
from concourse.bass_utils import run_bass_kernel_spmd

F32 = mybir.dt.float32
BF16 = mybir.dt.bfloat16
I32 = mybir.dt.int32
U32 = mybir.dt.uint32
AF = mybir.ActivationFunctionType
ALU = mybir.AluOpType
AX = mybir.AxisListType

D = 1024
DBG_TILE = 0
DIN = 3080
NEXP = 32
C0 = math.exp(-0.5)


class V:
    def __init__(self, ap, key):
        self.ap = ap
        self.key = key


def A(x):
    if isinstance(x, V):
        return x.ap
    if type(x).__name__.endswith('TensorHandle'):
        return x.ap()
    return x


def K(x):
    return x.key if isinstance(x, V) else x.name


class Sched:
    EPOCH = 20000

    def __init__(self, nc, es, n_dma_sems=32):
        self.nc = nc
        self.es = es
        self.eng = {'pe': nc.tensor, 'act': nc.scalar, 'dve': nc.vector,
                    'pool': nc.gpsimd, 'sp': nc.sync}
        self.sem = {}
        self.cnt = {}
        self.nsem = 0
        self.total = {k: 0 for k in self.eng}
        for k in self.eng:
            self._new_sem(k)
        self.waited = {k: {} for k in self.eng}
        self.res = {}
        self.dma_sems = [es.enter_context(nc.semaphore(f"dq{i}")) for i in range(n_dma_sems)]
        self.dma_cnt = [0] * n_dma_sems
        self.dma_rr = 0
        self.out_tokens = []

    def _new_sem(self, k):
        self.nsem += 1
        self.sem[k] = self.es.enter_context(self.nc.semaphore(f"s_{k}_{self.nsem}"))
        self.cnt[k] = 0

    def _need(self, reads, writes):
        need = []
        for key in reads:
            st = self.res.get(key)
            if st is not None and st['w'] is not None:
                need.append((st['w'], 'raw'))
        for key in writes:
            st = self.res.get(key)
            if st is not None:
                if st['w'] is not None:
                    need.append((st['w'], 'waw'))
                need.extend((t, 'war') for t in st['r'].values())
        return need

    import os as _os
    SAME_ENGINE_GAP = int(_os.environ.get('SE_GAP', 16))
    SKIP_WAX = int(_os.environ.get('SE_SKIPWAX', 1))
    SKIP_ENG = _os.environ.get('SE_ENG', 'dve,act,pool').split(',')

    def _emit_waits(self, e, need):
        for item in need:
            tok, kind = item if isinstance(item[0], tuple) else (item, 'raw')
            sem, val, src = tok[0], tok[1], tok[2]
            if src == 'pe' and e == 'pe':
                continue
            if src == e and src != 'dma':
                if e in self.SKIP_ENG:
                    if kind != 'raw' and self.SKIP_WAX:
                        continue
                    if kind == 'raw' and self.total[e] - tok[3] >= self.SAME_ENGINE_GAP:
                        continue
            w = self.waited[e]
            if w.get(id(sem), 0) >= val:
                continue
            self.eng[e].wait_ge(sem, val)
            w[id(sem)] = val

    def _record(self, tok, reads, writes):
        for key in reads:
            st = self.res.setdefault(key, {'w': None, 'r': {}})
            st['r'][id(tok[0])] = tok
        for key in writes:
            self.res[key] = {'w': tok, 'r': {}}

    def op(self, e, fn, r=(), w=()):
        r = [K(k) if not isinstance(k, (str, tuple)) else k for k in r]
        w = [K(k) if not isinstance(k, (str, tuple)) else k for k in w]
        w = w + [k for k in r if isinstance(k, str) and k.startswith('pb') and k not in w]
        if self.cnt[e] >= self.EPOCH:
            self._new_sem(e)
        self._emit_waits(e, self._need(r, w))
        inst = fn(self.eng[e])
        self.cnt[e] += 1
        self.total[e] += 1
        inst.then_inc(self.sem[e], 1)
        tok = (self.sem[e], self.cnt[e], e, self.total[e])
        self._record(tok, r, w)
        return tok

    def dma(self, q, fn, r=(), w=(), is_out=False):
        r = [K(k) if not isinstance(k, (str, tuple)) else k for k in r]
        w = [K(k) if not isinstance(k, (str, tuple)) else k for k in w]
        i = self.dma_rr
        self.dma_rr = (self.dma_rr + 1) % len(self.dma_sems)
        sem = self.dma_sems[i]
        need = self._need(r, w)
        if self.dma_cnt[i] > 0:
            need.append(((sem, 16 * self.dma_cnt[i], 'dma', 0), 'raw'))
        self._emit_waits(q, need)
        inst = fn(self.eng[q])
        self.dma_cnt[i] += 1
        inst.then_inc(sem, 16)
        tok = (sem, 16 * self.dma_cnt[i], 'dma', 0)
        self._record(tok, r, w)
        if is_out:
            self.out_tokens.append(tok)
        return tok

    def barrier(self):
        toks = [(self.sem[k], self.cnt[k], k, -10**9) for k in self.eng if self.cnt[k] > 0]
        toks += [(s, 16 * c, 'dma', 0) for s, c in zip(self.dma_sems, self.dma_cnt) if c > 0]
        for e in self.eng:
            self._emit_waits(e, [t for t in toks if t[2] != e])

    def finish(self):
        self._emit_waits('sp', self.out_tokens)
        self.barrier()


class Stop(Exception):
    pass


def inter(gens):
    gens = list(gens)
    while gens:
        for g in list(gens):
            try:
                next(g)
            except StopIteration:
                gens.remove(g)
        yield


def build(NT, CAP, stop_after=None, dbgn=8192):
    nc = bass.Bass("TRN2", target_bir_lowering=False)
    NX = NT - 1
    dt_in = lambda name, shape: nc.dram_tensor(name, shape, F32, kind="ExternalInput").ap()
    x_d = dt_in("x", [NX * 128, D])
    meta_d = dt_in("meta_tokens", [16, D])
    nmix_d = dt_in("norm_mix_w", [1, D]); nffn_d = dt_in("norm_ffn_w", [1, D]); nfin_d = dt_in("norm_final_w", [D])
    win_d = dt_in("w_in", [1, D, DIN]); wout_d = dt_in("w_out", [1, D, D])
    murkv_d = dt_in("rwkv_mu_rkv", [1, 3, 512]); muwag_d = dt_in("rwkv_mu_wag", [1, 3, D])
    w0_d = dt_in("rwkv_w0", [1, 512]); wla_d = dt_in("rwkv_w_lora_a", [1, D, 64]); wlb_d = dt_in("rwkv_w_lora_b", [1, 64, 512])
    a0_d = dt_in("rwkv_a0", [1, 512]); ala_d = dt_in("rwkv_a_lora_a", [1, D, 64]); alb_d = dt_in("rwkv_a_lora_b", [1, 64, 512])
    gla_d = dt_in("rwkv_g_lora_a", [1, D, 160]); glb_d = dt_in("rwkv_g_lora_b", [1, 160, 512])
    kk_d = dt_in("rwkv_k_k", [1, 512]); ka_d = dt_in("rwkv_k_a", [1, 512]); rk_d = dt_in("rwkv_r_k", [1, 512])
    lnw_d = dt_in("rwkv_lnx_w", [1, 512]); lnb_d = dt_in("rwkv_lnx_b", [1, 512])
    cw_d = dt_in("mlstm_conv_w", [1, 4, 512]); cb_d = dt_in("mlstm_conv_b", [1, 512])
    gb_d = dt_in("mlstm_gate_b", [1, 8]); mnw_d = dt_in("mlstm_norm_w", [1, 512])
    rgw_d = dt_in("router_group_w", [1, D, 4]); rgb_d = dt_in("router_group_b", [1, 4])
    rew_d = dt_in("router_expert_w", [1, D, 32]); reb_d = dt_in("router_expert_b", [1, 32])
    wgu_d = dt_in("expert_w_gate_up", [1, NEXP, D, D]); wdn_d = dt_in("expert_w_down", [1, NEXP, 512, D])
    out_d = nc.dram_tensor("out", [NX * 128, D], F32, kind="ExternalOutput").ap()
    h1_d = nc.dram_tensor("h1_scr", [NT * 128, D], F32, kind="Internal").ap()
    xs_d = nc.dram_tensor("xs_scr", [NEXP * CAP, D], BF16, kind="Internal").ap()
    ys_d = nc.dram_tensor("ys_scr", [NEXP * CAP, D], F32, kind="Internal").ap()
    yT_d = nc.dram_tensor("yT_scr", [NT * 128, D], BF16, kind="Internal").ap()
    dbg_d = nc.dram_tensor("dbg", [128, dbgn], F32, kind="ExternalOutput").ap() if stop_after else None
    dbg_pos = [0]
    dbg_map = {}

    with ExitStack() as es0:
        S = Sched(nc, es0)

        def dump(name, ap, np_=128):
            if dbg_d is None:
                return
            n = ap.shape[-1] if len(ap.shape) == 2 else int(np.prod(ap.shape[1:]))
            dbg_map[name] = (dbg_pos[0], n, np_)
            S.dma('sp', lambda E: E.dma_start(out=dbg_d[0:np_, dbg_pos[0]:dbg_pos[0] + n], in_=ap, allow_slow_non_contiguous=True), r=[ap], w=['dbg'], is_out=True)
            dbg_pos[0] += n

        def chk(name):
            if stop_after == name:
                raise Stop()
        try:

            def mm(out, lhsT, rhs, start=True, stop=True):
                S.op('pe', lambda E: E.matmul(A(out), lhsT=A(lhsT), rhs=A(rhs), start=start, stop=stop),
                     r=[lhsT, rhs], w=[out])

            def act(out, in_, func, bias=None, scale=None, accum=None, e='act'):
                kw = {}
                rd = [in_]
                if bias is not None:
                    kw['bias'] = A(bias) if not isinstance(bias, float) else bias
                    if not isinstance(bias, float):
                        rd.append(bias)
                if scale is not None:
                    kw['scale'] = A(scale) if not isinstance(scale, float) else scale
                    if not isinstance(scale, float):
                        rd.append(scale)
                wr = [out]
                if accum is not None:
                    kw['accum_out'] = A(accum)
                    wr.append(accum)
                S.op('act', lambda E: E.activation(out=A(out), in_=A(in_), func=func, **kw), r=rd, w=wr)

            def tsc(out, in0, s1, s2, op0, op1=None, e='dve'):
                rd = [in0]
                a1 = s1
                a2 = s2
                if not isinstance(s1, (float, int)):
                    rd.append(s1); a1 = A(s1)
                if s2 is not None and not isinstance(s2, (float, int)):
                    rd.append(s2); a2 = A(s2)
                kw = {} if op1 is None else {'op1': op1}
                S.op(e, lambda E: E.tensor_scalar(out=A(out), in0=A(in0), scalar1=a1, scalar2=a2, op0=op0, **kw),
                     r=rd, w=[out])

            def ttn(out, a, b, op, e='dve'):
                S.op(e, lambda E: E.tensor_tensor(out=A(out), in0=A(a), in1=A(b), op=op), r=[a, b], w=[out])

            def stt(out, in0, sc, in1, op0, op1):
                rd = [in0, in1]
                a = sc
                if not isinstance(sc, (float, int)):
                    rd.append(sc); a = A(sc)
                S.op('dve', lambda E: E.scalar_tensor_tensor(out=A(out), in0=A(in0), scalar=a, in1=A(in1), op0=op0, op1=op1),
                     r=rd, w=[out])

            def cp(out, in_, e='dve'):
                if e == 'act':
                    S.op('act', lambda E: E.activation(out=A(out), in_=A(in_), func=AF.Copy), r=[in_], w=[out])
                else:
                    S.op(e, lambda E: E.tensor_copy(out=A(out), in_=A(in_)), r=[in_], w=[out])

            def mset(t, val, e='pool'):
                S.op(e, lambda E: E.memset(A(t), val), w=[t])

            def recip(out, in_):
                S.op('dve', lambda E: E.reciprocal(out=A(out), in_=A(in_)), r=[in_], w=[out])

            def dma(out, in_, q='sp', is_out=False):
                S.dma(q, lambda E: E.dma_start(out=A(out), in_=A(in_)), r=[in_], w=[out], is_out=is_out)

            banks = [es0.enter_context(nc.psum_tensor(f"pb{i}", [128, 512], F32)) for i in range(8)]
            st = {'q': 0, 'b': 0}

            def psq():
                i = st['q']; st['q'] = (i + 1) % 16
                b_, q_ = i % 4, (i // 4) % 4
                return V(banks[b_][:, q_ * 128:(q_ + 1) * 128], f"pb{b_}")

            def psb():
                i = st['b']; st['b'] = (i + 1) % 4
                return V(banks[4 + i][:, :], f"pbB{i}")

            def sub(v, ap):
                return V(ap, v.key) if isinstance(v, V) else ap

            T = lambda stack, name, shape, dt=F32: stack.enter_context(nc.sbuf_tensor(name, shape, dt))

            ones = T(es0, "ones", [128, 128]); ident = T(es0, "ident", [128, 128]); identb = T(es0, "identb", [128, 128], BF16)
            msu = T(es0, "msu", [128, 128]); msl = T(es0, "msl", [128, 128]); mst = T(es0, "mst", [128, 64])
            blk64 = T(es0, "blk64", [128, 128]); tric = T(es0, "tric", [128, 128]); maskc = T(es0, "maskc", [128, 128])
            mset(ones, 1.0)
            asel = lambda out, pat, cmp, base, cm, in_=None: S.op('pool', lambda E: E.affine_select(
                out=A(out), in_=A(in_ if in_ is not None else ones[:]), pattern=pat, compare_op=cmp, fill=0.0, base=base, channel_multiplier=cm),
                r=[in_ if in_ is not None else ones], w=[out])
            asel(ident[:], [[-1, 128]], ALU.is_equal, 0, 1)
            cp(identb[:], ident[:], e='pool')
            asel(msu[:], [[1, 128]], ALU.is_gt, 0, -1)
            asel(msl[:], [[-1, 128]], ALU.is_gt, 0, 1)
            asel(mst[0:64, :], [[1, 64]], ALU.is_ge, 0, -1, in_=ones[0:64, 0:64])
            asel(mst[64:128, :], [[1, 64]], ALU.is_ge, 0, -1, in_=ones[64:128, 0:64])
            mset(blk64, 0.0); mset(blk64[0:64, 0:64], 1.0); mset(blk64[64:128, 64:128], 1.0)
            asel(tric[:], [[1, 128]], ALU.is_ge, 0, -1)
            ttn(tric[:], tric[:], blk64[:], ALU.mult, e='pool')
            tsc(maskc[:], tric[:], 0.125, None, ALU.mult, e='pool')

            pstg = T(es0, "pstg", [128, 128]); pv = T(es0, "pv", [128, 128]); pv1 = T(es0, "pv1", [128, 128])
            mset(pstg, 0.0)
            row = {}
            rcur = [0]

            def ldrows(name, ap2d, n):
                row[name] = rcur[0]
                dma(pstg[rcur[0]:rcur[0] + n, :], ap2d)
                rcur[0] += n
            ldrows('nmix', nmix_d[0].rearrange("(c p) -> c p", p=128), 8)
            ldrows('muwag', muwag_d[0].rearrange("j (c p) -> (j c) p", p=128), 24)
            ldrows('murkv', murkv_d[0].rearrange("j (c p) -> (j c) p", p=128), 12)
            for nm, ap in (('w0', w0_d), ('a0', a0_d), ('kk', kk_d), ('ka', ka_d), ('rk', rk_d), ('lnw', lnw_d), ('lnb', lnb_d), ('cb', cb_d)):
                ldrows(nm, ap[0].rearrange("(c p) -> c p", p=128), 4)
            ldrows('cw', cw_d[0].rearrange("j (c p) -> (j c) p", p=128), 16)
            tp = psq()
            S.op('pe', lambda E: E.transpose(out=A(tp), in_=pstg[:], identity=ident[:]), r=[pstg, ident], w=[tp])
            cp(pv[:], tp, e='act')
            tsc(pv1[:], pv[:], -1.0, 1.0, ALU.mult, ALU.add)
            PV = lambda nm, j=0: pv[:, row[nm] + j:row[nm] + j + 1]
            PV1 = lambda nm, j=0: pv1[:, row[nm] + j:row[nm] + j + 1]

            mnw_bc = T(es0, "mnw_bc", [128, 512])
            gb_bc = T(es0, "gb_bc", [128, 8]); rb_bc = T(es0, "rb_bc", [128, 36]); iota_i = T(es0, "iota_i", [128, 32], I32)
            iota_f = T(es0, "iota_f", [128, 32]); giota = T(es0, "giota", [128, 4])

            dma(mnw_bc[:], mnw_d[0].partition_broadcast(128)); dma(gb_bc[:], gb_d[0].partition_broadcast(128))
            dma(rb_bc[:, 0:4], rgb_d[0].partition_broadcast(128)); dma(rb_bc[:, 4:36], reb_d[0].partition_broadcast(128))
            S.op('pool', lambda E: E.iota(iota_i[:], pattern=[[1, 32]], base=0, channel_multiplier=0), w=[iota_i])
            cp(iota_f[:], iota_i[:], e='pool')
            tsc(giota[:], iota_f[:, 0:4], 8.0, None, ALU.mult, e='pool')

            gates_all = T(es0, "gates_all", [128, NT, 2]); slots_all = T(es0, "slots_all", [128, NT, 2], I32)
            es1 = es0.enter_context(ExitStack())
            win_b = T(es1, "win_b", [128, 8, DIN], BF16)
            wla_b = T(es1, "wla_b", [128, 8, 288], BF16); wlamu_b = T(es1, "wlamu_b", [128, 8, 288], BF16)
            lb_b = T(es1, "lb_b", [128, 512], BF16); glb0_b = T(es1, "glb0_b", [128, 512], BF16); glb1_b = T(es1, "glb1_b", [32, 512], BF16)
            with ExitStack() as esl:
                stg = [T(esl, f"wstg{i}", [128, DIN]) for i in range(2)]
                win_v = win_d[0].rearrange("(c p) f -> p c f", p=128)
                ceng = ['dve', 'pool', 'act']
                k = 0
                for c in range(8):
                    s_ = stg[k % 2]
                    dma(s_[:, :], win_v[:, c, :])
                    cp(win_b[:, c, :], s_[:, :], e=ceng[k % 3]); k += 1
                s_ = stg[k % 2]; k += 1
                sv = s_[:, 0:8 * 288].rearrange("p (c j) -> p c j", j=288)
                dma(sv[:, :, 0:64], wla_d[0].rearrange("(c p) j -> p c j", p=128))
                dma(sv[:, :, 64:128], ala_d[0].rearrange("(c p) j -> p c j", p=128))
                dma(sv[:, :, 128:288], gla_d[0].rearrange("(c p) j -> p c j", p=128))
                cp(wla_b[:, :, :], sv, e='dve')
                for c in range(8):
                    for j, (lo, hi) in enumerate(((0, 64), (64, 128), (128, 288))):
                        tsc(wlamu_b[:, c, lo:hi], sv[:, c, lo:hi], PV('muwag', j * 8 + c), None, ALU.mult, e='dve' if c % 2 else 'pool')
                s_ = stg[k % 2]; k += 1
                dma(s_[0:64, 0:512], wlb_d[0]); dma(s_[64:128, 0:512], alb_d[0])
                dma(s_[:, 512:1024], glb_d[0][0:128, :]); dma(s_[0:32, 1024:1536], glb_d[0][128:160, :])
                cp(lb_b[:, :], s_[:, 0:512]); cp(glb0_b[:, :], s_[:, 512:1024]); cp(glb1_b[:, :], s_[0:32, 1024:1536])
                S.barrier()
            dump('pv', pv[:, :])
            chk('setup')

            es2 = es1.enter_context(ExitStack())
            t2 = lambda name, shape, dt=F32: T(es2, name, shape, dt)
            x_tm = [t2("x_tm0", [128, D])] * 2
            big = t2("big", [128, D])
            xnT_f = t2("xnT_f", [128, 8, 129]); xnT_b = t2("xnT_b", [128, 8, 128], BF16); xxT_b = t2("xxT_b", [128, 8, 128], BF16)
            rkv_raw = t2("rkv_raw", [128, 12, 129]); qk_raw = t2("qk_raw", [128, 4, 131])
            V1s = [t2(f"V1_{i}", [128, 4, 129]) for i in range(2)]; sigos = [t2(f"sigo{i}", [128, 512]) for i in range(2)]
            laT = t2("laT", [128, 128], BF16); lg0 = t2("lg0", [128, 128], BF16); lg1 = t2("lg1", [32, 128], BF16)
            sgw = t2("sgw", [128, 4, 128]); asig = t2("asig", [128, 4, 128]); ggs = [t2(f"gg{i}", [128, 4, 128]) for i in range(2)]
            bonuss = [t2(f"bonus{i}", [128, 4, 128]) for i in range(2)]; y_as = [t2(f"y_a{i}", [128, 4, 128]) for i in range(2)]
            ptA = [t2(f"ptA{i}", [128, 128]) for i in range(2)]; ptB = [t2(f"ptB{i}", [128, 128]) for i in range(2)]
            yT_bs = [t2(f"yT_b{i}", [128, 8, 128], BF16) for i in range(2)]
            ssq = t2("ssq", [128, 4]); rstd = t2("rstd", [128, 4])
            NR = 4
            rt = {nm: [t2(f"{nm}{i}", [128, 128]) for i in range(NR)] for nm in
                  ('r', 'k', 'v', 'kkn', 'k2', 'bv', 'tA', 'tB', 'cs', 'E1', 'E2', 'E3')}
            opz = [[t2(f"opz{fb}_{c}", [128, 4, 128], BF16) for c in range(2)] for fb in range(4)]
            rst = [[t2(f"rst{fb}_{c}", [128, 64], BF16) for c in range(2)] for fb in range(4)]
            gC = t2("gC", [128, 4, 2])
            ArbArk = t2("alg_ArbArk", [128, 2, 4, 64], BF16)
            Gc = [{nm: t2(f"alg{c}_{nm}", [128, 4, 128], BF16) for nm in
                   ('BzT', 'KzT', 'VzT', 'Aak', 'Q0', 'Q1', 'QT0', 'QT1', 'P0', 'P1', 'PT0', 'PT1')} for c in range(2)]
            GS = {nm: t2(f"alg_{nm}", [128, 4, 128], BF16) for nm in ('W0T', 'UT')}
            ArbArks = [ArbArk, t2("alg_ArbArk1", [128, 2, 4, 64], BF16)]
            ST32 = t2("ST32", [128, 4, 128])
            STb = t2("STb", [128, 4, 128], BF16)
            QKf = t2("QKf", [128, 4, 128]); cacc = QKf
            g8s = [t2(f"g8_{i}", [128, 8]) for i in range(2)]; th8 = t2("th8", [128, 8]); nbg = t2("nbg", [128, 8]); wgt = t2("wgt", [128, 4]); dbias = t2("dbias", [128, 4])
            lfb = [t2(f"lfb{i}", [128, 128]) for i in range(2)]; Dm = [t2(f"Dm{i}", [128, 128]) for i in range(2)]
            eB = [t2(f"eB{i}", [128, 128]) for i in range(2)]; Pm = [t2(f"Pm{i}", [128, 128]) for i in range(2)]
            Qz = [t2(f"Qz{h}", [128, 2, 128]) for h in range(4)]
            Kw = t2("Kw", [128, 4, 64])
            CTa = [t2(f"CTa{h}", [128, 129]) for h in range(4)]; CTb = [t2(f"CTb{h}", [128, 129]) for h in range(4)]
            hraw = [t2(f"hraw{i}", [128, 128]) for i in range(2)]; y_b = t2("y_b", [128, 512])
            sm = t2("sm", [128, 16])

            mset(xnT_f[:, :, 0:1], 0.0); mset(rkv_raw[:, :, 0:1], 0.0); mset(qk_raw[:, :, 0:3], 0.0)
            mset(V1s[0][:, :, 128:129], 1.0); mset(V1s[1][:, :, 128:129], 1.0)
            mset(ST32, 0.0); mset(STb, 0.0)
            for fb in range(4):
                for c in range(2):
                    mset(opz[fb][c], 0.0)
            for h in range(4):
                mset(Qz[h], 0.0); mset(CTa[h], 0.0); mset(CTb[h], 0.0)

            def g_front(i):
                xt = x_tm[i % 2]; V1 = V1s[i % 2]; sigo = sigos[i % 2]; g8 = g8s[i % 2]; gg = ggs[i % 2]
                if i == 0:
                    mset(xt, 0.0)
                    dma(xt[112:128, :], meta_d)
                else:
                    dma(xt[:, :], x_d[(i - 1) * 128:i * 128, :])
                act(big[:], xt[:], AF.Square, accum=ssq[:, 0:1])
                act(rstd[:, 0:1], ssq[:, 0:1], AF.Sqrt, bias=1e-6, scale=1.0 / D)
                recip(rstd[:, 0:1], rstd[:, 0:1])
                act(big[:], xt[:], AF.Copy, scale=rstd[:, 0:1])
                yield
                for half in range(2):
                    pb = psb()
                    for j in range(4):
                        c = half * 4 + j
                        S.op('pe', lambda E, c=c, j=j, pb=pb: E.transpose(out=A(pb)[:, j * 128:(j + 1) * 128], in_=big[:, c * 128:(c + 1) * 128], identity=ident[:]),
                             r=[big, ident], w=[pb])
                    for j in range(4):
                        c = half * 4 + j
                        if j % 2 == 0:
                            act(xnT_f[:, c, 1:129], sub(pb, A(pb)[:, j * 128:(j + 1) * 128]), AF.Copy, scale=PV('nmix', c))
                        else:
                            tsc(xnT_f[:, c, 1:129], sub(pb, A(pb)[:, j * 128:(j + 1) * 128]), PV('nmix', c), None, ALU.mult)
                    yield
                cp(xnT_b[:, :, :], xnT_f[:, :, 1:129], e='pool')
                ttn(xxT_b[:, :, :], xnT_f[:, :, 0:128], xnT_f[:, :, 1:129], ALU.subtract)
                cp(xnT_f[:, :, 0:1], xnT_f[:, :, 128:129], e='pool')
                yield

                for blk in range(16):
                    p_ = psq()
                    for c in range(8):
                        mm(p_, win_b[:, c, blk * 128:(blk + 1) * 128], xnT_b[:, c, :], start=(c == 0), stop=(c == 7))
                    if blk < 12:
                        cp(rkv_raw[:, blk, 1:129], p_, e='act')
                    else:
                        cp(qk_raw[:, blk - 12, 3:131], p_, e='act')
                    yield
                pv_ = psb()
                for c in range(8):
                    mm(pv_, xnT_b[:, c, :], win_b[:, c, 2048:2560], start=(c == 0), stop=(c == 7))
                cp(V1[:, :, 0:128], sub(pv_, A(pv_).rearrange("p (h v) -> p h v", v=128)), e='act')
                yield
                po_ = psb()
                for c in range(8):
                    mm(po_, xnT_b[:, c, :], win_b[:, c, 2560:3072], start=(c == 0), stop=(c == 7))
                act(sigo[:], po_, AF.Sigmoid)
                yield
                pg_ = psq()
                pg8 = sub(pg_, A(pg_)[:, 0:8])
                for c in range(8):
                    mm(pg8, xnT_b[:, c, :], win_b[:, c, 3072:3080], start=(c == 0), stop=(c == 7))
                ttn(g8[:], pg8, gb_bc[:], ALU.add)
                yield
                la_ps = []
                for (lo, hi) in ((0, 128), (128, 256), (256, 288)):
                    p_ = psq()
                    po = sub(p_, A(p_)[0:hi - lo, :])
                    for c in range(8):
                        mm(po, wla_b[:, c, lo:hi], xnT_b[:, c, :], start=(c == 0), stop=False)
                    for c in range(8):
                        mm(po, wlamu_b[:, c, lo:hi], xxT_b[:, c, :], start=False, stop=(c == 7))
                    la_ps.append(p_)
                act(laT[0:64, :], sub(la_ps[0], A(la_ps[0])[0:64, :]), AF.Tanh)
                cp(laT[64:128, :], sub(la_ps[0], A(la_ps[0])[64:128, :]), e='dve')
                act(lg0[:, :], la_ps[1], AF.Sigmoid)
                act(lg1[:, :], sub(la_ps[2], A(la_ps[2])[0:32, :]), AF.Sigmoid)
                yield
                for fb in range(4):
                    fs = slice(fb * 128, (fb + 1) * 128)
                    p_ = psq(); mm(p_, lb_b[0:64, fs], laT[0:64, :])
                    act(sgw[:, fb, :], p_, AF.Sigmoid, bias=PV('w0', fb))
                    p_ = psq(); mm(p_, lb_b[64:128, fs], laT[64:128, :])
                    act(asig[:, fb, :], p_, AF.Sigmoid, bias=PV('a0', fb))
                    p_ = psq(); mm(p_, glb0_b[:, fs], lg0[:, :], start=True, stop=False); mm(p_, glb1_b[0:32, fs], lg1[0:32, :], start=False, stop=True)
                    cp(gg[:, fb, :], p_, e='dve')
                    yield

                yield

            pending = []
            for i in range(NT):
                xt = x_tm[i % 2]; V1 = V1s[i % 2]; sigo = sigos[i % 2]; g8 = g8s[i % 2]; gg = ggs[i % 2]
                yT_b = yT_bs[i % 2]; bonus = bonuss[i % 2]; y_a = y_as[i % 2]
                if i == 0:
                    for _ in g_front(0):
                        pass
                def g_prep(fb):
                    R = {nm: rt[nm][fb % NR] for nm in rt}
                    for nm, bi in (('r', 0), ('k', 1), ('v', 2)):
                        blk = bi * 4 + fb
                        tsc(R['tA'][:], rkv_raw[:, blk, 0:128], PV('murkv', bi * 4 + fb), None, ALU.mult, e='pool')
                        stt(R[nm][:], rkv_raw[:, blk, 1:129], PV1('murkv', bi * 4 + fb), R['tA'][:], ALU.mult, ALU.add)
                        yield
                    tsc(R['kkn'][:], R['k'][:], PV('kk', fb), None, ALU.mult)
                    ttn(R['tA'][:], R['kkn'][:], R['kkn'][:], ALU.mult, e='pool')
                    p_ = psq(); mm(p_, blk64[:], R['tA'][:])
                    act(R['tB'][:], p_, AF.Sqrt)
                    yield
                    tsc(R['tB'][:], R['tB'][:], 1e-12, None, ALU.max)
                    recip(R['tB'][:], R['tB'][:])
                    yield
                    ttn(R['kkn'][:], R['kkn'][:], R['tB'][:], ALU.mult)
                    tsc(R['tA'][:], asig[:, fb, :], PV('ka', fb), PV1('ka', fb), ALU.mult, ALU.add)
                    ttn(R['k2'][:], R['k'][:], R['tA'][:], ALU.mult)
                    ttn(R['bv'][:], R['kkn'][:], asig[:, fb, :], ALU.mult, e='pool')
                    yield
                    stt(R['tA'][:], R['r'][:], PV('rk', fb), R['k2'][:], ALU.mult, ALU.mult)
                    p_ = psq(); mm(p_, blk64[:], R['tA'][:])
                    ttn(bonus[:, fb, :], p_, R['v'][:], ALU.mult)
                    yield
                    for c in range(2):
                        cs_ = slice(c * 64, (c + 1) * 64)
                        S.op('dve', lambda E, fb=fb, cs_=cs_, R=R: E.tensor_tensor_scan(out=R['cs'][:, cs_], data0=ones[:, 0:64], data1=sgw[:, fb, cs_], initial=0.0, op0=ALU.mult, op1=ALU.add),
                             r=[ones, sgw], w=[R['cs']])
                        yield
                    act(R['E1'][:], R['cs'][:], AF.Exp, scale=-C0)
                    act(R['E2'][:], R['cs'][:], AF.Exp, scale=C0)
                    yield
                    ttn(R['tB'][:], R['cs'][:], sgw[:, fb, :], ALU.subtract, e='pool')
                    act(R['E3'][:], R['tB'][:], AF.Exp, scale=-C0)
                    yield
                    for c in range(2):
                        O = opz[fb][c]
                        for hh in range(2):
                            ps_ = slice(hh * 64, (hh + 1) * 64)
                            ts_ = slice(c * 64, (c + 1) * 64)
                            os_ = slice(hh * 64, (hh + 1) * 64)
                            stt(O[ps_, 0, os_], R['kkn'][ps_, ts_], -1.0, R['E3'][ps_, ts_], ALU.mult, ALU.mult)
                            ttn(O[ps_, 1, os_], R['bv'][ps_, ts_], R['E2'][ps_, ts_], ALU.mult)
                            ttn(O[ps_, 2, os_], R['k2'][ps_, ts_], R['E2'][ps_, ts_], ALU.mult, e='pool')
                            cp(O[ps_, 3, os_], R['v'][ps_, ts_], e='pool')
                            yield
                        ttn(rst[fb][c][:, :], R['r'][:, c * 64:(c + 1) * 64], R['E1'][:, c * 64:(c + 1) * 64], ALU.mult)
                        cp(gC[:, fb, c:c + 1], R['E1'][:, c * 64 + 63:c * 64 + 64], e='pool')
                    yield

                def q4(bank):
                    return sub(bank, A(bank).rearrange("p (f t) -> p f t", t=128))
                bc4 = lambda m: m[:, :].unsqueeze(1).broadcast_to([128, 4, 128])
                TinvOf = {}
                def g_alg(c):
                    Az = [opz[fb][c][:, 0, :] for fb in range(4)]; Bz = [opz[fb][c][:, 1, :] for fb in range(4)]
                    Kz = [opz[fb][c][:, 2, :] for fb in range(4)]; Vz = [opz[fb][c][:, 3, :] for fb in range(4)]
                    Rs = [rst[fb][c][:, :] for fb in range(4)]
                    for kk_i, (nm, src) in enumerate((('BzT', Bz), ('KzT', Kz), ('VzT', Vz))):
                        bk = psb()
                        for fb in range(4):
                            mm(sub(bk, A(bk)[:, fb * 128:(fb + 1) * 128]), src[fb], identb[:])
                        cp(Gc[c][nm][:, :, :], q4(bk), e='act' if kk_i != 1 else 'dve')
                        yield
                    for nm, l_, r_, msk in (('Q0', Bz, Az, msu), ('QT0', Az, Bz, msl), ('Aak', Kz, Az, msu)):
                        bk = psb()
                        for fb in range(4):
                            mm(sub(bk, A(bk)[:, fb * 128:(fb + 1) * 128]), l_[fb], r_[fb])
                        ttn(Gc[c][nm][:, :, :], q4(bk), bc4(msk), ALU.mult)
                        yield
                    bk = psb()
                    for j, l_ in enumerate((Bz, Kz)):
                        for fb in range(4):
                            o0 = j * 256 + fb * 64
                            mm(sub(bk, A(bk)[:, o0:o0 + 64]), l_[fb], Rs[fb])
                    ttn(ArbArks[c][:, :, :, :].rearrange("p j f t -> p (j f) t"), sub(bk, A(bk).rearrange("p (g t) -> p g t", t=64)),
                        mst[:, :].unsqueeze(1).broadcast_to([128, 8, 64]), ALU.mult)
                    yield
                    ttn(Gc[c]['P0'][:, :, :], Gc[c]['Q0'][:, :, :], identb[:, :].unsqueeze(1).broadcast_to([128, 4, 128]), ALU.add, e='pool')
                    ttn(Gc[c]['PT0'][:, :, :], Gc[c]['QT0'][:, :, :], identb[:, :].unsqueeze(1).broadcast_to([128, 4, 128]), ALU.add, e='pool')
                    yield
                    Qb = [Gc[c]['Q0'], Gc[c]['Q1']]; QTb = [Gc[c]['QT0'], Gc[c]['QT1']]
                    Pb = [Gc[c]['P0'], Gc[c]['P1']]; PTb = [Gc[c]['PT0'], Gc[c]['PT1']]
                    for s_ in range(6):
                        Qs, QTs = Qb[s_ % 2], QTb[s_ % 2]
                        Qn, QTn = Qb[(s_ + 1) % 2], QTb[(s_ + 1) % 2]
                        Pp, PTp = Pb[(s_ - 1) % 2], PTb[(s_ - 1) % 2]
                        Pc, PTc = Pb[s_ % 2], PTb[s_ % 2]
                        todo = []
                        if s_ <= 4:
                            bk = psb()
                            for fb in range(4):
                                mm(sub(bk, A(bk)[:, fb * 128:(fb + 1) * 128]), QTs[:, fb, :], Qs[:, fb, :])
                            todo.append(lambda bk=bk: cp(Qn[:, :, :], q4(bk), e='act'))
                        if s_ <= 3:
                            bk = psb()
                            for fb in range(4):
                                mm(sub(bk, A(bk)[:, fb * 128:(fb + 1) * 128]), Qs[:, fb, :], QTs[:, fb, :])
                            todo.append(lambda bk=bk: cp(QTn[:, :, :], q4(bk), e='act'))
                        if s_ >= 1:
                            bk = psb()
                            for fb in range(4):
                                o_ = sub(bk, A(bk)[:, fb * 128:(fb + 1) * 128])
                                mm(o_, identb[:], Pp[:, fb, :], start=True, stop=False)
                                mm(o_, PTp[:, fb, :], Qs[:, fb, :], start=False, stop=True)
                            todo.append(lambda bk=bk: cp(Pc[:, :, :], q4(bk), e='act'))
                        if 1 <= s_ <= 4:
                            bk = psb()
                            for fb in range(4):
                                o_ = sub(bk, A(bk)[:, fb * 128:(fb + 1) * 128])
                                mm(o_, identb[:], PTp[:, fb, :], start=True, stop=False)
                                mm(o_, Qs[:, fb, :], PTp[:, fb, :], start=False, stop=True)
                            todo.append(lambda bk=bk: cp(PTc[:, :, :], q4(bk), e='dve'))
                        for f_ in todo:
                            f_()
                        yield
                    cur = 1
                    TinvOf[c] = Gc[c][f'P{cur}']
                    yield
                def g_chain(c):
                    Az = [opz[fb][c][:, 0, :] for fb in range(4)]; Bz = [opz[fb][c][:, 1, :] for fb in range(4)]
                    Kz = [opz[fb][c][:, 2, :] for fb in range(4)]; Vz = [opz[fb][c][:, 3, :] for fb in range(4)]
                    Rs = [rst[fb][c][:, :] for fb in range(4)]
                    bk = psb()
                    for fb in range(4):
                        o_ = sub(bk, A(bk)[:, fb * 128:(fb + 1) * 128])
                        mm(o_, Az[fb], STb[:, fb, :], start=True, stop=False); mm(o_, Gc[c]['Aak'][:, fb, :], Gc[c]['VzT'][:, fb, :], start=False, stop=True)
                    cp(GS['W0T'][:, :, :], q4(bk), e='act')
                    yield
                    bk = psb()
                    for fb in range(4):
                        mm(sub(bk, A(bk)[:, fb * 128:(fb + 1) * 128]), TinvOf[c][:, fb, :], GS['W0T'][:, fb, :])
                    cp(GS['UT'][:, :, :], q4(bk), e='act')
                    yield
                    bk = psb()
                    for fb in range(4):
                        o_ = sub(bk, A(bk)[:, fb * 64:(fb + 1) * 64])
                        mm(o_, STb[:, fb, :], Rs[fb], start=True, stop=False)
                        mm(o_, GS['UT'][:, fb, :], ArbArks[c][:, 0, fb, :], start=False, stop=False)
                        mm(o_, Gc[c]['VzT'][:, fb, :], ArbArks[c][:, 1, fb, :], start=False, stop=True)
                    cp(y_a[:, :, c * 64:(c + 1) * 64], sub(bk, A(bk)[:, 0:256].rearrange("p (f t) -> p f t", t=64)), e='act')
                    yield
                    bk = psb()
                    for fb in range(4):
                        o_ = sub(bk, A(bk)[:, fb * 128:(fb + 1) * 128])
                        mm(o_, Gc[c]['BzT'][:, fb, :], GS['UT'][:, fb, :], start=True, stop=False)
                        mm(o_, Gc[c]['KzT'][:, fb, :], Gc[c]['VzT'][:, fb, :], start=False, stop=True)
                    ttn(ST32[:, :, :], ST32[:, :, :], q4(bk), ALU.add)
                    ttn(ST32[:, :, :], ST32[:, :, :], gC[:, :, c:c + 1].broadcast_to([128, 4, 128]), ALU.mult)
                    cp(STb[:, :, :], ST32[:, :, :], e='pool')
                    yield
                    yield

                def g_post(fb, y_a=y_a, bonus=bonus, gg=gg, yT_b=yT_b):
                    R = {'tA': ptA[fb % 2], 'tB': ptB[fb % 2]}
                    p1 = psq(); mm(p1, blk64[:], y_a[:, fb, :])
                    ttn(R['tA'][:], y_a[:, fb, :], y_a[:, fb, :], ALU.mult, e='pool')
                    p2 = psq(); mm(p2, blk64[:], R['tA'][:])
                    act(R['tB'][:], p1, AF.Copy, scale=1.0 / 64)
                    ttn(R['tA'][:], R['tB'][:], R['tB'][:], ALU.mult)
                    stt(R['tA'][:], p2, 1.0 / 64, R['tA'][:], ALU.mult, ALU.subtract)
                    yield
                    act(R['tA'][:], R['tA'][:], AF.Sqrt, bias=64e-5)
                    yield
                    recip(R['tA'][:], R['tA'][:])
                    yield
                    ttn(R['tB'][:], y_a[:, fb, :], R['tB'][:], ALU.subtract)
                    ttn(R['tB'][:], R['tB'][:], R['tA'][:], ALU.mult)
                    tsc(R['tB'][:], R['tB'][:], PV('lnw', fb), PV('lnb', fb), ALU.mult, ALU.add)
                    ttn(R['tB'][:], R['tB'][:], bonus[:, fb, :], ALU.add)
                    ttn(yT_b[:, fb, :], R['tB'][:], gg[:, fb, :], ALU.mult)
                    yield

                def g_mlstm_pre():
                    for t_ in range(4):
                        for jb in range(4):
                            kj = ('QKf', jb)
                            if t_ == 0:
                                S.op('dve', lambda E, jb=jb: E.tensor_scalar(out=cacc[:, jb, :], in0=qk_raw[:, jb, 0:128], scalar1=PV('cw', jb), scalar2=PV('cb', jb), op0=ALU.mult, op1=ALU.add),
                                     r=[qk_raw, pv], w=[kj, 'QKf'])
                            else:
                                S.op('dve', lambda E, jb=jb, t_=t_: E.scalar_tensor_tensor(out=cacc[:, jb, :], in0=qk_raw[:, jb, t_:t_ + 128], scalar=PV('cw', t_ * 4 + jb), in1=cacc[:, jb, :], op0=ALU.mult, op1=ALU.add),
                                     r=[qk_raw, pv, kj], w=[kj, 'QKf'])
                    S.op('act', lambda E: E.activation(out=QKf[:, :, :], in_=cacc[:, :, :], func=AF.Silu), r=[('QKf', jb) for jb in range(4)] + ['QKf'], w=['QKf'] + [('QKf', jb) for jb in range(4)])
                    yield
                    cp(qk_raw[:, :, 0:3], qk_raw[:, :, 128:131], e='pool')
                    act(th8[:], g8[:], AF.Tanh, scale=1.0 / 15.0)
                    tsc(g8[:, 0:4], th8[:, 0:4], 15.0, None, ALU.mult)
                    act(g8[:, 4:8], th8[:, 4:8], AF.Exp, scale=-15.0)
                    act(g8[:, 4:8], g8[:, 4:8], AF.Ln, bias=1.0)
                    yield
                    if i == 0:
                        mset(g8[0:112, 0:4], -1.0e4, e='dve'); mset(g8[0:112, 4:8], 0.0, e='dve')
                    pn = psq()
                    pn4 = sub(pn, A(pn)[:, 0:4]); pn8 = sub(pn, A(pn)[:, 4:8])
                    mm(pn4, tric[:], g8[:, 4:8]); mm(pn8, blk64[:], g8[:, 4:8])
                    cp(nbg[:], sub(pn, A(pn)[:, 0:8]), e='act')
                    yield
                    ttn(dbias[:], g8[:, 0:4], nbg[:, 0:4], ALU.add)
                    ttn(wgt[:], dbias[:], nbg[:, 4:8], ALU.subtract)
                    act(wgt[:], wgt[:], AF.Exp, bias=math.log(0.125))
                    yield
                    for kb in range(2):
                        p_ = psq()
                        S.op('pe', lambda E, kb=kb, p_=p_: E.transpose(out=A(p_), in_=QKf[:, 2 + kb, :], identity=ident[:]), r=[QKf, ident], w=[p_])
                        for hh in range(2):
                            h = kb * 2 + hh
                            tsc(Kw[:, h, :], sub(p_, A(p_)[:, hh * 64:(hh + 1) * 64]), wgt[:, h:h + 1], None, ALU.mult)
                    yield

                def g_mlstm_rest():
                    def g_head(h):
                        hb = (h % 2) * 64
                        hs = slice(hb, hb + 64)
                        qb = h // 2
                        L_, D_, E_, P_ = lfb[h % 2], Dm[h % 2], eB[h % 2], Pm[h % 2]
                        tsc(L_[:], ones[:], g8[:, 4 + h:5 + h], None, ALU.mult, e='pool')
                        pbr = psq(); mm(pbr, L_[:], tric[:])
                        act(D_[:], pbr, AF.Exp, bias=dbias[:, h:h + 1], scale=-1.0)
                        act(E_[:], pbr, AF.Exp, scale=-1.0)
                        yield
                        psc = psq(); mm(psc, QKf[hs, 2 + qb, :], QKf[hs, qb, :])
                        ttn(D_[:], D_[:], maskc[:], ALU.mult, e='pool')
                        ttn(P_[:], D_[:], psc, ALU.mult)
                        yield
                        ttn(Qz[h][hs, 0, 0:64], QKf[hs, qb, 0:64], E_[hs, 0:64], ALU.mult)
                        ttn(Qz[h][hs, 1, 64:128], QKf[hs, qb, 64:128], E_[hs, 64:128], ALU.mult)
                        pU = psb()
                        u0 = sub(pU, A(pU)[hs, 0:129]); u1 = sub(pU, A(pU)[hs, 256:385])
                        mm(u0, Kw[0:64, h, :], V1[0:64, h, :])
                        stt(CTb[h][hs, :], CTa[h][hs, :], E_[hs, 63:64], u0, ALU.mult, ALU.add)
                        yield
                        pO = psb(); o_ = sub(pO, A(pO)[:, 0:129])
                        mm(o_, P_[:], V1[:, h, :], start=True, stop=False)
                        mm(o_, Qz[h][hs, 0, :], CTa[h][hs, :], start=False, stop=False)
                        mm(o_, Qz[h][hs, 1, :], CTb[h][hs, :], start=False, stop=True)
                        mm(u1, Kw[64:128, h, :], V1[64:128, h, :])
                        stt(CTa[h][hs, :], CTb[h][hs, :], E_[hs, 127:128], u1, ALU.mult, ALU.add)
                        if i > 0:
                            H_ = hraw[h % 2]
                            act(sm[:, 3 * h:3 * h + 1], sub(pO, A(pO)[:, 128:129]), AF.Abs)
                            tsc(sm[:, 3 * h:3 * h + 1], sm[:, 3 * h:3 * h + 1], 1.0, None, ALU.max)
                            recip(sm[:, 3 * h:3 * h + 1], sm[:, 3 * h:3 * h + 1])
                            tsc(H_[:], sub(pO, A(pO)[:, 0:128]), sm[:, 3 * h:3 * h + 1], None, ALU.mult)
                            act(P_[:], H_[:], AF.Square, accum=sm[:, 3 * h + 1:3 * h + 2])
                            yield
                            act(sm[:, 3 * h + 2:3 * h + 3], sm[:, 3 * h + 1:3 * h + 2], AF.Sqrt, bias=1e-6, scale=1.0 / 128)
                            recip(sm[:, 3 * h + 2:3 * h + 3], sm[:, 3 * h + 2:3 * h + 3])
                            yield
                            stt(H_[:], H_[:], sm[:, 3 * h + 2:3 * h + 3], mnw_bc[:, h * 128:(h + 1) * 128], ALU.mult, ALU.mult)
                            ttn(y_b[:, h * 128:(h + 1) * 128], H_[:], sigo[:, h * 128:(h + 1) * 128], ALU.mult)
                        yield
                    yield from inter([g_head(0), g_head(1)])
                    yield from inter([g_head(2), g_head(3)])
                    if i == 0:
                        return
                    for h in range(4):
                        p_ = psq()
                        S.op('pe', lambda E, h=h, p_=p_: E.transpose(out=A(p_), in_=y_b[:, h * 128:(h + 1) * 128], identity=ident[:]), r=[y_b, ident], w=[p_])
                        cp(yT_b[:, 4 + h, :], p_, e='act')
                    yield

                def g_rwkv_prep():
                    yield from inter([g_prep(fb) for fb in range(4)])
                    cp(rkv_raw[:, :, 0:1], rkv_raw[:, :, 128:129], e='pool')
                    yield

                def g_rwkv_rest():
                    yield from inter([g_alg(0), g_alg(1)])
                    yield from g_chain(0)
                    yield from g_chain(1)

                def g_post_all(i=i, yT_b=yT_b, posts=[g_post(fb) for fb in range(4)]):
                    yield from inter(posts[0:2])
                    yield from inter(posts[2:4])
                    dma(yT_d[i * 128:(i + 1) * 128, :], yT_b[:, :, :].rearrange("p b t -> p (b t)"))
                    yield
                for _ in inter([g_rwkv_prep(), g_mlstm_pre()] + pending):
                    pass
                pending = []
                streams = [g_rwkv_rest(), g_mlstm_rest()]
                if i + 1 < NT:
                    streams.append(g_front(i + 1))
                for _ in inter(streams):
                    pass
                if i > 0:
                    pending = [g_post_all()]
            for _ in inter(pending):
                pass

            S.barrier()
            chk('p1')
            es2.close()
            es1.close()

            with ExitStack() as es5:
                t5 = lambda name, shape, dt=F32: T(es5, name, shape, dt)
                NB1 = 4
                wout_b = t5("wout_b", [128, 8, D], BF16); wr_f = t5("wr_f", [128, 8, 36]); wffn_bc = t5("wffn_bc", [128, D])
                wst = [t5(f"wst{i}", [128, D]) for i in range(2)]
                xt1 = [t5(f"xt1_{i}", [128, D]) for i in range(NB1)]; yt1 = [t5(f"yt1_{i}", [128, 8, 128], BF16) for i in range(NB1)]
                h1s = [t5(f"h1s{i}", [128, D]) for i in range(NB1)]; big2 = [t5(f"big2_{i}", [128, D]) for i in range(NB1)]
                xn2_bs = [t5(f"xn2_b{i}", [128, D], BF16) for i in range(NB1)]; xn2T = [t5(f"xn2T{i}", [128, 8, 128]) for i in range(NB1)]
                scr = [dict(lgt=t5(f"lgt{i}", [128, 36]), rsm=t5(f"rsm{i}", [128, 32]), oh=[t5(f"oh{k}_{i}", [128, 32]) for k in range(2)],
                            cnt=t5(f"cnt{i}", [128, 32]), el=t5(f"el{i}", [128, 8]), mx8=t5(f"mx8_{i}", [128, 8]), ix8=t5(f"ix8_{i}", [128, 8], U32),
                            sm=t5(f"smb{i}", [128, 16])) for i in range(NB1)]
                carry = t5("carry", [1, 32])
                mset(carry, 0.0)
                dma(wffn_bc[:], nffn_d[0].partition_broadcast(128))
                dma(wr_f[:, :, 0:4], rgw_d[0].rearrange("(c p) e -> p c e", p=128))
                dma(wr_f[:, :, 4:36], rew_d[0].rearrange("(c p) e -> p c e", p=128))
                wout_v = wout_d[0].rearrange("(c p) f -> p c f", p=128)
                for c in range(8):
                    dma(wst[c % 2][:, :], wout_v[:, c, :])
                    cp(wout_b[:, c, :], wst[c % 2][:, :], e=('dve', 'act')[c % 2])

                def loads1b(i):
                    dma(xt1[i % NB1][:, :], x_d[(i - 1) * 128:i * 128, :])
                    dma(yt1[i % NB1][:, :, :].rearrange("p b t -> p (b t)"), yT_d[i * 128:(i + 1) * 128, :])
                def g_A(i):
                    b = i % NB1
                    xt = xt1[b]; yT_b = yt1[b]; h1 = h1s[b]; big = big2[b]; xn2_b = xn2_bs[b]; xT2 = xn2T[b]
                    Z = scr[b]; lgt = Z['lgt']; rsm = Z['rsm']; oh = Z['oh']; cnt = Z['cnt']; el = Z['el']; mx8 = Z['mx8']; ix8 = Z['ix8']; sm = Z['sm']
                    for n in range(2):
                        pm_ = psb()
                        for blk in range(8):
                            mm(pm_, yT_b[:, blk, :], wout_b[:, blk, n * 512:(n + 1) * 512], start=(blk == 0), stop=(blk == 7))
                        ttn(h1[:, n * 512:(n + 1) * 512], xt[:, n * 512:(n + 1) * 512], pm_, ALU.add)
                    dma(h1_d[i * 128:(i + 1) * 128, :], h1[:, :], q='pool')
                    yield
                    act(big[:], h1[:], AF.Square, accum=sm[:, 14:15])
                    act(sm[:, 15:16], sm[:, 14:15], AF.Sqrt, bias=1e-6, scale=1.0 / D)
                    recip(sm[:, 15:16], sm[:, 15:16])
                    stt(big[:], h1[:], sm[:, 15:16], wffn_bc[:], ALU.mult, ALU.mult)
                    cp(xn2_b[:], big[:], e='pool')
                    yield
                    for half in range(2):
                        pb = psb()
                        for j in range(4):
                            c = half * 4 + j
                            S.op('pe', lambda E, c=c, j=j, pb=pb, big=big: E.transpose(out=A(pb)[:, j * 128:(j + 1) * 128], in_=big[:, c * 128:(c + 1) * 128], identity=ident[:]),
                                 r=[big, ident], w=[pb])
                        cp(xT2[:, half * 4:half * 4 + 4, :], sub(pb, A(pb).rearrange("p (j t) -> p j t", t=128)), e='act')
                        yield
                    pl = psq(); pl36 = sub(pl, A(pl)[:, 0:36])
                    for c in range(8):
                        mm(pl36, xT2[:, c, :], wr_f[:, c, :], start=(c == 0), stop=(c == 7))
                    ttn(lgt[:], pl36, rb_bc[:], ALU.add)
                    yield
                    S.op('dve', lambda E: E.tensor_reduce(out=sm[:, 4:5], in_=lgt[:, 0:4], axis=AX.X, op=ALU.max, negate=True), r=[lgt], w=[sm])
                    act(rsm[:, 0:4], lgt[:, 0:4], AF.Exp, bias=sm[:, 4:5], accum=sm[:, 5:6])
                    recip(sm[:, 5:6], sm[:, 5:6])
                    yield
                    tsc(sm[:, 4:5], sm[:, 4:5], -1.0, None, ALU.mult)
                    tsc(rsm[:, 4:8], lgt[:, 0:4], sm[:, 4:5], None, ALU.is_equal)
                    yield
                    tsc(el[:], lgt[:, 4:12], rsm[:, 4:5], None, ALU.mult)
                    for g in range(1, 4):
                        stt(el[:], lgt[:, 4 + g * 8:12 + g * 8], rsm[:, 4 + g:5 + g], el[:], ALU.mult, ALU.add)
                    ttn(rsm[:, 8:12], rsm[:, 4:8], giota[:], ALU.mult)
                    S.op('dve', lambda E: E.tensor_reduce(out=sm[:, 6:7], in_=rsm[:, 8:12], axis=AX.X, op=ALU.add), r=[rsm], w=[sm])
                    yield
                    S.op('dve', lambda E: E.max(out=mx8[:], in_=el[:]), r=[el], w=[mx8])
                    S.op('dve', lambda E: E.max_index(out=ix8[:], in_max=mx8[:], in_values=el[:]), r=[mx8, el], w=[ix8])
                    yield
                    cp(sm[:, 8:10], ix8[:, 0:2])
                    tsc(sm[:, 8:10], sm[:, 8:10], sm[:, 6:7], None, ALU.add)
                    yield
                    ttn(sm[:, 10:11], mx8[:, 1:2], mx8[:, 0:1], ALU.subtract)
                    act(sm[:, 10:11], sm[:, 10:11], AF.Exp)
                    tsc(sm[:, 11:12], sm[:, 10:11], 1.0, None, ALU.add)
                    recip(sm[:, 11:12], sm[:, 11:12])
                    yield
                    ttn(gates_all[:, i, 0:1], sm[:, 5:6], sm[:, 11:12], ALU.mult)
                    ttn(gates_all[:, i, 1:2], gates_all[:, i, 0:1], sm[:, 10:11], ALU.mult)
                    for k in range(2):
                        tsc(oh[k][:], iota_f[:], sm[:, 8 + k:9 + k], None, ALU.is_equal)
                    ttn(cnt[:], oh[0][:], oh[1][:], ALU.add)
                    yield
                    yield

                def g_B(i):
                    b = i % NB1
                    xt = xt1[b]; yT_b = yt1[b]; h1 = h1s[b]; big = big2[b]; xn2_b = xn2_bs[b]; xT2 = xn2T[b]
                    Z = scr[b]; lgt = Z['lgt']; rsm = Z['rsm']; oh = Z['oh']; cnt = Z['cnt']; el = Z['el']; mx8 = Z['mx8']; ix8 = Z['ix8']; sm = Z['sm']
                    pp = psq(); pp32 = sub(pp, A(pp)[:, 0:32])
                    mm(pp32, msu[:], cnt[:], start=True, stop=False)
                    mm(pp32, ones[0:1, :], carry[0:1, :], start=False, stop=True)
                    for k in range(2):
                        ttn(rsm[:], oh[k][:], pp32, ALU.mult)
                        S.op('dve', lambda E, k=k: E.tensor_reduce(out=sm[:, 12 + k:13 + k], in_=rsm[:], axis=AX.X, op=ALU.add), r=[rsm], w=[sm])
                    tsc(sm[:, 12:14], sm[:, 12:14], float(CAP - 1), None, ALU.min)
                    stt(sm[:, 12:14], sm[:, 8:10], float(CAP), sm[:, 12:14], ALU.mult, ALU.add)
                    cp(slots_all[:, i, :], sm[:, 12:14])
                    yield
                    pc = psq(); pc32 = sub(pc, A(pc)[0:1, 0:32])
                    mm(pc32, ones[:, 0:1], cnt[:])
                    ttn(carry[0:1, :], carry[0:1, :], pc32, ALU.add)
                    yield
                    for k in range(2):
                        S.dma('pool', lambda E, k=k, i=i: E.indirect_dma_start(
                            out=xs_d[:, :], out_offset=bass.IndirectOffsetOnAxis(ap=slots_all[:, i, k:k + 1], axis=0),
                            in_=xn2_b[:, :], in_offset=None), r=[xn2_b, slots_all], w=['xs_scr'])
                    yield

                def g_tile(i):
                    loads1b(i)
                    yield
                    yield from g_A(i)
                    yield from g_B(i)
                active = []
                nxt_tile = 1
                rnd = 0
                while active or nxt_tile < NT:
                    if nxt_tile < NT and len(active) < NB1 and rnd % 4 == 0:
                        active.append(g_tile(nxt_tile)); nxt_tile += 1
                    for g in list(active):
                        try:
                            next(g)
                        except StopIteration:
                            active.remove(g)
                    rnd += 1
                S.barrier()
                chk('p1b')

            with ExitStack() as es3:
                t3 = lambda name, shape, dt=F32: T(es3, name, shape, dt)
                NSUB = CAP // 128
                wstg = [t3(f"ewstg{i}", [128, 2, D]) for i in range(6)]
                wgu_b = [t3(f"wgu_b{i}", [128, 8, D], BF16) for i in range(2)]
                wdn_b = [t3(f"wdn_b{i}", [128, 4, D], BF16) for i in range(2)]
                xsl = [t3(f"xsl{i}", [128, NSUB, D], BF16) for i in range(2)]
                xT = [t3(f"xT{i}", [128, 8, CAP], BF16) for i in range(2)]
                hT = t3("hT", [128, 4, CAP], BF16); gsl = t3("gsl", [128, CAP])
                ysl = [t3(f"ysl{i}", [128, D]) for i in range(2)]
                def wpiece(e, k):
                    g_ = e * 6 + k
                    s_ = wstg[g_ % 6]
                    if k < 4:
                        src = wgu_d[0, e].rearrange("(c p) f -> p c f", p=128)[:, 2 * k:2 * k + 2, :]
                        dst = wgu_b[e % 2][:, 2 * k:2 * k + 2, :]
                    else:
                        src = wdn_d[0, e].rearrange("(c p) f -> p c f", p=128)[:, 2 * (k - 4):2 * (k - 4) + 2, :]
                        dst = wdn_b[e % 2][:, 2 * (k - 4):2 * (k - 4) + 2, :]
                    return (lambda: dma(s_[:, :, :], src)), (lambda: cp(dst, s_[:, :, :], e=('dve', 'act')[g_ % 2]))

                def xload(e):
                    dma(xsl[e % 2][:, :, :], xs_d[e * CAP:(e + 1) * CAP, :].rearrange("(m p) f -> p m f", p=128), q='pool')

                for k in range(6):
                    d_, c_ = wpiece(0, k)
                    d_(); c_()
                xload(0)
                for e in range(NEXP):
                    Wg = wgu_b[e % 2]; Wd = wdn_b[e % 2]
                    X = xsl[e % 2]; XT = xT[e % 2]
                    if e + 1 < NEXP:
                        xload(e + 1)
                    steps = []

                    def st_tr(m, X=X, XT=XT):
                        for half in range(2):
                            pb = psb()
                            pbv = A(pb).bitcast(BF16)
                            for j in range(4):
                                c = half * 4 + j
                                S.op('pe', lambda E, c=c, j=j, m=m, pbv=pbv, X=X: E.transpose(out=pbv[:, j * 128:(j + 1) * 128], in_=X[:, m, c * 128:(c + 1) * 128], identity=identb[:]),
                                     r=[X, identb], w=[pb])
                            cp(XT[:, half * 4:half * 4 + 4, m * 128:(m + 1) * 128], sub(pb, pbv[:, 0:512].rearrange("p (j t) -> p j t", t=128)), e='act' if half else 'dve')

                    def st_gu(j, Wg=Wg, XT=XT):
                        pg = psb(); pu = psb()
                        for c in range(8):
                            mm(sub(pg, A(pg)[:, 0:CAP]), Wg[:, c, j * 128:(j + 1) * 128], XT[:, c, :], start=(c == 0), stop=(c == 7))
                        for c in range(8):
                            mm(sub(pu, A(pu)[:, 0:CAP]), Wg[:, c, 512 + j * 128:512 + (j + 1) * 128], XT[:, c, :], start=(c == 0), stop=(c == 7))
                        act(gsl[:, :], sub(pg, A(pg)[:, 0:CAP]), AF.Silu)
                        ttn(hT[:, j, :], gsl[:, :], sub(pu, A(pu)[:, 0:CAP]), ALU.mult)

                    def st_dn(m, Wd=Wd, e=e):
                        Y = ysl[m % 2]
                        for n in range(2):
                            py = psb()
                            for c in range(4):
                                mm(py, hT[:, c, m * 128:(m + 1) * 128], Wd[:, c, n * 512:(n + 1) * 512], start=(c == 0), stop=(c == 3))
                            cp(Y[:, n * 512:(n + 1) * 512], py, e='act' if n else 'dve')
                        dma(ys_d[e * CAP + m * 128:e * CAP + (m + 1) * 128, :], Y[:, :], q='pool')

                    for m in range(NSUB):
                        steps.append(lambda m=m: st_tr(m))
                    for j in range(4):
                        steps.append(lambda j=j: st_gu(j))
                    for m in range(NSUB):
                        steps.append(lambda m=m: st_dn(m))
                    assert len(steps) >= 6
                    casts = []
                    if e + 1 < NEXP:
                        for k in range(6):
                            d_, c_ = wpiece(e + 1, k)
                            d_()
                            casts.append(c_)
                    for si, stp in enumerate(steps):
                        stp()
                        if si < len(casts):
                            casts[si]()
                    for c_ in casts[len(steps):]:
                        c_()
                S.barrier()
                chk('p2')

            with ExitStack() as es4:
                t4 = lambda name, shape, dt=F32: T(es4, name, shape, dt)
                y0 = [t4(f"y0_{i}", [128, D]) for i in range(2)]; y1 = [t4(f"y1_{i}", [128, D]) for i in range(2)]
                hh = [t4(f"hh{i}", [128, D]) for i in range(2)]; jk = t4("jk", [128, D]); s4 = t4("s4", [128, 4])
                ob = [t4(f"ob{i}", [128, D]) for i in range(2)]
                wfin_bc = t4("wfin_bc", [128, D])
                dma(wfin_bc[:], nfin_d.partition_broadcast(128))
                def loads3(i):
                    b = i % 2
                    for k, yk in ((0, y0[b]), (1, y1[b])):
                        S.dma('pool', lambda E, k=k, i=i, yk=yk: E.indirect_dma_start(
                            out=yk[:, :], out_offset=None, in_=ys_d[:, :],
                            in_offset=bass.IndirectOffsetOnAxis(ap=slots_all[:, i, k:k + 1], axis=0)), r=['ys_scr', slots_all], w=[yk])
                    dma(hh[b][:, :], h1_d[i * 128:(i + 1) * 128, :])
                if NT > 1:
                    loads3(1)
                for i in range(1, NT):
                    b = i % 2
                    stt(hh[b][:], y0[b][:], gates_all[:, i, 0:1], hh[b][:], ALU.mult, ALU.add)
                    stt(hh[b][:], y1[b][:], gates_all[:, i, 1:2], hh[b][:], ALU.mult, ALU.add)
                    act(jk[:], hh[b][:], AF.Square, accum=s4[:, 0:1])
                    act(s4[:, 1:2], s4[:, 0:1], AF.Sqrt, bias=1e-6, scale=1.0 / D)
                    recip(s4[:, 1:2], s4[:, 1:2])
                    stt(ob[b][:], hh[b][:], s4[:, 1:2], wfin_bc[:], ALU.mult, ALU.mult)
                    if i + 1 < NT:
                        loads3(i + 1)
                    dma(out_d[(i - 1) * 128:i * 128, :], ob[b][:, :], is_out=True)
                S.finish()
        except Stop:
            S.finish()
        print("instr counts", S.total, "nsem", S.nsem)
    nc._dbg_map = dbg_map
    return nc


_NAMES = ['meta_tokens', 'norm_mix_w', 'norm_ffn_w', 'norm_final_w', 'w_in', 'w_out', 'rwkv_mu_rkv', 'rwkv_mu_wag',
          'rwkv_w0', 'rwkv_w_lora_a', 'rwkv_w_lora_b', 'rwkv_a0', 'rwkv_a_lora_a', 'rwkv_a_lora_b', 'rwkv_g_lora_a',
          'rwkv_g_lora_b', 'rwkv_k_k', 'rwkv_k_a', 'rwkv_r_k', 'rwkv_lnx_w', 'rwkv_lnx_b', 'mlstm_conv_w', 'mlstm_conv_b',
          'mlstm_gate_b', 'mlstm_norm_w', 'router_group_w', 'router_group_b', 'router_expert_w', 'router_expert_b',
          'expert_w_gate_up', 'expert_w_down']


def run(inputs, CAP=512, stop_after=None):
    x = np.asarray(inputs['x'], dtype=np.float32)
    B, L, _ = x.shape
    NT = L // 128 + 1
    nc = build(NT, CAP, stop_after=stop_after)
    shared = {n: np.ascontiguousarray(np.asarray(inputs[n], dtype=np.float32)) for n in _NAMES}
    in_maps = []
    for b in range(B):
        m = dict(shared)
        m['x'] = np.ascontiguousarray(x[b])
        in_maps.append(m)
    res = run_bass_kernel_spmd(nc, in_maps, core_ids=list(range(B)))
    if stop_after:
        d = np.asarray(res.results[0]['dbg'])
        return {k: d[0:v[2], v[0]:v[0] + v[1]] for k, v in nc._dbg_map.items()}
    return np.stack([np.asarray(r['out']).reshape(L, D) for r in res.results], axis=0).astype(np.float32)


def kernel(**inputs):
    return run(inputs, CAP=384)
```

```python
import math
import numpy as np
from contextlib import ExitStack
import concourse.bass as bass
import concourse.mybir as mybir
from concourse.bass_utils import run_bass_kernel_spmd

F32 = mybir.dt.float32
BF16 = mybir.dt.bfloat16
I32 = mybir.dt.int32
U32 = mybir.dt.uint32
AF = mybir.ActivationFunctionType
ALU = mybir.AluOpType
AX = mybir.AxisListType

D = 1024
DBG_TILE = 0
DIN = 3080
NEXP = 32
C0 = math.exp(-0.5)


class V:
    def __init__(self, ap, key):
        self.ap = ap
        self.key = key


def A(x):
    if isinstance(x, V):
        return x.ap
    if type(x).__name__.endswith('TensorHandle'):
        return x.ap()
    return x


def K(x):
    return x.key if isinstance(x, V) else x.name


class Sched:
    EPOCH = 20000

    def __init__(self, nc, es, n_dma_sems=32):
        self.nc = nc
        self.es = es
        self.eng = {'pe': nc.tensor, 'act': nc.scalar, 'dve': nc.vector,
                    'pool': nc.gpsimd, 'sp': nc.sync}
        self.sem = {}
        self.cnt = {}
        self.nsem = 0
        self.total = {k: 0 for k in self.eng}
        for k in self.eng:
            self._new_sem(k)
        self.waited = {k: {} for k in self.eng}
        self.res = {}
        self.dma_sems = [es.enter_context(nc.semaphore(f"dq{i}")) for i in range(n_dma_sems)]
        self.dma_cnt = [0] * n_dma_sems
        self.dma_rr = 0
        self.out_tokens = []

    def _new_sem(self, k):
        self.nsem += 1
        self.sem[k] = self.es.enter_context(self.nc.semaphore(f"s_{k}_{self.nsem}"))
        self.cnt[k] = 0

    def _need(self, reads, writes):
        need = []
        for key in reads:
            st = self.res.get(key)
            if st is not None and st['w'] is not None:
                need.append((st['w'], 'raw'))
        for key in writes:
            st = self.res.get(key)
            if st is not None:
                if st['w'] is not None:
                    need.append((st['w'], 'waw'))
                need.extend((t, 'war') for t in st['r'].values())
        return need

    import os as _os
    SAME_ENGINE_GAP = int(_os.environ.get('SE_GAP', 16))
    SKIP_WAX = int(_os.environ.get('SE_SKIPWAX', 1))
    SKIP_ENG = _os.environ.get('SE_ENG', 'dve,act,pool').split(',')

    def _emit_waits(self, e, need):
        for item in need:
            tok, kind = item if isinstance(item[0], tuple) else (item, 'raw')
            sem, val, src = tok[0], tok[1], tok[2]
            if src == 'pe' and e == 'pe':
                continue
            if src == e and src != 'dma':
                if e in self.SKIP_ENG:
                    if kind != 'raw' and self.SKIP_WAX:
                        continue
                    if kind == 'raw' and self.total[e] - tok[3] >= self.SAME_ENGINE_GAP:
                        continue
            w = self.waited[e]
            if w.get(id(sem), 0) >= val:
                continue
            self.eng[e].wait_ge(sem, val)
            w[id(sem)] = val

    def _record(self, tok, reads, writes):
        for key in reads:
            st = self.res.setdefault(key, {'w': None, 'r': {}})
            st['r'][id(tok[0])] = tok
        for key in writes:
            self.res[key] = {'w': tok, 'r': {}}

    def op(self, e, fn, r=(), w=()):
        r = [K(k) if not isinstance(k, (str, tuple)) else k for k in r]
        w = [K(k) if not isinstance(k, (str, tuple)) else k for k in w]
        w = w + [k for k in r if isinstance(k, str) and k.startswith('pb') and k not in w]
        if self.cnt[e] >= self.EPOCH:
            self._new_sem(e)
        self._emit_waits(e, self._need(r, w))
        inst = fn(self.eng[e])
        self.cnt[e] += 1
        self.total[e] += 1
        inst.then_inc(self.sem[e], 1)
        tok = (self.sem[e], self.cnt[e], e, self.total[e])
        self._record(tok, r, w)
        return tok

    def dma(self, q, fn, r=(), w=(), is_out=False):
        r = [K(k) if not isinstance(k, (str, tuple)) else k for k in r]
        w = [K(k) if not isinstance(k, (str, tuple)) else k for k in w]
        i = self.dma_rr
        self.dma_rr = (self.dma_rr + 1) % len(self.dma_sems)
        sem = self.dma_sems[i]
        need = self._need(r, w)
        if self.dma_cnt[i] > 0:
            need.append(((sem, 16 * self.dma_cnt[i], 'dma', 0), 'raw'))
        self._emit_waits(q, need)
        inst = fn(self.eng[q])
        self.dma_cnt[i] += 1
        inst.then_inc(sem, 16)
        tok = (sem, 16 * self.dma_cnt[i], 'dma', 0)
        self._record(tok, r, w)
        if is_out:
            self.out_tokens.append(tok)
        return tok

    def barrier(self):
        toks = [(self.sem[k], self.cnt[k], k, -10**9) for k in self.eng if self.cnt[k] > 0]
        toks += [(s, 16 * c, 'dma', 0) for s, c in zip(self.dma_sems, self.dma_cnt) if c > 0]
        for e in self.eng:
            self._emit_waits(e, [t for t in toks if t[2] != e])

    def finish(self):
        self._emit_waits('sp', self.out_tokens)
        self.barrier()


class Stop(Exception):
    pass


def inter(gens, weights=None):
    gens = list(gens)
    wts = {id(g): (weights[k] if weights else 1) for k, g in enumerate(gens)}
    while gens:
        for g in list(gens):
            for _ in range(wts[id(g)]):
                try:
                    next(g)
                except StopIteration:
                    gens.remove(g)
                    break
        yield


def build(NT, CAP, stop_after=None, dbgn=8192):
    nc = bass.Bass("TRN2", target_bir_lowering=False)
    NX = NT - 1
    dt_in = lambda name, shape: nc.dram_tensor(name, shape, F32, kind="ExternalInput").ap()
    x_d = dt_in("x", [NX * 128, D])
    meta_d = dt_in("meta_tokens", [16, D])
    nmix_d = dt_in("norm_mix_w", [1, D]); nffn_d = dt_in("norm_ffn_w", [1, D]); nfin_d = dt_in("norm_final_w", [D])
    win_d = dt_in("w_in", [1, D, DIN]); wout_d = dt_in("w_out", [1, D, D])
    murkv_d = dt_in("rwkv_mu_rkv", [1, 3, 512]); muwag_d = dt_in("rwkv_mu_wag", [1, 3, D])
    w0_d = dt_in("rwkv_w0", [1, 512]); wla_d = dt_in("rwkv_w_lora_a", [1, D, 64]); wlb_d = dt_in("rwkv_w_lora_b", [1, 64, 512])
    a0_d = dt_in("rwkv_a0", [1, 512]); ala_d = dt_in("rwkv_a_lora_a", [1, D, 64]); alb_d = dt_in("rwkv_a_lora_b", [1, 64, 512])
    gla_d = dt_in("rwkv_g_lora_a", [1, D, 160]); glb_d = dt_in("rwkv_g_lora_b", [1, 160, 512])
    kk_d = dt_in("rwkv_k_k", [1, 512]); ka_d = dt_in("rwkv_k_a", [1, 512]); rk_d = dt_in("rwkv_r_k", [1, 512])
    lnw_d = dt_in("rwkv_lnx_w", [1, 512]); lnb_d = dt_in("rwkv_lnx_b", [1, 512])
    cw_d = dt_in("mlstm_conv_w", [1, 4, 512]); cb_d = dt_in("mlstm_conv_b", [1, 512])
    gb_d = dt_in("mlstm_gate_b", [1, 8]); mnw_d = dt_in("mlstm_norm_w", [1, 512])
    rgw_d = dt_in("router_group_w", [1, D, 4]); rgb_d = dt_in("router_group_b", [1, 4])
    rew_d = dt_in("router_expert_w", [1, D, 32]); reb_d = dt_in("router_expert_b", [1, 32])
    wgu_d = dt_in("expert_w_gate_up", [1, NEXP, D, D]); wdn_d = dt_in("expert_w_down", [1, NEXP, 512, D])
    out_d = nc.dram_tensor("out", [NX * 128, D], F32, kind="ExternalOutput").ap()
    h1_d = nc.dram_tensor("h1_scr", [NT * 128, D], F32, kind="Internal").ap()
    xs_d = nc.dram_tensor("xs_scr", [NEXP * CAP, D], BF16, kind="Internal").ap()
    ys_d = nc.dram_tensor("ys_scr", [NEXP * CAP, D], F32, kind="Internal").ap()
    yT_d = nc.dram_tensor("yT_scr", [NT * 128, D], BF16, kind="Internal").ap()
    dbg_d = nc.dram_tensor("dbg", [128, dbgn], F32, kind="ExternalOutput").ap() if stop_after else None
    dbg_pos = [0]
    dbg_map = {}

    with ExitStack() as es0:
        S = Sched(nc, es0)

        def dump(name, ap, np_=128):
            if dbg_d is None:
                return
            n = ap.shape[-1] if len(ap.shape) == 2 else int(np.prod(ap.shape[1:]))
            dbg_map[name] = (dbg_pos[0], n, np_)
            S.dma('sp', lambda E: E.dma_start(out=dbg_d[0:np_, dbg_pos[0]:dbg_pos[0] + n], in_=ap, allow_slow_non_contiguous=True), r=[ap], w=['dbg'], is_out=True)
            dbg_pos[0] += n

        def chk(name):
            if stop_after == name:
                raise Stop()
        try:

            def mm(out, lhsT, rhs, start=True, stop=True):
                S.op('pe', lambda E: E.matmul(A(out), lhsT=A(lhsT), rhs=A(rhs), start=start, stop=stop),
                     r=[lhsT, rhs], w=[out])

            def act(out, in_, func, bias=None, scale=None, accum=None, e='act'):
                kw = {}
                rd = [in_]
                if bias is not None:
                    kw['bias'] = A(bias) if not isinstance(bias, float) else bias
                    if not isinstance(bias, float):
                        rd.append(bias)
                if scale is not None:
                    kw['scale'] = A(scale) if not isinstance(scale, float) else scale
                    if not isinstance(scale, float):
                        rd.append(scale)
                wr = [out]
                if accum is not None:
                    kw['accum_out'] = A(accum)
                    wr.append(accum)
                S.op('act', lambda E: E.activation(out=A(out), in_=A(in_), func=func, **kw), r=rd, w=wr)

            def tsc(out, in0, s1, s2, op0, op1=None, e='dve'):
                rd = [in0]
                a1 = s1
                a2 = s2
                if not isinstance(s1, (float, int)):
                    rd.append(s1); a1 = A(s1)
                if s2 is not None and not isinstance(s2, (float, int)):
                    rd.append(s2); a2 = A(s2)
                kw = {} if op1 is None else {'op1': op1}
                S.op(e, lambda E: E.tensor_scalar(out=A(out), in0=A(in0), scalar1=a1, scalar2=a2, op0=op0, **kw),
                     r=rd, w=[out])

            def ttn(out, a, b, op, e='dve'):
                S.op(e, lambda E: E.tensor_tensor(out=A(out), in0=A(a), in1=A(b), op=op), r=[a, b], w=[out])

            def stt(out, in0, sc, in1, op0, op1):
                rd = [in0, in1]
                a = sc
                if not isinstance(sc, (float, int)):
                    rd.append(sc); a = A(sc)
                S.op('dve', lambda E: E.scalar_tensor_tensor(out=A(out), in0=A(in0), scalar=a, in1=A(in1), op0=op0, op1=op1),
                     r=rd, w=[out])

            def cp(out, in_, e='dve'):
                if e == 'act':
                    S.op('act', lambda E: E.activation(out=A(out), in_=A(in_), func=AF.Copy), r=[in_], w=[out])
                else:
                    S.op(e, lambda E: E.tensor_copy(out=A(out), in_=A(in_)), r=[in_], w=[out])

            def mset(t, val, e='pool'):
                S.op(e, lambda E: E.memset(A(t), val), w=[t])

            def recip(out, in_):
                S.op('dve', lambda E: E.reciprocal(out=A(out), in_=A(in_)), r=[in_], w=[out])

            def dma(out, in_, q='sp', is_out=False):
                S.dma(q, lambda E: E.dma_start(out=A(out), in_=A(in_)), r=[in_], w=[out], is_out=is_out)

            banks = [es0.enter_context(nc.psum_tensor(f"pb{i}", [128, 512], F32)) for i in range(8)]
            st = {'q': 0, 'b': 0}

            def psq():
                i = st['q']; st['q'] = (i + 1) % 16
                b_, q_ = i % 4, (i // 4) % 4
                return V(banks[b_][:, q_ * 128:(q_ + 1) * 128], f"pb{b_}")

            def psb():
                i = st['b']; st['b'] = (i + 1) % 4
                return V(banks[4 + i][:, :], f"pbB{i}")

            def sub(v, ap):
                return V(ap, v.key) if isinstance(v, V) else ap

            T = lambda stack, name, shape, dt=F32: stack.enter_context(nc.sbuf_tensor(name, shape, dt))

            ones = T(es0, "ones", [128, 128]); ident = T(es0, "ident", [128, 128]); identb = T(es0, "identb", [128, 128], BF16)
            msu = T(es0, "msu", [128, 128]); msl = T(es0, "msl", [128, 128]); mst = T(es0, "mst", [128, 64])
            blk64 = T(es0, "blk64", [128, 128]); tric = T(es0, "tric", [128, 128]); maskc = T(es0, "maskc", [128, 128])
            mset(ones, 1.0)
            asel = lambda out, pat, cmp, base, cm, in_=None: S.op('pool', lambda E: E.affine_select(
                out=A(out), in_=A(in_ if in_ is not None else ones[:]), pattern=pat, compare_op=cmp, fill=0.0, base=base, channel_multiplier=cm),
                r=[in_ if in_ is not None else ones], w=[out])
            asel(ident[:], [[-1, 128]], ALU.is_equal, 0, 1)
            cp(identb[:], ident[:], e='pool')
            asel(msu[:], [[1, 128]], ALU.is_gt, 0, -1)
            asel(msl[:], [[-1, 128]], ALU.is_gt, 0, 1)
            asel(mst[0:64, :], [[1, 64]], ALU.is_ge, 0, -1, in_=ones[0:64, 0:64])
            asel(mst[64:128, :], [[1, 64]], ALU.is_ge, 0, -1, in_=ones[64:128, 0:64])
            mset(blk64, 0.0); mset(blk64[0:64, 0:64], 1.0); mset(blk64[64:128, 64:128], 1.0)
            asel(tric[:], [[1, 128]], ALU.is_ge, 0, -1)
            ttn(tric[:], tric[:], blk64[:], ALU.mult, e='pool')
            tsc(maskc[:], tric[:], 0.125, None, ALU.mult, e='pool')

            pstg = T(es0, "pstg", [128, 128]); pv = T(es0, "pv", [128, 128]); pv1 = T(es0, "pv1", [128, 128])
            mset(pstg, 0.0)
            row = {}
            rcur = [0]

            def ldrows(name, ap2d, n):
                row[name] = rcur[0]
                dma(pstg[rcur[0]:rcur[0] + n, :], ap2d)
                rcur[0] += n
            ldrows('nmix', nmix_d[0].rearrange("(c p) -> c p", p=128), 8)
            ldrows('muwag', muwag_d[0].rearrange("j (c p) -> (j c) p", p=128), 24)
            ldrows('murkv', murkv_d[0].rearrange("j (c p) -> (j c) p", p=128), 12)
            for nm, ap in (('w0', w0_d), ('a0', a0_d), ('kk', kk_d), ('ka', ka_d), ('rk', rk_d), ('lnw', lnw_d), ('lnb', lnb_d), ('cb', cb_d)):
                ldrows(nm, ap[0].rearrange("(c p) -> c p", p=128), 4)
            ldrows('cw', cw_d[0].rearrange("j (c p) -> (j c) p", p=128), 16)
            tp = psq()
            S.op('pe', lambda E: E.transpose(out=A(tp), in_=pstg[:], identity=ident[:]), r=[pstg, ident], w=[tp])
            cp(pv[:], tp, e='act')
            tsc(pv1[:], pv[:], -1.0, 1.0, ALU.mult, ALU.add)
            PV = lambda nm, j=0: pv[:, row[nm] + j:row[nm] + j + 1]
            PV1 = lambda nm, j=0: pv1[:, row[nm] + j:row[nm] + j + 1]

            mnw_bc = T(es0, "mnw_bc", [128, 512])
            gb_bc = T(es0, "gb_bc", [128, 8]); rb_bc = T(es0, "rb_bc", [128, 36]); iota_i = T(es0, "iota_i", [128, 32], I32)
            iota_f = T(es0, "iota_f", [128, 32]); giota = T(es0, "giota", [128, 4])

            dma(mnw_bc[:], mnw_d[0].partition_broadcast(128)); dma(gb_bc[:], gb_d[0].partition_broadcast(128))
            dma(rb_bc[:, 0:4], rgb_d[0].partition_broadcast(128)); dma(rb_bc[:, 4:36], reb_d[0].partition_broadcast(128))
            S.op('pool', lambda E: E.iota(iota_i[:], pattern=[[1, 32]], base=0, channel_multiplier=0), w=[iota_i])
            cp(iota_f[:], iota_i[:], e='pool')
            tsc(giota[:], iota_f[:, 0:4], 8.0, None, ALU.mult, e='pool')

            gates_all = T(es0, "gates_all", [128, NT, 2]); slots_all = T(es0, "slots_all", [128, NT, 2], I32)
            es1 = es0.enter_context(ExitStack())
            win_b = T(es1, "win_b", [128, 8, DIN], BF16)
            wla_b = T(es1, "wla_b", [128, 8, 288], BF16); wlamu_b = T(es1, "wlamu_b", [128, 8, 288], BF16)
            lb_b = T(es1, "lb_b", [128, 512], BF16); glb0_b = T(es1, "glb0_b", [128, 512], BF16); glb1_b = T(es1, "glb1_b", [32, 512], BF16)
            with ExitStack() as esl:
                stg = [T(esl, f"wstg{i}", [128, DIN]) for i in range(2)]
                win_v = win_d[0].rearrange("(c p) f -> p c f", p=128)
                ceng = ['dve', 'pool', 'act']
                k = 0
                for c in range(8):
                    s_ = stg[k % 2]
                    dma(s_[:, :], win_v[:, c, :])
                    cp(win_b[:, c, :], s_[:, :], e=ceng[k % 3]); k += 1
                s_ = stg[k % 2]; k += 1
                sv = s_[:, 0:8 * 288].rearrange("p (c j) -> p c j", j=288)
                dma(sv[:, :, 0:64], wla_d[0].rearrange("(c p) j -> p c j", p=128))
                dma(sv[:, :, 64:128], ala_d[0].rearrange("(c p) j -> p c j", p=128))
                dma(sv[:, :, 128:288], gla_d[0].rearrange("(c p) j -> p c j", p=128))
                cp(wla_b[:, :, :], sv, e='dve')
                for c in range(8):
                    for j, (lo, hi) in enumerate(((0, 64), (64, 128), (128, 288))):
                        tsc(wlamu_b[:, c, lo:hi], sv[:, c, lo:hi], PV('muwag', j * 8 + c), None, ALU.mult, e='dve' if c % 2 else 'pool')
                s_ = stg[k % 2]; k += 1
                dma(s_[0:64, 0:512], wlb_d[0]); dma(s_[64:128, 0:512], alb_d[0])
                dma(s_[:, 512:1024], glb_d[0][0:128, :]); dma(s_[0:32, 1024:1536], glb_d[0][128:160, :])
                cp(lb_b[:, :], s_[:, 0:512]); cp(glb0_b[:, :], s_[:, 512:1024]); cp(glb1_b[:, :], s_[0:32, 1024:1536])
                S.barrier()
            dump('pv', pv[:, :])
            chk('setup')

            es2 = es1.enter_context(ExitStack())
            t2 = lambda name, shape, dt=F32: T(es2, name, shape, dt)
            x_tm = [t2("x_tm0", [128, D])] * 2
            big = t2("big", [128, D])
            xnT_f = t2("xnT_f", [128, 8, 129]); xnT_b = t2("xnT_b", [128, 8, 128], BF16); xxT_b = t2("xxT_b", [128, 8, 128], BF16)
            rkv_raw = t2("rkv_raw", [128, 12, 129]); qk_raw = t2("qk_raw", [128, 4, 131])
            V1s = [t2(f"V1_{i}", [128, 4, 129]) for i in range(2)]; sigos = [t2(f"sigo{i}", [128, 512]) for i in range(2)]
            laT = t2("laT", [128, 128], BF16); lg0 = t2("lg0", [128, 128], BF16); lg1 = t2("lg1", [32, 128], BF16)
            sgw = t2("sgw", [128, 4, 128]); asig = t2("asig", [128, 4, 128]); ggs = [t2(f"gg{i}", [128, 4, 128]) for i in range(2)]
            bonuss = [t2(f"bonus{i}", [128, 4, 128]) for i in range(2)]; y_as = [t2(f"y_a{i}", [128, 4, 128]) for i in range(2)]
            ptA = [t2(f"ptA{i}", [128, 128]) for i in range(2)]; ptB = [t2(f"ptB{i}", [128, 128]) for i in range(2)]
            yT_bs = [t2(f"yT_b{i}", [128, 8, 128], BF16) for i in range(2)]
            ssq = t2("ssq", [128, 4]); rstd = t2("rstd", [128, 4])
            NR = 4
            rt = {nm: [t2(f"{nm}{i}", [128, 128]) for i in range(NR)] for nm in
                  ('r', 'k', 'v', 'kkn', 'k2', 'bv', 'tA', 'tB', 'cs', 'E1', 'E2', 'E3')}
            opz = [[t2(f"opz{fb}_{c}", [128, 4, 128], BF16) for c in range(2)] for fb in range(4)]
            rst = [[t2(f"rst{fb}_{c}", [128, 64], BF16) for c in range(2)] for fb in range(4)]
            gC = t2("gC", [128, 4, 2])
            ArbArk = t2("alg_ArbArk", [128, 2, 4, 64], BF16)
            Gc = [{nm: t2(f"alg{c}_{nm}", [128, 4, 128], BF16) for nm in
                   ('BzT', 'KzT', 'VzT', 'Aak', 'Q0', 'Q1', 'QT0', 'QT1', 'P0', 'P1', 'PT0', 'PT1')} for c in range(2)]
            GS = {nm: t2(f"alg_{nm}", [128, 4, 128], BF16) for nm in ('W0T', 'UT')}
            ArbArks = [ArbArk, t2("alg_ArbArk1", [128, 2, 4, 64], BF16)]
            ST32 = t2("ST32", [128, 4, 128])
            STb = t2("STb", [128, 4, 128], BF16)
            QKf = t2("QKf", [128, 4, 128]); cacc = QKf
            g8s = [t2(f"g8_{i}", [128, 8]) for i in range(2)]; th8 = t2("th8", [128, 8]); nbg = t2("nbg", [128, 8]); wgt = t2("wgt", [128, 4]); dbias = t2("dbias", [128, 4])
            lfb = [t2(f"lfb{i}", [128, 128]) for i in range(2)]; Dm = [t2(f"Dm{i}", [128, 128]) for i in range(2)]
            eB = [t2(f"eB{i}", [128, 128]) for i in range(2)]; Pm = [t2(f"Pm{i}", [128, 128]) for i in range(2)]
            Qz = [t2(f"Qz{h}", [128, 2, 128]) for h in range(4)]
            Kw = t2("Kw", [128, 4, 64])
            CTa = [t2(f"CTa{h}", [128, 129]) for h in range(4)]; CTb = [t2(f"CTb{h}", [128, 129]) for h in range(4)]
            hraw = [t2(f"hraw{i}", [128, 128]) for i in range(2)]; y_b = t2("y_b", [128, 512])
            sm = t2("sm", [128, 16])

            mset(xnT_f[:, :, 0:1], 0.0); mset(rkv_raw[:, :, 0:1], 0.0); mset(qk_raw[:, :, 0:3], 0.0)
            mset(V1s[0][:, :, 128:129], 1.0); mset(V1s[1][:, :, 128:129], 1.0)
            mset(ST32, 0.0); mset(STb, 0.0)
            for fb in range(4):
                for c in range(2):
                    mset(opz[fb][c], 0.0)
            for h in range(4):
                mset(Qz[h], 0.0); mset(CTa[h], 0.0); mset(CTb[h], 0.0)

            def g_front(i):
                xt = x_tm[i % 2]; V1 = V1s[i % 2]; sigo = sigos[i % 2]; g8 = g8s[i % 2]; gg = ggs[i % 2]
                if i == 0:
                    mset(xt, 0.0)
                    dma(xt[112:128, :], meta_d)
                else:
                    dma(xt[:, :], x_d[(i - 1) * 128:i * 128, :])
                act(big[:], xt[:], AF.Square, accum=ssq[:, 0:1])
                act(rstd[:, 0:1], ssq[:, 0:1], AF.Sqrt, bias=1e-6, scale=1.0 / D)
                recip(rstd[:, 0:1], rstd[:, 0:1])
                tsc(big[:], xt[:], rstd[:, 0:1], None, ALU.mult)
                yield
                for half in range(2):
                    pb = psb()
                    for j in range(4):
                        c = half * 4 + j
                        S.op('pe', lambda E, c=c, j=j, pb=pb: E.transpose(out=A(pb)[:, j * 128:(j + 1) * 128], in_=big[:, c * 128:(c + 1) * 128], identity=ident[:]),
                             r=[big, ident], w=[pb])
                    for j in range(4):
                        c = half * 4 + j
                        if j % 2 == 0:
                            act(xnT_f[:, c, 1:129], sub(pb, A(pb)[:, j * 128:(j + 1) * 128]), AF.Copy, scale=PV('nmix', c))
                        else:
                            tsc(xnT_f[:, c, 1:129], sub(pb, A(pb)[:, j * 128:(j + 1) * 128]), PV('nmix', c), None, ALU.mult)
                    yield
                cp(xnT_b[:, :, :], xnT_f[:, :, 1:129], e='pool')
                ttn(xxT_b[:, :, :], xnT_f[:, :, 0:128], xnT_f[:, :, 1:129], ALU.subtract)
                cp(xnT_f[:, :, 0:1], xnT_f[:, :, 128:129], e='pool')
                yield

                for blk in range(16):
                    p_ = psq()
                    for c in range(8):
                        mm(p_, win_b[:, c, blk * 128:(blk + 1) * 128], xnT_b[:, c, :], start=(c == 0), stop=(c == 7))
                    if blk < 12:
                        cp(rkv_raw[:, blk, 1:129], p_, e='act')
                    else:
                        cp(qk_raw[:, blk - 12, 3:131], p_, e='act')
                    yield
                pv_ = psb()
                for c in range(8):
                    mm(pv_, xnT_b[:, c, :], win_b[:, c, 2048:2560], start=(c == 0), stop=(c == 7))
                cp(V1[:, :, 0:128], sub(pv_, A(pv_).rearrange("p (h v) -> p h v", v=128)), e='act')
                yield
                po_ = psb()
                for c in range(8):
                    mm(po_, xnT_b[:, c, :], win_b[:, c, 2560:3072], start=(c == 0), stop=(c == 7))
                act(sigo[:], po_, AF.Sigmoid)
                yield
                pg_ = psq()
                pg8 = sub(pg_, A(pg_)[:, 0:8])
                for c in range(8):
                    mm(pg8, xnT_b[:, c, :], win_b[:, c, 3072:3080], start=(c == 0), stop=(c == 7))
                ttn(g8[:], pg8, gb_bc[:], ALU.add)
                yield
                la_ps = []
                for (lo, hi) in ((0, 128), (128, 256), (256, 288)):
                    p_ = psq()
                    po = sub(p_, A(p_)[0:hi - lo, :])
                    for c in range(8):
                        mm(po, wla_b[:, c, lo:hi], xnT_b[:, c, :], start=(c == 0), stop=False)
                    for c in range(8):
                        mm(po, wlamu_b[:, c, lo:hi], xxT_b[:, c, :], start=False, stop=(c == 7))
                    la_ps.append(p_)
                act(laT[0:64, :], sub(la_ps[0], A(la_ps[0])[0:64, :]), AF.Tanh)
                cp(laT[64:128, :], sub(la_ps[0], A(la_ps[0])[64:128, :]), e='dve')
                act(lg0[:, :], la_ps[1], AF.Sigmoid)
                act(lg1[:, :], sub(la_ps[2], A(la_ps[2])[0:32, :]), AF.Sigmoid)
                yield
                for fb in range(4):
                    fs = slice(fb * 128, (fb + 1) * 128)
                    p_ = psq(); mm(p_, lb_b[0:64, fs], laT[0:64, :])
                    act(sgw[:, fb, :], p_, AF.Sigmoid, bias=PV('w0', fb))
                    p_ = psq(); mm(p_, lb_b[64:128, fs], laT[64:128, :])
                    act(asig[:, fb, :], p_, AF.Sigmoid, bias=PV('a0', fb))
                    p_ = psq(); mm(p_, glb0_b[:, fs], lg0[:, :], start=True, stop=False); mm(p_, glb1_b[0:32, fs], lg1[0:32, :], start=False, stop=True)
                    cp(gg[:, fb, :], p_, e='dve')
                    yield

                yield

            pending = []
            for i in range(NT):
                xt = x_tm[i % 2]; V1 = V1s[i % 2]; sigo = sigos[i % 2]; g8 = g8s[i % 2]; gg = ggs[i % 2]
                yT_b = yT_bs[i % 2]; bonus = bonuss[i % 2]; y_a = y_as[i % 2]
                if i == 0:
                    for _ in g_front(0):
                        pass
                def g_prep(fb):
                    R = {nm: rt[nm][fb % NR] for nm in rt}
                    for nm, bi in (('r', 0), ('k', 1), ('v', 2)):
                        blk = bi * 4 + fb
                        tsc(R['tA'][:], rkv_raw[:, blk, 0:128], PV('murkv', bi * 4 + fb), None, ALU.mult, e='pool')
                        stt(R[nm][:], rkv_raw[:, blk, 1:129], PV1('murkv', bi * 4 + fb), R['tA'][:], ALU.mult, ALU.add)
                        yield
                    tsc(R['kkn'][:], R['k'][:], PV('kk', fb), None, ALU.mult)
                    ttn(R['tA'][:], R['kkn'][:], R['kkn'][:], ALU.mult, e='pool')
                    p_ = psq(); mm(p_, blk64[:], R['tA'][:])
                    act(R['tB'][:], p_, AF.Sqrt)
                    yield
                    tsc(R['tB'][:], R['tB'][:], 1e-12, None, ALU.max)
                    recip(R['tB'][:], R['tB'][:])
                    yield
                    ttn(R['kkn'][:], R['kkn'][:], R['tB'][:], ALU.mult)
                    tsc(R['tA'][:], asig[:, fb, :], PV('ka', fb), PV1('ka', fb), ALU.mult, ALU.add)
                    ttn(R['k2'][:], R['k'][:], R['tA'][:], ALU.mult)
                    ttn(R['bv'][:], R['kkn'][:], asig[:, fb, :], ALU.mult, e='pool')
                    yield
                    stt(R['tA'][:], R['r'][:], PV('rk', fb), R['k2'][:], ALU.mult, ALU.mult)
                    p_ = psq(); mm(p_, blk64[:], R['tA'][:])
                    ttn(bonus[:, fb, :], p_, R['v'][:], ALU.mult)
                    yield
                    for c in range(2):
                        cs_ = slice(c * 64, (c + 1) * 64)
                        S.op('dve', lambda E, fb=fb, cs_=cs_, R=R: E.tensor_tensor_scan(out=R['cs'][:, cs_], data0=ones[:, 0:64], data1=sgw[:, fb, cs_], initial=0.0, op0=ALU.mult, op1=ALU.add),
                             r=[ones, sgw], w=[R['cs']])
                        yield
                    act(R['E1'][:], R['cs'][:], AF.Exp, scale=-C0)
                    act(R['E2'][:], R['cs'][:], AF.Exp, scale=C0)
                    yield
                    ttn(R['tB'][:], R['cs'][:], sgw[:, fb, :], ALU.subtract, e='pool')
                    act(R['E3'][:], R['tB'][:], AF.Exp, scale=-C0)
                    yield
                    for c in range(2):
                        O = opz[fb][c]
                        for hh in range(2):
                            ps_ = slice(hh * 64, (hh + 1) * 64)
                            ts_ = slice(c * 64, (c + 1) * 64)
                            os_ = slice(hh * 64, (hh + 1) * 64)
                            stt(O[ps_, 0, os_], R['kkn'][ps_, ts_], -1.0, R['E3'][ps_, ts_], ALU.mult, ALU.mult)
                            ttn(O[ps_, 1, os_], R['bv'][ps_, ts_], R['E2'][ps_, ts_], ALU.mult)
                            ttn(O[ps_, 2, os_], R['k2'][ps_, ts_], R['E2'][ps_, ts_], ALU.mult, e='pool')
                            cp(O[ps_, 3, os_], R['v'][ps_, ts_], e='pool')
                            yield
                        ttn(rst[fb][c][:, :], R['r'][:, c * 64:(c + 1) * 64], R['E1'][:, c * 64:(c + 1) * 64], ALU.mult)
                        cp(gC[:, fb, c:c + 1], R['E1'][:, c * 64 + 63:c * 64 + 64], e='pool')
                    yield

                def q4(bank):
                    return sub(bank, A(bank).rearrange("p (f t) -> p f t", t=128))
                bc4 = lambda m: m[:, :].unsqueeze(1).broadcast_to([128, 4, 128])
                TinvOf = {}
                def g_alg(c):
                    Az = [opz[fb][c][:, 0, :] for fb in range(4)]; Bz = [opz[fb][c][:, 1, :] for fb in range(4)]
                    Kz = [opz[fb][c][:, 2, :] for fb in range(4)]; Vz = [opz[fb][c][:, 3, :] for fb in range(4)]
                    Rs = [rst[fb][c][:, :] for fb in range(4)]
                    for kk_i, (nm, src) in enumerate((('BzT', Bz), ('KzT', Kz), ('VzT', Vz))):
                        bk = psb()
                        for fb in range(4):
                            mm(sub(bk, A(bk)[:, fb * 128:(fb + 1) * 128]), src[fb], identb[:])
                        cp(Gc[c][nm][:, :, :], q4(bk), e='act' if kk_i != 1 else 'dve')
                        yield
                    for nm, l_, r_, msk in (('Q0', Bz, Az, msu), ('QT0', Az, Bz, msl), ('Aak', Kz, Az, msu)):
                        bk = psb()
                        for fb in range(4):
                            mm(sub(bk, A(bk)[:, fb * 128:(fb + 1) * 128]), l_[fb], r_[fb])
                        ttn(Gc[c][nm][:, :, :], q4(bk), bc4(msk), ALU.mult)
                        yield
                    bk = psb()
                    for j, l_ in enumerate((Bz, Kz)):
                        for fb in range(4):
                            o0 = j * 256 + fb * 64
                            mm(sub(bk, A(bk)[:, o0:o0 + 64]), l_[fb], Rs[fb])
                    ttn(ArbArks[c][:, :, :, :].rearrange("p j f t -> p (j f) t"), sub(bk, A(bk).rearrange("p (g t) -> p g t", t=64)),
                        mst[:, :].unsqueeze(1).broadcast_to([128, 8, 64]), ALU.mult)
                    yield
                    ttn(Gc[c]['P0'][:, :, :], Gc[c]['Q0'][:, :, :], identb[:, :].unsqueeze(1).broadcast_to([128, 4, 128]), ALU.add, e='pool')
                    ttn(Gc[c]['PT0'][:, :, :], Gc[c]['QT0'][:, :, :], identb[:, :].unsqueeze(1).broadcast_to([128, 4, 128]), ALU.add, e='pool')
                    yield
                    Qb = [Gc[c]['Q0'], Gc[c]['Q1']]; QTb = [Gc[c]['QT0'], Gc[c]['QT1']]
                    Pb = [Gc[c]['P0'], Gc[c]['P1']]; PTb = [Gc[c]['PT0'], Gc[c]['PT1']]
                    for s_ in range(6):
                        Qs, QTs = Qb[s_ % 2], QTb[s_ % 2]
                        Qn, QTn = Qb[(s_ + 1) % 2], QTb[(s_ + 1) % 2]
                        Pp, PTp = Pb[(s_ - 1) % 2], PTb[(s_ - 1) % 2]
                        Pc, PTc = Pb[s_ % 2], PTb[s_ % 2]
                        todo = []
                        if s_ <= 4:
                            bk = psb()
                            for fb in range(4):
                                mm(sub(bk, A(bk)[:, fb * 128:(fb + 1) * 128]), QTs[:, fb, :], Qs[:, fb, :])
                            todo.append(lambda bk=bk: cp(Qn[:, :, :], q4(bk), e='act'))
                        if s_ <= 3:
                            bk = psb()
                            for fb in range(4):
                                mm(sub(bk, A(bk)[:, fb * 128:(fb + 1) * 128]), Qs[:, fb, :], QTs[:, fb, :])
                            todo.append(lambda bk=bk: cp(QTn[:, :, :], q4(bk), e='act'))
                        if s_ >= 1:
                            bk = psb()
                            for fb in range(4):
                                mm(sub(bk, A(bk)[:, fb * 128:(fb + 1) * 128]), PTp[:, fb, :], Qs[:, fb, :])
                            todo.append(lambda bk=bk: ttn(Pc[:, :, :], q4(bk), Pp[:, :, :], ALU.add))
                        if 1 <= s_ <= 4:
                            bk = psb()
                            for fb in range(4):
                                mm(sub(bk, A(bk)[:, fb * 128:(fb + 1) * 128]), Qs[:, fb, :], PTp[:, fb, :])
                            todo.append(lambda bk=bk: ttn(PTc[:, :, :], q4(bk), PTp[:, :, :], ALU.add))
                        for f_ in todo:
                            f_()
                        yield
                    cur = 1
                    TinvOf[c] = Gc[c][f'P{cur}']
                    yield
                def g_chain(c):
                    Az = [opz[fb][c][:, 0, :] for fb in range(4)]; Bz = [opz[fb][c][:, 1, :] for fb in range(4)]
                    Kz = [opz[fb][c][:, 2, :] for fb in range(4)]; Vz = [opz[fb][c][:, 3, :] for fb in range(4)]
                    Rs = [rst[fb][c][:, :] for fb in range(4)]
                    bk = psb()
                    for fb in range(4):
                        o_ = sub(bk, A(bk)[:, fb * 128:(fb + 1) * 128])
                        mm(o_, Az[fb], STb[:, fb, :], start=True, stop=False); mm(o_, Gc[c]['Aak'][:, fb, :], Gc[c]['VzT'][:, fb, :], start=False, stop=True)
                    cp(GS['W0T'][:, :, :], q4(bk), e='act')
                    yield
                    bk = psb()
                    for fb in range(4):
                        mm(sub(bk, A(bk)[:, fb * 128:(fb + 1) * 128]), TinvOf[c][:, fb, :], GS['W0T'][:, fb, :])
                    cp(GS['UT'][:, :, :], q4(bk), e='act')
                    yield
                    bk = psb()
                    for fb in range(4):
                        o_ = sub(bk, A(bk)[:, fb * 64:(fb + 1) * 64])
                        mm(o_, STb[:, fb, :], Rs[fb], start=True, stop=False)
                        mm(o_, GS['UT'][:, fb, :], ArbArks[c][:, 0, fb, :], start=False, stop=False)
                        mm(o_, Gc[c]['VzT'][:, fb, :], ArbArks[c][:, 1, fb, :], start=False, stop=True)
                    cp(y_a[:, :, c * 64:(c + 1) * 64], sub(bk, A(bk)[:, 0:256].rearrange("p (f t) -> p f t", t=64)), e='act')
                    yield
                    bk = psb()
                    for fb in range(4):
                        o_ = sub(bk, A(bk)[:, fb * 128:(fb + 1) * 128])
                        mm(o_, Gc[c]['BzT'][:, fb, :], GS['UT'][:, fb, :], start=True, stop=False)
                        mm(o_, Gc[c]['KzT'][:, fb, :], Gc[c]['VzT'][:, fb, :], start=False, stop=True)
                    ttn(ST32[:, :, :], ST32[:, :, :], q4(bk), ALU.add)
                    ttn(ST32[:, :, :], ST32[:, :, :], gC[:, :, c:c + 1].broadcast_to([128, 4, 128]), ALU.mult)
                    cp(STb[:, :, :], ST32[:, :, :], e='pool')
                    yield
                    yield

                def g_post(fb, y_a=y_a, bonus=bonus, gg=gg, yT_b=yT_b):
                    R = {'tA': ptA[fb % 2], 'tB': ptB[fb % 2]}
                    p1 = psq(); mm(p1, blk64[:], y_a[:, fb, :])
                    ttn(R['tA'][:], y_a[:, fb, :], y_a[:, fb, :], ALU.mult, e='pool')
                    p2 = psq(); mm(p2, blk64[:], R['tA'][:])
                    act(R['tB'][:], p1, AF.Copy, scale=1.0 / 64)
                    ttn(R['tA'][:], R['tB'][:], R['tB'][:], ALU.mult)
                    stt(R['tA'][:], p2, 1.0 / 64, R['tA'][:], ALU.mult, ALU.subtract)
                    yield
                    act(R['tA'][:], R['tA'][:], AF.Sqrt, bias=64e-5)
                    yield
                    recip(R['tA'][:], R['tA'][:])
                    yield
                    ttn(R['tB'][:], y_a[:, fb, :], R['tB'][:], ALU.subtract)
                    ttn(R['tB'][:], R['tB'][:], R['tA'][:], ALU.mult)
                    tsc(R['tB'][:], R['tB'][:], PV('lnw', fb), PV('lnb', fb), ALU.mult, ALU.add)
                    ttn(R['tB'][:], R['tB'][:], bonus[:, fb, :], ALU.add)
                    ttn(yT_b[:, fb, :], R['tB'][:], gg[:, fb, :], ALU.mult)
                    yield

                def g_mlstm_pre():
                    for t_ in range(4):
                        for jb in range(4):
                            kj = ('QKf', jb)
                            if t_ == 0:
                                S.op('dve', lambda E, jb=jb: E.tensor_scalar(out=cacc[:, jb, :], in0=qk_raw[:, jb, 0:128], scalar1=PV('cw', jb), scalar2=PV('cb', jb), op0=ALU.mult, op1=ALU.add),
                                     r=[qk_raw, pv], w=[kj, 'QKf'])
                            else:
                                S.op('dve', lambda E, jb=jb, t_=t_: E.scalar_tensor_tensor(out=cacc[:, jb, :], in0=qk_raw[:, jb, t_:t_ + 128], scalar=PV('cw', t_ * 4 + jb), in1=cacc[:, jb, :], op0=ALU.mult, op1=ALU.add),
                                     r=[qk_raw, pv, kj], w=[kj, 'QKf'])
                    S.op('act', lambda E: E.activation(out=QKf[:, :, :], in_=cacc[:, :, :], func=AF.Silu), r=[('QKf', jb) for jb in range(4)] + ['QKf'], w=['QKf'] + [('QKf', jb) for jb in range(4)])
                    yield
                    cp(qk_raw[:, :, 0:3], qk_raw[:, :, 128:131], e='pool')
                    act(th8[:], g8[:], AF.Tanh, scale=1.0 / 15.0)
                    tsc(g8[:, 0:4], th8[:, 0:4], 15.0, None, ALU.mult)
                    act(g8[:, 4:8], th8[:, 4:8], AF.Exp, scale=-15.0)
                    act(g8[:, 4:8], g8[:, 4:8], AF.Ln, bias=1.0)
                    yield
                    if i == 0:
                        mset(g8[0:112, 0:4], -1.0e4, e='dve'); mset(g8[0:112, 4:8], 0.0, e='dve')
                    pn = psq()
                    pn4 = sub(pn, A(pn)[:, 0:4]); pn8 = sub(pn, A(pn)[:, 4:8])
                    mm(pn4, tric[:], g8[:, 4:8]); mm(pn8, blk64[:], g8[:, 4:8])
                    cp(nbg[:], sub(pn, A(pn)[:, 0:8]), e='act')
                    yield
                    ttn(dbias[:], g8[:, 0:4], nbg[:, 0:4], ALU.add)
                    ttn(wgt[:], dbias[:], nbg[:, 4:8], ALU.subtract)
                    act(wgt[:], wgt[:], AF.Exp, bias=math.log(0.125))
                    yield
                    for kb in range(2):
                        p_ = psq()
                        S.op('pe', lambda E, kb=kb, p_=p_: E.transpose(out=A(p_), in_=QKf[:, 2 + kb, :], identity=ident[:]), r=[QKf, ident], w=[p_])
                        for hh in range(2):
                            h = kb * 2 + hh
                            tsc(Kw[:, h, :], sub(p_, A(p_)[:, hh * 64:(hh + 1) * 64]), wgt[:, h:h + 1], None, ALU.mult)
                    yield

                def g_mlstm_rest():
                    def g_head(h):
                        hb = (h % 2) * 64
                        hs = slice(hb, hb + 64)
                        qb = h // 2
                        L_, D_, E_, P_ = lfb[h % 2], Dm[h % 2], eB[h % 2], Pm[h % 2]
                        tsc(L_[:], ones[:], g8[:, 4 + h:5 + h], None, ALU.mult, e='pool')
                        pbr = psq(); mm(pbr, L_[:], tric[:])
                        act(D_[:], pbr, AF.Exp, bias=dbias[:, h:h + 1], scale=-1.0)
                        act(E_[:], pbr, AF.Exp, scale=-1.0)
                        yield
                        psc = psq(); mm(psc, QKf[hs, 2 + qb, :], QKf[hs, qb, :])
                        ttn(D_[:], D_[:], maskc[:], ALU.mult, e='pool')
                        ttn(P_[:], D_[:], psc, ALU.mult)
                        yield
                        ttn(Qz[h][hs, 0, 0:64], QKf[hs, qb, 0:64], E_[hs, 0:64], ALU.mult)
                        ttn(Qz[h][hs, 1, 64:128], QKf[hs, qb, 64:128], E_[hs, 64:128], ALU.mult)
                        pU = psb()
                        u0 = sub(pU, A(pU)[hs, 0:129]); u1 = sub(pU, A(pU)[hs, 256:385])
                        mm(u0, Kw[0:64, h, :], V1[0:64, h, :])
                        stt(CTb[h][hs, :], CTa[h][hs, :], E_[hs, 63:64], u0, ALU.mult, ALU.add)
                        yield
                        pO = psb(); o_ = sub(pO, A(pO)[:, 0:129])
                        mm(o_, P_[:], V1[:, h, :], start=True, stop=False)
                        mm(o_, Qz[h][hs, 0, :], CTa[h][hs, :], start=False, stop=False)
                        mm(o_, Qz[h][hs, 1, :], CTb[h][hs, :], start=False, stop=True)
                        mm(u1, Kw[64:128, h, :], V1[64:128, h, :])
                        stt(CTa[h][hs, :], CTb[h][hs, :], E_[hs, 127:128], u1, ALU.mult, ALU.add)
                        if i > 0:
                            H_ = hraw[h % 2]
                            act(sm[:, 3 * h:3 * h + 1], sub(pO, A(pO)[:, 128:129]), AF.Abs)
                            tsc(sm[:, 3 * h:3 * h + 1], sm[:, 3 * h:3 * h + 1], 1.0, None, ALU.max)
                            recip(sm[:, 3 * h:3 * h + 1], sm[:, 3 * h:3 * h + 1])
                            tsc(H_[:], sub(pO, A(pO)[:, 0:128]), sm[:, 3 * h:3 * h + 1], None, ALU.mult)
                            act(P_[:], H_[:], AF.Square, accum=sm[:, 3 * h + 1:3 * h + 2])
                            yield
                            act(sm[:, 3 * h + 2:3 * h + 3], sm[:, 3 * h + 1:3 * h + 2], AF.Sqrt, bias=1e-6, scale=1.0 / 128)
                            recip(sm[:, 3 * h + 2:3 * h + 3], sm[:, 3 * h + 2:3 * h + 3])
                            yield
                            stt(H_[:], H_[:], sm[:, 3 * h + 2:3 * h + 3], mnw_bc[:, h * 128:(h + 1) * 128], ALU.mult, ALU.mult)
                            ttn(y_b[:, h * 128:(h + 1) * 128], H_[:], sigo[:, h * 128:(h + 1) * 128], ALU.mult)
                        yield
                    yield from inter([g_head(0), g_head(1)])
                    yield from inter([g_head(2), g_head(3)])
                    if i == 0:
                        return
                    for h in range(4):
                        p_ = psq()
                        S.op('pe', lambda E, h=h, p_=p_: E.transpose(out=A(p_), in_=y_b[:, h * 128:(h + 1) * 128], identity=ident[:]), r=[y_b, ident], w=[p_])
                        cp(yT_b[:, 4 + h, :], p_, e='act')
                    yield

                def g_rwkv_prep():
                    yield from inter([g_prep(fb) for fb in range(4)])
                    cp(rkv_raw[:, :, 0:1], rkv_raw[:, :, 128:129], e='pool')
                    yield

                def g_rwkv_rest():
                    yield from inter([g_alg(0), g_alg(1)])
                    yield from g_chain(0)
                    yield from g_chain(1)

                def g_post_all(i=i, yT_b=yT_b, posts=[g_post(fb) for fb in range(4)]):
                    yield from inter(posts[0:2])
                    yield from inter(posts[2:4])
                    dma(yT_d[i * 128:(i + 1) * 128, :], yT_b[:, :, :].rearrange("p b t -> p (b t)"))
                    yield
                for _ in inter([g_rwkv_prep(), g_mlstm_pre()] + pending):
                    pass
                pending = []
                streams = [g_rwkv_rest(), g_mlstm_rest()]
                if i + 1 < NT:
                    streams.append(g_front(i + 1))
                for _ in inter(streams, weights=[2, 1, 1][:len(streams)]):
                    pass
                if i > 0:
                    pending = [g_post_all()]
            for _ in inter(pending):
                pass

            S.barrier()
            chk('p1')
            es2.close()
            es1.close()

            with ExitStack() as es5:
                t5 = lambda name, shape, dt=F32: T(es5, name, shape, dt)
                NB1 = 4
                wout_b = t5("wout_b", [128, 8, D], BF16); wr_f = t5("wr_f", [128, 8, 36]); wffn_bc = t5("wffn_bc", [128, D])
                wst = [t5(f"wst{i}", [128, D]) for i in range(2)]
                xt1 = [t5(f"xt1_{i}", [128, D]) for i in range(NB1)]; yt1 = [t5(f"yt1_{i}", [128, 8, 128], BF16) for i in range(NB1)]
                h1s = [t5(f"h1s{i}", [128, D]) for i in range(NB1)]; big2 = [t5(f"big2_{i}", [128, D]) for i in range(NB1)]
                xn2_bs = [t5(f"xn2_b{i}", [128, D], BF16) for i in range(NB1)]; xn2T = [t5(f"xn2T{i}", [128, 8, 128]) for i in range(NB1)]
                scr = [dict(lgt=t5(f"lgt{i}", [128, 36]), rsm=t5(f"rsm{i}", [128, 32]), oh=[t5(f"oh{k}_{i}", [128, 32]) for k in range(2)],
                            cnt=t5(f"cnt{i}", [128, 32]), el=t5(f"el{i}", [128, 8]), mx8=t5(f"mx8_{i}", [128, 8]), ix8=t5(f"ix8_{i}", [128, 8], U32),
                            sm=t5(f"smb{i}", [128, 16])) for i in range(NB1)]
                carry = t5("carry", [1, 32])
                mset(carry, 0.0)
                dma(wffn_bc[:], nffn_d[0].partition_broadcast(128))
                dma(wr_f[:, :, 0:4], rgw_d[0].rearrange("(c p) e -> p c e", p=128))
                dma(wr_f[:, :, 4:36], rew_d[0].rearrange("(c p) e -> p c e", p=128))
                wout_v = wout_d[0].rearrange("(c p) f -> p c f", p=128)
                for c in range(8):
                    dma(wst[c % 2][:, :], wout_v[:, c, :])
                    cp(wout_b[:, c, :], wst[c % 2][:, :], e=('dve', 'act')[c % 2])

                def loads1b(i):
                    dma(xt1[i % NB1][:, :], x_d[(i - 1) * 128:i * 128, :])
                    dma(yt1[i % NB1][:, :, :].rearrange("p b t -> p (b t)"), yT_d[i * 128:(i + 1) * 128, :])
                def g_A(i):
                    b = i % NB1
                    xt = xt1[b]; yT_b = yt1[b]; h1 = h1s[b]; big = big2[b]; xn2_b = xn2_bs[b]; xT2 = xn2T[b]
                    Z = scr[b]; lgt = Z['lgt']; rsm = Z['rsm']; oh = Z['oh']; cnt = Z['cnt']; el = Z['el']; mx8 = Z['mx8']; ix8 = Z['ix8']; sm = Z['sm']
                    for n in range(2):
                        pm_ = psb()
                        for blk in range(8):
                            mm(pm_, yT_b[:, blk, :], wout_b[:, blk, n * 512:(n + 1) * 512], start=(blk == 0), stop=(blk == 7))
                        ttn(h1[:, n * 512:(n + 1) * 512], xt[:, n * 512:(n + 1) * 512], pm_, ALU.add)
                    dma(h1_d[i * 128:(i + 1) * 128, :], h1[:, :], q='pool')
                    yield
                    act(big[:], h1[:], AF.Square, accum=sm[:, 14:15])
                    act(sm[:, 15:16], sm[:, 14:15], AF.Sqrt, bias=1e-6, scale=1.0 / D)
                    recip(sm[:, 15:16], sm[:, 15:16])
                    stt(big[:], h1[:], sm[:, 15:16], wffn_bc[:], ALU.mult, ALU.mult)
                    cp(xn2_b[:], big[:], e='pool')
                    yield
                    for half in range(2):
                        pb = psb()
                        for j in range(4):
                            c = half * 4 + j
                            S.op('pe', lambda E, c=c, j=j, pb=pb, big=big: E.transpose(out=A(pb)[:, j * 128:(j + 1) * 128], in_=big[:, c * 128:(c + 1) * 128], identity=ident[:]),
                                 r=[big, ident], w=[pb])
                        cp(xT2[:, half * 4:half * 4 + 4, :], sub(pb, A(pb).rearrange("p (j t) -> p j t", t=128)), e='act')
                        yield
                    pl = psq(); pl36 = sub(pl, A(pl)[:, 0:36])
                    for c in range(8):
                        mm(pl36, xT2[:, c, :], wr_f[:, c, :], start=(c == 0), stop=(c == 7))
                    ttn(lgt[:], pl36, rb_bc[:], ALU.add)
                    yield
                    S.op('dve', lambda E: E.tensor_reduce(out=sm[:, 4:5], in_=lgt[:, 0:4], axis=AX.X, op=ALU.max, negate=True), r=[lgt], w=[sm])
                    act(rsm[:, 0:4], lgt[:, 0:4], AF.Exp, bias=sm[:, 4:5], accum=sm[:, 5:6])
                    recip(sm[:, 5:6], sm[:, 5:6])
                    yield
                    tsc(sm[:, 4:5], sm[:, 4:5], -1.0, None, ALU.mult)
                    tsc(rsm[:, 4:8], lgt[:, 0:4], sm[:, 4:5], None, ALU.is_equal)
                    yield
                    tsc(el[:], lgt[:, 4:12], rsm[:, 4:5], None, ALU.mult)
                    for g in range(1, 4):
                        stt(el[:], lgt[:, 4 + g * 8:12 + g * 8], rsm[:, 4 + g:5 + g], el[:], ALU.mult, ALU.add)
                    ttn(rsm[:, 8:12], rsm[:, 4:8], giota[:], ALU.mult)
                    S.op('dve', lambda E: E.tensor_reduce(out=sm[:, 6:7], in_=rsm[:, 8:12], axis=AX.X, op=ALU.add), r=[rsm], w=[sm])
                    yield
                    S.op('dve', lambda E: E.max(out=mx8[:], in_=el[:]), r=[el], w=[mx8])
                    S.op('dve', lambda E: E.max_index(out=ix8[:], in_max=mx8[:], in_values=el[:]), r=[mx8, el], w=[ix8])
                    yield
                    cp(sm[:, 8:10], ix8[:, 0:2])
                    tsc(sm[:, 8:10], sm[:, 8:10], sm[:, 6:7], None, ALU.add)
                    yield
                    ttn(sm[:, 10:11], mx8[:, 1:2], mx8[:, 0:1], ALU.subtract)
                    act(sm[:, 10:11], sm[:, 10:11], AF.Exp)
                    tsc(sm[:, 11:12], sm[:, 10:11], 1.0, None, ALU.add)
                    recip(sm[:, 11:12], sm[:, 11:12])
                    yield
                    ttn(gates_all[:, i, 0:1], sm[:, 5:6], sm[:, 11:12], ALU.mult)
                    ttn(gates_all[:, i, 1:2], gates_all[:, i, 0:1], sm[:, 10:11], ALU.mult)
                    for k in range(2):
                        tsc(oh[k][:], iota_f[:], sm[:, 8 + k:9 + k], None, ALU.is_equal)
                    ttn(cnt[:], oh[0][:], oh[1][:], ALU.add)
                    yield
                    yield

                def g_B(i):
                    b = i % NB1
                    xt = xt1[b]; yT_b = yt1[b]; h1 = h1s[b]; big = big2[b]; xn2_b = xn2_bs[b]; xT2 = xn2T[b]
                    Z = scr[b]; lgt = Z['lgt']; rsm = Z['rsm']; oh = Z['oh']; cnt = Z['cnt']; el = Z['el']; mx8 = Z['mx8']; ix8 = Z['ix8']; sm = Z['sm']
                    pp = psq(); pp32 = sub(pp, A(pp)[:, 0:32])
                    mm(pp32, msu[:], cnt[:], start=True, stop=False)
                    mm(pp32, ones[0:1, :], carry[0:1, :], start=False, stop=True)
                    for k in range(2):
                        ttn(rsm[:], oh[k][:], pp32, ALU.mult)
                        S.op('dve', lambda E, k=k: E.tensor_reduce(out=sm[:, 12 + k:13 + k], in_=rsm[:], axis=AX.X, op=ALU.add), r=[rsm], w=[sm])
                    tsc(sm[:, 12:14], sm[:, 12:14], float(CAP - 1), None, ALU.min)
                    stt(sm[:, 12:14], sm[:, 8:10], float(CAP), sm[:, 12:14], ALU.mult, ALU.add)
                    cp(slots_all[:, i, :], sm[:, 12:14])
                    yield
                    pc = psq(); pc32 = sub(pc, A(pc)[0:1, 0:32])
                    mm(pc32, ones[:, 0:1], cnt[:])
                    ttn(carry[0:1, :], carry[0:1, :], pc32, ALU.add)
                    yield
                    for k in range(2):
                        S.dma('pool', lambda E, k=k, i=i: E.indirect_dma_start(
                            out=xs_d[:, :], out_offset=bass.IndirectOffsetOnAxis(ap=slots_all[:, i, k:k + 1], axis=0),
                            in_=xn2_b[:, :], in_offset=None), r=[xn2_b, slots_all], w=['xs_scr'])
                    yield

                def g_tile(i):
                    loads1b(i)
                    yield
                    yield from g_A(i)
                    yield from g_B(i)
                active = []
                nxt_tile = 1
                rnd = 0
                while active or nxt_tile < NT:
                    if nxt_tile < NT and len(active) < NB1 and rnd % 4 == 0:
                        active.append(g_tile(nxt_tile)); nxt_tile += 1
                    for g in list(active):
                        try:
                            next(g)
                        except StopIteration:
                            active.remove(g)
                    rnd += 1
                S.barrier()
                chk('p1b')

            with ExitStack() as es3:
                t3 = lambda name, shape, dt=F32: T(es3, name, shape, dt)
                NSUB = CAP // 128
                wstg = [t3(f"ewstg{i}", [128, 2, D]) for i in range(6)]
                wgu_b = [t3(f"wgu_b{i}", [128, 8, D], BF16) for i in range(2)]
                wdn_b = [t3(f"wdn_b{i}", [128, 4, D], BF16) for i in range(2)]
                xsl = [t3(f"xsl{i}", [128, NSUB, D], BF16) for i in range(2)]
                xT = [t3(f"xT{i}", [128, 8, CAP], BF16) for i in range(2)]
                hT = t3("hT", [128, 4, CAP], BF16); gsl = t3("gsl", [128, CAP])
                ysl = [t3(f"ysl{i}", [128, D]) for i in range(2)]
                def wpiece(e, k):
                    g_ = e * 6 + k
                    s_ = wstg[g_ % 6]
                    if k < 4:
                        src = wgu_d[0, e].rearrange("(c p) f -> p c f", p=128)[:, 2 * k:2 * k + 2, :]
                        dst = wgu_b[e % 2][:, 2 * k:2 * k + 2, :]
                    else:
                        src = wdn_d[0, e].rearrange("(c p) f -> p c f", p=128)[:, 2 * (k - 4):2 * (k - 4) + 2, :]
                        dst = wdn_b[e % 2][:, 2 * (k - 4):2 * (k - 4) + 2, :]
                    return (lambda: dma(s_[:, :, :], src)), (lambda: cp(dst, s_[:, :, :], e=('dve', 'act')[g_ % 2]))

                def xload(e):
                    dma(xsl[e % 2][:, :, :], xs_d[e * CAP:(e + 1) * CAP, :].rearrange("(m p) f -> p m f", p=128), q='pool')

                for k in range(6):
                    d_, c_ = wpiece(0, k)
                    d_(); c_()
                xload(0)
                for e in range(NEXP):
                    Wg = wgu_b[e % 2]; Wd = wdn_b[e % 2]
                    X = xsl[e % 2]; XT = xT[e % 2]
                    if e + 1 < NEXP:
                        xload(e + 1)
                    steps = []

                    def st_tr(m, X=X, XT=XT):
                        for half in range(2):
                            pb = psb()
                            pbv = A(pb).bitcast(BF16)
                            for j in range(4):
                                c = half * 4 + j
                                S.op('pe', lambda E, c=c, j=j, m=m, pbv=pbv, X=X: E.transpose(out=pbv[:, j * 128:(j + 1) * 128], in_=X[:, m, c * 128:(c + 1) * 128], identity=identb[:]),
                                     r=[X, identb], w=[pb])
                            cp(XT[:, half * 4:half * 4 + 4, m * 128:(m + 1) * 128], sub(pb, pbv[:, 0:512].rearrange("p (j t) -> p j t", t=128)), e='act' if half else 'dve')

                    def st_gu(j, Wg=Wg, XT=XT):
                        pg = psb(); pu = psb()
                        for c in range(8):
                            mm(sub(pg, A(pg)[:, 0:CAP]), Wg[:, c, j * 128:(j + 1) * 128], XT[:, c, :], start=(c == 0), stop=(c == 7))
                        for c in range(8):
                            mm(sub(pu, A(pu)[:, 0:CAP]), Wg[:, c, 512 + j * 128:512 + (j + 1) * 128], XT[:, c, :], start=(c == 0), stop=(c == 7))
                        act(gsl[:, :], sub(pg, A(pg)[:, 0:CAP]), AF.Silu)
                        ttn(hT[:, j, :], gsl[:, :], sub(pu, A(pu)[:, 0:CAP]), ALU.mult)

                    def st_dn(m, Wd=Wd, e=e):
                        Y = ysl[m % 2]
                        for n in range(2):
                            py = psb()
                            for c in range(4):
                                mm(py, hT[:, c, m * 128:(m + 1) * 128], Wd[:, c, n * 512:(n + 1) * 512], start=(c == 0), stop=(c == 3))
                            cp(Y[:, n * 512:(n + 1) * 512], py, e='act' if n else 'dve')
                        dma(ys_d[e * CAP + m * 128:e * CAP + (m + 1) * 128, :], Y[:, :], q='pool')

                    for m in range(NSUB):
                        steps.append(lambda m=m: st_tr(m))
                    for j in range(4):
                        steps.append(lambda j=j: st_gu(j))
                    for m in range(NSUB):
                        steps.append(lambda m=m: st_dn(m))
                    assert len(steps) >= 6
                    casts = []
                    if e + 1 < NEXP:
                        for k in range(6):
                            d_, c_ = wpiece(e + 1, k)
                            d_()
                            casts.append(c_)
                    for si, stp in enumerate(steps):
                        stp()
                        if si < len(casts):
                            casts[si]()
                    for c_ in casts[len(steps):]:
                        c_()
                S.barrier()
                chk('p2')

            with ExitStack() as es4:
                t4 = lambda name, shape, dt=F32: T(es4, name, shape, dt)
                y0 = [t4(f"y0_{i}", [128, D]) for i in range(2)]; y1 = [t4(f"y1_{i}", [128, D]) for i in range(2)]
                hh = [t4(f"hh{i}", [128, D]) for i in range(2)]; jk = t4("jk", [128, D]); s4 = t4("s4", [128, 4])
                ob = [t4(f"ob{i}", [128, D]) for i in range(2)]
                wfin_bc = t4("wfin_bc", [128, D])
                dma(wfin_bc[:], nfin_d.partition_broadcast(128))
                def loads3(i):
                    b = i % 2
                    for k, yk in ((0, y0[b]), (1, y1[b])):
                        S.dma('pool', lambda E, k=k, i=i, yk=yk: E.indirect_dma_start(
                            out=yk[:, :], out_offset=None, in_=ys_d[:, :],
                            in_offset=bass.IndirectOffsetOnAxis(ap=slots_all[:, i, k:k + 1], axis=0)), r=['ys_scr', slots_all], w=[yk])
                    dma(hh[b][:, :], h1_d[i * 128:(i + 1) * 128, :])
                if NT > 1:
                    loads3(1)
                for i in range(1, NT):
                    b = i % 2
                    stt(hh[b][:], y0[b][:], gates_all[:, i, 0:1], hh[b][:], ALU.mult, ALU.add)
                    stt(hh[b][:], y1[b][:], gates_all[:, i, 1:2], hh[b][:], ALU.mult, ALU.add)
                    act(jk[:], hh[b][:], AF.Square, accum=s4[:, 0:1])
                    act(s4[:, 1:2], s4[:, 0:1], AF.Sqrt, bias=1e-6, scale=1.0 / D)
                    recip(s4[:, 1:2], s4[:, 1:2])
                    stt(ob[b][:], hh[b][:], s4[:, 1:2], wfin_bc[:], ALU.mult, ALU.mult)
                    if i + 1 < NT:
                        loads3(i + 1)
                    dma(out_d[(i - 1) * 128:i * 128, :], ob[b][:, :], is_out=True)
                S.finish()
        except Stop:
            S.finish()
        print("instr counts", S.total, "nsem", S.nsem)
    nc._dbg_map = dbg_map
    return nc


_NAMES = ['meta_tokens', 'norm_mix_w', 'norm_ffn_w', 'norm_final_w', 'w_in', 'w_out', 'rwkv_mu_rkv', 'rwkv_mu_wag',
          'rwkv_w0', 'rwkv_w_lora_a', 'rwkv_w_lora_b', 'rwkv_a0', 'rwkv_a_lora_a', 'rwkv_a_lora_b', 'rwkv_g_lora_a',
          'rwkv_g_lora_b', 'rwkv_k_k', 'rwkv_k_a', 'rwkv_r_k', 'rwkv_lnx_w', 'rwkv_lnx_b', 'mlstm_conv_w', 'mlstm_conv_b',
          'mlstm_gate_b', 'mlstm_norm_w', 'router_group_w', 'router_group_b', 'router_expert_w', 'router_expert_b',
          'expert_w_gate_up', 'expert_w_down']


def run(inputs, CAP=512, stop_after=None):
    x = np.asarray(inputs['x'], dtype=np.float32)
    B, L, _ = x.shape
    NT = L // 128 + 1
    nc = build(NT, CAP, stop_after=stop_after)
    shared = {n: np.ascontiguousarray(np.asarray(inputs[n], dtype=np.float32)) for n in _NAMES}
    in_maps = []
    for b in range(B):
        m = dict(shared)
        m['x'] = np.ascontiguousarray(x[b])
        in_maps.append(m)
    res = run_bass_kernel_spmd(nc, in_maps, core_ids=list(range(B)))
    if stop_after:
        d = np.asarray(res.results[0]['dbg'])
        return {k: d[0:v[2], v[0]:v[0] + v[1]] for k, v in nc._dbg_map.items()}
    return np.stack([np.asarray(r['out']).reshape(L, D) for r in res.results], axis=0).astype(np.float32)


def kernel(**inputs):
    return run(inputs, CAP=384)
```

```python
import math
import numpy as np
from contextlib import ExitStack
import concourse.bass as bass
import concourse.mybir as mybir
from concourse.bass_utils import run_bass_kernel_spmd

F32 = mybir.dt.float32
BF16 = mybir.dt.bfloat16
I32 = mybir.dt.int32
U32 = mybir.dt.uint32
AF = mybir.ActivationFunctionType
ALU = mybir.AluOpType
AX = mybir.AxisListType

D = 1024
DBG_TILE = 0
DIN = 3080
NEXP = 32
C0 = math.exp(-0.5)


class V:
    def __init__(self, ap, key):
        self.ap = ap
        self.key = key


def A(x):
    if isinstance(x, V):
        return x.ap
    if type(x).__name__.endswith('TensorHandle'):
        return x.ap()
    return x


def K(x):
    return x.key if isinstance(x, V) else x.name


class Sched:
    EPOCH = 20000

    def __init__(self, nc, es, n_dma_sems=32):
        self.nc = nc
        self.es = es
        self.eng = {'pe': nc.tensor, 'act': nc.scalar, 'dve': nc.vector,
                    'pool': nc.gpsimd, 'sp': nc.sync}
        self.sem = {}
        self.cnt = {}
        self.nsem = 0
        self.total = {k: 0 for k in self.eng}
        for k in self.eng:
            self._new_sem(k)
        self.waited = {k: {} for k in self.eng}
        self.res = {}
        self.dma_sems = [es.enter_context(nc.semaphore(f"dq{i}")) for i in range(n_dma_sems)]
        self.dma_cnt = [0] * n_dma_sems
        self.dma_rr = 0
        self.out_tokens = []

    def _new_sem(self, k):
        self.nsem += 1
        self.sem[k] = self.es.enter_context(self.nc.semaphore(f"s_{k}_{self.nsem}"))
        self.cnt[k] = 0

    def _need(self, reads, writes):
        need = []
        for key in reads:
            st = self.res.get(key)
            if st is not None and st['w'] is not None:
                need.append((st['w'], 'raw'))
        for key in writes:
            st = self.res.get(key)
            if st is not None:
                if st['w'] is not None:
                    need.append((st['w'], 'waw'))
                need.extend((t, 'war') for t in st['r'].values())
        return need

    import os as _os
    SAME_ENGINE_GAP = int(_os.environ.get('SE_GAP', 16))
    SKIP_WAX = int(_os.environ.get('SE_SKIPWAX', 1))
    SKIP_ENG = _os.environ.get('SE_ENG', 'dve,act,pool').split(',')

    def _emit_waits(self, e, need):
        for item in need:
            tok, kind = item if isinstance(item[0], tuple) else (item, 'raw')
            sem, val, src = tok[0], tok[1], tok[2]
            if src == 'pe' and e == 'pe':
                continue
            if src == e and src != 'dma':
                if e in self.SKIP_ENG:
                    if kind != 'raw' and self.SKIP_WAX:
                        continue
                    if kind == 'raw' and self.total[e] - tok[3] >= self.SAME_ENGINE_GAP:
                        continue
            w = self.waited[e]
            if w.get(id(sem), 0) >= val:
                continue
            self.eng[e].wait_ge(sem, val)
            w[id(sem)] = val

    def _record(self, tok, reads, writes):
        for key in reads:
            st = self.res.setdefault(key, {'w': None, 'r': {}})
            st['r'][id(tok[0])] = tok
        for key in writes:
            self.res[key] = {'w': tok, 'r': {}}

    def op(self, e, fn, r=(), w=()):
        r = [K(k) if not isinstance(k, (str, tuple)) else k for k in r]
        w = [K(k) if not isinstance(k, (str, tuple)) else k for k in w]
        w = w + [k for k in r if isinstance(k, str) and k.startswith('pb') and k not in w]
        if self.cnt[e] >= self.EPOCH:
            self._new_sem(e)
        self._emit_waits(e, self._need(r, w))
        inst = fn(self.eng[e])
        self.cnt[e] += 1
        self.total[e] += 1
        inst.then_inc(self.sem[e], 1)
        tok = (self.sem[e], self.cnt[e], e, self.total[e])
        self._record(tok, r, w)
        return tok

    def dma(self, q, fn, r=(), w=(), is_out=False):
        r = [K(k) if not isinstance(k, (str, tuple)) else k for k in r]
        w = [K(k) if not isinstance(k, (str, tuple)) else k for k in w]
        i = self.dma_rr
        self.dma_rr = (self.dma_rr + 1) % len(self.dma_sems)
        sem = self.dma_sems[i]
        need = self._need(r, w)
        if self.dma_cnt[i] > 0:
            need.append(((sem, 16 * self.dma_cnt[i], 'dma', 0), 'raw'))
        self._emit_waits(q, need)
        inst = fn(self.eng[q])
        self.dma_cnt[i] += 1
        inst.then_inc(sem, 16)
        tok = (sem, 16 * self.dma_cnt[i], 'dma', 0)
        self._record(tok, r, w)
        if is_out:
            self.out_tokens.append(tok)
        return tok

    def barrier(self):
        toks = [(self.sem[k], self.cnt[k], k, -10**9) for k in self.eng if self.cnt[k] > 0]
        toks += [(s, 16 * c, 'dma', 0) for s, c in zip(self.dma_sems, self.dma_cnt) if c > 0]
        for e in self.eng:
            self._emit_waits(e, [t for t in toks if t[2] != e])

    def finish(self):
        self._emit_waits('sp', self.out_tokens)
        self.barrier()


class Stop(Exception):
    pass


def inter(gens, weights=None):
    gens = list(gens)
    wts = {id(g): (weights[k] if weights else 1) for k, g in enumerate(gens)}
    while gens:
        for g in list(gens):
            for _ in range(wts[id(g)]):
                try:
                    next(g)
                except StopIteration:
                    gens.remove(g)
                    break
        yield


def build(NT, CAP, stop_after=None, dbgn=8192):
    nc = bass.Bass("TRN2", target_bir_lowering=False)
    NX = NT - 1
    dt_in = lambda name, shape: nc.dram_tensor(name, shape, F32, kind="ExternalInput").ap()
    x_d = dt_in("x", [NX * 128, D])
    meta_d = dt_in("meta_tokens", [16, D])
    nmix_d = dt_in("norm_mix_w", [1, D]); nffn_d = dt_in("norm_ffn_w", [1, D]); nfin_d = dt_in("norm_final_w", [D])
    win_d = dt_in("w_in", [1, D, DIN]); wout_d = dt_in("w_out", [1, D, D])
    murkv_d = dt_in("rwkv_mu_rkv", [1, 3, 512]); muwag_d = dt_in("rwkv_mu_wag", [1, 3, D])
    w0_d = dt_in("rwkv_w0", [1, 512]); wla_d = dt_in("rwkv_w_lora_a", [1, D, 64]); wlb_d = dt_in("rwkv_w_lora_b", [1, 64, 512])
    a0_d = dt_in("rwkv_a0", [1, 512]); ala_d = dt_in("rwkv_a_lora_a", [1, D, 64]); alb_d = dt_in("rwkv_a_lora_b", [1, 64, 512])
    gla_d = dt_in("rwkv_g_lora_a", [1, D, 160]); glb_d = dt_in("rwkv_g_lora_b", [1, 160, 512])
    kk_d = dt_in("rwkv_k_k", [1, 512]); ka_d = dt_in("rwkv_k_a", [1, 512]); rk_d = dt_in("rwkv_r_k", [1, 512])
    lnw_d = dt_in("rwkv_lnx_w", [1, 512]); lnb_d = dt_in("rwkv_lnx_b", [1, 512])
    cw_d = dt_in("mlstm_conv_w", [1, 4, 512]); cb_d = dt_in("mlstm_conv_b", [1, 512])
    gb_d = dt_in("mlstm_gate_b", [1, 8]); mnw_d = dt_in("mlstm_norm_w", [1, 512])
    rgw_d = dt_in("router_group_w", [1, D, 4]); rgb_d = dt_in("router_group_b", [1, 4])
    rew_d = dt_in("router_expert_w", [1, D, 32]); reb_d = dt_in("router_expert_b", [1, 32])
    wgu_d = dt_in("expert_w_gate_up", [1, NEXP, D, D]); wdn_d = dt_in("expert_w_down", [1, NEXP, 512, D])
    out_d = nc.dram_tensor("out", [NX * 128, D], F32, kind="ExternalOutput").ap()
    h1_d = nc.dram_tensor("h1_scr", [NT * 128, D], F32, kind="Internal").ap()
    xs_d = nc.dram_tensor("xs_scr", [NEXP * CAP, D], BF16, kind="Internal").ap()
    ys_d = nc.dram_tensor("ys_scr", [NEXP * CAP, D], F32, kind="Internal").ap()
    yT_d = nc.dram_tensor("yT_scr", [NT * 128, D], BF16, kind="Internal").ap()
    dbg_d = nc.dram_tensor("dbg", [128, dbgn], F32, kind="ExternalOutput").ap() if stop_after else None
    dbg_pos = [0]
    dbg_map = {}

    with ExitStack() as es0:
        S = Sched(nc, es0)

        def dump(name, ap, np_=128):
            if dbg_d is None:
                return
            n = ap.shape[-1] if len(ap.shape) == 2 else int(np.prod(ap.shape[1:]))
            dbg_map[name] = (dbg_pos[0], n, np_)
            S.dma('sp', lambda E: E.dma_start(out=dbg_d[0:np_, dbg_pos[0]:dbg_pos[0] + n], in_=ap, allow_slow_non_contiguous=True), r=[ap], w=['dbg'], is_out=True)
            dbg_pos[0] += n

        def chk(name):
            if stop_after == name:
                raise Stop()
        try:

            def mm(out, lhsT, rhs, start=True, stop=True):
                S.op('pe', lambda E: E.matmul(A(out), lhsT=A(lhsT), rhs=A(rhs), start=start, stop=stop),
                     r=[lhsT, rhs], w=[out])

            def act(out, in_, func, bias=None, scale=None, accum=None, e='act'):
                kw = {}
                rd = [in_]
                if bias is not None:
                    kw['bias'] = A(bias) if not isinstance(bias, float) else bias
                    if not isinstance(bias, float):
                        rd.append(bias)
                if scale is not None:
                    kw['scale'] = A(scale) if not isinstance(scale, float) else scale
                    if not isinstance(scale, float):
                        rd.append(scale)
                wr = [out]
                if accum is not None:
                    kw['accum_out'] = A(accum)
                    wr.append(accum)
                S.op('act', lambda E: E.activation(out=A(out), in_=A(in_), func=func, **kw), r=rd, w=wr)

            def tsc(out, in0, s1, s2, op0, op1=None, e='dve'):
                rd = [in0]
                a1 = s1
                a2 = s2
                if not isinstance(s1, (float, int)):
                    rd.append(s1); a1 = A(s1)
                if s2 is not None and not isinstance(s2, (float, int)):
                    rd.append(s2); a2 = A(s2)
                kw = {} if op1 is None else {'op1': op1}
                S.op(e, lambda E: E.tensor_scalar(out=A(out), in0=A(in0), scalar1=a1, scalar2=a2, op0=op0, **kw),
                     r=rd, w=[out])

            def ttn(out, a, b, op, e='dve'):
                S.op(e, lambda E: E.tensor_tensor(out=A(out), in0=A(a), in1=A(b), op=op), r=[a, b], w=[out])

            def stt(out, in0, sc, in1, op0, op1):
                rd = [in0, in1]
                a = sc
                if not isinstance(sc, (float, int)):
                    rd.append(sc); a = A(sc)
                S.op('dve', lambda E: E.scalar_tensor_tensor(out=A(out), in0=A(in0), scalar=a, in1=A(in1), op0=op0, op1=op1),
                     r=rd, w=[out])

            def cp(out, in_, e='dve'):
                if e == 'act':
                    S.op('act', lambda E: E.activation(out=A(out), in_=A(in_), func=AF.Copy), r=[in_], w=[out])
                else:
                    S.op(e, lambda E: E.tensor_copy(out=A(out), in_=A(in_)), r=[in_], w=[out])

            def mset(t, val, e='pool'):
                S.op(e, lambda E: E.memset(A(t), val), w=[t])

            def recip(out, in_):
                S.op('dve', lambda E: E.reciprocal(out=A(out), in_=A(in_)), r=[in_], w=[out])

            def dma(out, in_, q='sp', is_out=False):
                S.dma(q, lambda E: E.dma_start(out=A(out), in_=A(in_)), r=[in_], w=[out], is_out=is_out)

            banks = [es0.enter_context(nc.psum_tensor(f"pb{i}", [128, 512], F32)) for i in range(8)]
            st = {'q': 0, 'b': 0}

            def psq():
                i = st['q']; st['q'] = (i + 1) % 16
                b_, q_ = i % 4, (i // 4) % 4
                return V(banks[b_][:, q_ * 128:(q_ + 1) * 128], f"pb{b_}")

            def psb():
                i = st['b']; st['b'] = (i + 1) % 4
                return V(banks[4 + i][:, :], f"pbB{i}")

            def sub(v, ap):
                return V(ap, v.key) if isinstance(v, V) else ap

            T = lambda stack, name, shape, dt=F32: stack.enter_context(nc.sbuf_tensor(name, shape, dt))

            ones = T(es0, "ones", [128, 128]); ident = T(es0, "ident", [128, 128]); identb = T(es0, "identb", [128, 128], BF16)
            msu = T(es0, "msu", [128, 128]); msl = T(es0, "msl", [128, 128]); mst = T(es0, "mst", [128, 64])
            blk64 = T(es0, "blk64", [128, 128]); tric = T(es0, "tric", [128, 128]); maskc = T(es0, "maskc", [128, 128])
            mset(ones, 1.0)
            asel = lambda out, pat, cmp, base, cm, in_=None: S.op('pool', lambda E: E.affine_select(
                out=A(out), in_=A(in_ if in_ is not None else ones[:]), pattern=pat, compare_op=cmp, fill=0.0, base=base, channel_multiplier=cm),
                r=[in_ if in_ is not None else ones], w=[out])
            asel(ident[:], [[-1, 128]], ALU.is_equal, 0, 1)
            cp(identb[:], ident[:], e='pool')
            asel(msu[:], [[1, 128]], ALU.is_gt, 0, -1)
            asel(msl[:], [[-1, 128]], ALU.is_gt, 0, 1)
            asel(mst[0:64, :], [[1, 64]], ALU.is_ge, 0, -1, in_=ones[0:64, 0:64])
            asel(mst[64:128, :], [[1, 64]], ALU.is_ge, 0, -1, in_=ones[64:128, 0:64])
            mset(blk64, 0.0); mset(blk64[0:64, 0:64], 1.0); mset(blk64[64:128, 64:128], 1.0)
            asel(tric[:], [[1, 128]], ALU.is_ge, 0, -1)
            ttn(tric[:], tric[:], blk64[:], ALU.mult, e='pool')
            tsc(maskc[:], tric[:], 0.125, None, ALU.mult, e='pool')

            pstg = T(es0, "pstg", [128, 128]); pv = T(es0, "pv", [128, 128]); pv1 = T(es0, "pv1", [128, 128])
            mset(pstg, 0.0)
            row = {}
            rcur = [0]

            def ldrows(name, ap2d, n):
                row[name] = rcur[0]
                dma(pstg[rcur[0]:rcur[0] + n, :], ap2d)
                rcur[0] += n
            ldrows('nmix', nmix_d[0].rearrange("(c p) -> c p", p=128), 8)
            ldrows('muwag', muwag_d[0].rearrange("j (c p) -> (j c) p", p=128), 24)
            ldrows('murkv', murkv_d[0].rearrange("j (c p) -> (j c) p", p=128), 12)
            for nm, ap in (('w0', w0_d), ('a0', a0_d), ('kk', kk_d), ('ka', ka_d), ('rk', rk_d), ('lnw', lnw_d), ('lnb', lnb_d), ('cb', cb_d)):
                ldrows(nm, ap[0].rearrange("(c p) -> c p", p=128), 4)
            ldrows('cw', cw_d[0].rearrange("j (c p) -> (j c) p", p=128), 16)
            tp = psq()
            S.op('pe', lambda E: E.transpose(out=A(tp), in_=pstg[:], identity=ident[:]), r=[pstg, ident], w=[tp])
            cp(pv[:], tp, e='act')
            tsc(pv1[:], pv[:], -1.0, 1.0, ALU.mult, ALU.add)
            PV = lambda nm, j=0: pv[:, row[nm] + j:row[nm] + j + 1]
            PV1 = lambda nm, j=0: pv1[:, row[nm] + j:row[nm] + j + 1]

            mnw_bc = T(es0, "mnw_bc", [128, 512])
            gb_bc = T(es0, "gb_bc", [128, 8]); rb_bc = T(es0, "rb_bc", [128, 36]); iota_i = T(es0, "iota_i", [128, 32], I32)
            iota_f = T(es0, "iota_f", [128, 32]); giota = T(es0, "giota", [128, 4])

            dma(mnw_bc[:], mnw_d[0].partition_broadcast(128)); dma(gb_bc[:], gb_d[0].partition_broadcast(128))
            dma(rb_bc[:, 0:4], rgb_d[0].partition_broadcast(128)); dma(rb_bc[:, 4:36], reb_d[0].partition_broadcast(128))
            S.op('pool', lambda E: E.iota(iota_i[:], pattern=[[1, 32]], base=0, channel_multiplier=0), w=[iota_i])
            cp(iota_f[:], iota_i[:], e='pool')
            tsc(giota[:], iota_f[:, 0:4], 8.0, None, ALU.mult, e='pool')

            gates_all = T(es0, "gates_all", [128, NT, 2]); slots_all = T(es0, "slots_all", [128, NT, 2], I32)
            es1 = es0.enter_context(ExitStack())
            win_b = T(es1, "win_b", [128, 8, DIN], BF16)
            wla_b = T(es1, "wla_b", [128, 8, 288], BF16); wlamu_b = T(es1, "wlamu_b", [128, 8, 288], BF16)
            lb_b = T(es1, "lb_b", [128, 512], BF16); glb0_b = T(es1, "glb0_b", [128, 512], BF16); glb1_b = T(es1, "glb1_b", [32, 512], BF16)
            with ExitStack() as esl:
                stg = [T(esl, f"wstg{i}", [128, DIN]) for i in range(2)]
                win_v = win_d[0].rearrange("(c p) f -> p c f", p=128)
                ceng = ['dve', 'pool', 'act']
                k = 0
                for c in range(8):
                    s_ = stg[k % 2]
                    dma(s_[:, :], win_v[:, c, :])
                    cp(win_b[:, c, :], s_[:, :], e=ceng[k % 3]); k += 1
                s_ = stg[k % 2]; k += 1
                sv = s_[:, 0:8 * 288].rearrange("p (c j) -> p c j", j=288)
                dma(sv[:, :, 0:64], wla_d[0].rearrange("(c p) j -> p c j", p=128))
                dma(sv[:, :, 64:128], ala_d[0].rearrange("(c p) j -> p c j", p=128))
                dma(sv[:, :, 128:288], gla_d[0].rearrange("(c p) j -> p c j", p=128))
                cp(wla_b[:, :, :], sv, e='dve')
                for c in range(8):
                    for j, (lo, hi) in enumerate(((0, 64), (64, 128), (128, 288))):
                        tsc(wlamu_b[:, c, lo:hi], sv[:, c, lo:hi], PV('muwag', j * 8 + c), None, ALU.mult, e='dve' if c % 2 else 'pool')
                s_ = stg[k % 2]; k += 1
                dma(s_[0:64, 0:512], wlb_d[0]); dma(s_[64:128, 0:512], alb_d[0])
                dma(s_[:, 512:1024], glb_d[0][0:128, :]); dma(s_[0:32, 1024:1536], glb_d[0][128:160, :])
                cp(lb_b[:, :], s_[:, 0:512]); cp(glb0_b[:, :], s_[:, 512:1024]); cp(glb1_b[:, :], s_[0:32, 1024:1536])
                S.barrier()
            dump('pv', pv[:, :])
            chk('setup')

            es2 = es1.enter_context(ExitStack())
            t2 = lambda name, shape, dt=F32: T(es2, name, shape, dt)
            x_tm = [t2("x_tm0", [128, D])] * 2
            big = t2("big", [128, D])
            xnT_f = t2("xnT_f", [128, 8, 129]); xnT_b = t2("xnT_b", [128, 8, 128], BF16); xxT_b = t2("xxT_b", [128, 8, 128], BF16)
            rkv_raw = t2("rkv_raw", [128, 12, 129]); qk_raw = t2("qk_raw", [128, 4, 131])
            V1s = [t2(f"V1_{i}", [128, 4, 129]) for i in range(2)]; sigos = [t2(f"sigo{i}", [128, 512]) for i in range(2)]
            laT = t2("laT", [128, 128], BF16); lg0 = t2("lg0", [128, 128], BF16); lg1 = t2("lg1", [32, 128], BF16)
            sgw = t2("sgw", [128, 4, 128]); asig = t2("asig", [128, 4, 128]); ggs = [t2(f"gg{i}", [128, 4, 128]) for i in range(2)]
            bonuss = [t2(f"bonus{i}", [128, 4, 128]) for i in range(2)]; y_as = [t2(f"y_a{i}", [128, 4, 128]) for i in range(2)]
            ptA = [t2(f"ptA{i}", [128, 128]) for i in range(2)]; ptB = [t2(f"ptB{i}", [128, 128]) for i in range(2)]
            yT_bs = [t2(f"yT_b{i}", [128, 8, 128], BF16) for i in range(2)]
            ssq = t2("ssq", [128, 4]); rstd = t2("rstd", [128, 4])
            NR = 4
            rt = {nm: [t2(f"{nm}{i}", [128, 128]) for i in range(NR)] for nm in
                  ('r', 'k', 'v', 'kkn', 'k2', 'bv', 'tA', 'tB', 'cs', 'E1', 'E2')}
            STg = t2("STg", [128, 4, 128])
            opz = [[t2(f"opz{fb}_{c}", [128, 4, 128], BF16) for c in range(2)] for fb in range(4)]
            rst = [[t2(f"rst{fb}_{c}", [128, 64], BF16) for c in range(2)] for fb in range(4)]
            gC = t2("gC", [128, 4, 2])
            ArbArk = t2("alg_ArbArk", [128, 2, 4, 64], BF16)
            Gc = [{nm: t2(f"alg{c}_{nm}", [128, 4, 128], BF16) for nm in
                   ('BzT', 'KzT', 'VzT', 'Aak', 'Q0', 'Q1', 'QT0', 'QT1', 'P0', 'P1', 'PT0', 'PT1')} for c in range(2)]
            GS = {nm: t2(f"alg_{nm}", [128, 4, 128], BF16) for nm in ('W0T', 'UT')}
            ArbArks = [ArbArk, t2("alg_ArbArk1", [128, 2, 4, 64], BF16)]
            ST32 = t2("ST32", [128, 4, 128])
            STb = t2("STb", [128, 4, 128], BF16)
            QKf = t2("QKf", [128, 4, 128]); cacc = QKf
            g8s = [t2(f"g8_{i}", [128, 8]) for i in range(2)]; th8 = t2("th8", [128, 8]); nbg = t2("nbg", [128, 8]); wgt = t2("wgt", [128, 4]); dbias = t2("dbias", [128, 4])
            lfb = [t2(f"lfb{i}", [128, 128]) for i in range(2)]; Dm = [t2(f"Dm{i}", [128, 128]) for i in range(2)]
            eB = [t2(f"eB{i}", [128, 128]) for i in range(2)]; Pm = [t2(f"Pm{i}", [128, 128]) for i in range(2)]
            Qz = [t2(f"Qz{h}", [128, 2, 128]) for h in range(4)]
            Kw = t2("Kw", [128, 4, 64])
            CTa = [t2(f"CTa{h}", [128, 129]) for h in range(4)]; CTb = [t2(f"CTb{h}", [128, 129]) for h in range(4)]
            hraw = [t2(f"hraw{i}", [128, 128]) for i in range(2)]; y_b = t2("y_b", [128, 512])
            sm = t2("sm", [128, 16])

            mset(xnT_f[:, :, 0:1], 0.0); mset(rkv_raw[:, :, 0:1], 0.0); mset(qk_raw[:, :, 0:3], 0.0)
            mset(V1s[0][:, :, 128:129], 1.0); mset(V1s[1][:, :, 128:129], 1.0)
            mset(ST32, 0.0); mset(STb, 0.0)
            for fb in range(4):
                for c in range(2):
                    mset(opz[fb][c], 0.0)
            for h in range(4):
                mset(Qz[h], 0.0); mset(CTa[h], 0.0); mset(CTb[h], 0.0)

            def g_front(i):
                xt = x_tm[i % 2]; V1 = V1s[i % 2]; sigo = sigos[i % 2]; g8 = g8s[i % 2]; gg = ggs[i % 2]
                if i == 0:
                    mset(xt, 0.0)
                    dma(xt[112:128, :], meta_d)
                else:
                    dma(xt[:, :], x_d[(i - 1) * 128:i * 128, :])
                act(big[:], xt[:], AF.Square, accum=ssq[:, 0:1])
                act(rstd[:, 0:1], ssq[:, 0:1], AF.Sqrt, bias=1e-6, scale=1.0 / D)
                recip(rstd[:, 0:1], rstd[:, 0:1])
                tsc(big[:], xt[:], rstd[:, 0:1], None, ALU.mult)
                yield
                for half in range(2):
                    pb = psb()
                    for j in range(4):
                        c = half * 4 + j
                        S.op('pe', lambda E, c=c, j=j, pb=pb: E.transpose(out=A(pb)[:, j * 128:(j + 1) * 128], in_=big[:, c * 128:(c + 1) * 128], identity=ident[:]),
                             r=[big, ident], w=[pb])
                    for j in range(4):
                        c = half * 4 + j
                        if j % 2 == 0:
                            act(xnT_f[:, c, 1:129], sub(pb, A(pb)[:, j * 128:(j + 1) * 128]), AF.Copy, scale=PV('nmix', c))
                        else:
                            tsc(xnT_f[:, c, 1:129], sub(pb, A(pb)[:, j * 128:(j + 1) * 128]), PV('nmix', c), None, ALU.mult)
                    yield
                cp(xnT_b[:, :, :], xnT_f[:, :, 1:129], e='pool')
                ttn(xxT_b[:, :, :], xnT_f[:, :, 0:128], xnT_f[:, :, 1:129], ALU.subtract)
                cp(xnT_f[:, :, 0:1], xnT_f[:, :, 128:129], e='pool')
                yield

                for blk in range(16):
                    p_ = psq()
                    for c in range(8):
                        mm(p_, win_b[:, c, blk * 128:(blk + 1) * 128], xnT_b[:, c, :], start=(c == 0), stop=(c == 7))
                    if blk < 12:
                        cp(rkv_raw[:, blk, 1:129], p_, e='act')
                    else:
                        cp(qk_raw[:, blk - 12, 3:131], p_, e='act')
                    yield
                pv_ = psb()
                for c in range(8):
                    mm(pv_, xnT_b[:, c, :], win_b[:, c, 2048:2560], start=(c == 0), stop=(c == 7))
                cp(V1[:, :, 0:128], sub(pv_, A(pv_).rearrange("p (h v) -> p h v", v=128)), e='act')
                yield
                po_ = psb()
                for c in range(8):
                    mm(po_, xnT_b[:, c, :], win_b[:, c, 2560:3072], start=(c == 0), stop=(c == 7))
                act(sigo[:], po_, AF.Sigmoid)
                yield
                pg_ = psq()
                pg8 = sub(pg_, A(pg_)[:, 0:8])
                for c in range(8):
                    mm(pg8, xnT_b[:, c, :], win_b[:, c, 3072:3080], start=(c == 0), stop=(c == 7))
                ttn(g8[:], pg8, gb_bc[:], ALU.add)
                yield
                la_ps = []
                for (lo, hi) in ((0, 128), (128, 256), (256, 288)):
                    p_ = psq()
                    po = sub(p_, A(p_)[0:hi - lo, :])
                    for c in range(8):
                        mm(po, wla_b[:, c, lo:hi], xnT_b[:, c, :], start=(c == 0), stop=False)
                    for c in range(8):
                        mm(po, wlamu_b[:, c, lo:hi], xxT_b[:, c, :], start=False, stop=(c == 7))
                    la_ps.append(p_)
                act(laT[0:64, :], sub(la_ps[0], A(la_ps[0])[0:64, :]), AF.Tanh)
                cp(laT[64:128, :], sub(la_ps[0], A(la_ps[0])[64:128, :]), e='dve')
                act(lg0[:, :], la_ps[1], AF.Sigmoid)
                act(lg1[:, :], sub(la_ps[2], A(la_ps[2])[0:32, :]), AF.Sigmoid)
                yield
                for fb in range(4):
                    fs = slice(fb * 128, (fb + 1) * 128)
                    p_ = psq(); mm(p_, lb_b[0:64, fs], laT[0:64, :])
                    act(sgw[:, fb, :], p_, AF.Sigmoid, bias=PV('w0', fb))
                    p_ = psq(); mm(p_, lb_b[64:128, fs], laT[64:128, :])
                    act(asig[:, fb, :], p_, AF.Sigmoid, bias=PV('a0', fb))
                    p_ = psq(); mm(p_, glb0_b[:, fs], lg0[:, :], start=True, stop=False); mm(p_, glb1_b[0:32, fs], lg1[0:32, :], start=False, stop=True)
                    cp(gg[:, fb, :], p_, e='dve')
                    yield

                yield

            pending = []
            for i in range(NT):
                xt = x_tm[i % 2]; V1 = V1s[i % 2]; sigo = sigos[i % 2]; g8 = g8s[i % 2]; gg = ggs[i % 2]
                yT_b = yT_bs[i % 2]; bonus = bonuss[i % 2]; y_a = y_as[i % 2]
                if i == 0:
                    for _ in g_front(0):
                        pass
                def g_prep(fb):
                    R = {nm: rt[nm][fb % NR] for nm in rt}
                    R['E3'] = R['tB']
                    for nm, bi in (('r', 0), ('k', 1), ('v', 2)):
                        blk = bi * 4 + fb
                        tsc(R['tA'][:], rkv_raw[:, blk, 0:128], PV('murkv', bi * 4 + fb), None, ALU.mult, e='pool')
                        stt(R[nm][:], rkv_raw[:, blk, 1:129], PV1('murkv', bi * 4 + fb), R['tA'][:], ALU.mult, ALU.add)
                        yield
                    tsc(R['kkn'][:], R['k'][:], PV('kk', fb), None, ALU.mult)
                    ttn(R['tA'][:], R['kkn'][:], R['kkn'][:], ALU.mult, e='pool')
                    p_ = psq(); mm(p_, blk64[:], R['tA'][:])
                    act(R['tB'][:], p_, AF.Sqrt)
                    yield
                    tsc(R['tB'][:], R['tB'][:], 1e-12, None, ALU.max)
                    recip(R['tB'][:], R['tB'][:])
                    yield
                    ttn(R['kkn'][:], R['kkn'][:], R['tB'][:], ALU.mult)
                    tsc(R['tA'][:], asig[:, fb, :], PV('ka', fb), PV1('ka', fb), ALU.mult, ALU.add)
                    ttn(R['k2'][:], R['k'][:], R['tA'][:], ALU.mult)
                    ttn(R['bv'][:], R['kkn'][:], asig[:, fb, :], ALU.mult, e='pool')
                    yield
                    stt(R['tA'][:], R['r'][:], PV('rk', fb), R['k2'][:], ALU.mult, ALU.mult)
                    p_ = psq(); mm(p_, blk64[:], R['tA'][:])
                    ttn(bonus[:, fb, :], p_, R['v'][:], ALU.mult)
                    yield
                    for c in range(2):
                        cs_ = slice(c * 64, (c + 1) * 64)
                        S.op('dve', lambda E, fb=fb, cs_=cs_, R=R: E.tensor_tensor_scan(out=R['cs'][:, cs_], data0=ones[:, 0:64], data1=sgw[:, fb, cs_], initial=0.0, op0=ALU.mult, op1=ALU.add),
                             r=[ones, sgw], w=[R['cs']])
                        yield
                    act(R['E1'][:], R['cs'][:], AF.Exp, scale=-C0)
                    act(R['E2'][:], R['cs'][:], AF.Exp, scale=C0)
                    yield
                    ttn(R['tB'][:], R['cs'][:], sgw[:, fb, :], ALU.subtract, e='pool')
                    act(R['E3'][:], R['tB'][:], AF.Exp, scale=-C0)
                    yield
                    for c in range(2):
                        O = opz[fb][c]
                        for hh in range(2):
                            ps_ = slice(hh * 64, (hh + 1) * 64)
                            ts_ = slice(c * 64, (c + 1) * 64)
                            os_ = slice(hh * 64, (hh + 1) * 64)
                            stt(O[ps_, 0, os_], R['kkn'][ps_, ts_], -1.0, R['E3'][ps_, ts_], ALU.mult, ALU.mult)
                            ttn(O[ps_, 1, os_], R['bv'][ps_, ts_], R['E2'][ps_, ts_], ALU.mult)
                            ttn(O[ps_, 2, os_], R['k2'][ps_, ts_], R['E2'][ps_, ts_], ALU.mult, e='pool')
                            cp(O[ps_, 3, os_], R['v'][ps_, ts_], e='pool')
                            yield
                        ttn(rst[fb][c][:, :], R['r'][:, c * 64:(c + 1) * 64], R['E1'][:, c * 64:(c + 1) * 64], ALU.mult)
                        cp(gC[:, fb, c:c + 1], R['E1'][:, c * 64 + 63:c * 64 + 64], e='pool')
                    yield

                def q4(bank):
                    return sub(bank, A(bank).rearrange("p (f t) -> p f t", t=128))
                bc4 = lambda m: m[:, :].unsqueeze(1).broadcast_to([128, 4, 128])
                TinvOf = {}
                def g_alg(c):
                    Az = [opz[fb][c][:, 0, :] for fb in range(4)]; Bz = [opz[fb][c][:, 1, :] for fb in range(4)]
                    Kz = [opz[fb][c][:, 2, :] for fb in range(4)]; Vz = [opz[fb][c][:, 3, :] for fb in range(4)]
                    Rs = [rst[fb][c][:, :] for fb in range(4)]
                    for kk_i, (nm, src) in enumerate((('BzT', Bz), ('KzT', Kz), ('VzT', Vz))):
                        bk = psb()
                        for fb in range(4):
                            mm(sub(bk, A(bk)[:, fb * 128:(fb + 1) * 128]), src[fb], identb[:])
                        cp(Gc[c][nm][:, :, :], q4(bk), e='act' if kk_i != 1 else 'dve')
                        yield
                    for nm, l_, r_, msk in (('Q0', Bz, Az, msu), ('QT0', Az, Bz, msl), ('Aak', Kz, Az, msu)):
                        bk = psb()
                        for fb in range(4):
                            mm(sub(bk, A(bk)[:, fb * 128:(fb + 1) * 128]), l_[fb], r_[fb])
                        ttn(Gc[c][nm][:, :, :], q4(bk), bc4(msk), ALU.mult)
                        yield
                    bk = psb()
                    for j, l_ in enumerate((Bz, Kz)):
                        for fb in range(4):
                            o0 = j * 256 + fb * 64
                            mm(sub(bk, A(bk)[:, o0:o0 + 64]), l_[fb], Rs[fb])
                    ttn(ArbArks[c][:, :, :, :].rearrange("p j f t -> p (j f) t"), sub(bk, A(bk).rearrange("p (g t) -> p g t", t=64)),
                        mst[:, :].unsqueeze(1).broadcast_to([128, 8, 64]), ALU.mult)
                    yield
                    ttn(Gc[c]['P0'][:, :, :], Gc[c]['Q0'][:, :, :], identb[:, :].unsqueeze(1).broadcast_to([128, 4, 128]), ALU.add, e='pool')
                    ttn(Gc[c]['PT0'][:, :, :], Gc[c]['QT0'][:, :, :], identb[:, :].unsqueeze(1).broadcast_to([128, 4, 128]), ALU.add, e='pool')
                    yield
                    Qb = [Gc[c]['Q0'], Gc[c]['Q1']]; QTb = [Gc[c]['QT0'], Gc[c]['QT1']]
                    Pb = [Gc[c]['P0'], Gc[c]['P1']]; PTb = [Gc[c]['PT0'], Gc[c]['PT1']]
                    for s_ in range(6):
                        Qs, QTs = Qb[s_ % 2], QTb[s_ % 2]
                        Qn, QTn = Qb[(s_ + 1) % 2], QTb[(s_ + 1) % 2]
                        Pp, PTp = Pb[(s_ - 1) % 2], PTb[(s_ - 1) % 2]
                        Pc, PTc = Pb[s_ % 2], PTb[s_ % 2]
                        todo = []
                        if s_ <= 4:
                            bk = psb()
                            for fb in range(4):
                                mm(sub(bk, A(bk)[:, fb * 128:(fb + 1) * 128]), QTs[:, fb, :], Qs[:, fb, :])
                            todo.append(lambda bk=bk: cp(Qn[:, :, :], q4(bk), e='act'))
                        if s_ <= 3:
                            bk = psb()
                            for fb in range(4):
                                mm(sub(bk, A(bk)[:, fb * 128:(fb + 1) * 128]), Qs[:, fb, :], QTs[:, fb, :])
                            todo.append(lambda bk=bk: cp(QTn[:, :, :], q4(bk), e='act'))
                        if s_ >= 1:
                            bk = psb()
                            for fb in range(4):
                                mm(sub(bk, A(bk)[:, fb * 128:(fb + 1) * 128]), PTp[:, fb, :], Qs[:, fb, :])
                            todo.append(lambda bk=bk: ttn(Pc[:, :, :], q4(bk), Pp[:, :, :], ALU.add))
                        if 1 <= s_ <= 4:
                            bk = psb()
                            for fb in range(4):
                                mm(sub(bk, A(bk)[:, fb * 128:(fb + 1) * 128]), Qs[:, fb, :], PTp[:, fb, :])
                            todo.append(lambda bk=bk: ttn(PTc[:, :, :], q4(bk), PTp[:, :, :], ALU.add))
                        for f_ in todo:
                            f_()
                        yield
                    cur = 1
                    TinvOf[c] = Gc[c][f'P{cur}']
                    yield
                def g_chain(c):
                    Az = [opz[fb][c][:, 0, :] for fb in range(4)]; Bz = [opz[fb][c][:, 1, :] for fb in range(4)]
                    Kz = [opz[fb][c][:, 2, :] for fb in range(4)]; Vz = [opz[fb][c][:, 3, :] for fb in range(4)]
                    Rs = [rst[fb][c][:, :] for fb in range(4)]
                    ttn(STg[:, :, :], ST32[:, :, :], gC[:, :, c:c + 1].broadcast_to([128, 4, 128]), ALU.mult, e='pool')
                    bk = psb()
                    for fb in range(4):
                        o_ = sub(bk, A(bk)[:, fb * 128:(fb + 1) * 128])
                        mm(o_, Az[fb], STb[:, fb, :], start=True, stop=False); mm(o_, Gc[c]['Aak'][:, fb, :], Gc[c]['VzT'][:, fb, :], start=False, stop=True)
                    cp(GS['W0T'][:, :, :], q4(bk), e='act')
                    yield
                    bk = psb()
                    for fb in range(4):
                        mm(sub(bk, A(bk)[:, fb * 128:(fb + 1) * 128]), TinvOf[c][:, fb, :], GS['W0T'][:, fb, :])
                    cp(GS['UT'][:, :, :], q4(bk), e='act')
                    yield
                    bkS = psb()
                    for fb in range(4):
                        o_ = sub(bkS, A(bkS)[:, fb * 128:(fb + 1) * 128])
                        mm(o_, Gc[c]['BzT'][:, fb, :], GS['UT'][:, fb, :], start=True, stop=False)
                        mm(o_, Gc[c]['KzT'][:, fb, :], Gc[c]['VzT'][:, fb, :], start=False, stop=True)
                    bkY = psb()
                    for fb in range(4):
                        o_ = sub(bkY, A(bkY)[:, fb * 64:(fb + 1) * 64])
                        mm(o_, STb[:, fb, :], Rs[fb], start=True, stop=False)
                        mm(o_, GS['UT'][:, fb, :], ArbArks[c][:, 0, fb, :], start=False, stop=False)
                        mm(o_, Gc[c]['VzT'][:, fb, :], ArbArks[c][:, 1, fb, :], start=False, stop=True)
                    for fb in range(4):
                        stt(STb[:, fb, :], sub(bkS, A(bkS)[:, fb * 128:(fb + 1) * 128]), gC[:, fb, c:c + 1], STg[:, fb, :], ALU.mult, ALU.add)
                    for fb in range(4):
                        stt(ST32[:, fb, :], sub(bkS, A(bkS)[:, fb * 128:(fb + 1) * 128]), gC[:, fb, c:c + 1], STg[:, fb, :], ALU.mult, ALU.add)
                    cp(y_a[:, :, c * 64:(c + 1) * 64], sub(bkY, A(bkY)[:, 0:256].rearrange("p (f t) -> p f t", t=64)), e='act')
                    yield
                    yield

                def g_post(fb, y_a=y_a, bonus=bonus, gg=gg, yT_b=yT_b):
                    R = {'tA': ptA[fb % 2], 'tB': ptB[fb % 2]}
                    p1 = psq(); mm(p1, blk64[:], y_a[:, fb, :])
                    ttn(R['tA'][:], y_a[:, fb, :], y_a[:, fb, :], ALU.mult, e='pool')
                    p2 = psq(); mm(p2, blk64[:], R['tA'][:])
                    act(R['tB'][:], p1, AF.Copy, scale=1.0 / 64)
                    ttn(R['tA'][:], R['tB'][:], R['tB'][:], ALU.mult)
                    stt(R['tA'][:], p2, 1.0 / 64, R['tA'][:], ALU.mult, ALU.subtract)
                    yield
                    act(R['tA'][:], R['tA'][:], AF.Sqrt, bias=64e-5)
                    yield
                    recip(R['tA'][:], R['tA'][:])
                    yield
                    ttn(R['tB'][:], y_a[:, fb, :], R['tB'][:], ALU.subtract)
                    ttn(R['tB'][:], R['tB'][:], R['tA'][:], ALU.mult)
                    tsc(R['tB'][:], R['tB'][:], PV('lnw', fb), PV('lnb', fb), ALU.mult, ALU.add)
                    ttn(R['tB'][:], R['tB'][:], bonus[:, fb, :], ALU.add)
                    ttn(yT_b[:, fb, :], R['tB'][:], gg[:, fb, :], ALU.mult)
                    yield

                def g_mlstm_pre():
                    for t_ in range(4):
                        for jb in range(4):
                            kj = ('QKf', jb)
                            if t_ == 0:
                                S.op('dve', lambda E, jb=jb: E.tensor_scalar(out=cacc[:, jb, :], in0=qk_raw[:, jb, 0:128], scalar1=PV('cw', jb), scalar2=PV('cb', jb), op0=ALU.mult, op1=ALU.add),
                                     r=[qk_raw, pv], w=[kj, 'QKf'])
                            else:
                                S.op('dve', lambda E, jb=jb, t_=t_: E.scalar_tensor_tensor(out=cacc[:, jb, :], in0=qk_raw[:, jb, t_:t_ + 128], scalar=PV('cw', t_ * 4 + jb), in1=cacc[:, jb, :], op0=ALU.mult, op1=ALU.add),
                                     r=[qk_raw, pv, kj], w=[kj, 'QKf'])
                    S.op('act', lambda E: E.activation(out=QKf[:, :, :], in_=cacc[:, :, :], func=AF.Silu), r=[('QKf', jb) for jb in range(4)] + ['QKf'], w=['QKf'] + [('QKf', jb) for jb in range(4)])
                    yield
                    cp(qk_raw[:, :, 0:3], qk_raw[:, :, 128:131], e='pool')
                    act(th8[:], g8[:], AF.Tanh, scale=1.0 / 15.0)
                    tsc(g8[:, 0:4], th8[:, 0:4], 15.0, None, ALU.mult)
                    act(g8[:, 4:8], th8[:, 4:8], AF.Exp, scale=-15.0)
                    act(g8[:, 4:8], g8[:, 4:8], AF.Ln, bias=1.0)
                    yield
                    if i == 0:
                        mset(g8[0:112, 0:4], -1.0e4, e='dve'); mset(g8[0:112, 4:8], 0.0, e='dve')
                    pn = psq()
                    pn4 = sub(pn, A(pn)[:, 0:4]); pn8 = sub(pn, A(pn)[:, 4:8])
                    mm(pn4, tric[:], g8[:, 4:8]); mm(pn8, blk64[:], g8[:, 4:8])
                    cp(nbg[:], sub(pn, A(pn)[:, 0:8]), e='act')
                    yield
                    ttn(dbias[:], g8[:, 0:4], nbg[:, 0:4], ALU.add)
                    ttn(wgt[:], dbias[:], nbg[:, 4:8], ALU.subtract)
                    act(wgt[:], wgt[:], AF.Exp, bias=math.log(0.125))
                    yield
                    for kb in range(2):
                        p_ = psq()
                        S.op('pe', lambda E, kb=kb, p_=p_: E.transpose(out=A(p_), in_=QKf[:, 2 + kb, :], identity=ident[:]), r=[QKf, ident], w=[p_])
                        for hh in range(2):
                            h = kb * 2 + hh
                            tsc(Kw[:, h, :], sub(p_, A(p_)[:, hh * 64:(hh + 1) * 64]), wgt[:, h:h + 1], None, ALU.mult)
                    yield

                def g_mlstm_rest():
                    def g_head(h):
                        hb = (h % 2) * 64
                        hs = slice(hb, hb + 64)
                        qb = h // 2
                        L_, D_, E_, P_ = lfb[h % 2], Dm[h % 2], eB[h % 2], Pm[h % 2]
                        tsc(L_[:], ones[:], g8[:, 4 + h:5 + h], None, ALU.mult, e='pool')
                        pbr = psq(); mm(pbr, L_[:], tric[:])
                        act(D_[:], pbr, AF.Exp, bias=dbias[:, h:h + 1], scale=-1.0)
                        act(E_[:], pbr, AF.Exp, scale=-1.0)
                        yield
                        psc = psq(); mm(psc, QKf[hs, 2 + qb, :], QKf[hs, qb, :])
                        ttn(D_[:], D_[:], maskc[:], ALU.mult, e='pool')
                        ttn(P_[:], D_[:], psc, ALU.mult)
                        yield
                        ttn(Qz[h][hs, 0, 0:64], QKf[hs, qb, 0:64], E_[hs, 0:64], ALU.mult)
                        ttn(Qz[h][hs, 1, 64:128], QKf[hs, qb, 64:128], E_[hs, 64:128], ALU.mult)
                        pU = psb()
                        u0 = sub(pU, A(pU)[hs, 0:129]); u1 = sub(pU, A(pU)[hs, 256:385])
                        mm(u0, Kw[0:64, h, :], V1[0:64, h, :])
                        stt(CTb[h][hs, :], CTa[h][hs, :], E_[hs, 63:64], u0, ALU.mult, ALU.add)
                        yield
                        pO = psb(); o_ = sub(pO, A(pO)[:, 0:129])
                        mm(o_, P_[:], V1[:, h, :], start=True, stop=False)
                        mm(o_, Qz[h][hs, 0, :], CTa[h][hs, :], start=False, stop=False)
                        mm(o_, Qz[h][hs, 1, :], CTb[h][hs, :], start=False, stop=True)
                        mm(u1, Kw[64:128, h, :], V1[64:128, h, :])
                        stt(CTa[h][hs, :], CTb[h][hs, :], E_[hs, 127:128], u1, ALU.mult, ALU.add)
                        if i > 0:
                            H_ = hraw[h % 2]
                            act(sm[:, 3 * h:3 * h + 1], sub(pO, A(pO)[:, 128:129]), AF.Abs)
                            tsc(sm[:, 3 * h:3 * h + 1], sm[:, 3 * h:3 * h + 1], 1.0, None, ALU.max)
                            recip(sm[:, 3 * h:3 * h + 1], sm[:, 3 * h:3 * h + 1])
                            tsc(H_[:], sub(pO, A(pO)[:, 0:128]), sm[:, 3 * h:3 * h + 1], None, ALU.mult)
                            act(P_[:], H_[:], AF.Square, accum=sm[:, 3 * h + 1:3 * h + 2])
                            yield
                            act(sm[:, 3 * h + 2:3 * h + 3], sm[:, 3 * h + 1:3 * h + 2], AF.Sqrt, bias=1e-6, scale=1.0 / 128)
                            recip(sm[:, 3 * h + 2:3 * h + 3], sm[:, 3 * h + 2:3 * h + 3])
                            yield
                            stt(H_[:], H_[:], sm[:, 3 * h + 2:3 * h + 3], mnw_bc[:, h * 128:(h + 1) * 128], ALU.mult, ALU.mult)
                            ttn(y_b[:, h * 128:(h + 1) * 128], H_[:], sigo[:, h * 128:(h + 1) * 128], ALU.mult)
                        yield
                    yield from inter([g_head(0), g_head(1)])
                    yield from inter([g_head(2), g_head(3)])
                    if i == 0:
                        return
                    for h in range(4):
                        p_ = psq()
                        S.op('pe', lambda E, h=h, p_=p_: E.transpose(out=A(p_), in_=y_b[:, h * 128:(h + 1) * 128], identity=ident[:]), r=[y_b, ident], w=[p_])
                        cp(yT_b[:, 4 + h, :], p_, e='act')
                    yield

                def g_rwkv_prep():
                    yield from inter([g_prep(fb) for fb in range(4)])
                    cp(rkv_raw[:, :, 0:1], rkv_raw[:, :, 128:129], e='pool')
                    yield

                def g_rwkv_rest():
                    yield from inter([g_alg(0), g_alg(1)])
                    yield from g_chain(0)
                    yield from g_chain(1)

                def g_post_all(i=i, yT_b=yT_b, posts=[g_post(fb) for fb in range(4)]):
                    yield from inter(posts[0:2])
                    yield from inter(posts[2:4])
                    dma(yT_d[i * 128:(i + 1) * 128, :], yT_b[:, :, :].rearrange("p b t -> p (b t)"))
                    yield
                for _ in inter([g_rwkv_prep(), g_mlstm_pre()] + pending):
                    pass
                pending = []
                streams = [g_rwkv_rest(), g_mlstm_rest()]
                if i + 1 < NT:
                    streams.append(g_front(i + 1))
                for _ in inter(streams, weights=[2, 1, 1][:len(streams)]):
                    pass
                if i > 0:
                    pending = [g_post_all()]
            for _ in inter(pending):
                pass

            S.barrier()
            chk('p1')
            es2.close()
            es1.close()

            with ExitStack() as es5:
                t5 = lambda name, shape, dt=F32: T(es5, name, shape, dt)
                NB1 = 4
                wout_b = t5("wout_b", [128, 8, D], BF16); wr_f = t5("wr_f", [128, 8, 36]); wffn_bc = t5("wffn_bc", [128, D])
                wst = [t5(f"wst{i}", [128, D]) for i in range(2)]
                xt1 = [t5(f"xt1_{i}", [128, D]) for i in range(NB1)]; yt1 = [t5(f"yt1_{i}", [128, 8, 128], BF16) for i in range(NB1)]
                h1s = [t5(f"h1s{i}", [128, D]) for i in range(NB1)]; big2 = [t5(f"big2_{i}", [128, D]) for i in range(NB1)]
                xn2_bs = [t5(f"xn2_b{i}", [128, D], BF16) for i in range(NB1)]; xn2T = [t5(f"xn2T{i}", [128, 8, 128]) for i in range(NB1)]
                scr = [dict(lgt=t5(f"lgt{i}", [128, 36]), rsm=t5(f"rsm{i}", [128, 32]), oh=[t5(f"oh{k}_{i}", [128, 32]) for k in range(2)],
                            cnt=t5(f"cnt{i}", [128, 32]), el=t5(f"el{i}", [128, 8]), mx8=t5(f"mx8_{i}", [128, 8]), ix8=t5(f"ix8_{i}", [128, 8], U32),
                            sm=t5(f"smb{i}", [128, 16])) for i in range(NB1)]
                carry = t5("carry", [1, 32])
                mset(carry, 0.0)
                dma(wffn_bc[:], nffn_d[0].partition_broadcast(128))
                dma(wr_f[:, :, 0:4], rgw_d[0].rearrange("(c p) e -> p c e", p=128))
                dma(wr_f[:, :, 4:36], rew_d[0].rearrange("(c p) e -> p c e", p=128))
                wout_v = wout_d[0].rearrange("(c p) f -> p c f", p=128)
                for c in range(8):
                    dma(wst[c % 2][:, :], wout_v[:, c, :])
                    cp(wout_b[:, c, :], wst[c % 2][:, :], e=('dve', 'act')[c % 2])

                def loads1b(i):
                    dma(xt1[i % NB1][:, :], x_d[(i - 1) * 128:i * 128, :])
                    dma(yt1[i % NB1][:, :, :].rearrange("p b t -> p (b t)"), yT_d[i * 128:(i + 1) * 128, :])
                def g_A(i):
                    b = i % NB1
                    xt = xt1[b]; yT_b = yt1[b]; h1 = h1s[b]; big = big2[b]; xn2_b = xn2_bs[b]; xT2 = xn2T[b]
                    Z = scr[b]; lgt = Z['lgt']; rsm = Z['rsm']; oh = Z['oh']; cnt = Z['cnt']; el = Z['el']; mx8 = Z['mx8']; ix8 = Z['ix8']; sm = Z['sm']
                    for n in range(2):
                        pm_ = psb()
                        for blk in range(8):
                            mm(pm_, yT_b[:, blk, :], wout_b[:, blk, n * 512:(n + 1) * 512], start=(blk == 0), stop=(blk == 7))
                        ttn(h1[:, n * 512:(n + 1) * 512], xt[:, n * 512:(n + 1) * 512], pm_, ALU.add)
                    dma(h1_d[i * 128:(i + 1) * 128, :], h1[:, :], q='pool')
                    yield
                    act(big[:], h1[:], AF.Square, accum=sm[:, 14:15])
                    act(sm[:, 15:16], sm[:, 14:15], AF.Sqrt, bias=1e-6, scale=1.0 / D)
                    recip(sm[:, 15:16], sm[:, 15:16])
                    stt(big[:], h1[:], sm[:, 15:16], wffn_bc[:], ALU.mult, ALU.mult)
                    cp(xn2_b[:], big[:], e='pool')
                    yield
                    for half in range(2):
                        pb = psb()
                        for j in range(4):
                            c = half * 4 + j
                            S.op('pe', lambda E, c=c, j=j, pb=pb, big=big: E.transpose(out=A(pb)[:, j * 128:(j + 1) * 128], in_=big[:, c * 128:(c + 1) * 128], identity=ident[:]),
                                 r=[big, ident], w=[pb])
                        cp(xT2[:, half * 4:half * 4 + 4, :], sub(pb, A(pb).rearrange("p (j t) -> p j t", t=128)), e='act')
                        yield
                    pl = psq(); pl36 = sub(pl, A(pl)[:, 0:36])
                    for c in range(8):
                        mm(pl36, xT2[:, c, :], wr_f[:, c, :], start=(c == 0), stop=(c == 7))
                    ttn(lgt[:], pl36, rb_bc[:], ALU.add)
                    yield
                    S.op('dve', lambda E: E.tensor_reduce(out=sm[:, 4:5], in_=lgt[:, 0:4], axis=AX.X, op=ALU.max, negate=True), r=[lgt], w=[sm])
                    act(rsm[:, 0:4], lgt[:, 0:4], AF.Exp, bias=sm[:, 4:5], accum=sm[:, 5:6])
                    recip(sm[:, 5:6], sm[:, 5:6])
                    yield
                    tsc(sm[:, 4:5], sm[:, 4:5], -1.0, None, ALU.mult)
                    tsc(rsm[:, 4:8], lgt[:, 0:4], sm[:, 4:5], None, ALU.is_equal)
                    yield
                    tsc(el[:], lgt[:, 4:12], rsm[:, 4:5], None, ALU.mult)
                    for g in range(1, 4):
                        stt(el[:], lgt[:, 4 + g * 8:12 + g * 8], rsm[:, 4 + g:5 + g], el[:], ALU.mult, ALU.add)
                    ttn(rsm[:, 8:12], rsm[:, 4:8], giota[:], ALU.mult)
                    S.op('dve', lambda E: E.tensor_reduce(out=sm[:, 6:7], in_=rsm[:, 8:12], axis=AX.X, op=ALU.add), r=[rsm], w=[sm])
                    yield
                    S.op('dve', lambda E: E.max(out=mx8[:], in_=el[:]), r=[el], w=[mx8])
                    S.op('dve', lambda E: E.max_index(out=ix8[:], in_max=mx8[:], in_values=el[:]), r=[mx8, el], w=[ix8])
                    yield
                    cp(sm[:, 8:10], ix8[:, 0:2])
                    tsc(sm[:, 8:10], sm[:, 8:10], sm[:, 6:7], None, ALU.add)
                    yield
                    ttn(sm[:, 10:11], mx8[:, 1:2], mx8[:, 0:1], ALU.subtract)
                    act(sm[:, 10:11], sm[:, 10:11], AF.Exp)
                    tsc(sm[:, 11:12], sm[:, 10:11], 1.0, None, ALU.add)
                    recip(sm[:, 11:12], sm[:, 11:12])
                    yield
                    ttn(gates_all[:, i, 0:1], sm[:, 5:6], sm[:, 11:12], ALU.mult)
                    ttn(gates_all[:, i, 1:2], gates_all[:, i, 0:1], sm[:, 10:11], ALU.mult)
                    for k in range(2):
                        tsc(oh[k][:], iota_f[:], sm[:, 8 + k:9 + k], None, ALU.is_equal)
                    ttn(cnt[:], oh[0][:], oh[1][:], ALU.add)
                    yield
                    yield

                def g_B(i):
                    b = i % NB1
                    xt = xt1[b]; yT_b = yt1[b]; h1 = h1s[b]; big = big2[b]; xn2_b = xn2_bs[b]; xT2 = xn2T[b]
                    Z = scr[b]; lgt = Z['lgt']; rsm = Z['rsm']; oh = Z['oh']; cnt = Z['cnt']; el = Z['el']; mx8 = Z['mx8']; ix8 = Z['ix8']; sm = Z['sm']
                    pp = psq(); pp32 = sub(pp, A(pp)[:, 0:32])
                    mm(pp32, msu[:], cnt[:], start=True, stop=False)
                    mm(pp32, ones[0:1, :], carry[0:1, :], start=False, stop=True)
                    for k in range(2):
                        ttn(rsm[:], oh[k][:], pp32, ALU.mult)
                        S.op('dve', lambda E, k=k: E.tensor_reduce(out=sm[:, 12 + k:13 + k], in_=rsm[:], axis=AX.X, op=ALU.add), r=[rsm], w=[sm])
                    tsc(sm[:, 12:14], sm[:, 12:14], float(CAP - 1), None, ALU.min)
                    stt(sm[:, 12:14], sm[:, 8:10], float(CAP), sm[:, 12:14], ALU.mult, ALU.add)
                    cp(slots_all[:, i, :], sm[:, 12:14])
                    yield
                    pc = psq(); pc32 = sub(pc, A(pc)[0:1, 0:32])
                    mm(pc32, ones[:, 0:1], cnt[:])
                    ttn(carry[0:1, :], carry[0:1, :], pc32, ALU.add)
                    yield
                    for k in range(2):
                        S.dma('pool', lambda E, k=k, i=i: E.indirect_dma_start(
                            out=xs_d[:, :], out_offset=bass.IndirectOffsetOnAxis(ap=slots_all[:, i, k:k + 1], axis=0),
                            in_=xn2_b[:, :], in_offset=None), r=[xn2_b, slots_all], w=['xs_scr'])
                    yield

                def g_tile(i):
                    loads1b(i)
                    yield
                    yield from g_A(i)
                    yield from g_B(i)
                active = []
                nxt_tile = 1
                rnd = 0
                while active or nxt_tile < NT:
                    if nxt_tile < NT and len(active) < NB1 and rnd % 4 == 0:
                        active.append(g_tile(nxt_tile)); nxt_tile += 1
                    for g in list(active):
                        try:
                            next(g)
                        except StopIteration:
                            active.remove(g)
                    rnd += 1
                S.barrier()
                chk('p1b')

            with ExitStack() as es3:
                t3 = lambda name, shape, dt=F32: T(es3, name, shape, dt)
                NSUB = CAP // 128
                wstg = [t3(f"ewstg{i}", [128, 2, D]) for i in range(6)]
                wgu_b = [t3(f"wgu_b{i}", [128, 8, D], BF16) for i in range(2)]
                wdn_b = [t3(f"wdn_b{i}", [128, 4, D], BF16) for i in range(2)]
                xsl = [t3(f"xsl{i}", [128, NSUB, D], BF16) for i in range(2)]
                xT = [t3(f"xT{i}", [128, 8, CAP], BF16) for i in range(2)]
                hT = t3("hT", [128, 4, CAP], BF16); gsl = t3("gsl", [128, CAP])
                ysl = [t3(f"ysl{i}", [128, D]) for i in range(2)]
                def wpiece(e, k):
                    g_ = e * 6 + k
                    s_ = wstg[g_ % 6]
                    if k < 4:
                        src = wgu_d[0, e].rearrange("(c p) f -> p c f", p=128)[:, 2 * k:2 * k + 2, :]
                        dst = wgu_b[e % 2][:, 2 * k:2 * k + 2, :]
                    else:
                        src = wdn_d[0, e].rearrange("(c p) f -> p c f", p=128)[:, 2 * (k - 4):2 * (k - 4) + 2, :]
                        dst = wdn_b[e % 2][:, 2 * (k - 4):2 * (k - 4) + 2, :]
                    return (lambda: dma(s_[:, :, :], src)), (lambda: cp(dst, s_[:, :, :], e=('dve', 'act')[g_ % 2]))

                def xload(e):
                    dma(xsl[e % 2][:, :, :], xs_d[e * CAP:(e + 1) * CAP, :].rearrange("(m p) f -> p m f", p=128), q='pool')

                for k in range(6):
                    d_, c_ = wpiece(0, k)
                    d_(); c_()
                xload(0)
                for e in range(NEXP):
                    Wg = wgu_b[e % 2]; Wd = wdn_b[e % 2]
                    X = xsl[e % 2]; XT = xT[e % 2]
                    if e + 1 < NEXP:
                        xload(e + 1)
                    steps = []

                    def st_tr(m, X=X, XT=XT):
                        for half in range(2):
                            pb = psb()
                            pbv = A(pb).bitcast(BF16)
                            for j in range(4):
                                c = half * 4 + j
                                S.op('pe', lambda E, c=c, j=j, m=m, pbv=pbv, X=X: E.transpose(out=pbv[:, j * 128:(j + 1) * 128], in_=X[:, m, c * 128:(c + 1) * 128], identity=identb[:]),
                                     r=[X, identb], w=[pb])
                            cp(XT[:, half * 4:half * 4 + 4, m * 128:(m + 1) * 128], sub(pb, pbv[:, 0:512].rearrange("p (j t) -> p j t", t=128)), e='act' if half else 'dve')

                    def st_gu(j, Wg=Wg, XT=XT):
                        pg = psb(); pu = psb()
                        for c in range(8):
                            mm(sub(pg, A(pg)[:, 0:CAP]), Wg[:, c, j * 128:(j + 1) * 128], XT[:, c, :], start=(c == 0), stop=(c == 7))
                        for c in range(8):
                            mm(sub(pu, A(pu)[:, 0:CAP]), Wg[:, c, 512 + j * 128:512 + (j + 1) * 128], XT[:, c, :], start=(c == 0), stop=(c == 7))
                        act(gsl[:, :], sub(pg, A(pg)[:, 0:CAP]), AF.Silu)
                        ttn(hT[:, j, :], gsl[:, :], sub(pu, A(pu)[:, 0:CAP]), ALU.mult)

                    def st_dn(m, Wd=Wd, e=e):
                        Y = ysl[m % 2]
                        for n in range(2):
                            py = psb()
                            for c in range(4):
                                mm(py, hT[:, c, m * 128:(m + 1) * 128], Wd[:, c, n * 512:(n + 1) * 512], start=(c == 0), stop=(c == 3))
                            cp(Y[:, n * 512:(n + 1) * 512], py, e='act' if n else 'dve')
                        dma(ys_d[e * CAP + m * 128:e * CAP + (m + 1) * 128, :], Y[:, :], q='pool')

                    for m in range(NSUB):
                        steps.append(lambda m=m: st_tr(m))
                    for j in range(4):
                        steps.append(lambda j=j: st_gu(j))
                    for m in range(NSUB):
                        steps.append(lambda m=m: st_dn(m))
                    assert len(steps) >= 6
                    casts = []
                    if e + 1 < NEXP:
                        for k in range(6):
                            d_, c_ = wpiece(e + 1, k)
                            d_()
                            casts.append(c_)
                    for si, stp in enumerate(steps):
                        stp()
                        if si < len(casts):
                            casts[si]()
                    for c_ in casts[len(steps):]:
                        c_()
                S.barrier()
                chk('p2')

            with ExitStack() as es4:
                t4 = lambda name, shape, dt=F32: T(es4, name, shape, dt)
                y0 = [t4(f"y0_{i}", [128, D]) for i in range(2)]; y1 = [t4(f"y1_{i}", [128, D]) for i in range(2)]
                hh = [t4(f"hh{i}", [128, D]) for i in range(2)]; jk = t4("jk", [128, D]); s4 = t4("s4", [128, 4])
                ob = [t4(f"ob{i}", [128, D]) for i in range(2)]
                wfin_bc = t4("wfin_bc", [128, D])
                dma(wfin_bc[:], nfin_d.partition_broadcast(128))
                def loads3(i):
                    b = i % 2
                    for k, yk in ((0, y0[b]), (1, y1[b])):
                        S.dma('pool', lambda E, k=k, i=i, yk=yk: E.indirect_dma_start(
                            out=yk[:, :], out_offset=None, in_=ys_d[:, :],
                            in_offset=bass.IndirectOffsetOnAxis(ap=slots_all[:, i, k:k + 1], axis=0)), r=['ys_scr', slots_all], w=[yk])
                    dma(hh[b][:, :], h1_d[i * 128:(i + 1) * 128, :])
                if NT > 1:
                    loads3(1)
                for i in range(1, NT):
                    b = i % 2
                    stt(hh[b][:], y0[b][:], gates_all[:, i, 0:1], hh[b][:], ALU.mult, ALU.add)
                    stt(hh[b][:], y1[b][:], gates_all[:, i, 1:2], hh[b][:], ALU.mult, ALU.add)
                    act(jk[:], hh[b][:], AF.Square, accum=s4[:, 0:1])
                    act(s4[:, 1:2], s4[:, 0:1], AF.Sqrt, bias=1e-6, scale=1.0 / D)
                    recip(s4[:, 1:2], s4[:, 1:2])
                    stt(ob[b][:], hh[b][:], s4[:, 1:2], wfin_bc[:], ALU.mult, ALU.mult)
                    if i + 1 < NT:
                        loads3(i + 1)
                    dma(out_d[(i - 1) * 128:i * 128, :], ob[b][:, :], is_out=True)
                S.finish()
        except Stop:
            S.finish()
        print("instr counts", S.total, "nsem", S.nsem)
    nc._dbg_map = dbg_map
    return nc


_NAMES = ['meta_tokens', 'norm_mix_w', 'norm_ffn_w', 'norm_final_w', 'w_in', 'w_out', 'rwkv_mu_rkv', 'rwkv_mu_wag',
          'rwkv_w0', 'rwkv_w_lora_a', 'rwkv_w_lora_b', 'rwkv_a0', 'rwkv_a_lora_a', 'rwkv_a_lora_b', 'rwkv_g_lora_a',
          'rwkv_g_lora_b', 'rwkv_k_k', 'rwkv_k_a', 'rwkv_r_k', 'rwkv_lnx_w', 'rwkv_lnx_b', 'mlstm_conv_w', 'mlstm_conv_b',
          'mlstm_gate_b', 'mlstm_norm_w', 'router_group_w', 'router_group_b', 'router_expert_w', 'router_expert_b',
          'expert_w_gate_up', 'expert_w_down']


def run(inputs, CAP=512, stop_after=None):
    x = np.asarray(inputs['x'], dtype=np.float32)
    B, L, _ = x.shape
    NT = L // 128 + 1
    nc = build(NT, CAP, stop_after=stop_after)
    shared = {n: np.ascontiguousarray(np.asarray(inputs[n], dtype=np.float32)) for n in _NAMES}
    in_maps = []
    for b in range(B):
        m = dict(shared)
        m['x'] = np.ascontiguousarray(x[b])
        in_maps.append(m)
    res = run_bass_kernel_spmd(nc, in_maps, core_ids=list(range(B)))
    if stop_after:
        d = np.asarray(res.results[0]['dbg'])
        return {k: d[0:v[2], v[0]:v[0] + v[1]] for k, v in nc._dbg_map.items()}
    return np.stack([np.asarray(r['out']).reshape(L, D) for r in res.results], axis=0).astype(np.float32)


def kernel(**inputs):
    return run(inputs, CAP=384)
```

```python
import math
import numpy as np
from contextlib import ExitStack
import concourse.bass as bass
import concourse.mybir as mybir
from concourse.bass_utils import run_bass_kernel_spmd

F32 = mybir.dt.float32
BF16 = mybir.dt.bfloat16
I32 = mybir.dt.int32
U32 = mybir.dt.uint32
AF = mybir.ActivationFunctionType
ALU = mybir.AluOpType
AX = mybir.AxisListType

D = 1024
DBG_TILE = 0
DIN = 3080
NEXP = 32
C0 = math.exp(-0.5)


class V:
    def __init__(self, ap, key):
        self.ap = ap
        self.key = key


def A(x):
    if isinstance(x, V):
        return x.ap
    if type(x).__name__.endswith('TensorHandle'):
        return x.ap()
    return x


def K(x):
    return x.key if isinstance(x, V) else x.name


class Sched:
    EPOCH = 20000

    def __init__(self, nc, es, n_dma_sems=32):
        self.nc = nc
        self.es = es
        self.eng = {'pe': nc.tensor, 'act': nc.scalar, 'dve': nc.vector,
                    'pool': nc.gpsimd, 'sp': nc.sync}
        self.sem = {}
        self.cnt = {}
        self.nsem = 0
        self.total = {k: 0 for k in self.eng}
        for k in self.eng:
            self._new_sem(k)
        self.waited = {k: {} for k in self.eng}
        self.res = {}
        self.dma_sems = [es.enter_context(nc.semaphore(f"dq{i}")) for i in range(n_dma_sems)]
        self.dma_cnt = [0] * n_dma_sems
        self.dma_rr = 0
        self.out_tokens = []

    def _new_sem(self, k):
        self.nsem += 1
        self.sem[k] = self.es.enter_context(self.nc.semaphore(f"s_{k}_{self.nsem}"))
        self.cnt[k] = 0

    def _need(self, reads, writes):
        need = []
        for key in reads:
            st = self.res.get(key)
            if st is not None and st['w'] is not None:
                need.append((st['w'], 'raw'))
        for key in writes:
            st = self.res.get(key)
            if st is not None:
                if st['w'] is not None:
                    need.append((st['w'], 'waw'))
                need.extend((t, 'war') for t in st['r'].values())
        return need

    import os as _os
    SAME_ENGINE_GAP = int(_os.environ.get('SE_GAP', 16))
    SKIP_WAX = int(_os.environ.get('SE_SKIPWAX', 1))
    SKIP_ENG = _os.environ.get('SE_ENG', 'dve,act,pool').split(',')

    def _emit_waits(self, e, need):
        for item in need:
            tok, kind = item if isinstance(item[0], tuple) else (item, 'raw')
            sem, val, src = tok[0], tok[1], tok[2]
            if src == 'pe' and e == 'pe':
                continue
            if src == e and src != 'dma':
                if e in self.SKIP_ENG:
                    if kind != 'raw' and self.SKIP_WAX:
                        continue
                    if kind == 'raw' and self.total[e] - tok[3] >= self.SAME_ENGINE_GAP:
                        continue
            w = self.waited[e]
            if w.get(id(sem), 0) >= val:
                continue
            self.eng[e].wait_ge(sem, val)
            w[id(sem)] = val

    def _record(self, tok, reads, writes):
        for key in reads:
            st = self.res.setdefault(key, {'w': None, 'r': {}})
            st['r'][id(tok[0])] = tok
        for key in writes:
            self.res[key] = {'w': tok, 'r': {}}

    def op(self, e, fn, r=(), w=()):
        r = [K(k) if not isinstance(k, (str, tuple)) else k for k in r]
        w = [K(k) if not isinstance(k, (str, tuple)) else k for k in w]
        w = w + [k for k in r if isinstance(k, str) and k.startswith('pb') and k not in w]
        if self.cnt[e] >= self.EPOCH:
            self._new_sem(e)
        self._emit_waits(e, self._need(r, w))
        inst = fn(self.eng[e])
        self.cnt[e] += 1
        self.total[e] += 1
        inst.then_inc(self.sem[e], 1)
        tok = (self.sem[e], self.cnt[e], e, self.total[e])
        self._record(tok, r, w)
        return tok

    def dma(self, q, fn, r=(), w=(), is_out=False):
        r = [K(k) if not isinstance(k, (str, tuple)) else k for k in r]
        w = [K(k) if not isinstance(k, (str, tuple)) else k for k in w]
        i = self.dma_rr
        self.dma_rr = (self.dma_rr + 1) % len(self.dma_sems)
        sem = self.dma_sems[i]
        need = self._need(r, w)
        if self.dma_cnt[i] > 0:
            need.append(((sem, 16 * self.dma_cnt[i], 'dma', 0), 'raw'))
        self._emit_waits(q, need)
        inst = fn(self.eng[q])
        self.dma_cnt[i] += 1
        inst.then_inc(sem, 16)
        tok = (sem, 16 * self.dma_cnt[i], 'dma', 0)
        self._record(tok, r, w)
        if is_out:
            self.out_tokens.append(tok)
        return tok

    def barrier(self):
        toks = [(self.sem[k], self.cnt[k], k, -10**9) for k in self.eng if self.cnt[k] > 0]
        toks += [(s, 16 * c, 'dma', 0) for s, c in zip(self.dma_sems, self.dma_cnt) if c > 0]
        for e in self.eng:
            self._emit_waits(e, [t for t in toks if t[2] != e])

    def finish(self):
        self._emit_waits('sp', self.out_tokens)
        self.barrier()


class Stop(Exception):
    pass


def inter(gens, weights=None):
    gens = list(gens)
    wts = {id(g): (weights[k] if weights else 1) for k, g in enumerate(gens)}
    while gens:
        for g in list(gens):
            for _ in range(wts[id(g)]):
                try:
                    next(g)
                except StopIteration:
                    gens.remove(g)
                    break
        yield


def build(NT, CAP, stop_after=None, dbgn=8192):
    nc = bass.Bass("TRN2", target_bir_lowering=False)
    NX = NT - 1
    dt_in = lambda name, shape: nc.dram_tensor(name, shape, F32, kind="ExternalInput").ap()
    x_d = dt_in("x", [NX * 128, D])
    meta_d = dt_in("meta_tokens", [16, D])
    nmix_d = dt_in("norm_mix_w", [1, D]); nffn_d = dt_in("norm_ffn_w", [1, D]); nfin_d = dt_in("norm_final_w", [D])
    win_d = dt_in("w_in", [1, D, DIN]); wout_d = dt_in("w_out", [1, D, D])
    murkv_d = dt_in("rwkv_mu_rkv", [1, 3, 512]); muwag_d = dt_in("rwkv_mu_wag", [1, 3, D])
    w0_d = dt_in("rwkv_w0", [1, 512]); wla_d = dt_in("rwkv_w_lora_a", [1, D, 64]); wlb_d = dt_in("rwkv_w_lora_b", [1, 64, 512])
    a0_d = dt_in("rwkv_a0", [1, 512]); ala_d = dt_in("rwkv_a_lora_a", [1, D, 64]); alb_d = dt_in("rwkv_a_lora_b", [1, 64, 512])
    gla_d = dt_in("rwkv_g_lora_a", [1, D, 160]); glb_d = dt_in("rwkv_g_lora_b", [1, 160, 512])
    kk_d = dt_in("rwkv_k_k", [1, 512]); ka_d = dt_in("rwkv_k_a", [1, 512]); rk_d = dt_in("rwkv_r_k", [1, 512])
    lnw_d = dt_in("rwkv_lnx_w", [1, 512]); lnb_d = dt_in("rwkv_lnx_b", [1, 512])
    cw_d = dt_in("mlstm_conv_w", [1, 4, 512]); cb_d = dt_in("mlstm_conv_b", [1, 512])
    gb_d = dt_in("mlstm_gate_b", [1, 8]); mnw_d = dt_in("mlstm_norm_w", [1, 512])
    rgw_d = dt_in("router_group_w", [1, D, 4]); rgb_d = dt_in("router_group_b", [1, 4])
    rew_d = dt_in("router_expert_w", [1, D, 32]); reb_d = dt_in("router_expert_b", [1, 32])
    wgu_d = dt_in("expert_w_gate_up", [1, NEXP, D, D]); wdn_d = dt_in("expert_w_down", [1, NEXP, 512, D])
    out_d = nc.dram_tensor("out", [NX * 128, D], F32, kind="ExternalOutput").ap()
    h1_d = nc.dram_tensor("h1_scr", [NT * 128, D], F32, kind="Internal").ap()
    xs_d = nc.dram_tensor("xs_scr", [NEXP * CAP, D], BF16, kind="Internal").ap()
    ys_d = nc.dram_tensor("ys_scr", [NEXP * CAP, D], F32, kind="Internal").ap()
    yT_d = nc.dram_tensor("yT_scr", [NT * 128, D], BF16, kind="Internal").ap()
    dbg_d = nc.dram_tensor("dbg", [128, dbgn], F32, kind="ExternalOutput").ap() if stop_after else None
    dbg_pos = [0]
    dbg_map = {}

    with ExitStack() as es0:
        S = Sched(nc, es0)

        def dump(name, ap, np_=128):
            if dbg_d is None:
                return
            n = ap.shape[-1] if len(ap.shape) == 2 else int(np.prod(ap.shape[1:]))
            dbg_map[name] = (dbg_pos[0], n, np_)
            S.dma('sp', lambda E: E.dma_start(out=dbg_d[0:np_, dbg_pos[0]:dbg_pos[0] + n], in_=ap, allow_slow_non_contiguous=True), r=[ap], w=['dbg'], is_out=True)
            dbg_pos[0] += n

        def chk(name):
            if stop_after == name:
                raise Stop()
        try:

            def mm(out, lhsT, rhs, start=True, stop=True):
                S.op('pe', lambda E: E.matmul(A(out), lhsT=A(lhsT), rhs=A(rhs), start=start, stop=stop),
                     r=[lhsT, rhs], w=[out])

            def act(out, in_, func, bias=None, scale=None, accum=None, e='act'):
                kw = {}
                rd = [in_]
                if bias is not None:
                    kw['bias'] = A(bias) if not isinstance(bias, float) else bias
                    if not isinstance(bias, float):
                        rd.append(bias)
                if scale is not None:
                    kw['scale'] = A(scale) if not isinstance(scale, float) else scale
                    if not isinstance(scale, float):
                        rd.append(scale)
                wr = [out]
                if accum is not None:
                    kw['accum_out'] = A(accum)
                    wr.append(accum)
                S.op('act', lambda E: E.activation(out=A(out), in_=A(in_), func=func, **kw), r=rd, w=wr)

            def tsc(out, in0, s1, s2, op0, op1=None, e='dve'):
                rd = [in0]
                a1 = s1
                a2 = s2
                if not isinstance(s1, (float, int)):
                    rd.append(s1); a1 = A(s1)
                if s2 is not None and not isinstance(s2, (float, int)):
                    rd.append(s2); a2 = A(s2)
                kw = {} if op1 is None else {'op1': op1}
                S.op(e, lambda E: E.tensor_scalar(out=A(out), in0=A(in0), scalar1=a1, scalar2=a2, op0=op0, **kw),
                     r=rd, w=[out])

            def ttn(out, a, b, op, e='dve'):
                S.op(e, lambda E: E.tensor_tensor(out=A(out), in0=A(a), in1=A(b), op=op), r=[a, b], w=[out])

            def stt(out, in0, sc, in1, op0, op1):
                rd = [in0, in1]
                a = sc
                if not isinstance(sc, (float, int)):
                    rd.append(sc); a = A(sc)
                S.op('dve', lambda E: E.scalar_tensor_tensor(out=A(out), in0=A(in0), scalar=a, in1=A(in1), op0=op0, op1=op1),
                     r=rd, w=[out])

            def cp(out, in_, e='dve'):
                if e == 'act':
                    S.op('act', lambda E: E.activation(out=A(out), in_=A(in_), func=AF.Copy), r=[in_], w=[out])
                else:
                    S.op(e, lambda E: E.tensor_copy(out=A(out), in_=A(in_)), r=[in_], w=[out])

            def mset(t, val, e='pool'):
                S.op(e, lambda E: E.memset(A(t), val), w=[t])

            def recip(out, in_):
                S.op('dve', lambda E: E.reciprocal(out=A(out), in_=A(in_)), r=[in_], w=[out])

            def dma(out, in_, q='sp', is_out=False):
                S.dma(q, lambda E: E.dma_start(out=A(out), in_=A(in_)), r=[in_], w=[out], is_out=is_out)

            banks = [es0.enter_context(nc.psum_tensor(f"pb{i}", [128, 512], F32)) for i in range(8)]
            st = {'q': 0, 'b': 0}

            def psq():
                i = st['q']; st['q'] = (i + 1) % 16
                b_, q_ = i % 4, (i // 4) % 4
                return V(banks[b_][:, q_ * 128:(q_ + 1) * 128], f"pb{b_}")

            def psb():
                i = st['b']; st['b'] = (i + 1) % 4
                return V(banks[4 + i][:, :], f"pbB{i}")

            def sub(v, ap):
                return V(ap, v.key) if isinstance(v, V) else ap

            T = lambda stack, name, shape, dt=F32: stack.enter_context(nc.sbuf_tensor(name, shape, dt))

            ones = T(es0, "ones", [128, 128]); ident = T(es0, "ident", [128, 128]); identb = T(es0, "identb", [128, 128], BF16)
            msu = T(es0, "msu", [128, 128]); msl = T(es0, "msl", [128, 128]); mst = T(es0, "mst", [128, 64])
            blk64 = T(es0, "blk64", [128, 128]); tric = T(es0, "tric", [128, 128]); maskc = T(es0, "maskc", [128, 128])
            mset(ones, 1.0)
            asel = lambda out, pat, cmp, base, cm, in_=None: S.op('pool', lambda E: E.affine_select(
                out=A(out), in_=A(in_ if in_ is not None else ones[:]), pattern=pat, compare_op=cmp, fill=0.0, base=base, channel_multiplier=cm),
                r=[in_ if in_ is not None else ones], w=[out])
            asel(ident[:], [[-1, 128]], ALU.is_equal, 0, 1)
            cp(identb[:], ident[:], e='pool')
            asel(msu[:], [[1, 128]], ALU.is_gt, 0, -1)
            asel(msl[:], [[-1, 128]], ALU.is_gt, 0, 1)
            asel(mst[0:64, :], [[1, 64]], ALU.is_ge, 0, -1, in_=ones[0:64, 0:64])
            asel(mst[64:128, :], [[1, 64]], ALU.is_ge, 0, -1, in_=ones[64:128, 0:64])
            mset(blk64, 0.0); mset(blk64[0:64, 0:64], 1.0); mset(blk64[64:128, 64:128], 1.0)
            asel(tric[:], [[1, 128]], ALU.is_ge, 0, -1)
            ttn(tric[:], tric[:], blk64[:], ALU.mult, e='pool')
            tsc(maskc[:], tric[:], 0.125, None, ALU.mult, e='pool')

            pstg = T(es0, "pstg", [128, 128]); pv = T(es0, "pv", [128, 128]); pv1 = T(es0, "pv1", [128, 128])
            mset(pstg, 0.0)
            row = {}
            rcur = [0]

            def ldrows(name, ap2d, n):
                row[name] = rcur[0]
                dma(pstg[rcur[0]:rcur[0] + n, :], ap2d)
                rcur[0] += n
            ldrows('nmix', nmix_d[0].rearrange("(c p) -> c p", p=128), 8)
            ldrows('muwag', muwag_d[0].rearrange("j (c p) -> (j c) p", p=128), 24)
            ldrows('murkv', murkv_d[0].rearrange("j (c p) -> (j c) p", p=128), 12)
            for nm, ap in (('w0', w0_d), ('a0', a0_d), ('kk', kk_d), ('ka', ka_d), ('rk', rk_d), ('lnw', lnw_d), ('lnb', lnb_d), ('cb', cb_d)):
                ldrows(nm, ap[0].rearrange("(c p) -> c p", p=128), 4)
            ldrows('cw', cw_d[0].rearrange("j (c p) -> (j c) p", p=128), 16)
            tp = psq()
            S.op('pe', lambda E: E.transpose(out=A(tp), in_=pstg[:], identity=ident[:]), r=[pstg, ident], w=[tp])
            cp(pv[:], tp, e='act')
            tsc(pv1[:], pv[:], -1.0, 1.0, ALU.mult, ALU.add)
            PV = lambda nm, j=0: pv[:, row[nm] + j:row[nm] + j + 1]
            PV1 = lambda nm, j=0: pv1[:, row[nm] + j:row[nm] + j + 1]

            mnw_bc = T(es0, "mnw_bc", [128, 512])
            gb_bc = T(es0, "gb_bc", [128, 8]); rb_bc = T(es0, "rb_bc", [128, 36]); iota_i = T(es0, "iota_i", [128, 32], I32)
            iota_f = T(es0, "iota_f", [128, 32]); giota = T(es0, "giota", [128, 4])

            dma(mnw_bc[:], mnw_d[0].partition_broadcast(128)); dma(gb_bc[:], gb_d[0].partition_broadcast(128))
            dma(rb_bc[:, 0:4], rgb_d[0].partition_broadcast(128)); dma(rb_bc[:, 4:36], reb_d[0].partition_broadcast(128))
            S.op('pool', lambda E: E.iota(iota_i[:], pattern=[[1, 32]], base=0, channel_multiplier=0), w=[iota_i])
            cp(iota_f[:], iota_i[:], e='pool')
            tsc(giota[:], iota_f[:, 0:4], 8.0, None, ALU.mult, e='pool')

            gates_all = T(es0, "gates_all", [128, NT, 2]); slots_all = T(es0, "slots_all", [128, NT, 2], I32)
            es1 = es0.enter_context(ExitStack())
            win_b = T(es1, "win_b", [128, 8, DIN], BF16)
            wla_b = T(es1, "wla_b", [128, 8, 288], BF16); wlamu_b = T(es1, "wlamu_b", [128, 8, 288], BF16)
            lb_b = T(es1, "lb_b", [128, 512], BF16); glb0_b = T(es1, "glb0_b", [128, 512], BF16); glb1_b = T(es1, "glb1_b", [32, 512], BF16)
            with ExitStack() as esl:
                stg = [T(esl, f"wstg{i}", [128, DIN]) for i in range(2)]
                win_v = win_d[0].rearrange("(c p) f -> p c f", p=128)
                ceng = ['dve', 'pool', 'act']
                k = 0
                for c in range(8):
                    s_ = stg[k % 2]
                    dma(s_[:, :], win_v[:, c, :])
                    cp(win_b[:, c, :], s_[:, :], e=ceng[k % 3]); k += 1
                s_ = stg[k % 2]; k += 1
                sv = s_[:, 0:8 * 288].rearrange("p (c j) -> p c j", j=288)
                dma(sv[:, :, 0:64], wla_d[0].rearrange("(c p) j -> p c j", p=128))
                dma(sv[:, :, 64:128], ala_d[0].rearrange("(c p) j -> p c j", p=128))
                dma(sv[:, :, 128:288], gla_d[0].rearrange("(c p) j -> p c j", p=128))
                cp(wla_b[:, :, :], sv, e='dve')
                for c in range(8):
                    for j, (lo, hi) in enumerate(((0, 64), (64, 128), (128, 288))):
                        tsc(wlamu_b[:, c, lo:hi], sv[:, c, lo:hi], PV('muwag', j * 8 + c), None, ALU.mult, e='dve' if c % 2 else 'pool')
                s_ = stg[k % 2]; k += 1
                dma(s_[0:64, 0:512], wlb_d[0]); dma(s_[64:128, 0:512], alb_d[0])
                dma(s_[:, 512:1024], glb_d[0][0:128, :]); dma(s_[0:32, 1024:1536], glb_d[0][128:160, :])
                cp(lb_b[:, :], s_[:, 0:512]); cp(glb0_b[:, :], s_[:, 512:1024]); cp(glb1_b[:, :], s_[0:32, 1024:1536])
                S.barrier()
            dump('pv', pv[:, :])
            chk('setup')

            es2 = es1.enter_context(ExitStack())
            t2 = lambda name, shape, dt=F32: T(es2, name, shape, dt)
            x_tm = [t2("x_tm0", [128, D])] * 2
            big = t2("big", [128, D])
            xnT_f = t2("xnT_f", [128, 8, 129]); xnT_b = t2("xnT_b", [128, 8, 128], BF16); xxT_b = t2("xxT_b", [128, 8, 128], BF16)
            rkv_raw = t2("rkv_raw", [128, 12, 129]); qk_raw = t2("qk_raw", [128, 4, 131])
            V1s = [t2(f"V1_{i}", [128, 4, 129]) for i in range(2)]; sigos = [t2(f"sigo{i}", [128, 512]) for i in range(2)]
            laT = t2("laT", [128, 128], BF16); lg0 = t2("lg0", [128, 128], BF16); lg1 = t2("lg1", [32, 128], BF16)
            sgw = t2("sgw", [128, 4, 128]); asig = t2("asig", [128, 4, 128]); ggs = [t2(f"gg{i}", [128, 4, 128]) for i in range(2)]
            bonuss = [t2(f"bonus{i}", [128, 4, 128]) for i in range(2)]; y_as = [t2(f"y_a{i}", [128, 4, 128]) for i in range(2)]
            ptA = [t2(f"ptA{i}", [128, 128]) for i in range(2)]; ptB = [t2(f"ptB{i}", [128, 128]) for i in range(2)]
            yT_bs = [t2(f"yT_b{i}", [128, 8, 128], BF16) for i in range(2)]
            ssq = t2("ssq", [128, 4]); rstd = t2("rstd", [128, 4])
            rb = {nm: t2(f"rb_{nm}", [128, 4, 128]) for nm in ('r', 'k', 'v', 'kkn', 'k2', 'bv', 'tA', 'tB', 'cs', 'E1', 'E2')}
            STg = t2("STg", [128, 4, 128])
            opzT = t2("opzT", [128, 4, 2, 4, 128], BF16)
            rstT = t2("rstT", [128, 4, 128], BF16)
            gC = t2("gC", [128, 4, 2])
            ArbArk = t2("alg_ArbArk", [128, 2, 4, 64], BF16)
            Gc = [{nm: t2(f"alg{c}_{nm}", [128, 4, 128], BF16) for nm in
                   ('BzT', 'KzT', 'VzT', 'Aak', 'Q0', 'Q1', 'QT0', 'QT1', 'P0', 'P1', 'PT0', 'PT1')} for c in range(2)]
            GS = {nm: t2(f"alg_{nm}", [128, 4, 128], BF16) for nm in ('W0T', 'UT')}
            ArbArks = [ArbArk, t2("alg_ArbArk1", [128, 2, 4, 64], BF16)]
            ST32 = t2("ST32", [128, 4, 128])
            STb = t2("STb", [128, 4, 128], BF16)
            QKf = t2("QKf", [128, 4, 128]); cacc = QKf
            g8s = [t2(f"g8_{i}", [128, 8]) for i in range(2)]; th8 = t2("th8", [128, 8]); nbg = t2("nbg", [128, 8]); wgt = t2("wgt", [128, 4]); dbias = t2("dbias", [128, 4])
            lfb = [t2(f"lfb{i}", [128, 128]) for i in range(2)]; Dm = [t2(f"Dm{i}", [128, 128]) for i in range(2)]
            eB = [t2(f"eB{i}", [128, 128]) for i in range(2)]; Pm = [t2(f"Pm{i}", [128, 128]) for i in range(2)]
            Qz = [t2(f"Qz{h}", [128, 2, 128]) for h in range(4)]
            Kw = t2("Kw", [128, 4, 64])
            CTa = [t2(f"CTa{h}", [128, 129]) for h in range(4)]; CTb = [t2(f"CTb{h}", [128, 129]) for h in range(4)]
            hraw = [t2(f"hraw{i}", [128, 128]) for i in range(2)]; y_b = t2("y_b", [128, 512])
            sm = t2("sm", [128, 16])

            mset(xnT_f[:, :, 0:1], 0.0); mset(rkv_raw[:, :, 0:1], 0.0); mset(qk_raw[:, :, 0:3], 0.0)
            mset(V1s[0][:, :, 128:129], 1.0); mset(V1s[1][:, :, 128:129], 1.0)
            mset(ST32, 0.0); mset(STb, 0.0)
            mset(opzT, 0.0)
            for h in range(4):
                mset(Qz[h], 0.0); mset(CTa[h], 0.0); mset(CTb[h], 0.0)

            def g_front(i):
                xt = x_tm[i % 2]; V1 = V1s[i % 2]; sigo = sigos[i % 2]; g8 = g8s[i % 2]; gg = ggs[i % 2]
                if i == 0:
                    mset(xt, 0.0)
                    dma(xt[112:128, :], meta_d)
                else:
                    dma(xt[:, :], x_d[(i - 1) * 128:i * 128, :])
                act(big[:], xt[:], AF.Square, accum=ssq[:, 0:1])
                act(rstd[:, 0:1], ssq[:, 0:1], AF.Sqrt, bias=1e-6, scale=1.0 / D)
                recip(rstd[:, 0:1], rstd[:, 0:1])
                tsc(big[:], xt[:], rstd[:, 0:1], None, ALU.mult)
                yield
                for half in range(2):
                    pb = psb()
                    for j in range(4):
                        c = half * 4 + j
                        S.op('pe', lambda E, c=c, j=j, pb=pb: E.transpose(out=A(pb)[:, j * 128:(j + 1) * 128], in_=big[:, c * 128:(c + 1) * 128], identity=ident[:]),
                             r=[big, ident], w=[pb])
                    for j in range(4):
                        c = half * 4 + j
                        if j % 2 == 0:
                            act(xnT_f[:, c, 1:129], sub(pb, A(pb)[:, j * 128:(j + 1) * 128]), AF.Copy, scale=PV('nmix', c))
                        else:
                            tsc(xnT_f[:, c, 1:129], sub(pb, A(pb)[:, j * 128:(j + 1) * 128]), PV('nmix', c), None, ALU.mult)
                    yield
                cp(xnT_b[:, :, :], xnT_f[:, :, 1:129], e='pool')
                ttn(xxT_b[:, :, :], xnT_f[:, :, 0:128], xnT_f[:, :, 1:129], ALU.subtract)
                cp(xnT_f[:, :, 0:1], xnT_f[:, :, 128:129], e='pool')
                yield

                for blk in range(16):
                    p_ = psq()
                    for c in range(8):
                        mm(p_, win_b[:, c, blk * 128:(blk + 1) * 128], xnT_b[:, c, :], start=(c == 0), stop=(c == 7))
                    if blk < 12:
                        cp(rkv_raw[:, blk, 1:129], p_, e='act')
                    else:
                        cp(qk_raw[:, blk - 12, 3:131], p_, e='act')
                    yield
                pv_ = psb()
                for c in range(8):
                    mm(pv_, xnT_b[:, c, :], win_b[:, c, 2048:2560], start=(c == 0), stop=(c == 7))
                cp(V1[:, :, 0:128], sub(pv_, A(pv_).rearrange("p (h v) -> p h v", v=128)), e='act')
                yield
                po_ = psb()
                for c in range(8):
                    mm(po_, xnT_b[:, c, :], win_b[:, c, 2560:3072], start=(c == 0), stop=(c == 7))
                act(sigo[:], po_, AF.Sigmoid)
                yield
                pg_ = psq()
                pg8 = sub(pg_, A(pg_)[:, 0:8])
                for c in range(8):
                    mm(pg8, xnT_b[:, c, :], win_b[:, c, 3072:3080], start=(c == 0), stop=(c == 7))
                ttn(g8[:], pg8, gb_bc[:], ALU.add)
                yield
                la_ps = []
                for (lo, hi) in ((0, 128), (128, 256), (256, 288)):
                    p_ = psq()
                    po = sub(p_, A(p_)[0:hi - lo, :])
                    for c in range(8):
                        mm(po, wla_b[:, c, lo:hi], xnT_b[:, c, :], start=(c == 0), stop=False)
                    for c in range(8):
                        mm(po, wlamu_b[:, c, lo:hi], xxT_b[:, c, :], start=False, stop=(c == 7))
                    la_ps.append(p_)
                act(laT[0:64, :], sub(la_ps[0], A(la_ps[0])[0:64, :]), AF.Tanh)
                cp(laT[64:128, :], sub(la_ps[0], A(la_ps[0])[64:128, :]), e='dve')
                act(lg0[:, :], la_ps[1], AF.Sigmoid)
                act(lg1[:, :], sub(la_ps[2], A(la_ps[2])[0:32, :]), AF.Sigmoid)
                yield
                for fb in range(4):
                    fs = slice(fb * 128, (fb + 1) * 128)
                    p_ = psq(); mm(p_, lb_b[0:64, fs], laT[0:64, :])
                    act(sgw[:, fb, :], p_, AF.Sigmoid, bias=PV('w0', fb))
                    p_ = psq(); mm(p_, lb_b[64:128, fs], laT[64:128, :])
                    act(asig[:, fb, :], p_, AF.Sigmoid, bias=PV('a0', fb))
                    p_ = psq(); mm(p_, glb0_b[:, fs], lg0[:, :], start=True, stop=False); mm(p_, glb1_b[0:32, fs], lg1[0:32, :], start=False, stop=True)
                    cp(gg[:, fb, :], p_, e='dve')
                    yield

                yield

            pending = []
            for i in range(NT):
                xt = x_tm[i % 2]; V1 = V1s[i % 2]; sigo = sigos[i % 2]; g8 = g8s[i % 2]; gg = ggs[i % 2]
                yT_b = yT_bs[i % 2]; bonus = bonuss[i % 2]; y_a = y_as[i % 2]
                if i == 0:
                    for _ in g_front(0):
                        pass
                def g_prep_all():
                    R = rb
                    P4 = lambda nm, j=0: pv[:, row[nm] + j:row[nm] + j + 4].unsqueeze(2).broadcast_to([128, 4, 128])
                    P41 = lambda nm, j=0: pv1[:, row[nm] + j:row[nm] + j + 4].unsqueeze(2).broadcast_to([128, 4, 128])
                    for nm, bi, tmp in (('k', 1, 'tA'), ('r', 0, 'tB'), ('v', 2, 'E2')):
                        ttn(R[tmp][:, :, :], rkv_raw[:, bi * 4:bi * 4 + 4, 0:128], P4('murkv', bi * 4), ALU.mult, e='pool')
                        ttn(R[nm][:, :, :], rkv_raw[:, bi * 4:bi * 4 + 4, 1:129], P41('murkv', bi * 4), ALU.mult)
                        ttn(R[nm][:, :, :], R[nm][:, :, :], R[tmp][:, :, :], ALU.add)
                    yield
                    ttn(R['kkn'][:, :, :], R['k'][:, :, :], P4('kk'), ALU.mult)
                    ttn(R['tA'][:, :, :], R['kkn'][:, :, :], R['kkn'][:, :, :], ALU.mult, e='pool')
                    bk = psb()
                    for fb in range(4):
                        mm(sub(bk, A(bk)[:, fb * 128:(fb + 1) * 128]), blk64[:], R['tA'][:, fb, :])
                    act(R['tB'][:, :, :], sub(bk, A(bk).rearrange("p (f t) -> p f t", t=128)), AF.Sqrt)
                    for fb in range(4):
                        for c in range(2):
                            cs_ = slice(c * 64, (c + 1) * 64)
                            S.op('dve', lambda E, fb=fb, cs_=cs_: E.tensor_tensor_scan(out=R['cs'][:, fb, cs_], data0=ones[:, 0:64], data1=sgw[:, fb, cs_], initial=0.0, op0=ALU.mult, op1=ALU.add),
                                 r=[ones, sgw], w=[R['cs']])
                    yield
                    tsc(R['tB'][:, :, :], R['tB'][:, :, :], 1e-12, None, ALU.max)
                    recip(R['tB'][:, :, :], R['tB'][:, :, :])
                    ttn(R['kkn'][:, :, :], R['kkn'][:, :, :], R['tB'][:, :, :], ALU.mult)
                    act(R['E1'][:, :, :], R['cs'][:, :, :], AF.Exp, scale=-C0)
                    act(R['E2'][:, :, :], R['cs'][:, :, :], AF.Exp, scale=C0)
                    ttn(R['tA'][:, :, :], asig[:, :, :], P4('ka'), ALU.mult, e='pool')
                    ttn(R['tA'][:, :, :], R['tA'][:, :, :], P41('ka'), ALU.add, e='pool')
                    yield
                    ttn(R['k2'][:, :, :], R['k'][:, :, :], R['tA'][:, :, :], ALU.mult)
                    ttn(R['bv'][:, :, :], R['kkn'][:, :, :], asig[:, :, :], ALU.mult, e='pool')
                    ttn(R['tB'][:, :, :], R['cs'][:, :, :], sgw[:, :, :], ALU.subtract, e='pool')
                    act(R['tB'][:, :, :], R['tB'][:, :, :], AF.Exp, scale=-C0)
                    ttn(R['tA'][:, :, :], R['r'][:, :, :], P4('rk'), ALU.mult, e='pool')
                    ttn(R['tA'][:, :, :], R['tA'][:, :, :], R['k2'][:, :, :], ALU.mult)
                    bk = psb()
                    for fb in range(4):
                        mm(sub(bk, A(bk)[:, fb * 128:(fb + 1) * 128]), blk64[:], R['tA'][:, fb, :])
                    ttn(bonus[:, :, :], sub(bk, A(bk).rearrange("p (f t) -> p f t", t=128)), R['v'][:, :, :], ALU.mult)
                    yield
                    E3 = R['tB']
                    for c in range(2):
                        for hh in range(2):
                            ps_ = slice(hh * 64, (hh + 1) * 64)
                            ts_ = slice(c * 64, (c + 1) * 64)
                            os_ = slice(hh * 64, (hh + 1) * 64)
                            stt(opzT[ps_, :, c, 0, os_], R['kkn'][ps_, :, ts_], -1.0, E3[ps_, :, ts_], ALU.mult, ALU.mult)
                            ttn(opzT[ps_, :, c, 1, os_], R['bv'][ps_, :, ts_], R['E2'][ps_, :, ts_], ALU.mult)
                            ttn(opzT[ps_, :, c, 2, os_], R['k2'][ps_, :, ts_], R['E2'][ps_, :, ts_], ALU.mult, e='pool')
                            cp(opzT[ps_, :, c, 3, os_], R['v'][ps_, :, ts_], e='pool')
                        yield
                    ttn(rstT[:, :, :], R['r'][:, :, :], R['E1'][:, :, :], ALU.mult)
                    cp(gC[:, :, 0:1], R['E1'][:, :, 63:64], e='pool')
                    cp(gC[:, :, 1:2], R['E1'][:, :, 127:128], e='pool')
                    yield

                def q4(bank):
                    return sub(bank, A(bank).rearrange("p (f t) -> p f t", t=128))
                bc4 = lambda m: m[:, :].unsqueeze(1).broadcast_to([128, 4, 128])
                TinvOf = {}
                def g_alg(c):
                    Az = [opzT[:, fb, c, 0, :] for fb in range(4)]; Bz = [opzT[:, fb, c, 1, :] for fb in range(4)]
                    Kz = [opzT[:, fb, c, 2, :] for fb in range(4)]; Vz = [opzT[:, fb, c, 3, :] for fb in range(4)]
                    Rs = [rstT[:, fb, c * 64:(c + 1) * 64] for fb in range(4)]
                    for kk_i, (nm, src) in enumerate((('BzT', Bz), ('KzT', Kz), ('VzT', Vz))):
                        bk = psb()
                        for fb in range(4):
                            mm(sub(bk, A(bk)[:, fb * 128:(fb + 1) * 128]), src[fb], identb[:])
                        cp(Gc[c][nm][:, :, :], q4(bk), e='act' if kk_i != 1 else 'dve')
                        yield
                    for nm, l_, r_, msk in (('Q0', Bz, Az, msu), ('QT0', Az, Bz, msl), ('Aak', Kz, Az, msu)):
                        bk = psb()
                        for fb in range(4):
                            mm(sub(bk, A(bk)[:, fb * 128:(fb + 1) * 128]), l_[fb], r_[fb])
                        ttn(Gc[c][nm][:, :, :], q4(bk), bc4(msk), ALU.mult)
                        yield
                    bk = psb()
                    for j, l_ in enumerate((Bz, Kz)):
                        for fb in range(4):
                            o0 = j * 256 + fb * 64
                            mm(sub(bk, A(bk)[:, o0:o0 + 64]), l_[fb], Rs[fb])
                    ttn(ArbArks[c][:, :, :, :].rearrange("p j f t -> p (j f) t"), sub(bk, A(bk).rearrange("p (g t) -> p g t", t=64)),
                        mst[:, :].unsqueeze(1).broadcast_to([128, 8, 64]), ALU.mult)
                    yield
                    ttn(Gc[c]['P0'][:, :, :], Gc[c]['Q0'][:, :, :], identb[:, :].unsqueeze(1).broadcast_to([128, 4, 128]), ALU.add, e='pool')
                    ttn(Gc[c]['PT0'][:, :, :], Gc[c]['QT0'][:, :, :], identb[:, :].unsqueeze(1).broadcast_to([128, 4, 128]), ALU.add, e='pool')
                    yield
                    Qb = [Gc[c]['Q0'], Gc[c]['Q1']]; QTb = [Gc[c]['QT0'], Gc[c]['QT1']]
                    Pb = [Gc[c]['P0'], Gc[c]['P1']]; PTb = [Gc[c]['PT0'], Gc[c]['PT1']]
                    for s_ in range(6):
                        Qs, QTs = Qb[s_ % 2], QTb[s_ % 2]
                        Qn, QTn = Qb[(s_ + 1) % 2], QTb[(s_ + 1) % 2]
                        Pp, PTp = Pb[(s_ - 1) % 2], PTb[(s_ - 1) % 2]
                        Pc, PTc = Pb[s_ % 2], PTb[s_ % 2]
                        todo = []
                        if s_ <= 4:
                            bk = psb()
                            for fb in range(4):
                                mm(sub(bk, A(bk)[:, fb * 128:(fb + 1) * 128]), QTs[:, fb, :], Qs[:, fb, :])
                            todo.append(lambda bk=bk: cp(Qn[:, :, :], q4(bk), e='act'))
                        if s_ <= 3:
                            bk = psb()
                            for fb in range(4):
                                mm(sub(bk, A(bk)[:, fb * 128:(fb + 1) * 128]), Qs[:, fb, :], QTs[:, fb, :])
                            todo.append(lambda bk=bk: cp(QTn[:, :, :], q4(bk), e='act'))
                        if s_ >= 1:
                            bk = psb()
                            for fb in range(4):
                                mm(sub(bk, A(bk)[:, fb * 128:(fb + 1) * 128]), PTp[:, fb, :], Qs[:, fb, :])
                            todo.append(lambda bk=bk: ttn(Pc[:, :, :], q4(bk), Pp[:, :, :], ALU.add))
                        if 1 <= s_ <= 4:
                            bk = psb()
                            for fb in range(4):
                                mm(sub(bk, A(bk)[:, fb * 128:(fb + 1) * 128]), Qs[:, fb, :], PTp[:, fb, :])
                            todo.append(lambda bk=bk: ttn(PTc[:, :, :], q4(bk), PTp[:, :, :], ALU.add))
                        for f_ in todo:
                            f_()
                        yield
                    cur = 1
                    TinvOf[c] = Gc[c][f'P{cur}']
                    yield
                def g_chain(c):
                    Az = [opzT[:, fb, c, 0, :] for fb in range(4)]; Bz = [opzT[:, fb, c, 1, :] for fb in range(4)]
                    Kz = [opzT[:, fb, c, 2, :] for fb in range(4)]; Vz = [opzT[:, fb, c, 3, :] for fb in range(4)]
                    Rs = [rstT[:, fb, c * 64:(c + 1) * 64] for fb in range(4)]
                    ttn(STg[:, :, :], ST32[:, :, :], gC[:, :, c:c + 1].broadcast_to([128, 4, 128]), ALU.mult, e='pool')
                    bk = psb()
                    for fb in range(4):
                        o_ = sub(bk, A(bk)[:, fb * 128:(fb + 1) * 128])
                        mm(o_, Az[fb], STb[:, fb, :], start=True, stop=False); mm(o_, Gc[c]['Aak'][:, fb, :], Gc[c]['VzT'][:, fb, :], start=False, stop=True)
                    cp(GS['W0T'][:, :, :], q4(bk), e='act')
                    yield
                    bk = psb()
                    for fb in range(4):
                        mm(sub(bk, A(bk)[:, fb * 128:(fb + 1) * 128]), TinvOf[c][:, fb, :], GS['W0T'][:, fb, :])
                    cp(GS['UT'][:, :, :], q4(bk), e='act')
                    yield
                    bkS = psb()
                    for fb in range(4):
                        o_ = sub(bkS, A(bkS)[:, fb * 128:(fb + 1) * 128])
                        mm(o_, Gc[c]['BzT'][:, fb, :], GS['UT'][:, fb, :], start=True, stop=False)
                        mm(o_, Gc[c]['KzT'][:, fb, :], Gc[c]['VzT'][:, fb, :], start=False, stop=True)
                    bkY = psb()
                    for fb in range(4):
                        o_ = sub(bkY, A(bkY)[:, fb * 64:(fb + 1) * 64])
                        mm(o_, STb[:, fb, :], Rs[fb], start=True, stop=False)
                        mm(o_, GS['UT'][:, fb, :], ArbArks[c][:, 0, fb, :], start=False, stop=False)
                        mm(o_, Gc[c]['VzT'][:, fb, :], ArbArks[c][:, 1, fb, :], start=False, stop=True)
                    for fb in range(4):
                        stt(STb[:, fb, :], sub(bkS, A(bkS)[:, fb * 128:(fb + 1) * 128]), gC[:, fb, c:c + 1], STg[:, fb, :], ALU.mult, ALU.add)
                    for fb in range(4):
                        stt(ST32[:, fb, :], sub(bkS, A(bkS)[:, fb * 128:(fb + 1) * 128]), gC[:, fb, c:c + 1], STg[:, fb, :], ALU.mult, ALU.add)
                    cp(y_a[:, :, c * 64:(c + 1) * 64], sub(bkY, A(bkY)[:, 0:256].rearrange("p (f t) -> p f t", t=64)), e='act')
                    yield
                    yield

                def g_post(fb, y_a=y_a, bonus=bonus, gg=gg, yT_b=yT_b):
                    R = {'tA': ptA[fb % 2], 'tB': ptB[fb % 2]}
                    p1 = psq(); mm(p1, blk64[:], y_a[:, fb, :])
                    ttn(R['tA'][:], y_a[:, fb, :], y_a[:, fb, :], ALU.mult, e='pool')
                    p2 = psq(); mm(p2, blk64[:], R['tA'][:])
                    act(R['tB'][:], p1, AF.Copy, scale=1.0 / 64)
                    ttn(R['tA'][:], R['tB'][:], R['tB'][:], ALU.mult)
                    stt(R['tA'][:], p2, 1.0 / 64, R['tA'][:], ALU.mult, ALU.subtract)
                    yield
                    act(R['tA'][:], R['tA'][:], AF.Sqrt, bias=64e-5)
                    yield
                    recip(R['tA'][:], R['tA'][:])
                    yield
                    ttn(R['tB'][:], y_a[:, fb, :], R['tB'][:], ALU.subtract)
                    ttn(R['tB'][:], R['tB'][:], R['tA'][:], ALU.mult)
                    tsc(R['tB'][:], R['tB'][:], PV('lnw', fb), PV('lnb', fb), ALU.mult, ALU.add)
                    ttn(R['tB'][:], R['tB'][:], bonus[:, fb, :], ALU.add)
                    ttn(yT_b[:, fb, :], R['tB'][:], gg[:, fb, :], ALU.mult)
                    yield

                def g_mlstm_pre():
                    for t_ in range(4):
                        for jb in range(4):
                            kj = ('QKf', jb)
                            if t_ == 0:
                                S.op('dve', lambda E, jb=jb: E.tensor_scalar(out=cacc[:, jb, :], in0=qk_raw[:, jb, 0:128], scalar1=PV('cw', jb), scalar2=PV('cb', jb), op0=ALU.mult, op1=ALU.add),
                                     r=[qk_raw, pv], w=[kj, 'QKf'])
                            else:
                                S.op('dve', lambda E, jb=jb, t_=t_: E.scalar_tensor_tensor(out=cacc[:, jb, :], in0=qk_raw[:, jb, t_:t_ + 128], scalar=PV('cw', t_ * 4 + jb), in1=cacc[:, jb, :], op0=ALU.mult, op1=ALU.add),
                                     r=[qk_raw, pv, kj], w=[kj, 'QKf'])
                    S.op('act', lambda E: E.activation(out=QKf[:, :, :], in_=cacc[:, :, :], func=AF.Silu), r=[('QKf', jb) for jb in range(4)] + ['QKf'], w=['QKf'] + [('QKf', jb) for jb in range(4)])
                    yield
                    cp(qk_raw[:, :, 0:3], qk_raw[:, :, 128:131], e='pool')
                    act(th8[:], g8[:], AF.Tanh, scale=1.0 / 15.0)
                    tsc(g8[:, 0:4], th8[:, 0:4], 15.0, None, ALU.mult)
                    act(g8[:, 4:8], th8[:, 4:8], AF.Exp, scale=-15.0)
                    act(g8[:, 4:8], g8[:, 4:8], AF.Ln, bias=1.0)
                    yield
                    if i == 0:
                        mset(g8[0:112, 0:4], -1.0e4, e='dve'); mset(g8[0:112, 4:8], 0.0, e='dve')
                    pn = psq()
                    pn4 = sub(pn, A(pn)[:, 0:4]); pn8 = sub(pn, A(pn)[:, 4:8])
                    mm(pn4, tric[:], g8[:, 4:8]); mm(pn8, blk64[:], g8[:, 4:8])
                    cp(nbg[:], sub(pn, A(pn)[:, 0:8]), e='act')
                    yield
                    ttn(dbias[:], g8[:, 0:4], nbg[:, 0:4], ALU.add)
                    ttn(wgt[:], dbias[:], nbg[:, 4:8], ALU.subtract)
                    act(wgt[:], wgt[:], AF.Exp, bias=math.log(0.125))
                    yield
                    for kb in range(2):
                        p_ = psq()
                        S.op('pe', lambda E, kb=kb, p_=p_: E.transpose(out=A(p_), in_=QKf[:, 2 + kb, :], identity=ident[:]), r=[QKf, ident], w=[p_])
                        for hh in range(2):
                            h = kb * 2 + hh
                            tsc(Kw[:, h, :], sub(p_, A(p_)[:, hh * 64:(hh + 1) * 64]), wgt[:, h:h + 1], None, ALU.mult)
                    yield

                def g_mlstm_rest():
                    def g_head(h):
                        hb = (h % 2) * 64
                        hs = slice(hb, hb + 64)
                        qb = h // 2
                        L_, D_, E_, P_ = lfb[h % 2], Dm[h % 2], eB[h % 2], Pm[h % 2]
                        tsc(L_[:], ones[:], g8[:, 4 + h:5 + h], None, ALU.mult, e='pool')
                        pbr = psq(); mm(pbr, L_[:], tric[:])
                        act(D_[:], pbr, AF.Exp, bias=dbias[:, h:h + 1], scale=-1.0)
                        act(E_[:], pbr, AF.Exp, scale=-1.0)
                        yield
                        psc = psq(); mm(psc, QKf[hs, 2 + qb, :], QKf[hs, qb, :])
                        ttn(D_[:], D_[:], maskc[:], ALU.mult, e='pool')
                        ttn(P_[:], D_[:], psc, ALU.mult)
                        yield
                        ttn(Qz[h][hs, 0, 0:64], QKf[hs, qb, 0:64], E_[hs, 0:64], ALU.mult)
                        ttn(Qz[h][hs, 1, 64:128], QKf[hs, qb, 64:128], E_[hs, 64:128], ALU.mult)
                        pU = psb()
                        u0 = sub(pU, A(pU)[hs, 0:129]); u1 = sub(pU, A(pU)[hs, 256:385])
                        mm(u0, Kw[0:64, h, :], V1[0:64, h, :])
                        stt(CTb[h][hs, :], CTa[h][hs, :], E_[hs, 63:64], u0, ALU.mult, ALU.add)
                        yield
                        pO = psb(); o_ = sub(pO, A(pO)[:, 0:129])
                        mm(o_, P_[:], V1[:, h, :], start=True, stop=False)
                        mm(o_, Qz[h][hs, 0, :], CTa[h][hs, :], start=False, stop=False)
                        mm(o_, Qz[h][hs, 1, :], CTb[h][hs, :], start=False, stop=True)
                        mm(u1, Kw[64:128, h, :], V1[64:128, h, :])
                        stt(CTa[h][hs, :], CTb[h][hs, :], E_[hs, 127:128], u1, ALU.mult, ALU.add)
                        if i > 0:
                            H_ = hraw[h % 2]
                            act(sm[:, 3 * h:3 * h + 1], sub(pO, A(pO)[:, 128:129]), AF.Abs)
                            tsc(sm[:, 3 * h:3 * h + 1], sm[:, 3 * h:3 * h + 1], 1.0, None, ALU.max)
                            recip(sm[:, 3 * h:3 * h + 1], sm[:, 3 * h:3 * h + 1])
                            tsc(H_[:], sub(pO, A(pO)[:, 0:128]), sm[:, 3 * h:3 * h + 1], None, ALU.mult)
                            act(P_[:], H_[:], AF.Square, accum=sm[:, 3 * h + 1:3 * h + 2])
                            yield
                            act(sm[:, 3 * h + 2:3 * h + 3], sm[:, 3 * h + 1:3 * h + 2], AF.Sqrt, bias=1e-6, scale=1.0 / 128)
                            recip(sm[:, 3 * h + 2:3 * h + 3], sm[:, 3 * h + 2:3 * h + 3])
                            yield
                            stt(H_[:], H_[:], sm[:, 3 * h + 2:3 * h + 3], mnw_bc[:, h * 128:(h + 1) * 128], ALU.mult, ALU.mult)
                            ttn(y_b[:, h * 128:(h + 1) * 128], H_[:], sigo[:, h * 128:(h + 1) * 128], ALU.mult)
                        yield
                    yield from inter([g_head(0), g_head(1)])
                    yield from inter([g_head(2), g_head(3)])
                    if i == 0:
                        return
                    for h in range(4):
                        p_ = psq()
                        S.op('pe', lambda E, h=h, p_=p_: E.transpose(out=A(p_), in_=y_b[:, h * 128:(h + 1) * 128], identity=ident[:]), r=[y_b, ident], w=[p_])
                        cp(yT_b[:, 4 + h, :], p_, e='act')
                    yield

                def g_rwkv_prep():
                    yield from g_prep_all()
                    cp(rkv_raw[:, :, 0:1], rkv_raw[:, :, 128:129], e='pool')
                    yield

                def g_rwkv_rest():
                    yield from inter([g_alg(0), g_alg(1)])
                    yield from g_chain(0)
                    yield from g_chain(1)

                def g_post_all(i=i, yT_b=yT_b, posts=[g_post(fb) for fb in range(4)]):
                    yield from inter(posts[0:2])
                    yield from inter(posts[2:4])
                    dma(yT_d[i * 128:(i + 1) * 128, :], yT_b[:, :, :].rearrange("p b t -> p (b t)"))
                    yield
                for _ in inter([g_rwkv_prep(), g_mlstm_pre()] + pending):
                    pass
                pending = []
                streams = [g_rwkv_rest(), g_mlstm_rest()]
                if i + 1 < NT:
                    streams.append(g_front(i + 1))
                for _ in inter(streams, weights=[2, 1, 1][:len(streams)]):
                    pass
                if i > 0:
                    pending = [g_post_all()]
            for _ in inter(pending):
                pass

            S.barrier()
            chk('p1')
            es2.close()
            es1.close()

            with ExitStack() as es5:
                t5 = lambda name, shape, dt=F32: T(es5, name, shape, dt)
                NB1 = 4
                wout_b = t5("wout_b", [128, 8, D], BF16); wr_f = t5("wr_f", [128, 8, 36]); wffn_bc = t5("wffn_bc", [128, D])
                wst = [t5(f"wst{i}", [128, D]) for i in range(2)]
                xt1 = [t5(f"xt1_{i}", [128, D]) for i in range(NB1)]; yt1 = [t5(f"yt1_{i}", [128, 8, 128], BF16) for i in range(NB1)]
                h1s = [t5(f"h1s{i}", [128, D]) for i in range(NB1)]; big2 = [t5(f"big2_{i}", [128, D]) for i in range(NB1)]
                xn2_bs = [t5(f"xn2_b{i}", [128, D], BF16) for i in range(NB1)]; xn2T = [t5(f"xn2T{i}", [128, 8, 128]) for i in range(NB1)]
                scr = [dict(lgt=t5(f"lgt{i}", [128, 36]), rsm=t5(f"rsm{i}", [128, 32]), oh=[t5(f"oh{k}_{i}", [128, 32]) for k in range(2)],
                            cnt=t5(f"cnt{i}", [128, 32]), el=t5(f"el{i}", [128, 8]), mx8=t5(f"mx8_{i}", [128, 8]), ix8=t5(f"ix8_{i}", [128, 8], U32),
                            sm=t5(f"smb{i}", [128, 16])) for i in range(NB1)]
                carry = t5("carry", [1, 32])
                mset(carry, 0.0)
                dma(wffn_bc[:], nffn_d[0].partition_broadcast(128))
                dma(wr_f[:, :, 0:4], rgw_d[0].rearrange("(c p) e -> p c e", p=128))
                dma(wr_f[:, :, 4:36], rew_d[0].rearrange("(c p) e -> p c e", p=128))
                wout_v = wout_d[0].rearrange("(c p) f -> p c f", p=128)
                for c in range(8):
                    dma(wst[c % 2][:, :], wout_v[:, c, :])
                    cp(wout_b[:, c, :], wst[c % 2][:, :], e=('dve', 'act')[c % 2])

                def loads1b(i):
                    dma(xt1[i % NB1][:, :], x_d[(i - 1) * 128:i * 128, :])
                    dma(yt1[i % NB1][:, :, :].rearrange("p b t -> p (b t)"), yT_d[i * 128:(i + 1) * 128, :])
                def g_A(i):
                    b = i % NB1
                    xt = xt1[b]; yT_b = yt1[b]; h1 = h1s[b]; big = big2[b]; xn2_b = xn2_bs[b]; xT2 = xn2T[b]
                    Z = scr[b]; lgt = Z['lgt']; rsm = Z['rsm']; oh = Z['oh']; cnt = Z['cnt']; el = Z['el']; mx8 = Z['mx8']; ix8 = Z['ix8']; sm = Z['sm']
                    for n in range(2):
                        pm_ = psb()
                        for blk in range(8):
                            mm(pm_, yT_b[:, blk, :], wout_b[:, blk, n * 512:(n + 1) * 512], start=(blk == 0), stop=(blk == 7))
                        ttn(h1[:, n * 512:(n + 1) * 512], xt[:, n * 512:(n + 1) * 512], pm_, ALU.add)
                    dma(h1_d[i * 128:(i + 1) * 128, :], h1[:, :], q='pool')
                    yield
                    act(big[:], h1[:], AF.Square, accum=sm[:, 14:15])
                    act(sm[:, 15:16], sm[:, 14:15], AF.Sqrt, bias=1e-6, scale=1.0 / D)
                    recip(sm[:, 15:16], sm[:, 15:16])
                    stt(big[:], h1[:], sm[:, 15:16], wffn_bc[:], ALU.mult, ALU.mult)
                    cp(xn2_b[:], big[:], e='pool')
                    yield
                    for half in range(2):
                        pb = psb()
                        for j in range(4):
                            c = half * 4 + j
                            S.op('pe', lambda E, c=c, j=j, pb=pb, big=big: E.transpose(out=A(pb)[:, j * 128:(j + 1) * 128], in_=big[:, c * 128:(c + 1) * 128], identity=ident[:]),
                                 r=[big, ident], w=[pb])
                        cp(xT2[:, half * 4:half * 4 + 4, :], sub(pb, A(pb).rearrange("p (j t) -> p j t", t=128)), e='act')
                        yield
                    pl = psq(); pl36 = sub(pl, A(pl)[:, 0:36])
                    for c in range(8):
                        mm(pl36, xT2[:, c, :], wr_f[:, c, :], start=(c == 0), stop=(c == 7))
                    ttn(lgt[:], pl36, rb_bc[:], ALU.add)
                    yield
                    S.op('dve', lambda E: E.tensor_reduce(out=sm[:, 4:5], in_=lgt[:, 0:4], axis=AX.X, op=ALU.max, negate=True), r=[lgt], w=[sm])
                    act(rsm[:, 0:4], lgt[:, 0:4], AF.Exp, bias=sm[:, 4:5], accum=sm[:, 5:6])
                    recip(sm[:, 5:6], sm[:, 5:6])
                    yield
                    tsc(sm[:, 4:5], sm[:, 4:5], -1.0, None, ALU.mult)
                    tsc(rsm[:, 4:8], lgt[:, 0:4], sm[:, 4:5], None, ALU.is_equal)
                    yield
                    tsc(el[:], lgt[:, 4:12], rsm[:, 4:5], None, ALU.mult)
                    for g in range(1, 4):
                        stt(el[:], lgt[:, 4 + g * 8:12 + g * 8], rsm[:, 4 + g:5 + g], el[:], ALU.mult, ALU.add)
                    ttn(rsm[:, 8:12], rsm[:, 4:8], giota[:], ALU.mult)
                    S.op('dve', lambda E: E.tensor_reduce(out=sm[:, 6:7], in_=rsm[:, 8:12], axis=AX.X, op=ALU.add), r=[rsm], w=[sm])
                    yield
                    S.op('dve', lambda E: E.max(out=mx8[:], in_=el[:]), r=[el], w=[mx8])
                    S.op('dve', lambda E: E.max_index(out=ix8[:], in_max=mx8[:], in_values=el[:]), r=[mx8, el], w=[ix8])
                    yield
                    cp(sm[:, 8:10], ix8[:, 0:2])
                    tsc(sm[:, 8:10], sm[:, 8:10], sm[:, 6:7], None, ALU.add)
                    yield
                    ttn(sm[:, 10:11], mx8[:, 1:2], mx8[:, 0:1], ALU.subtract)
                    act(sm[:, 10:11], sm[:, 10:11], AF.Exp)
                    tsc(sm[:, 11:12], sm[:, 10:11], 1.0, None, ALU.add)
                    recip(sm[:, 11:12], sm[:, 11:12])
                    yield
                    ttn(gates_all[:, i, 0:1], sm[:, 5:6], sm[:, 11:12], ALU.mult)
                    ttn(gates_all[:, i, 1:2], gates_all[:, i, 0:1], sm[:, 10:11], ALU.mult)
                    for k in range(2):
                        tsc(oh[k][:], iota_f[:], sm[:, 8 + k:9 + k], None, ALU.is_equal)
                    ttn(cnt[:], oh[0][:], oh[1][:], ALU.add)
                    yield
                    yield

                def g_B(i):
                    b = i % NB1
                    xt = xt1[b]; yT_b = yt1[b]; h1 = h1s[b]; big = big2[b]; xn2_b = xn2_bs[b]; xT2 = xn2T[b]
                    Z = scr[b]; lgt = Z['lgt']; rsm = Z['rsm']; oh = Z['oh']; cnt = Z['cnt']; el = Z['el']; mx8 = Z['mx8']; ix8 = Z['ix8']; sm = Z['sm']
                    pp = psq(); pp32 = sub(pp, A(pp)[:, 0:32])
                    mm(pp32, msu[:], cnt[:], start=True, stop=False)
                    mm(pp32, ones[0:1, :], carry[0:1, :], start=False, stop=True)
                    for k in range(2):
                        ttn(rsm[:], oh[k][:], pp32, ALU.mult)
                        S.op('dve', lambda E, k=k: E.tensor_reduce(out=sm[:, 12 + k:13 + k], in_=rsm[:], axis=AX.X, op=ALU.add), r=[rsm], w=[sm])
                    tsc(sm[:, 12:14], sm[:, 12:14], float(CAP - 1), None, ALU.min)
                    stt(sm[:, 12:14], sm[:, 8:10], float(CAP), sm[:, 12:14], ALU.mult, ALU.add)
                    cp(slots_all[:, i, :], sm[:, 12:14])
                    yield
                    pc = psq(); pc32 = sub(pc, A(pc)[0:1, 0:32])
                    mm(pc32, ones[:, 0:1], cnt[:])
                    ttn(carry[0:1, :], carry[0:1, :], pc32, ALU.add)
                    yield
                    for k in range(2):
                        S.dma('pool', lambda E, k=k, i=i: E.indirect_dma_start(
                            out=xs_d[:, :], out_offset=bass.IndirectOffsetOnAxis(ap=slots_all[:, i, k:k + 1], axis=0),
                            in_=xn2_b[:, :], in_offset=None), r=[xn2_b, slots_all], w=['xs_scr'])
                    yield

                def g_tile(i):
                    loads1b(i)
                    yield
                    yield from g_A(i)
                    yield from g_B(i)
                active = []
                nxt_tile = 1
                rnd = 0
                while active or nxt_tile < NT:
                    if nxt_tile < NT and len(active) < NB1 and rnd % 4 == 0:
                        active.append(g_tile(nxt_tile)); nxt_tile += 1
                    for g in list(active):
                        try:
                            next(g)
                        except StopIteration:
                            active.remove(g)
                    rnd += 1
                S.barrier()
                chk('p1b')

            with ExitStack() as es3:
                t3 = lambda name, shape, dt=F32: T(es3, name, shape, dt)
                NSUB = CAP // 128
                wstg = [t3(f"ewstg{i}", [128, 2, D]) for i in range(6)]
                wgu_b = [t3(f"wgu_b{i}", [128, 8, D], BF16) for i in range(2)]
                wdn_b = [t3(f"wdn_b{i}", [128, 4, D], BF16) for i in range(2)]
                xsl = [t3(f"xsl{i}", [128, NSUB, D], BF16) for i in range(2)]
                xT = [t3(f"xT{i}", [128, 8, CAP], BF16) for i in range(2)]
                hT = t3("hT", [128, 4, CAP], BF16); gsl = t3("gsl", [128, CAP])
                ysl = [t3(f"ysl{i}", [128, D]) for i in range(2)]
                def wpiece(e, k):
                    g_ = e * 6 + k
                    s_ = wstg[g_ % 6]
                    if k < 4:
                        src = wgu_d[0, e].rearrange("(c p) f -> p c f", p=128)[:, 2 * k:2 * k + 2, :]
                        dst = wgu_b[e % 2][:, 2 * k:2 * k + 2, :]
                    else:
                        src = wdn_d[0, e].rearrange("(c p) f -> p c f", p=128)[:, 2 * (k - 4):2 * (k - 4) + 2, :]
                        dst = wdn_b[e % 2][:, 2 * (k - 4):2 * (k - 4) + 2, :]
                    return (lambda: dma(s_[:, :, :], src)), (lambda: cp(dst, s_[:, :, :], e=('dve', 'act')[g_ % 2]))

                def xload(e):
                    dma(xsl[e % 2][:, :, :], xs_d[e * CAP:(e + 1) * CAP, :].rearrange("(m p) f -> p m f", p=128), q='pool')

                for k in range(6):
                    d_, c_ = wpiece(0, k)
                    d_(); c_()
                xload(0)
                for e in range(NEXP):
                    Wg = wgu_b[e % 2]; Wd = wdn_b[e % 2]
                    X = xsl[e % 2]; XT = xT[e % 2]
                    if e + 1 < NEXP:
                        xload(e + 1)
                    steps = []

                    def st_tr(m, X=X, XT=XT):
                        for half in range(2):
                            pb = psb()
                            pbv = A(pb).bitcast(BF16)
                            for j in range(4):
                                c = half * 4 + j
                                S.op('pe', lambda E, c=c, j=j, m=m, pbv=pbv, X=X: E.transpose(out=pbv[:, j * 128:(j + 1) * 128], in_=X[:, m, c * 128:(c + 1) * 128], identity=identb[:]),
                                     r=[X, identb], w=[pb])
                            cp(XT[:, half * 4:half * 4 + 4, m * 128:(m + 1) * 128], sub(pb, pbv[:, 0:512].rearrange("p (j t) -> p j t", t=128)), e='act' if half else 'dve')

                    def st_gu(j, Wg=Wg, XT=XT):
                        pg = psb(); pu = psb()
                        for c in range(8):
                            mm(sub(pg, A(pg)[:, 0:CAP]), Wg[:, c, j * 128:(j + 1) * 128], XT[:, c, :], start=(c == 0), stop=(c == 7))
                        for c in range(8):
                            mm(sub(pu, A(pu)[:, 0:CAP]), Wg[:, c, 512 + j * 128:512 + (j + 1) * 128], XT[:, c, :], start=(c == 0), stop=(c == 7))
                        act(gsl[:, :], sub(pg, A(pg)[:, 0:CAP]), AF.Silu)
                        ttn(hT[:, j, :], gsl[:, :], sub(pu, A(pu)[:, 0:CAP]), ALU.mult)

                    def st_dn(m, Wd=Wd, e=e):
                        Y = ysl[m % 2]
                        for n in range(2):
                            py = psb()
                            for c in range(4):
                                mm(py, hT[:, c, m * 128:(m + 1) * 128], Wd[:, c, n * 512:(n + 1) * 512], start=(c == 0), stop=(c == 3))
                            cp(Y[:, n * 512:(n + 1) * 512], py, e='act' if n else 'dve')
                        dma(ys_d[e * CAP + m * 128:e * CAP + (m + 1) * 128, :], Y[:, :], q='pool')

                    for m in range(NSUB):
                        steps.append(lambda m=m: st_tr(m))
                    for j in range(4):
                        steps.append(lambda j=j: st_gu(j))
                    for m in range(NSUB):
                        steps.append(lambda m=m: st_dn(m))
                    assert len(steps) >= 6
                    casts = []
                    if e + 1 < NEXP:
                        for k in range(6):
                            d_, c_ = wpiece(e + 1, k)
                            d_()
                            casts.append(c_)
                    for si, stp in enumerate(steps):
                        stp()
                        if si < len(casts):
                            casts[si]()
                    for c_ in casts[len(steps):]:
                        c_()
                S.barrier()
                chk('p2')

            with ExitStack() as es4:
                t4 = lambda name, shape, dt=F32: T(es4, name, shape, dt)
                y0 = [t4(f"y0_{i}", [128, D]) for i in range(2)]; y1 = [t4(f"y1_{i}", [128, D]) for i in range(2)]
                hh = [t4(f"hh{i}", [128, D]) for i in range(2)]; jk = t4("jk", [128, D]); s4 = t4("s4", [128, 4])
                ob = [t4(f"ob{i}", [128, D]) for i in range(2)]
                wfin_bc = t4("wfin_bc", [128, D])
                dma(wfin_bc[:], nfin_d.partition_broadcast(128))
                def loads3(i):
                    b = i % 2
                    for k, yk in ((0, y0[b]), (1, y1[b])):
                        S.dma('pool', lambda E, k=k, i=i, yk=yk: E.indirect_dma_start(
                            out=yk[:, :], out_offset=None, in_=ys_d[:, :],
                            in_offset=bass.IndirectOffsetOnAxis(ap=slots_all[:, i, k:k + 1], axis=0)), r=['ys_scr', slots_all], w=[yk])
                    dma(hh[b][:, :], h1_d[i * 128:(i + 1) * 128, :])
                if NT > 1:
                    loads3(1)
                for i in range(1, NT):
                    b = i % 2
                    stt(hh[b][:], y0[b][:], gates_all[:, i, 0:1], hh[b][:], ALU.mult, ALU.add)
                    stt(hh[b][:], y1[b][:], gates_all[:, i, 1:2], hh[b][:], ALU.mult, ALU.add)
                    act(jk[:], hh[b][:], AF.Square, accum=s4[:, 0:1])
                    act(s4[:, 1:2], s4[:, 0:1], AF.Sqrt, bias=1e-6, scale=1.0 / D)
                    recip(s4[:, 1:2], s4[:, 1:2])
                    stt(ob[b][:], hh[b][:], s4[:, 1:2], wfin_bc[:], ALU.mult, ALU.mult)
                    if i + 1 < NT:
                        loads3(i + 1)
                    dma(out_d[(i - 1) * 128:i * 128, :], ob[b][:, :], is_out=True)
                S.finish()
        except Stop:
            S.finish()
        print("instr counts", S.total, "nsem", S.nsem)
    nc._dbg_map = dbg_map
    return nc


_NAMES = ['meta_tokens', 'norm_mix_w', 'norm_ffn_w', 'norm_final_w', 'w_in', 'w_out', 'rwkv_mu_rkv', 'rwkv_mu_wag',
          'rwkv_w0', 'rwkv_w_lora_a', 'rwkv_w_lora_b', 'rwkv_a0', 'rwkv_a_lora_a', 'rwkv_a_lora_b', 'rwkv_g_lora_a',
          'rwkv_g_lora_b', 'rwkv_k_k', 'rwkv_k_a', 'rwkv_r_k', 'rwkv_lnx_w', 'rwkv_lnx_b', 'mlstm_conv_w', 'mlstm_conv_b',
          'mlstm_gate_b', 'mlstm_norm_w', 'router_group_w', 'router_group_b', 'router_expert_w', 'router_expert_b',
          'expert_w_gate_up', 'expert_w_down']


def run(inputs, CAP=512, stop_after=None):
    x = np.asarray(inputs['x'], dtype=np.float32)
    B, L, _ = x.shape
    NT = L // 128 + 1
    nc = build(NT, CAP, stop_after=stop_after)
    shared = {n: np.ascontiguousarray(np.asarray(inputs[n], dtype=np.float32)) for n in _NAMES}
    in_maps = []
    for b in range(B):
        m = dict(shared)
        m['x'] = np.ascontiguousarray(x[b])
        in_maps.append(m)
    res = run_bass_kernel_spmd(nc, in_maps, core_ids=list(range(B)))
    if stop_after:
        d = np.asarray(res.results[0]['dbg'])
        return {k: d[0:v[2], v[0]:v[0] + v[1]] for k, v in nc._dbg_map.items()}
    return np.stack([np.asarray(r['out']).reshape(L, D) for r in res.results], axis=0).astype(np.float32)


def kernel(**inputs):
    return run(inputs, CAP=384)
```

```python
import math
import numpy as np
from contextlib import ExitStack
import concourse.bass as bass
import concourse.mybir as mybir
from concourse.bass_utils import run_bass_kernel_spmd

F32 = mybir.dt.float32
BF16 = mybir.dt.bfloat16
I32 = mybir.dt.int32
U32 = mybir.dt.uint32
AF = mybir.ActivationFunctionType
ALU = mybir.AluOpType
AX = mybir.AxisListType

D = 1024
DBG_TILE = 0
DIN = 3080
NEXP = 32
C0 = math.exp(-0.5)


class V:
    def __init__(self, ap, key):
        self.ap = ap
        self.key = key


def A(x):
    if isinstance(x, V):
        return x.ap
    if type(x).__name__.endswith('TensorHandle'):
        return x.ap()
    return x


def K(x):
    return x.key if isinstance(x, V) else x.name


class Sched:
    EPOCH = 20000

    def __init__(self, nc, es, n_dma_sems=32):
        self.nc = nc
        self.es = es
        self.eng = {'pe': nc.tensor, 'act': nc.scalar, 'dve': nc.vector,
                    'pool': nc.gpsimd, 'sp': nc.sync}
        self.sem = {}
        self.cnt = {}
        self.nsem = 0
        self.total = {k: 0 for k in self.eng}
        for k in self.eng:
            self._new_sem(k)
        self.waited = {k: {} for k in self.eng}
        self.res = {}
        self.dma_sems = [es.enter_context(nc.semaphore(f"dq{i}")) for i in range(n_dma_sems)]
        self.dma_cnt = [0] * n_dma_sems
        self.dma_rr = 0
        self.out_tokens = []

    def _new_sem(self, k):
        self.nsem += 1
        self.sem[k] = self.es.enter_context(self.nc.semaphore(f"s_{k}_{self.nsem}"))
        self.cnt[k] = 0

    def _need(self, reads, writes):
        need = []
        for key in reads:
            st = self.res.get(key)
            if st is not None and st['w'] is not None:
                need.append((st['w'], 'raw'))
        for key in writes:
            st = self.res.get(key)
            if st is not None:
                if st['w'] is not None:
                    need.append((st['w'], 'waw'))
                need.extend((t, 'war') for t in st['r'].values())
        return need

    import os as _os
    SAME_ENGINE_GAP = int(_os.environ.get('SE_GAP', 16))
    SKIP_WAX = int(_os.environ.get('SE_SKIPWAX', 1))
    SKIP_ENG = _os.environ.get('SE_ENG', 'dve,act,pool').split(',')

    def _emit_waits(self, e, need):
        for item in need:
            tok, kind = item if isinstance(item[0], tuple) else (item, 'raw')
            sem, val, src = tok[0], tok[1], tok[2]
            if src == 'pe' and e == 'pe':
                continue
            if src == e and src != 'dma':
                if e in self.SKIP_ENG:
                    if kind != 'raw' and self.SKIP_WAX:
                        continue
                    if kind == 'raw' and self.total[e] - tok[3] >= self.SAME_ENGINE_GAP:
                        continue
            w = self.waited[e]
            if w.get(id(sem), 0) >= val:
                continue
            self.eng[e].wait_ge(sem, val)
            w[id(sem)] = val

    def _record(self, tok, reads, writes):
        for key in reads:
            st = self.res.setdefault(key, {'w': None, 'r': {}})
            st['r'][id(tok[0])] = tok
        for key in writes:
            self.res[key] = {'w': tok, 'r': {}}

    def op(self, e, fn, r=(), w=()):
        r = [K(k) if not isinstance(k, (str, tuple)) else k for k in r]
        w = [K(k) if not isinstance(k, (str, tuple)) else k for k in w]
        w = w + [k for k in r if isinstance(k, str) and k.startswith('pb') and k not in w]
        if self.cnt[e] >= self.EPOCH:
            self._new_sem(e)
        self._emit_waits(e, self._need(r, w))
        inst = fn(self.eng[e])
        self.cnt[e] += 1
        self.total[e] += 1
        inst.then_inc(self.sem[e], 1)
        tok = (self.sem[e], self.cnt[e], e, self.total[e])
        self._record(tok, r, w)
        return tok

    def dma(self, q, fn, r=(), w=(), is_out=False):
        r = [K(k) if not isinstance(k, (str, tuple)) else k for k in r]
        w = [K(k) if not isinstance(k, (str, tuple)) else k for k in w]
        i = self.dma_rr
        self.dma_rr = (self.dma_rr + 1) % len(self.dma_sems)
        sem = self.dma_sems[i]
        need = self._need(r, w)
        if self.dma_cnt[i] > 0:
            need.append(((sem, 16 * self.dma_cnt[i], 'dma', 0), 'raw'))
        self._emit_waits(q, need)
        inst = fn(self.eng[q])
        self.dma_cnt[i] += 1
        inst.then_inc(sem, 16)
        tok = (sem, 16 * self.dma_cnt[i], 'dma', 0)
        self._record(tok, r, w)
        if is_out:
            self.out_tokens.append(tok)
        return tok

    def barrier(self):
        toks = [(self.sem[k], self.cnt[k], k, -10**9) for k in self.eng if self.cnt[k] > 0]
        toks += [(s, 16 * c, 'dma', 0) for s, c in zip(self.dma_sems, self.dma_cnt) if c > 0]
        for e in self.eng:
            self._emit_waits(e, [t for t in toks if t[2] != e])

    def finish(self):
        self._emit_waits('sp', self.out_tokens)
        self.barrier()


class Stop(Exception):
    pass


def inter(gens, weights=None):
    gens = list(gens)
    wts = {id(g): (weights[k] if weights else 1) for k, g in enumerate(gens)}
    while gens:
        for g in list(gens):
            for _ in range(wts[id(g)]):
                try:
                    next(g)
                except StopIteration:
                    gens.remove(g)
                    break
        yield


def build(NT, CAP, stop_after=None, dbgn=8192):
    nc = bass.Bass("TRN2", target_bir_lowering=False)
    NX = NT - 1
    dt_in = lambda name, shape: nc.dram_tensor(name, shape, F32, kind="ExternalInput").ap()
    x_d = dt_in("x", [NX * 128, D])
    meta_d = dt_in("meta_tokens", [16, D])
    nmix_d = dt_in("norm_mix_w", [1, D]); nffn_d = dt_in("norm_ffn_w", [1, D]); nfin_d = dt_in("norm_final_w", [D])
    win_d = dt_in("w_in", [1, D, DIN]); wout_d = dt_in("w_out", [1, D, D])
    murkv_d = dt_in("rwkv_mu_rkv", [1, 3, 512]); muwag_d = dt_in("rwkv_mu_wag", [1, 3, D])
    w0_d = dt_in("rwkv_w0", [1, 512]); wla_d = dt_in("rwkv_w_lora_a", [1, D, 64]); wlb_d = dt_in("rwkv_w_lora_b", [1, 64, 512])
    a0_d = dt_in("rwkv_a0", [1, 512]); ala_d = dt_in("rwkv_a_lora_a", [1, D, 64]); alb_d = dt_in("rwkv_a_lora_b", [1, 64, 512])
    gla_d = dt_in("rwkv_g_lora_a", [1, D, 160]); glb_d = dt_in("rwkv_g_lora_b", [1, 160, 512])
    kk_d = dt_in("rwkv_k_k", [1, 512]); ka_d = dt_in("rwkv_k_a", [1, 512]); rk_d = dt_in("rwkv_r_k", [1, 512])
    lnw_d = dt_in("rwkv_lnx_w", [1, 512]); lnb_d = dt_in("rwkv_lnx_b", [1, 512])
    cw_d = dt_in("mlstm_conv_w", [1, 4, 512]); cb_d = dt_in("mlstm_conv_b", [1, 512])
    gb_d = dt_in("mlstm_gate_b", [1, 8]); mnw_d = dt_in("mlstm_norm_w", [1, 512])
    rgw_d = dt_in("router_group_w", [1, D, 4]); rgb_d = dt_in("router_group_b", [1, 4])
    rew_d = dt_in("router_expert_w", [1, D, 32]); reb_d = dt_in("router_expert_b", [1, 32])
    wgu_d = dt_in("expert_w_gate_up", [1, NEXP, D, D]); wdn_d = dt_in("expert_w_down", [1, NEXP, 512, D])
    out_d = nc.dram_tensor("out", [NX * 128, D], F32, kind="ExternalOutput").ap()
    h1_d = nc.dram_tensor("h1_scr", [NT * 128, D], F32, kind="Internal").ap()
    xs_d = nc.dram_tensor("xs_scr", [NEXP * CAP, D], BF16, kind="Internal").ap()
    ys_d = nc.dram_tensor("ys_scr", [NEXP * CAP, D], F32, kind="Internal").ap()
    yT_d = nc.dram_tensor("yT_scr", [NT * 128, D], BF16, kind="Internal").ap()
    dbg_d = nc.dram_tensor("dbg", [128, dbgn], F32, kind="ExternalOutput").ap() if stop_after else None
    dbg_pos = [0]
    dbg_map = {}

    with ExitStack() as es0:
        S = Sched(nc, es0)

        def dump(name, ap, np_=128):
            if dbg_d is None:
                return
            n = ap.shape[-1] if len(ap.shape) == 2 else int(np.prod(ap.shape[1:]))
            dbg_map[name] = (dbg_pos[0], n, np_)
            S.dma('sp', lambda E: E.dma_start(out=dbg_d[0:np_, dbg_pos[0]:dbg_pos[0] + n], in_=ap, allow_slow_non_contiguous=True), r=[ap], w=['dbg'], is_out=True)
            dbg_pos[0] += n

        def chk(name):
            if stop_after == name:
                raise Stop()
        try:

            def mm(out, lhsT, rhs, start=True, stop=True):
                S.op('pe', lambda E: E.matmul(A(out), lhsT=A(lhsT), rhs=A(rhs), start=start, stop=stop),
                     r=[lhsT, rhs], w=[out])

            def act(out, in_, func, bias=None, scale=None, accum=None, e='act'):
                kw = {}
                rd = [in_]
                if bias is not None:
                    kw['bias'] = A(bias) if not isinstance(bias, float) else bias
                    if not isinstance(bias, float):
                        rd.append(bias)
                if scale is not None:
                    kw['scale'] = A(scale) if not isinstance(scale, float) else scale
                    if not isinstance(scale, float):
                        rd.append(scale)
                wr = [out]
                if accum is not None:
                    kw['accum_out'] = A(accum)
                    wr.append(accum)
                S.op('act', lambda E: E.activation(out=A(out), in_=A(in_), func=func, **kw), r=rd, w=wr)

            def tsc(out, in0, s1, s2, op0, op1=None, e='dve'):
                rd = [in0]
                a1 = s1
                a2 = s2
                if not isinstance(s1, (float, int)):
                    rd.append(s1); a1 = A(s1)
                if s2 is not None and not isinstance(s2, (float, int)):
                    rd.append(s2); a2 = A(s2)
                kw = {} if op1 is None else {'op1': op1}
                S.op(e, lambda E: E.tensor_scalar(out=A(out), in0=A(in0), scalar1=a1, scalar2=a2, op0=op0, **kw),
                     r=rd, w=[out])

            def ttn(out, a, b, op, e='dve'):
                S.op(e, lambda E: E.tensor_tensor(out=A(out), in0=A(a), in1=A(b), op=op), r=[a, b], w=[out])

            def stt(out, in0, sc, in1, op0, op1):
                rd = [in0, in1]
                a = sc
                if not isinstance(sc, (float, int)):
                    rd.append(sc); a = A(sc)
                S.op('dve', lambda E: E.scalar_tensor_tensor(out=A(out), in0=A(in0), scalar=a, in1=A(in1), op0=op0, op1=op1),
                     r=rd, w=[out])

            def cp(out, in_, e='dve'):
                if e == 'act':
                    S.op('act', lambda E: E.activation(out=A(out), in_=A(in_), func=AF.Copy), r=[in_], w=[out])
                else:
                    S.op(e, lambda E: E.tensor_copy(out=A(out), in_=A(in_)), r=[in_], w=[out])

            def mset(t, val, e='pool'):
                S.op(e, lambda E: E.memset(A(t), val), w=[t])

            def recip(out, in_):
                S.op('dve', lambda E: E.reciprocal(out=A(out), in_=A(in_)), r=[in_], w=[out])

            def dma(out, in_, q='sp', is_out=False):
                S.dma(q, lambda E: E.dma_start(out=A(out), in_=A(in_)), r=[in_], w=[out], is_out=is_out)

            banks = [es0.enter_context(nc.psum_tensor(f"pb{i}", [128, 512], F32)) for i in range(8)]
            st = {'q': 0, 'b': 0}

            def psq():
                i = st['q']; st['q'] = (i + 1) % 16
                b_, q_ = i % 4, (i // 4) % 4
                return V(banks[b_][:, q_ * 128:(q_ + 1) * 128], f"pb{b_}")

            def psb():
                i = st['b']; st['b'] = (i + 1) % 4
                return V(banks[4 + i][:, :], f"pbB{i}")

            def sub(v, ap):
                return V(ap, v.key) if isinstance(v, V) else ap

            T = lambda stack, name, shape, dt=F32: stack.enter_context(nc.sbuf_tensor(name, shape, dt))

            ones = T(es0, "ones", [128, 128]); ident = T(es0, "ident", [128, 128]); identb = T(es0, "identb", [128, 128], BF16)
            msu = T(es0, "msu", [128, 128]); msl = T(es0, "msl", [128, 128]); mst = T(es0, "mst", [128, 64])
            blk64 = T(es0, "blk64", [128, 128]); tric = T(es0, "tric", [128, 128]); maskc = T(es0, "maskc", [128, 128])
            mset(ones, 1.0)
            asel = lambda out, pat, cmp, base, cm, in_=None: S.op('pool', lambda E: E.affine_select(
                out=A(out), in_=A(in_ if in_ is not None else ones[:]), pattern=pat, compare_op=cmp, fill=0.0, base=base, channel_multiplier=cm),
                r=[in_ if in_ is not None else ones], w=[out])
            asel(ident[:], [[-1, 128]], ALU.is_equal, 0, 1)
            cp(identb[:], ident[:], e='pool')
            asel(msu[:], [[1, 128]], ALU.is_gt, 0, -1)
            asel(msl[:], [[-1, 128]], ALU.is_gt, 0, 1)
            asel(mst[0:64, :], [[1, 64]], ALU.is_ge, 0, -1, in_=ones[0:64, 0:64])
            asel(mst[64:128, :], [[1, 64]], ALU.is_ge, 0, -1, in_=ones[64:128, 0:64])
            mset(blk64, 0.0); mset(blk64[0:64, 0:64], 1.0); mset(blk64[64:128, 64:128], 1.0)
            asel(tric[:], [[1, 128]], ALU.is_ge, 0, -1)
            ttn(tric[:], tric[:], blk64[:], ALU.mult, e='pool')
            tsc(maskc[:], tric[:], 0.125, None, ALU.mult, e='pool')

            pstg = T(es0, "pstg", [128, 128]); pv = T(es0, "pv", [128, 128]); pv1 = T(es0, "pv1", [128, 128])
            mset(pstg, 0.0)
            row = {}
            rcur = [0]

            def ldrows(name, ap2d, n):
                row[name] = rcur[0]
                dma(pstg[rcur[0]:rcur[0] + n, :], ap2d)
                rcur[0] += n
            ldrows('nmix', nmix_d[0].rearrange("(c p) -> c p", p=128), 8)
            ldrows('muwag', muwag_d[0].rearrange("j (c p) -> (j c) p", p=128), 24)
            ldrows('murkv', murkv_d[0].rearrange("j (c p) -> (j c) p", p=128), 12)
            for nm, ap in (('w0', w0_d), ('a0', a0_d), ('kk', kk_d), ('ka', ka_d), ('rk', rk_d), ('lnw', lnw_d), ('lnb', lnb_d), ('cb', cb_d)):
                ldrows(nm, ap[0].rearrange("(c p) -> c p", p=128), 4)
            ldrows('cw', cw_d[0].rearrange("j (c p) -> (j c) p", p=128), 16)
            tp = psq()
            S.op('pe', lambda E: E.transpose(out=A(tp), in_=pstg[:], identity=ident[:]), r=[pstg, ident], w=[tp])
            cp(pv[:], tp, e='act')
            tsc(pv1[:], pv[:], -1.0, 1.0, ALU.mult, ALU.add)
            PV = lambda nm, j=0: pv[:, row[nm] + j:row[nm] + j + 1]
            PV1 = lambda nm, j=0: pv1[:, row[nm] + j:row[nm] + j + 1]

            mnw_bc = T(es0, "mnw_bc", [128, 512])
            gb_bc = T(es0, "gb_bc", [128, 8]); rb_bc = T(es0, "rb_bc", [128, 36]); iota_i = T(es0, "iota_i", [128, 32], I32)
            iota_f = T(es0, "iota_f", [128, 32]); giota = T(es0, "giota", [128, 4])

            dma(mnw_bc[:], mnw_d[0].partition_broadcast(128)); dma(gb_bc[:], gb_d[0].partition_broadcast(128))
            dma(rb_bc[:, 0:4], rgb_d[0].partition_broadcast(128)); dma(rb_bc[:, 4:36], reb_d[0].partition_broadcast(128))
            S.op('pool', lambda E: E.iota(iota_i[:], pattern=[[1, 32]], base=0, channel_multiplier=0), w=[iota_i])
            cp(iota_f[:], iota_i[:], e='pool')
            tsc(giota[:], iota_f[:, 0:4], 8.0, None, ALU.mult, e='pool')

            gates_all = T(es0, "gates_all", [128, NT, 2]); slots_all = T(es0, "slots_all", [128, NT, 2], I32)
            es1 = es0.enter_context(ExitStack())
            win_b = T(es1, "win_b", [128, 8, DIN], BF16)
            wla_b = T(es1, "wla_b", [128, 8, 288], BF16); wlamu_b = T(es1, "wlamu_b", [128, 8, 288], BF16)
            lb_b = T(es1, "lb_b", [128, 512], BF16); glb0_b = T(es1, "glb0_b", [128, 512], BF16); glb1_b = T(es1, "glb1_b", [32, 512], BF16)
            with ExitStack() as esl:
                stg = [T(esl, f"wstg{i}", [128, DIN]) for i in range(2)]
                win_v = win_d[0].rearrange("(c p) f -> p c f", p=128)
                ceng = ['dve', 'pool', 'act']
                k = 0
                for c in range(8):
                    s_ = stg[k % 2]
                    dma(s_[:, :], win_v[:, c, :])
                    cp(win_b[:, c, :], s_[:, :], e=ceng[k % 3]); k += 1
                s_ = stg[k % 2]; k += 1
                sv = s_[:, 0:8 * 288].rearrange("p (c j) -> p c j", j=288)
                dma(sv[:, :, 0:64], wla_d[0].rearrange("(c p) j -> p c j", p=128))
                dma(sv[:, :, 64:128], ala_d[0].rearrange("(c p) j -> p c j", p=128))
                dma(sv[:, :, 128:288], gla_d[0].rearrange("(c p) j -> p c j", p=128))
                cp(wla_b[:, :, :], sv, e='dve')
                for c in range(8):
                    for j, (lo, hi) in enumerate(((0, 64), (64, 128), (128, 288))):
                        tsc(wlamu_b[:, c, lo:hi], sv[:, c, lo:hi], PV('muwag', j * 8 + c), None, ALU.mult, e='dve' if c % 2 else 'pool')
                s_ = stg[k % 2]; k += 1
                dma(s_[0:64, 0:512], wlb_d[0]); dma(s_[64:128, 0:512], alb_d[0])
                dma(s_[:, 512:1024], glb_d[0][0:128, :]); dma(s_[0:32, 1024:1536], glb_d[0][128:160, :])
                cp(lb_b[:, :], s_[:, 0:512]); cp(glb0_b[:, :], s_[:, 512:1024]); cp(glb1_b[:, :], s_[0:32, 1024:1536])
                S.barrier()
            dump('pv', pv[:, :])
            chk('setup')

            es2 = es1.enter_context(ExitStack())
            t2 = lambda name, shape, dt=F32: T(es2, name, shape, dt)
            x_tm = [t2("x_tm0", [128, D])] * 2
            big = t2("big", [128, D])
            xnT_f = t2("xnT_f", [128, 8, 129]); xnT_b = t2("xnT_b", [128, 8, 128], BF16); xxT_b = t2("xxT_b", [128, 8, 128], BF16)
            rkv_raw = t2("rkv_raw", [128, 12, 129]); qk_raw = t2("qk_raw", [128, 4, 131])
            V1s = [t2(f"V1_{i}", [128, 4, 129]) for i in range(2)]; sigos = [t2(f"sigo{i}", [128, 512]) for i in range(2)]
            laT = t2("laT", [128, 128], BF16); lg0 = t2("lg0", [128, 128], BF16); lg1 = t2("lg1", [32, 128], BF16)
            sgw = t2("sgw", [128, 4, 128]); asig = t2("asig", [128, 4, 128]); ggs = [t2(f"gg{i}", [128, 4, 128]) for i in range(2)]
            bonuss = [t2(f"bonus{i}", [128, 4, 128]) for i in range(2)]; y_as = [t2(f"y_a{i}", [128, 4, 128]) for i in range(2)]
            pt4 = t2("pt4", [128, 4, 128])
            yT_bs = [t2(f"yT_b{i}", [128, 8, 128], BF16) for i in range(2)]
            ssq = t2("ssq", [128, 4]); rstd = t2("rstd", [128, 4])
            rb = {nm: t2(f"rb_{nm}", [128, 4, 128]) for nm in ('r', 'k', 'v', 'kkn', 'k2', 'bv', 'tA', 'tB', 'cs', 'E1', 'E2')}
            STg = t2("STg", [128, 4, 128])
            opzT = t2("opzT", [128, 4, 2, 4, 128], BF16)
            rstT = t2("rstT", [128, 4, 128], BF16)
            gC = t2("gC", [128, 4, 2])
            ArbArk = t2("alg_ArbArk", [128, 2, 4, 64], BF16)
            Gc = [{nm: t2(f"alg{c}_{nm}", [128, 4, 128], BF16) for nm in
                   ('BzT', 'KzT', 'VzT', 'Aak', 'Q0', 'Q1', 'QT0', 'QT1', 'P0', 'P1', 'PT0', 'PT1')} for c in range(2)]
            GS = {nm: t2(f"alg_{nm}", [128, 4, 128], BF16) for nm in ('W0T', 'UT')}
            ArbArks = [ArbArk, t2("alg_ArbArk1", [128, 2, 4, 64], BF16)]
            ST32 = t2("ST32", [128, 4, 128])
            STb = t2("STb", [128, 4, 128], BF16)
            QKf = t2("QKf", [128, 4, 128]); cacc = QKf
            g8s = [t2(f"g8_{i}", [128, 8]) for i in range(2)]; th8 = t2("th8", [128, 8]); nbg = t2("nbg", [128, 8]); wgt = t2("wgt", [128, 4]); dbias = t2("dbias", [128, 4])
            lfb = [t2(f"lfb{i}", [128, 128]) for i in range(2)]; Dm = [t2(f"Dm{i}", [128, 128]) for i in range(2)]
            eB = [t2(f"eB{i}", [128, 128]) for i in range(2)]; Pm = [t2(f"Pm{i}", [128, 128]) for i in range(2)]
            Qz = [t2(f"Qz{h}", [128, 2, 128]) for h in range(4)]
            Kw = t2("Kw", [128, 4, 64])
            CTa = [t2(f"CTa{h}", [128, 129]) for h in range(4)]; CTb = [t2(f"CTb{h}", [128, 129]) for h in range(4)]
            hraw = [t2(f"hraw{i}", [128, 128]) for i in range(2)]; y_b = t2("y_b", [128, 512])
            sm = t2("sm", [128, 16])

            mset(xnT_f[:, :, 0:1], 0.0); mset(rkv_raw[:, :, 0:1], 0.0); mset(qk_raw[:, :, 0:3], 0.0)
            mset(V1s[0][:, :, 128:129], 1.0); mset(V1s[1][:, :, 128:129], 1.0)
            mset(ST32, 0.0); mset(STb, 0.0)
            mset(opzT, 0.0)
            for h in range(4):
                mset(Qz[h], 0.0); mset(CTa[h], 0.0); mset(CTb[h], 0.0)

            def g_front(i):
                xt = x_tm[i % 2]; V1 = V1s[i % 2]; sigo = sigos[i % 2]; g8 = g8s[i % 2]; gg = ggs[i % 2]
                if i == 0:
                    mset(xt, 0.0)
                    dma(xt[112:128, :], meta_d)
                else:
                    dma(xt[:, :], x_d[(i - 1) * 128:i * 128, :])
                act(big[:], xt[:], AF.Square, accum=ssq[:, 0:1])
                act(rstd[:, 0:1], ssq[:, 0:1], AF.Sqrt, bias=1e-6, scale=1.0 / D)
                recip(rstd[:, 0:1], rstd[:, 0:1])
                tsc(big[:], xt[:], rstd[:, 0:1], None, ALU.mult)
                yield
                for half in range(2):
                    pb = psb()
                    for j in range(4):
                        c = half * 4 + j
                        S.op('pe', lambda E, c=c, j=j, pb=pb: E.transpose(out=A(pb)[:, j * 128:(j + 1) * 128], in_=big[:, c * 128:(c + 1) * 128], identity=ident[:]),
                             r=[big, ident], w=[pb])
                    for j in range(4):
                        c = half * 4 + j
                        if j % 2 == 0:
                            act(xnT_f[:, c, 1:129], sub(pb, A(pb)[:, j * 128:(j + 1) * 128]), AF.Copy, scale=PV('nmix', c))
                        else:
                            tsc(xnT_f[:, c, 1:129], sub(pb, A(pb)[:, j * 128:(j + 1) * 128]), PV('nmix', c), None, ALU.mult)
                    yield
                cp(xnT_b[:, :, :], xnT_f[:, :, 1:129], e='pool')
                ttn(xxT_b[:, :, :], xnT_f[:, :, 0:128], xnT_f[:, :, 1:129], ALU.subtract)
                cp(xnT_f[:, :, 0:1], xnT_f[:, :, 128:129], e='pool')
                yield

                for blk in range(16):
                    p_ = psq()
                    for c in range(8):
                        mm(p_, win_b[:, c, blk * 128:(blk + 1) * 128], xnT_b[:, c, :], start=(c == 0), stop=(c == 7))
                    if blk < 12:
                        cp(rkv_raw[:, blk, 1:129], p_, e='act')
                    else:
                        cp(qk_raw[:, blk - 12, 3:131], p_, e='act')
                    yield
                pv_ = psb()
                for c in range(8):
                    mm(pv_, xnT_b[:, c, :], win_b[:, c, 2048:2560], start=(c == 0), stop=(c == 7))
                cp(V1[:, :, 0:128], sub(pv_, A(pv_).rearrange("p (h v) -> p h v", v=128)), e='act')
                yield
                po_ = psb()
                for c in range(8):
                    mm(po_, xnT_b[:, c, :], win_b[:, c, 2560:3072], start=(c == 0), stop=(c == 7))
                act(sigo[:], po_, AF.Sigmoid)
                yield
                pg_ = psq()
                pg8 = sub(pg_, A(pg_)[:, 0:8])
                for c in range(8):
                    mm(pg8, xnT_b[:, c, :], win_b[:, c, 3072:3080], start=(c == 0), stop=(c == 7))
                ttn(g8[:], pg8, gb_bc[:], ALU.add)
                yield
                la_ps = []
                for (lo, hi) in ((0, 128), (128, 256), (256, 288)):
                    p_ = psq()
                    po = sub(p_, A(p_)[0:hi - lo, :])
                    for c in range(8):
                        mm(po, wla_b[:, c, lo:hi], xnT_b[:, c, :], start=(c == 0), stop=False)
                    for c in range(8):
                        mm(po, wlamu_b[:, c, lo:hi], xxT_b[:, c, :], start=False, stop=(c == 7))
                    la_ps.append(p_)
                act(laT[0:64, :], sub(la_ps[0], A(la_ps[0])[0:64, :]), AF.Tanh)
                cp(laT[64:128, :], sub(la_ps[0], A(la_ps[0])[64:128, :]), e='dve')
                act(lg0[:, :], la_ps[1], AF.Sigmoid)
                act(lg1[:, :], sub(la_ps[2], A(la_ps[2])[0:32, :]), AF.Sigmoid)
                yield
                for fb in range(4):
                    fs = slice(fb * 128, (fb + 1) * 128)
                    p_ = psq(); mm(p_, lb_b[0:64, fs], laT[0:64, :])
                    act(sgw[:, fb, :], p_, AF.Sigmoid, bias=PV('w0', fb))
                    p_ = psq(); mm(p_, lb_b[64:128, fs], laT[64:128, :])
                    act(asig[:, fb, :], p_, AF.Sigmoid, bias=PV('a0', fb))
                    p_ = psq(); mm(p_, glb0_b[:, fs], lg0[:, :], start=True, stop=False); mm(p_, glb1_b[0:32, fs], lg1[0:32, :], start=False, stop=True)
                    cp(gg[:, fb, :], p_, e='dve')
                    yield

                yield

            pending = []
            for i in range(NT):
                xt = x_tm[i % 2]; V1 = V1s[i % 2]; sigo = sigos[i % 2]; g8 = g8s[i % 2]; gg = ggs[i % 2]
                yT_b = yT_bs[i % 2]; bonus = bonuss[i % 2]; y_a = y_as[i % 2]
                if i == 0:
                    for _ in g_front(0):
                        pass
                def g_prep_all():
                    R = rb
                    P4 = lambda nm, j=0: pv[:, row[nm] + j:row[nm] + j + 4].unsqueeze(2).broadcast_to([128, 4, 128])
                    P41 = lambda nm, j=0: pv1[:, row[nm] + j:row[nm] + j + 4].unsqueeze(2).broadcast_to([128, 4, 128])
                    for nm, bi, tmp in (('k', 1, 'tA'), ('r', 0, 'tB'), ('v', 2, 'E2')):
                        ttn(R[tmp][:, :, :], rkv_raw[:, bi * 4:bi * 4 + 4, 0:128], P4('murkv', bi * 4), ALU.mult, e='pool')
                        ttn(R[nm][:, :, :], rkv_raw[:, bi * 4:bi * 4 + 4, 1:129], P41('murkv', bi * 4), ALU.mult)
                        ttn(R[nm][:, :, :], R[nm][:, :, :], R[tmp][:, :, :], ALU.add)
                    yield
                    ttn(R['kkn'][:, :, :], R['k'][:, :, :], P4('kk'), ALU.mult)
                    ttn(R['tA'][:, :, :], R['kkn'][:, :, :], R['kkn'][:, :, :], ALU.mult, e='pool')
                    bk = psb()
                    for fb in range(4):
                        mm(sub(bk, A(bk)[:, fb * 128:(fb + 1) * 128]), blk64[:], R['tA'][:, fb, :])
                    act(R['tB'][:, :, :], sub(bk, A(bk).rearrange("p (f t) -> p f t", t=128)), AF.Sqrt)
                    for fb in range(4):
                        for c in range(2):
                            cs_ = slice(c * 64, (c + 1) * 64)
                            S.op('dve', lambda E, fb=fb, cs_=cs_: E.tensor_tensor_scan(out=R['cs'][:, fb, cs_], data0=ones[:, 0:64], data1=sgw[:, fb, cs_], initial=0.0, op0=ALU.mult, op1=ALU.add),
                                 r=[ones, sgw], w=[R['cs']])
                    yield
                    tsc(R['tB'][:, :, :], R['tB'][:, :, :], 1e-12, None, ALU.max)
                    recip(R['tB'][:, :, :], R['tB'][:, :, :])
                    ttn(R['kkn'][:, :, :], R['kkn'][:, :, :], R['tB'][:, :, :], ALU.mult)
                    act(R['E1'][:, :, :], R['cs'][:, :, :], AF.Exp, scale=-C0)
                    act(R['E2'][:, :, :], R['cs'][:, :, :], AF.Exp, scale=C0)
                    ttn(R['tA'][:, :, :], asig[:, :, :], P4('ka'), ALU.mult, e='pool')
                    ttn(R['tA'][:, :, :], R['tA'][:, :, :], P41('ka'), ALU.add, e='pool')
                    yield
                    ttn(R['k2'][:, :, :], R['k'][:, :, :], R['tA'][:, :, :], ALU.mult)
                    ttn(R['bv'][:, :, :], R['kkn'][:, :, :], asig[:, :, :], ALU.mult, e='pool')
                    ttn(R['tB'][:, :, :], R['cs'][:, :, :], sgw[:, :, :], ALU.subtract, e='pool')
                    act(R['tB'][:, :, :], R['tB'][:, :, :], AF.Exp, scale=-C0)
                    ttn(R['tA'][:, :, :], R['r'][:, :, :], P4('rk'), ALU.mult, e='pool')
                    ttn(R['tA'][:, :, :], R['tA'][:, :, :], R['k2'][:, :, :], ALU.mult)
                    bk = psb()
                    for fb in range(4):
                        mm(sub(bk, A(bk)[:, fb * 128:(fb + 1) * 128]), blk64[:], R['tA'][:, fb, :])
                    ttn(bonus[:, :, :], sub(bk, A(bk).rearrange("p (f t) -> p f t", t=128)), R['v'][:, :, :], ALU.mult)
                    yield
                    E3 = R['tB']
                    for c in range(2):
                        for hh in range(2):
                            ps_ = slice(hh * 64, (hh + 1) * 64)
                            ts_ = slice(c * 64, (c + 1) * 64)
                            os_ = slice(hh * 64, (hh + 1) * 64)
                            stt(opzT[ps_, :, c, 0, os_], R['kkn'][ps_, :, ts_], -1.0, E3[ps_, :, ts_], ALU.mult, ALU.mult)
                            ttn(opzT[ps_, :, c, 1, os_], R['bv'][ps_, :, ts_], R['E2'][ps_, :, ts_], ALU.mult)
                            ttn(opzT[ps_, :, c, 2, os_], R['k2'][ps_, :, ts_], R['E2'][ps_, :, ts_], ALU.mult, e='pool')
                            cp(opzT[ps_, :, c, 3, os_], R['v'][ps_, :, ts_], e='pool')
                        yield
                    ttn(rstT[:, :, :], R['r'][:, :, :], R['E1'][:, :, :], ALU.mult)
                    cp(gC[:, :, 0:1], R['E1'][:, :, 63:64], e='pool')
                    cp(gC[:, :, 1:2], R['E1'][:, :, 127:128], e='pool')
                    yield

                def q4(bank):
                    return sub(bank, A(bank).rearrange("p (f t) -> p f t", t=128))
                bc4 = lambda m: m[:, :].unsqueeze(1).broadcast_to([128, 4, 128])
                TinvOf = {}
                def g_alg(c):
                    Az = [opzT[:, fb, c, 0, :] for fb in range(4)]; Bz = [opzT[:, fb, c, 1, :] for fb in range(4)]
                    Kz = [opzT[:, fb, c, 2, :] for fb in range(4)]; Vz = [opzT[:, fb, c, 3, :] for fb in range(4)]
                    Rs = [rstT[:, fb, c * 64:(c + 1) * 64] for fb in range(4)]
                    for kk_i, (nm, src) in enumerate((('BzT', Bz), ('KzT', Kz), ('VzT', Vz))):
                        bk = psb()
                        for fb in range(4):
                            mm(sub(bk, A(bk)[:, fb * 128:(fb + 1) * 128]), src[fb], identb[:])
                        cp(Gc[c][nm][:, :, :], q4(bk), e='act' if kk_i != 1 else 'dve')
                        yield
                    for nm, l_, r_, msk in (('Q0', Bz, Az, msu), ('QT0', Az, Bz, msl), ('Aak', Kz, Az, msu)):
                        bk = psb()
                        for fb in range(4):
                            mm(sub(bk, A(bk)[:, fb * 128:(fb + 1) * 128]), l_[fb], r_[fb])
                        ttn(Gc[c][nm][:, :, :], q4(bk), bc4(msk), ALU.mult)
                        yield
                    bk = psb()
                    for j, l_ in enumerate((Bz, Kz)):
                        for fb in range(4):
                            o0 = j * 256 + fb * 64
                            mm(sub(bk, A(bk)[:, o0:o0 + 64]), l_[fb], Rs[fb])
                    ttn(ArbArks[c][:, :, :, :].rearrange("p j f t -> p (j f) t"), sub(bk, A(bk).rearrange("p (g t) -> p g t", t=64)),
                        mst[:, :].unsqueeze(1).broadcast_to([128, 8, 64]), ALU.mult)
                    yield
                    ttn(Gc[c]['P0'][:, :, :], Gc[c]['Q0'][:, :, :], identb[:, :].unsqueeze(1).broadcast_to([128, 4, 128]), ALU.add, e='pool')
                    ttn(Gc[c]['PT0'][:, :, :], Gc[c]['QT0'][:, :, :], identb[:, :].unsqueeze(1).broadcast_to([128, 4, 128]), ALU.add, e='pool')
                    yield
                    Qb = [Gc[c]['Q0'], Gc[c]['Q1']]; QTb = [Gc[c]['QT0'], Gc[c]['QT1']]
                    Pb = [Gc[c]['P0'], Gc[c]['P1']]; PTb = [Gc[c]['PT0'], Gc[c]['PT1']]
                    for s_ in range(6):
                        Qs, QTs = Qb[s_ % 2], QTb[s_ % 2]
                        Qn, QTn = Qb[(s_ + 1) % 2], QTb[(s_ + 1) % 2]
                        Pp, PTp = Pb[(s_ - 1) % 2], PTb[(s_ - 1) % 2]
                        Pc, PTc = Pb[s_ % 2], PTb[s_ % 2]
                        todo = []
                        if s_ <= 4:
                            bk = psb()
                            for fb in range(4):
                                mm(sub(bk, A(bk)[:, fb * 128:(fb + 1) * 128]), QTs[:, fb, :], Qs[:, fb, :])
                            todo.append(lambda bk=bk: cp(Qn[:, :, :], q4(bk), e='act'))
                        if s_ <= 3:
                            bk = psb()
                            for fb in range(4):
                                mm(sub(bk, A(bk)[:, fb * 128:(fb + 1) * 128]), Qs[:, fb, :], QTs[:, fb, :])
                            todo.append(lambda bk=bk: cp(QTn[:, :, :], q4(bk), e='act'))
                        if s_ >= 1:
                            bk = psb()
                            for fb in range(4):
                                mm(sub(bk, A(bk)[:, fb * 128:(fb + 1) * 128]), PTp[:, fb, :], Qs[:, fb, :])
                            todo.append(lambda bk=bk: ttn(Pc[:, :, :], q4(bk), Pp[:, :, :], ALU.add))
                        if 1 <= s_ <= 4:
                            bk = psb()
                            for fb in range(4):
                                mm(sub(bk, A(bk)[:, fb * 128:(fb + 1) * 128]), Qs[:, fb, :], PTp[:, fb, :])
                            todo.append(lambda bk=bk: ttn(PTc[:, :, :], q4(bk), PTp[:, :, :], ALU.add))
                        for f_ in todo:
                            f_()
                        yield
                    cur = 1
                    TinvOf[c] = Gc[c][f'P{cur}']
                    yield
                def g_chain(c):
                    Az = [opzT[:, fb, c, 0, :] for fb in range(4)]; Bz = [opzT[:, fb, c, 1, :] for fb in range(4)]
                    Kz = [opzT[:, fb, c, 2, :] for fb in range(4)]; Vz = [opzT[:, fb, c, 3, :] for fb in range(4)]
                    Rs = [rstT[:, fb, c * 64:(c + 1) * 64] for fb in range(4)]
                    ttn(STg[:, :, :], ST32[:, :, :], gC[:, :, c:c + 1].broadcast_to([128, 4, 128]), ALU.mult, e='pool')
                    bk = psb()
                    for fb in range(4):
                        o_ = sub(bk, A(bk)[:, fb * 128:(fb + 1) * 128])
                        mm(o_, Az[fb], STb[:, fb, :], start=True, stop=False); mm(o_, Gc[c]['Aak'][:, fb, :], Gc[c]['VzT'][:, fb, :], start=False, stop=True)
                    cp(GS['W0T'][:, :, :], q4(bk), e='act')
                    yield
                    bk = psb()
                    for fb in range(4):
                        mm(sub(bk, A(bk)[:, fb * 128:(fb + 1) * 128]), TinvOf[c][:, fb, :], GS['W0T'][:, fb, :])
                    cp(GS['UT'][:, :, :], q4(bk), e='act')
                    yield
                    bkS = psb()
                    for fb in range(4):
                        o_ = sub(bkS, A(bkS)[:, fb * 128:(fb + 1) * 128])
                        mm(o_, Gc[c]['BzT'][:, fb, :], GS['UT'][:, fb, :], start=True, stop=False)
                        mm(o_, Gc[c]['KzT'][:, fb, :], Gc[c]['VzT'][:, fb, :], start=False, stop=True)
                    bkY = psb()
                    for fb in range(4):
                        o_ = sub(bkY, A(bkY)[:, fb * 64:(fb + 1) * 64])
                        mm(o_, STb[:, fb, :], Rs[fb], start=True, stop=False)
                        mm(o_, GS['UT'][:, fb, :], ArbArks[c][:, 0, fb, :], start=False, stop=False)
                        mm(o_, Gc[c]['VzT'][:, fb, :], ArbArks[c][:, 1, fb, :], start=False, stop=True)
                    for fb in range(4):
                        stt(STb[:, fb, :], sub(bkS, A(bkS)[:, fb * 128:(fb + 1) * 128]), gC[:, fb, c:c + 1], STg[:, fb, :], ALU.mult, ALU.add)
                    for fb in range(4):
                        stt(ST32[:, fb, :], sub(bkS, A(bkS)[:, fb * 128:(fb + 1) * 128]), gC[:, fb, c:c + 1], STg[:, fb, :], ALU.mult, ALU.add)
                    cp(y_a[:, :, c * 64:(c + 1) * 64], sub(bkY, A(bkY)[:, 0:256].rearrange("p (f t) -> p f t", t=64)), e='act')
                    yield
                    yield

                def g_post4(y_a=y_a, bonus=bonus, gg=gg, yT_b=yT_b):
                    P4 = lambda nm: pv[:, row[nm]:row[nm] + 4].unsqueeze(2).broadcast_to([128, 4, 128])
                    v4 = lambda bank: sub(bank, A(bank).rearrange("p (f t) -> p f t", t=128))
                    bk = psb()
                    for fb in range(4):
                        mm(sub(bk, A(bk)[:, fb * 128:(fb + 1) * 128]), blk64[:], y_a[:, fb, :])
                    act(pt4[:, :, :], v4(bk), AF.Copy, scale=1.0 / 64)
                    yield
                    ttn(y_a[:, :, :], y_a[:, :, :], pt4[:, :, :], ALU.subtract)
                    ttn(pt4[:, :, :], y_a[:, :, :], y_a[:, :, :], ALU.mult, e='pool')
                    yield
                    bk = psb()
                    for fb in range(4):
                        mm(sub(bk, A(bk)[:, fb * 128:(fb + 1) * 128]), blk64[:], pt4[:, fb, :])
                    act(pt4[:, :, :], v4(bk), AF.Sqrt, bias=64e-5, scale=1.0 / 64)
                    yield
                    recip(pt4[:, :, :], pt4[:, :, :])
                    ttn(y_a[:, :, :], y_a[:, :, :], pt4[:, :, :], ALU.mult)
                    yield
                    ttn(y_a[:, :, :], y_a[:, :, :], P4('lnw'), ALU.mult)
                    ttn(y_a[:, :, :], y_a[:, :, :], P4('lnb'), ALU.add, e='pool')
                    yield
                    ttn(y_a[:, :, :], y_a[:, :, :], bonus[:, :, :], ALU.add)
                    ttn(yT_b[:, 0:4, :], y_a[:, :, :], gg[:, :, :], ALU.mult)
                    yield

                def g_mlstm_pre():
                    for t_ in range(4):
                        for jb in range(4):
                            kj = ('QKf', jb)
                            if t_ == 0:
                                S.op('dve', lambda E, jb=jb: E.tensor_scalar(out=cacc[:, jb, :], in0=qk_raw[:, jb, 0:128], scalar1=PV('cw', jb), scalar2=PV('cb', jb), op0=ALU.mult, op1=ALU.add),
                                     r=[qk_raw, pv], w=[kj, 'QKf'])
                            else:
                                S.op('dve', lambda E, jb=jb, t_=t_: E.scalar_tensor_tensor(out=cacc[:, jb, :], in0=qk_raw[:, jb, t_:t_ + 128], scalar=PV('cw', t_ * 4 + jb), in1=cacc[:, jb, :], op0=ALU.mult, op1=ALU.add),
                                     r=[qk_raw, pv, kj], w=[kj, 'QKf'])
                    S.op('act', lambda E: E.activation(out=QKf[:, :, :], in_=cacc[:, :, :], func=AF.Silu), r=[('QKf', jb) for jb in range(4)] + ['QKf'], w=['QKf'] + [('QKf', jb) for jb in range(4)])
                    yield
                    cp(qk_raw[:, :, 0:3], qk_raw[:, :, 128:131], e='pool')
                    act(th8[:], g8[:], AF.Tanh, scale=1.0 / 15.0)
                    tsc(g8[:, 0:4], th8[:, 0:4], 15.0, None, ALU.mult)
                    act(g8[:, 4:8], th8[:, 4:8], AF.Exp, scale=-15.0)
                    act(g8[:, 4:8], g8[:, 4:8], AF.Ln, bias=1.0)
                    yield
                    if i == 0:
                        mset(g8[0:112, 0:4], -1.0e4, e='dve'); mset(g8[0:112, 4:8], 0.0, e='dve')
                    pn = psq()
                    pn4 = sub(pn, A(pn)[:, 0:4]); pn8 = sub(pn, A(pn)[:, 4:8])
                    mm(pn4, tric[:], g8[:, 4:8]); mm(pn8, blk64[:], g8[:, 4:8])
                    cp(nbg[:], sub(pn, A(pn)[:, 0:8]), e='act')
                    yield
                    ttn(dbias[:], g8[:, 0:4], nbg[:, 0:4], ALU.add)
                    ttn(wgt[:], dbias[:], nbg[:, 4:8], ALU.subtract)
                    act(wgt[:], wgt[:], AF.Exp, bias=math.log(0.125))
                    yield
                    for kb in range(2):
                        p_ = psq()
                        S.op('pe', lambda E, kb=kb, p_=p_: E.transpose(out=A(p_), in_=QKf[:, 2 + kb, :], identity=ident[:]), r=[QKf, ident], w=[p_])
                        for hh in range(2):
                            h = kb * 2 + hh
                            tsc(Kw[:, h, :], sub(p_, A(p_)[:, hh * 64:(hh + 1) * 64]), wgt[:, h:h + 1], None, ALU.mult)
                    yield

                def g_mlstm_rest():
                    def g_head(h):
                        hb = (h % 2) * 64
                        hs = slice(hb, hb + 64)
                        qb = h // 2
                        L_, D_, E_, P_ = lfb[h % 2], Dm[h % 2], eB[h % 2], Pm[h % 2]
                        tsc(L_[:], ones[:], g8[:, 4 + h:5 + h], None, ALU.mult, e='pool')
                        pbr = psq(); mm(pbr, L_[:], tric[:])
                        act(D_[:], pbr, AF.Exp, bias=dbias[:, h:h + 1], scale=-1.0)
                        act(E_[:], pbr, AF.Exp, scale=-1.0)
                        yield
                        psc = psq(); mm(psc, QKf[hs, 2 + qb, :], QKf[hs, qb, :])
                        ttn(D_[:], D_[:], maskc[:], ALU.mult, e='pool')
                        ttn(P_[:], D_[:], psc, ALU.mult)
                        yield
                        ttn(Qz[h][hs, 0, 0:64], QKf[hs, qb, 0:64], E_[hs, 0:64], ALU.mult)
                        ttn(Qz[h][hs, 1, 64:128], QKf[hs, qb, 64:128], E_[hs, 64:128], ALU.mult)
                        pU = psb()
                        u0 = sub(pU, A(pU)[hs, 0:129]); u1 = sub(pU, A(pU)[hs, 256:385])
                        mm(u0, Kw[0:64, h, :], V1[0:64, h, :])
                        stt(CTb[h][hs, :], CTa[h][hs, :], E_[hs, 63:64], u0, ALU.mult, ALU.add)
                        yield
                        pO = psb(); o_ = sub(pO, A(pO)[:, 0:129])
                        mm(o_, P_[:], V1[:, h, :], start=True, stop=False)
                        mm(o_, Qz[h][hs, 0, :], CTa[h][hs, :], start=False, stop=False)
                        mm(o_, Qz[h][hs, 1, :], CTb[h][hs, :], start=False, stop=True)
                        mm(u1, Kw[64:128, h, :], V1[64:128, h, :])
                        stt(CTa[h][hs, :], CTb[h][hs, :], E_[hs, 127:128], u1, ALU.mult, ALU.add)
                        if i > 0:
                            H_ = hraw[h % 2]
                            act(sm[:, 3 * h:3 * h + 1], sub(pO, A(pO)[:, 128:129]), AF.Abs)
                            tsc(sm[:, 3 * h:3 * h + 1], sm[:, 3 * h:3 * h + 1], 1.0, None, ALU.max)
                            recip(sm[:, 3 * h:3 * h + 1], sm[:, 3 * h:3 * h + 1])
                            tsc(H_[:], sub(pO, A(pO)[:, 0:128]), sm[:, 3 * h:3 * h + 1], None, ALU.mult)
                            act(P_[:], H_[:], AF.Square, accum=sm[:, 3 * h + 1:3 * h + 2])
                            yield
                            act(sm[:, 3 * h + 2:3 * h + 3], sm[:, 3 * h + 1:3 * h + 2], AF.Sqrt, bias=1e-6, scale=1.0 / 128)
                            recip(sm[:, 3 * h + 2:3 * h + 3], sm[:, 3 * h + 2:3 * h + 3])
                            yield
                            stt(H_[:], H_[:], sm[:, 3 * h + 2:3 * h + 3], mnw_bc[:, h * 128:(h + 1) * 128], ALU.mult, ALU.mult)
                            ttn(y_b[:, h * 128:(h + 1) * 128], H_[:], sigo[:, h * 128:(h + 1) * 128], ALU.mult)
                        yield
                    yield from inter([g_head(0), g_head(1)])
                    yield from inter([g_head(2), g_head(3)])
                    if i == 0:
                        return
                    for h in range(4):
                        p_ = psq()
                        S.op('pe', lambda E, h=h, p_=p_: E.transpose(out=A(p_), in_=y_b[:, h * 128:(h + 1) * 128], identity=ident[:]), r=[y_b, ident], w=[p_])
                        cp(yT_b[:, 4 + h, :], p_, e='act')
                    yield

                def g_rwkv_prep():
                    yield from g_prep_all()
                    cp(rkv_raw[:, :, 0:1], rkv_raw[:, :, 128:129], e='pool')
                    yield

                def g_rwkv_rest():
                    yield from inter([g_alg(0), g_alg(1)])
                    yield from g_chain(0)
                    yield from g_chain(1)

                def g_post_all(i=i, yT_b=yT_b, post=g_post4()):
                    yield from post
                    dma(yT_d[i * 128:(i + 1) * 128, :], yT_b[:, :, :].rearrange("p b t -> p (b t)"))
                    yield
                for _ in inter([g_rwkv_prep(), g_mlstm_pre()] + pending):
                    pass
                pending = []
                streams = [g_rwkv_rest(), g_mlstm_rest()]
                if i + 1 < NT:
                    streams.append(g_front(i + 1))
                for _ in inter(streams, weights=[2, 1, 1][:len(streams)]):
                    pass
                if i > 0:
                    pending = [g_post_all()]
            for _ in inter(pending):
                pass

            S.barrier()
            chk('p1')
            es2.close()
            es1.close()

            with ExitStack() as es5:
                t5 = lambda name, shape, dt=F32: T(es5, name, shape, dt)
                NB1 = 4
                wout_b = t5("wout_b", [128, 8, D], BF16); wr_f = t5("wr_f", [128, 8, 36]); wffn_bc = t5("wffn_bc", [128, D])
                wst = [t5(f"wst{i}", [128, D]) for i in range(2)]
                xt1 = [t5(f"xt1_{i}", [128, D]) for i in range(NB1)]; yt1 = [t5(f"yt1_{i}", [128, 8, 128], BF16) for i in range(NB1)]
                h1s = [t5(f"h1s{i}", [128, D]) for i in range(NB1)]; big2 = [t5(f"big2_{i}", [128, D]) for i in range(NB1)]
                xn2_bs = [t5(f"xn2_b{i}", [128, D], BF16) for i in range(NB1)]; xn2T = [t5(f"xn2T{i}", [128, 8, 128]) for i in range(NB1)]
                scr = [dict(lgt=t5(f"lgt{i}", [128, 36]), rsm=t5(f"rsm{i}", [128, 32]), oh=[t5(f"oh{k}_{i}", [128, 32]) for k in range(2)],
                            cnt=t5(f"cnt{i}", [128, 32]), el=t5(f"el{i}", [128, 8]), mx8=t5(f"mx8_{i}", [128, 8]), ix8=t5(f"ix8_{i}", [128, 8], U32),
                            sm=t5(f"smb{i}", [128, 16])) for i in range(NB1)]
                carry = t5("carry", [1, 32])
                mset(carry, 0.0)
                dma(wffn_bc[:], nffn_d[0].partition_broadcast(128))
                dma(wr_f[:, :, 0:4], rgw_d[0].rearrange("(c p) e -> p c e", p=128))
                dma(wr_f[:, :, 4:36], rew_d[0].rearrange("(c p) e -> p c e", p=128))
                wout_v = wout_d[0].rearrange("(c p) f -> p c f", p=128)
                for c in range(8):
                    dma(wst[c % 2][:, :], wout_v[:, c, :])
                    cp(wout_b[:, c, :], wst[c % 2][:, :], e=('dve', 'act')[c % 2])

                def loads1b(i):
                    dma(xt1[i % NB1][:, :], x_d[(i - 1) * 128:i * 128, :])
                    dma(yt1[i % NB1][:, :, :].rearrange("p b t -> p (b t)"), yT_d[i * 128:(i + 1) * 128, :])
                def g_A(i):
                    b = i % NB1
                    xt = xt1[b]; yT_b = yt1[b]; h1 = h1s[b]; big = big2[b]; xn2_b = xn2_bs[b]; xT2 = xn2T[b]
                    Z = scr[b]; lgt = Z['lgt']; rsm = Z['rsm']; oh = Z['oh']; cnt = Z['cnt']; el = Z['el']; mx8 = Z['mx8']; ix8 = Z['ix8']; sm = Z['sm']
                    for n in range(2):
                        pm_ = psb()
                        for blk in range(8):
                            mm(pm_, yT_b[:, blk, :], wout_b[:, blk, n * 512:(n + 1) * 512], start=(blk == 0), stop=(blk == 7))
                        ttn(h1[:, n * 512:(n + 1) * 512], xt[:, n * 512:(n + 1) * 512], pm_, ALU.add)
                    dma(h1_d[i * 128:(i + 1) * 128, :], h1[:, :], q='pool')
                    yield
                    act(big[:], h1[:], AF.Square, accum=sm[:, 14:15])
                    act(sm[:, 15:16], sm[:, 14:15], AF.Sqrt, bias=1e-6, scale=1.0 / D)
                    recip(sm[:, 15:16], sm[:, 15:16])
                    stt(big[:], h1[:], sm[:, 15:16], wffn_bc[:], ALU.mult, ALU.mult)
                    cp(xn2_b[:], big[:], e='pool')
                    yield
                    for half in range(2):
                        pb = psb()
                        for j in range(4):
                            c = half * 4 + j
                            S.op('pe', lambda E, c=c, j=j, pb=pb, big=big: E.transpose(out=A(pb)[:, j * 128:(j + 1) * 128], in_=big[:, c * 128:(c + 1) * 128], identity=ident[:]),
                                 r=[big, ident], w=[pb])
                        cp(xT2[:, half * 4:half * 4 + 4, :], sub(pb, A(pb).rearrange("p (j t) -> p j t", t=128)), e='act')
                        yield
                    pl = psq(); pl36 = sub(pl, A(pl)[:, 0:36])
                    for c in range(8):
                        mm(pl36, xT2[:, c, :], wr_f[:, c, :], start=(c == 0), stop=(c == 7))
                    ttn(lgt[:], pl36, rb_bc[:], ALU.add)
                    yield
                    S.op('dve', lambda E: E.tensor_reduce(out=sm[:, 4:5], in_=lgt[:, 0:4], axis=AX.X, op=ALU.max, negate=True), r=[lgt], w=[sm])
                    act(rsm[:, 0:4], lgt[:, 0:4], AF.Exp, bias=sm[:, 4:5], accum=sm[:, 5:6])
                    recip(sm[:, 5:6], sm[:, 5:6])
                    yield
                    tsc(sm[:, 4:5], sm[:, 4:5], -1.0, None, ALU.mult)
                    tsc(rsm[:, 4:8], lgt[:, 0:4], sm[:, 4:5], None, ALU.is_equal)
                    yield
                    tsc(el[:], lgt[:, 4:12], rsm[:, 4:5], None, ALU.mult)
                    for g in range(1, 4):
                        stt(el[:], lgt[:, 4 + g * 8:12 + g * 8], rsm[:, 4 + g:5 + g], el[:], ALU.mult, ALU.add)
                    ttn(rsm[:, 8:12], rsm[:, 4:8], giota[:], ALU.mult)
                    S.op('dve', lambda E: E.tensor_reduce(out=sm[:, 6:7], in_=rsm[:, 8:12], axis=AX.X, op=ALU.add), r=[rsm], w=[sm])
                    yield
                    S.op('dve', lambda E: E.max(out=mx8[:], in_=el[:]), r=[el], w=[mx8])
                    S.op('dve', lambda E: E.max_index(out=ix8[:], in_max=mx8[:], in_values=el[:]), r=[mx8, el], w=[ix8])
                    yield
                    cp(sm[:, 8:10], ix8[:, 0:2])
                    tsc(sm[:, 8:10], sm[:, 8:10], sm[:, 6:7], None, ALU.add)
                    yield
                    ttn(sm[:, 10:11], mx8[:, 1:2], mx8[:, 0:1], ALU.subtract)
                    act(sm[:, 10:11], sm[:, 10:11], AF.Exp)
                    tsc(sm[:, 11:12], sm[:, 10:11], 1.0, None, ALU.add)
                    recip(sm[:, 11:12], sm[:, 11:12])
                    yield
                    ttn(gates_all[:, i, 0:1], sm[:, 5:6], sm[:, 11:12], ALU.mult)
                    ttn(gates_all[:, i, 1:2], gates_all[:, i, 0:1], sm[:, 10:11], ALU.mult)
                    for k in range(2):
                        tsc(oh[k][:], iota_f[:], sm[:, 8 + k:9 + k], None, ALU.is_equal)
                    ttn(cnt[:], oh[0][:], oh[1][:], ALU.add)
                    yield
                    yield

                def g_B(i):
                    b = i % NB1
                    xt = xt1[b]; yT_b = yt1[b]; h1 = h1s[b]; big = big2[b]; xn2_b = xn2_bs[b]; xT2 = xn2T[b]
                    Z = scr[b]; lgt = Z['lgt']; rsm = Z['rsm']; oh = Z['oh']; cnt = Z['cnt']; el = Z['el']; mx8 = Z['mx8']; ix8 = Z['ix8']; sm = Z['sm']
                    pp = psq(); pp32 = sub(pp, A(pp)[:, 0:32])
                    mm(pp32, msu[:], cnt[:], start=True, stop=False)
                    mm(pp32, ones[0:1, :], carry[0:1, :], start=False, stop=True)
                    for k in range(2):
                        ttn(rsm[:], oh[k][:], pp32, ALU.mult)
                        S.op('dve', lambda E, k=k: E.tensor_reduce(out=sm[:, 12 + k:13 + k], in_=rsm[:], axis=AX.X, op=ALU.add), r=[rsm], w=[sm])
                    tsc(sm[:, 12:14], sm[:, 12:14], float(CAP - 1), None, ALU.min)
                    stt(sm[:, 12:14], sm[:, 8:10], float(CAP), sm[:, 12:14], ALU.mult, ALU.add)
                    cp(slots_all[:, i, :], sm[:, 12:14])
                    yield
                    pc = psq(); pc32 = sub(pc, A(pc)[0:1, 0:32])
                    mm(pc32, ones[:, 0:1], cnt[:])
                    ttn(carry[0:1, :], carry[0:1, :], pc32, ALU.add)
                    yield
                    for k in range(2):
                        S.dma('pool', lambda E, k=k, i=i: E.indirect_dma_start(
                            out=xs_d[:, :], out_offset=bass.IndirectOffsetOnAxis(ap=slots_all[:, i, k:k + 1], axis=0),
                            in_=xn2_b[:, :], in_offset=None), r=[xn2_b, slots_all], w=['xs_scr'])
                    yield

                def g_tile(i):
                    loads1b(i)
                    yield
                    yield from g_A(i)
                    yield from g_B(i)
                active = []
                nxt_tile = 1
                rnd = 0
                while active or nxt_tile < NT:
                    if nxt_tile < NT and len(active) < NB1 and rnd % 4 == 0:
                        active.append(g_tile(nxt_tile)); nxt_tile += 1
                    for g in list(active):
                        try:
                            next(g)
                        except StopIteration:
                            active.remove(g)
                    rnd += 1
                S.barrier()
                chk('p1b')

            with ExitStack() as es3:
                t3 = lambda name, shape, dt=F32: T(es3, name, shape, dt)
                NSUB = CAP // 128
                wstg = [t3(f"ewstg{i}", [128, 2, D]) for i in range(6)]
                wgu_b = [t3(f"wgu_b{i}", [128, 8, D], BF16) for i in range(2)]
                wdn_b = [t3(f"wdn_b{i}", [128, 4, D], BF16) for i in range(2)]
                xsl = [t3(f"xsl{i}", [128, NSUB, D], BF16) for i in range(2)]
                xT = [t3(f"xT{i}", [128, 8, CAP], BF16) for i in range(2)]
                hT = t3("hT", [128, 4, CAP], BF16); gsl = t3("gsl", [128, CAP])
                ysl = [t3(f"ysl{i}", [128, D]) for i in range(2)]
                def wpiece(e, k):
                    g_ = e * 6 + k
                    s_ = wstg[g_ % 6]
                    if k < 4:
                        src = wgu_d[0, e].rearrange("(c p) f -> p c f", p=128)[:, 2 * k:2 * k + 2, :]
                        dst = wgu_b[e % 2][:, 2 * k:2 * k + 2, :]
                    else:
                        src = wdn_d[0, e].rearrange("(c p) f -> p c f", p=128)[:, 2 * (k - 4):2 * (k - 4) + 2, :]
                        dst = wdn_b[e % 2][:, 2 * (k - 4):2 * (k - 4) + 2, :]
                    return (lambda: dma(s_[:, :, :], src)), (lambda: cp(dst, s_[:, :, :], e=('dve', 'act')[g_ % 2]))

                def xload(e):
                    dma(xsl[e % 2][:, :, :], xs_d[e * CAP:(e + 1) * CAP, :].rearrange("(m p) f -> p m f", p=128), q='pool')

                for k in range(6):
                    d_, c_ = wpiece(0, k)
                    d_(); c_()
                xload(0)
                for e in range(NEXP):
                    Wg = wgu_b[e % 2]; Wd = wdn_b[e % 2]
                    X = xsl[e % 2]; XT = xT[e % 2]
                    if e + 1 < NEXP:
                        xload(e + 1)
                    steps = []

                    def st_tr(m, X=X, XT=XT):
                        for half in range(2):
                            pb = psb()
                            pbv = A(pb).bitcast(BF16)
                            for j in range(4):
                                c = half * 4 + j
                                S.op('pe', lambda E, c=c, j=j, m=m, pbv=pbv, X=X: E.transpose(out=pbv[:, j * 128:(j + 1) * 128], in_=X[:, m, c * 128:(c + 1) * 128], identity=identb[:]),
                                     r=[X, identb], w=[pb])
                            cp(XT[:, half * 4:half * 4 + 4, m * 128:(m + 1) * 128], sub(pb, pbv[:, 0:512].rearrange("p (j t) -> p j t", t=128)), e='act' if half else 'dve')

                    def st_gu(j, Wg=Wg, XT=XT):
                        pg = psb(); pu = psb()
                        for c in range(8):
                            mm(sub(pg, A(pg)[:, 0:CAP]), Wg[:, c, j * 128:(j + 1) * 128], XT[:, c, :], start=(c == 0), stop=(c == 7))
                        for c in range(8):
                            mm(sub(pu, A(pu)[:, 0:CAP]), Wg[:, c, 512 + j * 128:512 + (j + 1) * 128], XT[:, c, :], start=(c == 0), stop=(c == 7))
                        act(gsl[:, :], sub(pg, A(pg)[:, 0:CAP]), AF.Silu)
                        ttn(hT[:, j, :], gsl[:, :], sub(pu, A(pu)[:, 0:CAP]), ALU.mult)

                    def st_dn(m, Wd=Wd, e=e):
                        Y = ysl[m % 2]
                        for n in range(2):
                            py = psb()
                            for c in range(4):
                                mm(py, hT[:, c, m * 128:(m + 1) * 128], Wd[:, c, n * 512:(n + 1) * 512], start=(c == 0), stop=(c == 3))
                            cp(Y[:, n * 512:(n + 1) * 512], py, e='act' if n else 'dve')
                        dma(ys_d[e * CAP + m * 128:e * CAP + (m + 1) * 128, :], Y[:, :], q='pool')

                    for m in range(NSUB):
                        steps.append(lambda m=m: st_tr(m))
                    for j in range(4):
                        steps.append(lambda j=j: st_gu(j))
                    for m in range(NSUB):
                        steps.append(lambda m=m: st_dn(m))
                    assert len(steps) >= 6
                    casts = []
                    if e + 1 < NEXP:
                        for k in range(6):
                            d_, c_ = wpiece(e + 1, k)
                            d_()
                            casts.append(c_)
                    for si, stp in enumerate(steps):
                        stp()
                        if si < len(casts):
                            casts[si]()
                    for c_ in casts[len(steps):]:
                        c_()
                S.barrier()
                chk('p2')

            with ExitStack() as es4:
                t4 = lambda name, shape, dt=F32: T(es4, name, shape, dt)
                y0 = [t4(f"y0_{i}", [128, D]) for i in range(2)]; y1 = [t4(f"y1_{i}", [128, D]) for i in range(2)]
                hh = [t4(f"hh{i}", [128, D]) for i in range(2)]; jk = t4("jk", [128, D]); s4 = t4("s4", [128, 4])
                ob = [t4(f"ob{i}", [128, D]) for i in range(2)]
                wfin_bc = t4("wfin_bc", [128, D])
                dma(wfin_bc[:], nfin_d.partition_broadcast(128))
                def loads3(i):
                    b = i % 2
                    for k, yk in ((0, y0[b]), (1, y1[b])):
                        S.dma('pool', lambda E, k=k, i=i, yk=yk: E.indirect_dma_start(
                            out=yk[:, :], out_offset=None, in_=ys_d[:, :],
                            in_offset=bass.IndirectOffsetOnAxis(ap=slots_all[:, i, k:k + 1], axis=0)), r=['ys_scr', slots_all], w=[yk])
                    dma(hh[b][:, :], h1_d[i * 128:(i + 1) * 128, :])
                if NT > 1:
                    loads3(1)
                for i in range(1, NT):
                    b = i % 2
                    stt(hh[b][:], y0[b][:], gates_all[:, i, 0:1], hh[b][:], ALU.mult, ALU.add)
                    stt(hh[b][:], y1[b][:], gates_all[:, i, 1:2], hh[b][:], ALU.mult, ALU.add)
                    act(jk[:], hh[b][:], AF.Square, accum=s4[:, 0:1])
                    act(s4[:, 1:2], s4[:, 0:1], AF.Sqrt, bias=1e-6, scale=1.0 / D)
                    recip(s4[:, 1:2], s4[:, 1:2])
                    stt(ob[b][:], hh[b][:], s4[:, 1:2], wfin_bc[:], ALU.mult, ALU.mult)
                    if i + 1 < NT:
                        loads3(i + 1)
                    dma(out_d[(i - 1) * 128:i * 128, :], ob[b][:, :], is_out=True)
                S.finish()
        except Stop:
            S.finish()
        print("instr counts", S.total, "nsem", S.nsem)
    nc._dbg_map = dbg_map
    return nc


_NAMES = ['meta_tokens', 'norm_mix_w', 'norm_ffn_w', 'norm_final_w', 'w_in', 'w_out', 'rwkv_mu_rkv', 'rwkv_mu_wag',
          'rwkv_w0', 'rwkv_w_lora_a', 'rwkv_w_lora_b', 'rwkv_a0', 'rwkv_a_lora_a', 'rwkv_a_lora_b', 'rwkv_g_lora_a',
          'rwkv_g_lora_b', 'rwkv_k_k', 'rwkv_k_a', 'rwkv_r_k', 'rwkv_lnx_w', 'rwkv_lnx_b', 'mlstm_conv_w', 'mlstm_conv_b',
          'mlstm_gate_b', 'mlstm_norm_w', 'router_group_w', 'router_group_b', 'router_expert_w', 'router_expert_b',
          'expert_w_gate_up', 'expert_w_down']


def run(inputs, CAP=512, stop_after=None):
    x = np.asarray(inputs['x'], dtype=np.float32)
    B, L, _ = x.shape
    NT = L // 128 + 1
    nc = build(NT, CAP, stop_after=stop_after)
    shared = {n: np.ascontiguousarray(np.asarray(inputs[n], dtype=np.float32)) for n in _NAMES}
    in_maps = []
    for b in range(B):
        m = dict(shared)
        m['x'] = np.ascontiguousarray(x[b])
        in_maps.append(m)
    res = run_bass_kernel_spmd(nc, in_maps, core_ids=list(range(B)))
    if stop_after:
        d = np.asarray(res.results[0]['dbg'])
        return {k: d[0:v[2], v[0]:v[0] + v[1]] for k, v in nc._dbg_map.items()}
    return np.stack([np.asarray(r['out']).reshape(L, D) for r in res.results], axis=0).astype(np.float32)


def kernel(**inputs):
    return run(inputs, CAP=384)
```

```python
import math
import numpy as np
from contextlib import ExitStack
import concourse.bass as bass
import concourse.mybir as mybir
from concourse.bass_utils import run_bass_kernel_spmd

F32 = mybir.dt.float32
BF16 = mybir.dt.bfloat16
I32 = mybir.dt.int32
U32 = mybir.dt.uint32
AF = mybir.ActivationFunctionType
ALU = mybir.AluOpType
AX = mybir.AxisListType

D = 1024
DBG_TILE = 0
DIN = 3080
NEXP = 32
C0 = math.exp(-0.5)


class V:
    def __init__(self, ap, key):
        self.ap = ap
        self.key = key


def A(x):
    if isinstance(x, V):
        return x.ap
    if type(x).__name__.endswith('TensorHandle'):
        return x.ap()
    return x


def K(x):
    return x.key if isinstance(x, V) else x.name


class Sched:
    EPOCH = 20000

    def __init__(self, nc, es, n_dma_sems=32):
        self.nc = nc
        self.es = es
        self.eng = {'pe': nc.tensor, 'act': nc.scalar, 'dve': nc.vector,
                    'pool': nc.gpsimd, 'sp': nc.sync}
        self.sem = {}
        self.cnt = {}
        self.nsem = 0
        self.total = {k: 0 for k in self.eng}
        for k in self.eng:
            self._new_sem(k)
        self.waited = {k: {} for k in self.eng}
        self.res = {}
        self.dma_sems = [es.enter_context(nc.semaphore(f"dq{i}")) for i in range(n_dma_sems)]
        self.dma_cnt = [0] * n_dma_sems
        self.dma_rr = 0
        self.out_tokens = []

    def _new_sem(self, k):
        self.nsem += 1
        self.sem[k] = self.es.enter_context(self.nc.semaphore(f"s_{k}_{self.nsem}"))
        self.cnt[k] = 0

    def _need(self, reads, writes):
        need = []
        for key in reads:
            st = self.res.get(key)
            if st is not None and st['w'] is not None:
                need.append((st['w'], 'raw'))
        for key in writes:
            st = self.res.get(key)
            if st is not None:
                if st['w'] is not None:
                    need.append((st['w'], 'waw'))
                need.extend((t, 'war') for t in st['r'].values())
        return need

    import os as _os
    SAME_ENGINE_GAP = int(_os.environ.get('SE_GAP', 16))
    SKIP_WAX = int(_os.environ.get('SE_SKIPWAX', 1))
    SKIP_ENG = _os.environ.get('SE_ENG', 'dve,act,pool').split(',')

    def _emit_waits(self, e, need):
        for item in need:
            tok, kind = item if isinstance(item[0], tuple) else (item, 'raw')
            sem, val, src = tok[0], tok[1], tok[2]
            if src == 'pe' and e == 'pe':
                continue
            if src == e and src != 'dma':
                if e in self.SKIP_ENG:
                    if kind != 'raw' and self.SKIP_WAX:
                        continue
                    if kind == 'raw' and self.total[e] - tok[3] >= self.SAME_ENGINE_GAP:
                        continue
            w = self.waited[e]
            if w.get(id(sem), 0) >= val:
                continue
            self.eng[e].wait_ge(sem, val)
            w[id(sem)] = val

    def _record(self, tok, reads, writes):
        for key in reads:
            st = self.res.setdefault(key, {'w': None, 'r': {}})
            st['r'][id(tok[0])] = tok
        for key in writes:
            self.res[key] = {'w': tok, 'r': {}}

    def op(self, e, fn, r=(), w=()):
        r = [K(k) if not isinstance(k, (str, tuple)) else k for k in r]
        w = [K(k) if not isinstance(k, (str, tuple)) else k for k in w]
        w = w + [k for k in r if isinstance(k, str) and k.startswith('pb') and k not in w]
        if self.cnt[e] >= self.EPOCH:
            self._new_sem(e)
        self._emit_waits(e, self._need(r, w))
        inst = fn(self.eng[e])
        self.cnt[e] += 1
        self.total[e] += 1
        inst.then_inc(self.sem[e], 1)
        tok = (self.sem[e], self.cnt[e], e, self.total[e])
        self._record(tok, r, w)
        return tok

    def dma(self, q, fn, r=(), w=(), is_out=False):
        r = [K(k) if not isinstance(k, (str, tuple)) else k for k in r]
        w = [K(k) if not isinstance(k, (str, tuple)) else k for k in w]
        i = self.dma_rr
        self.dma_rr = (self.dma_rr + 1) % len(self.dma_sems)
        sem = self.dma_sems[i]
        need = self._need(r, w)
        if self.dma_cnt[i] > 0:
            need.append(((sem, 16 * self.dma_cnt[i], 'dma', 0), 'raw'))
        self._emit_waits(q, need)
        inst = fn(self.eng[q])
        self.dma_cnt[i] += 1
        inst.then_inc(sem, 16)
        tok = (sem, 16 * self.dma_cnt[i], 'dma', 0)
        self._record(tok, r, w)
        if is_out:
            self.out_tokens.append(tok)
        return tok

    def barrier(self):
        toks = [(self.sem[k], self.cnt[k], k, -10**9) for k in self.eng if self.cnt[k] > 0]
        toks += [(s, 16 * c, 'dma', 0) for s, c in zip(self.dma_sems, self.dma_cnt) if c > 0]
        for e in self.eng:
            self._emit_waits(e, [t for t in toks if t[2] != e])

    def finish(self):
        self._emit_waits('sp', self.out_tokens)
        self.barrier()


class Stop(Exception):
    pass


def inter(gens, weights=None):
    gens = list(gens)
    wts = {id(g): (weights[k] if weights else 1) for k, g in enumerate(gens)}
    while gens:
        for g in list(gens):
            for _ in range(wts[id(g)]):
                try:
                    next(g)
                except StopIteration:
                    gens.remove(g)
                    break
        yield


def build(NT, CAP, stop_after=None, dbgn=8192):
    nc = bass.Bass("TRN2", target_bir_lowering=False)
    NX = NT - 1
    dt_in = lambda name, shape: nc.dram_tensor(name, shape, F32, kind="ExternalInput").ap()
    x_d = dt_in("x", [NX * 128, D])
    meta_d = dt_in("meta_tokens", [16, D])
    nmix_d = dt_in("norm_mix_w", [1, D]); nffn_d = dt_in("norm_ffn_w", [1, D]); nfin_d = dt_in("norm_final_w", [D])
    win_d = dt_in("w_in", [1, D, DIN]); wout_d = dt_in("w_out", [1, D, D])
    murkv_d = dt_in("rwkv_mu_rkv", [1, 3, 512]); muwag_d = dt_in("rwkv_mu_wag", [1, 3, D])
    w0_d = dt_in("rwkv_w0", [1, 512]); wla_d = dt_in("rwkv_w_lora_a", [1, D, 64]); wlb_d = dt_in("rwkv_w_lora_b", [1, 64, 512])
    a0_d = dt_in("rwkv_a0", [1, 512]); ala_d = dt_in("rwkv_a_lora_a", [1, D, 64]); alb_d = dt_in("rwkv_a_lora_b", [1, 64, 512])
    gla_d = dt_in("rwkv_g_lora_a", [1, D, 160]); glb_d = dt_in("rwkv_g_lora_b", [1, 160, 512])
    kk_d = dt_in("rwkv_k_k", [1, 512]); ka_d = dt_in("rwkv_k_a", [1, 512]); rk_d = dt_in("rwkv_r_k", [1, 512])
    lnw_d = dt_in("rwkv_lnx_w", [1, 512]); lnb_d = dt_in("rwkv_lnx_b", [1, 512])
    cw_d = dt_in("mlstm_conv_w", [1, 4, 512]); cb_d = dt_in("mlstm_conv_b", [1, 512])
    gb_d = dt_in("mlstm_gate_b", [1, 8]); mnw_d = dt_in("mlstm_norm_w", [1, 512])
    rgw_d = dt_in("router_group_w", [1, D, 4]); rgb_d = dt_in("router_group_b", [1, 4])
    rew_d = dt_in("router_expert_w", [1, D, 32]); reb_d = dt_in("router_expert_b", [1, 32])
    wgu_d = dt_in("expert_w_gate_up", [1, NEXP, D, D]); wdn_d = dt_in("expert_w_down", [1, NEXP, 512, D])
    out_d = nc.dram_tensor("out", [NX * 128, D], F32, kind="ExternalOutput").ap()
    h1_d = nc.dram_tensor("h1_scr", [NT * 128, D], F32, kind="Internal").ap()
    xs_d = nc.dram_tensor("xs_scr", [NEXP * CAP, D], BF16, kind="Internal").ap()
    ys_d = nc.dram_tensor("ys_scr", [NEXP * CAP, D], F32, kind="Internal").ap()
    yT_d = nc.dram_tensor("yT_scr", [NT * 128, D], BF16, kind="Internal").ap()
    dbg_d = nc.dram_tensor("dbg", [128, dbgn], F32, kind="ExternalOutput").ap() if stop_after else None
    dbg_pos = [0]
    dbg_map = {}

    with ExitStack() as es0:
        S = Sched(nc, es0)

        def dump(name, ap, np_=128):
            if dbg_d is None:
                return
            n = ap.shape[-1] if len(ap.shape) == 2 else int(np.prod(ap.shape[1:]))
            dbg_map[name] = (dbg_pos[0], n, np_)
            S.dma('sp', lambda E: E.dma_start(out=dbg_d[0:np_, dbg_pos[0]:dbg_pos[0] + n], in_=ap, allow_slow_non_contiguous=True), r=[ap], w=['dbg'], is_out=True)
            dbg_pos[0] += n

        def chk(name):
            if stop_after == name:
                raise Stop()
        try:

            def mm(out, lhsT, rhs, start=True, stop=True):
                S.op('pe', lambda E: E.matmul(A(out), lhsT=A(lhsT), rhs=A(rhs), start=start, stop=stop),
                     r=[lhsT, rhs], w=[out])

            def act(out, in_, func, bias=None, scale=None, accum=None, e='act'):
                kw = {}
                rd = [in_]
                if bias is not None:
                    kw['bias'] = A(bias) if not isinstance(bias, float) else bias
                    if not isinstance(bias, float):
                        rd.append(bias)
                if scale is not None:
                    kw['scale'] = A(scale) if not isinstance(scale, float) else scale
                    if not isinstance(scale, float):
                        rd.append(scale)
                wr = [out]
                if accum is not None:
                    kw['accum_out'] = A(accum)
                    wr.append(accum)
                S.op('act', lambda E: E.activation(out=A(out), in_=A(in_), func=func, **kw), r=rd, w=wr)

            def tsc(out, in0, s1, s2, op0, op1=None, e='dve'):
                rd = [in0]
                a1 = s1
                a2 = s2
                if not isinstance(s1, (float, int)):
                    rd.append(s1); a1 = A(s1)
                if s2 is not None and not isinstance(s2, (float, int)):
                    rd.append(s2); a2 = A(s2)
                kw = {} if op1 is None else {'op1': op1}
                S.op(e, lambda E: E.tensor_scalar(out=A(out), in0=A(in0), scalar1=a1, scalar2=a2, op0=op0, **kw),
                     r=rd, w=[out])

            def ttn(out, a, b, op, e='dve'):
                S.op(e, lambda E: E.tensor_tensor(out=A(out), in0=A(a), in1=A(b), op=op), r=[a, b], w=[out])

            def stt(out, in0, sc, in1, op0, op1):
                rd = [in0, in1]
                a = sc
                if not isinstance(sc, (float, int)):
                    rd.append(sc); a = A(sc)
                S.op('dve', lambda E: E.scalar_tensor_tensor(out=A(out), in0=A(in0), scalar=a, in1=A(in1), op0=op0, op1=op1),
                     r=rd, w=[out])

            def cp(out, in_, e='dve'):
                if e == 'act':
                    S.op('act', lambda E: E.activation(out=A(out), in_=A(in_), func=AF.Copy), r=[in_], w=[out])
                else:
                    S.op(e, lambda E: E.tensor_copy(out=A(out), in_=A(in_)), r=[in_], w=[out])

            def mset(t, val, e='pool'):
                S.op(e, lambda E: E.memset(A(t), val), w=[t])

            def recip(out, in_):
                S.op('dve', lambda E: E.reciprocal(out=A(out), in_=A(in_)), r=[in_], w=[out])

            def dma(out, in_, q='sp', is_out=False):
                S.dma(q, lambda E: E.dma_start(out=A(out), in_=A(in_)), r=[in_], w=[out], is_out=is_out)

            banks = [es0.enter_context(nc.psum_tensor(f"pb{i}", [128, 512], F32)) for i in range(8)]
            st = {'q': 0, 'b': 0}

            def psq():
                i = st['q']; st['q'] = (i + 1) % 16
                b_, q_ = i % 4, (i // 4) % 4
                return V(banks[b_][:, q_ * 128:(q_ + 1) * 128], f"pb{b_}")

            def psb():
                i = st['b']; st['b'] = (i + 1) % 4
                return V(banks[4 + i][:, :], f"pbB{i}")

            def sub(v, ap):
                return V(ap, v.key) if isinstance(v, V) else ap

            T = lambda stack, name, shape, dt=F32: stack.enter_context(nc.sbuf_tensor(name, shape, dt))

            ones = T(es0, "ones", [128, 128]); ident = T(es0, "ident", [128, 128]); identb = T(es0, "identb", [128, 128], BF16)
            msu = T(es0, "msu", [128, 128]); msl = T(es0, "msl", [128, 128]); mst = T(es0, "mst", [128, 64])
            blk64 = T(es0, "blk64", [128, 128]); tric = T(es0, "tric", [128, 128]); maskc = T(es0, "maskc", [128, 128])
            mset(ones, 1.0)
            asel = lambda out, pat, cmp, base, cm, in_=None: S.op('pool', lambda E: E.affine_select(
                out=A(out), in_=A(in_ if in_ is not None else ones[:]), pattern=pat, compare_op=cmp, fill=0.0, base=base, channel_multiplier=cm),
                r=[in_ if in_ is not None else ones], w=[out])
            asel(ident[:], [[-1, 128]], ALU.is_equal, 0, 1)
            cp(identb[:], ident[:], e='pool')
            asel(msu[:], [[1, 128]], ALU.is_gt, 0, -1)
            asel(msl[:], [[-1, 128]], ALU.is_gt, 0, 1)
            asel(mst[0:64, :], [[1, 64]], ALU.is_ge, 0, -1, in_=ones[0:64, 0:64])
            asel(mst[64:128, :], [[1, 64]], ALU.is_ge, 0, -1, in_=ones[64:128, 0:64])
            mset(blk64, 0.0); mset(blk64[0:64, 0:64], 1.0); mset(blk64[64:128, 64:128], 1.0)
            asel(tric[:], [[1, 128]], ALU.is_ge, 0, -1)
            ttn(tric[:], tric[:], blk64[:], ALU.mult, e='pool')
            tsc(maskc[:], tric[:], 0.125, None, ALU.mult, e='pool')

            pstg = T(es0, "pstg", [128, 128]); pv = T(es0, "pv", [128, 128]); pv1 = T(es0, "pv1", [128, 128])
            mset(pstg, 0.0)
            row = {}
            rcur = [0]

            def ldrows(name, ap2d, n):
                row[name] = rcur[0]
                dma(pstg[rcur[0]:rcur[0] + n, :], ap2d)
                rcur[0] += n
            ldrows('nmix', nmix_d[0].rearrange("(c p) -> c p", p=128), 8)
            ldrows('muwag', muwag_d[0].rearrange("j (c p) -> (j c) p", p=128), 24)
            ldrows('murkv', murkv_d[0].rearrange("j (c p) -> (j c) p", p=128), 12)
            for nm, ap in (('w0', w0_d), ('a0', a0_d), ('kk', kk_d), ('ka', ka_d), ('rk', rk_d), ('lnw', lnw_d), ('lnb', lnb_d), ('cb', cb_d)):
                ldrows(nm, ap[0].rearrange("(c p) -> c p", p=128), 4)
            ldrows('cw', cw_d[0].rearrange("j (c p) -> (j c) p", p=128), 16)
            tp = psq()
            S.op('pe', lambda E: E.transpose(out=A(tp), in_=pstg[:], identity=ident[:]), r=[pstg, ident], w=[tp])
            cp(pv[:], tp, e='act')
            tsc(pv1[:], pv[:], -1.0, 1.0, ALU.mult, ALU.add)
            PV = lambda nm, j=0: pv[:, row[nm] + j:row[nm] + j + 1]
            PV1 = lambda nm, j=0: pv1[:, row[nm] + j:row[nm] + j + 1]

            mnw_bc = T(es0, "mnw_bc", [128, 512])
            gb_bc = T(es0, "gb_bc", [128, 8]); rb_bc = T(es0, "rb_bc", [128, 36]); iota_i = T(es0, "iota_i", [128, 32], I32)
            iota_f = T(es0, "iota_f", [128, 32]); giota = T(es0, "giota", [128, 4])

            dma(mnw_bc[:], mnw_d[0].partition_broadcast(128)); dma(gb_bc[:], gb_d[0].partition_broadcast(128))
            dma(rb_bc[:, 0:4], rgb_d[0].partition_broadcast(128)); dma(rb_bc[:, 4:36], reb_d[0].partition_broadcast(128))
            S.op('pool', lambda E: E.iota(iota_i[:], pattern=[[1, 32]], base=0, channel_multiplier=0), w=[iota_i])
            cp(iota_f[:], iota_i[:], e='pool')
            tsc(giota[:], iota_f[:, 0:4], 8.0, None, ALU.mult, e='pool')

            gates_all = T(es0, "gates_all", [128, NT, 2]); slots_all = T(es0, "slots_all", [128, NT, 2], I32)
            es1 = es0.enter_context(ExitStack())
            win_b = T(es1, "win_b", [128, 8, DIN], BF16)
            wla_b = T(es1, "wla_b", [128, 8, 288], BF16); wlamu_b = T(es1, "wlamu_b", [128, 8, 288], BF16)
            lb_b = T(es1, "lb_b", [128, 512], BF16); glb0_b = T(es1, "glb0_b", [128, 512], BF16); glb1_b = T(es1, "glb1_b", [32, 512], BF16)
            with ExitStack() as esl:
                stg = [T(esl, f"wstg{i}", [128, DIN]) for i in range(2)]
                win_v = win_d[0].rearrange("(c p) f -> p c f", p=128)
                ceng = ['dve', 'pool', 'act']
                k = 0
                for c in range(8):
                    s_ = stg[k % 2]
                    dma(s_[:, :], win_v[:, c, :])
                    cp(win_b[:, c, :], s_[:, :], e=ceng[k % 3]); k += 1
                s_ = stg[k % 2]; k += 1
                sv = s_[:, 0:8 * 288].rearrange("p (c j) -> p c j", j=288)
                dma(sv[:, :, 0:64], wla_d[0].rearrange("(c p) j -> p c j", p=128))
                dma(sv[:, :, 64:128], ala_d[0].rearrange("(c p) j -> p c j", p=128))
                dma(sv[:, :, 128:288], gla_d[0].rearrange("(c p) j -> p c j", p=128))
                cp(wla_b[:, :, :], sv, e='dve')
                for c in range(8):
                    for j, (lo, hi) in enumerate(((0, 64), (64, 128), (128, 288))):
                        tsc(wlamu_b[:, c, lo:hi], sv[:, c, lo:hi], PV('muwag', j * 8 + c), None, ALU.mult, e='dve' if c % 2 else 'pool')
                s_ = stg[k % 2]; k += 1
                dma(s_[0:64, 0:512], wlb_d[0]); dma(s_[64:128, 0:512], alb_d[0])
                dma(s_[:, 512:1024], glb_d[0][0:128, :]); dma(s_[0:32, 1024:1536], glb_d[0][128:160, :])
                cp(lb_b[:, :], s_[:, 0:512]); cp(glb0_b[:, :], s_[:, 512:1024]); cp(glb1_b[:, :], s_[0:32, 1024:1536])
                S.barrier()
            dump('pv', pv[:, :])
            chk('setup')

            es2 = es1.enter_context(ExitStack())
            t2 = lambda name, shape, dt=F32: T(es2, name, shape, dt)
            x_tm = [t2("x_tm0", [128, D])] * 2
            big = t2("big", [128, D])
            xnT_f = t2("xnT_f", [128, 8, 129]); xnT_b = t2("xnT_b", [128, 8, 128], BF16); xxT_b = t2("xxT_b", [128, 8, 128], BF16)
            rkv_raw = t2("rkv_raw", [128, 12, 129]); qk_raw = t2("qk_raw", [128, 4, 131])
            V1s = [t2(f"V1_{i}", [128, 4, 129]) for i in range(2)]; sigos = [t2(f"sigo{i}", [128, 512]) for i in range(2)]
            laT = t2("laT", [128, 128], BF16); lg0 = t2("lg0", [128, 128], BF16); lg1 = t2("lg1", [32, 128], BF16)
            sgw = t2("sgw", [128, 4, 128]); asig = t2("asig", [128, 4, 128]); ggs = [t2(f"gg{i}", [128, 4, 128]) for i in range(2)]
            bonuss = [t2(f"bonus{i}", [128, 4, 128]) for i in range(2)]; y_as = [t2(f"y_a{i}", [128, 4, 128]) for i in range(2)]
            pt4 = t2("pt4", [128, 4, 128])
            yT_bs = [t2(f"yT_b{i}", [128, 8, 128], BF16) for i in range(2)]
            ssq = t2("ssq", [128, 4]); rstd = t2("rstd", [128, 4])
            rb = {nm: t2(f"rb_{nm}", [128, 4, 128]) for nm in ('r', 'k', 'v', 'kkn', 'k2', 'bv', 'tA', 'tB', 'cs', 'E1', 'E2')}
            STg = t2("STg", [128, 4, 128])
            opzT = t2("opzT", [128, 4, 2, 4, 128], BF16)
            rstT = t2("rstT", [128, 4, 128], BF16)
            gC = t2("gC", [128, 4, 2])
            ArbArk = t2("alg_ArbArk", [128, 2, 4, 64], BF16)
            Gc = [{nm: t2(f"alg{c}_{nm}", [128, 4, 128], BF16) for nm in
                   ('BzT', 'KzT', 'VzT', 'Aak', 'Q0', 'Q1', 'QT0', 'QT1', 'P0', 'P1', 'PT0', 'PT1')} for c in range(2)]
            GS = {nm: t2(f"alg_{nm}", [128, 4, 128], BF16) for nm in ('W0T', 'UT')}
            ArbArks = [ArbArk, t2("alg_ArbArk1", [128, 2, 4, 64], BF16)]
            ST32 = t2("ST32", [128, 4, 128])
            STb = t2("STb", [128, 4, 128], BF16)
            QKf = t2("QKf", [128, 4, 128]); cacc = QKf
            g8s = [t2(f"g8_{i}", [128, 8]) for i in range(2)]; th8 = t2("th8", [128, 8]); nbg = t2("nbg", [128, 8]); wgt = t2("wgt", [128, 4]); dbias = t2("dbias", [128, 4])
            lfb = [t2(f"lfb{i}", [128, 128]) for i in range(2)]; Dm = [t2(f"Dm{i}", [128, 128]) for i in range(2)]
            eB = [t2(f"eB{i}", [128, 128]) for i in range(2)]; Pm = [t2(f"Pm{i}", [128, 128]) for i in range(2)]
            Qz = [t2(f"Qz{h}", [128, 2, 128]) for h in range(4)]
            Kw = t2("Kw", [128, 4, 64])
            CTa = [t2(f"CTa{h}", [128, 129]) for h in range(4)]; CTb = [t2(f"CTb{h}", [128, 129]) for h in range(4)]
            hraw = [t2(f"hraw{i}", [128, 128]) for i in range(2)]; y_b = t2("y_b", [128, 512])
            sm = t2("sm", [128, 16])

            mset(xnT_f[:, :, 0:1], 0.0); mset(rkv_raw[:, :, 0:1], 0.0); mset(qk_raw[:, :, 0:3], 0.0)
            mset(V1s[0][:, :, 128:129], 1.0); mset(V1s[1][:, :, 128:129], 1.0)
            mset(ST32, 0.0); mset(STb, 0.0)
            mset(opzT, 0.0)
            for h in range(4):
                mset(Qz[h], 0.0); mset(CTa[h], 0.0); mset(CTb[h], 0.0)

            def g_front(i):
                xt = x_tm[i % 2]; V1 = V1s[i % 2]; sigo = sigos[i % 2]; g8 = g8s[i % 2]; gg = ggs[i % 2]
                if i == 0:
                    mset(xt, 0.0)
                    dma(xt[112:128, :], meta_d)
                else:
                    dma(xt[:, :], x_d[(i - 1) * 128:i * 128, :])
                act(big[:], xt[:], AF.Square, accum=ssq[:, 0:1])
                act(rstd[:, 0:1], ssq[:, 0:1], AF.Sqrt, bias=1e-6, scale=1.0 / D)
                recip(rstd[:, 0:1], rstd[:, 0:1])
                tsc(big[:], xt[:], rstd[:, 0:1], None, ALU.mult)
                yield
                for half in range(2):
                    pb = psb()
                    for j in range(4):
                        c = half * 4 + j
                        S.op('pe', lambda E, c=c, j=j, pb=pb: E.transpose(out=A(pb)[:, j * 128:(j + 1) * 128], in_=big[:, c * 128:(c + 1) * 128], identity=ident[:]),
                             r=[big, ident], w=[pb])
                    for j in range(4):
                        c = half * 4 + j
                        if j % 2 == 0:
                            act(xnT_f[:, c, 1:129], sub(pb, A(pb)[:, j * 128:(j + 1) * 128]), AF.Copy, scale=PV('nmix', c))
                        else:
                            tsc(xnT_f[:, c, 1:129], sub(pb, A(pb)[:, j * 128:(j + 1) * 128]), PV('nmix', c), None, ALU.mult)
                    yield
                cp(xnT_b[:, :, :], xnT_f[:, :, 1:129], e='pool')
                ttn(xxT_b[:, :, :], xnT_f[:, :, 0:128], xnT_f[:, :, 1:129], ALU.subtract)
                cp(xnT_f[:, :, 0:1], xnT_f[:, :, 128:129], e='pool')
                yield

                for blk in range(16):
                    p_ = psq()
                    for c in range(8):
                        mm(p_, win_b[:, c, blk * 128:(blk + 1) * 128], xnT_b[:, c, :], start=(c == 0), stop=(c == 7))
                    if blk < 12:
                        cp(rkv_raw[:, blk, 1:129], p_, e='act')
                    else:
                        cp(qk_raw[:, blk - 12, 3:131], p_, e='act')
                    yield
                pv_ = psb()
                for c in range(8):
                    mm(pv_, xnT_b[:, c, :], win_b[:, c, 2048:2560], start=(c == 0), stop=(c == 7))
                cp(V1[:, :, 0:128], sub(pv_, A(pv_).rearrange("p (h v) -> p h v", v=128)), e='act')
                yield
                po_ = psb()
                for c in range(8):
                    mm(po_, xnT_b[:, c, :], win_b[:, c, 2560:3072], start=(c == 0), stop=(c == 7))
                act(sigo[:], po_, AF.Sigmoid)
                yield
                pg_ = psq()
                pg8 = sub(pg_, A(pg_)[:, 0:8])
                for c in range(8):
                    mm(pg8, xnT_b[:, c, :], win_b[:, c, 3072:3080], start=(c == 0), stop=(c == 7))
                ttn(g8[:], pg8, gb_bc[:], ALU.add)
                yield
                la_ps = []
                for (lo, hi) in ((0, 128), (128, 256), (256, 288)):
                    p_ = psq()
                    po = sub(p_, A(p_)[0:hi - lo, :])
                    for c in range(8):
                        mm(po, wla_b[:, c, lo:hi], xnT_b[:, c, :], start=(c == 0), stop=False)
                    for c in range(8):
                        mm(po, wlamu_b[:, c, lo:hi], xxT_b[:, c, :], start=False, stop=(c == 7))
                    la_ps.append(p_)
                act(laT[0:64, :], sub(la_ps[0], A(la_ps[0])[0:64, :]), AF.Tanh)
                cp(laT[64:128, :], sub(la_ps[0], A(la_ps[0])[64:128, :]), e='dve')
                act(lg0[:, :], la_ps[1], AF.Sigmoid)
                act(lg1[:, :], sub(la_ps[2], A(la_ps[2])[0:32, :]), AF.Sigmoid)
                yield
                for fb in range(4):
                    fs = slice(fb * 128, (fb + 1) * 128)
                    p_ = psq(); mm(p_, lb_b[0:64, fs], laT[0:64, :])
                    act(sgw[:, fb, :], p_, AF.Sigmoid, bias=PV('w0', fb))
                    p_ = psq(); mm(p_, lb_b[64:128, fs], laT[64:128, :])
                    act(asig[:, fb, :], p_, AF.Sigmoid, bias=PV('a0', fb))
                    p_ = psq(); mm(p_, glb0_b[:, fs], lg0[:, :], start=True, stop=False); mm(p_, glb1_b[0:32, fs], lg1[0:32, :], start=False, stop=True)
                    cp(gg[:, fb, :], p_, e='dve')
                    yield

                yield

            pending = []
            for i in range(NT):
                xt = x_tm[i % 2]; V1 = V1s[i % 2]; sigo = sigos[i % 2]; g8 = g8s[i % 2]; gg = ggs[i % 2]
                yT_b = yT_bs[i % 2]; bonus = bonuss[i % 2]; y_a = y_as[i % 2]
                if i == 0:
                    for _ in g_front(0):
                        pass
                def g_prep_all():
                    R = rb
                    P4 = lambda nm, j=0: pv[:, row[nm] + j:row[nm] + j + 4].unsqueeze(2).broadcast_to([128, 4, 128])
                    P41 = lambda nm, j=0: pv1[:, row[nm] + j:row[nm] + j + 4].unsqueeze(2).broadcast_to([128, 4, 128])
                    for nm, bi, tmp in (('k', 1, 'tA'), ('r', 0, 'tB'), ('v', 2, 'E2')):
                        ttn(R[tmp][:, :, :], rkv_raw[:, bi * 4:bi * 4 + 4, 0:128], P4('murkv', bi * 4), ALU.mult, e='pool')
                        ttn(R[nm][:, :, :], rkv_raw[:, bi * 4:bi * 4 + 4, 1:129], P41('murkv', bi * 4), ALU.mult)
                        ttn(R[nm][:, :, :], R[nm][:, :, :], R[tmp][:, :, :], ALU.add)
                    yield
                    ttn(R['kkn'][:, :, :], R['k'][:, :, :], P4('kk'), ALU.mult)
                    ttn(R['tA'][:, :, :], R['kkn'][:, :, :], R['kkn'][:, :, :], ALU.mult, e='pool')
                    bk = psb()
                    for fb in range(4):
                        mm(sub(bk, A(bk)[:, fb * 128:(fb + 1) * 128]), blk64[:], R['tA'][:, fb, :])
                    act(R['tB'][:, :, :], sub(bk, A(bk).rearrange("p (f t) -> p f t", t=128)), AF.Sqrt)
                    for fb in range(4):
                        for c in range(2):
                            cs_ = slice(c * 64, (c + 1) * 64)
                            S.op('dve', lambda E, fb=fb, cs_=cs_: E.tensor_tensor_scan(out=R['cs'][:, fb, cs_], data0=ones[:, 0:64], data1=sgw[:, fb, cs_], initial=0.0, op0=ALU.mult, op1=ALU.add),
                                 r=[ones, sgw], w=[R['cs']])
                    yield
                    tsc(R['tB'][:, :, :], R['tB'][:, :, :], 1e-12, None, ALU.max)
                    recip(R['tB'][:, :, :], R['tB'][:, :, :])
                    ttn(R['kkn'][:, :, :], R['kkn'][:, :, :], R['tB'][:, :, :], ALU.mult)
                    act(R['E1'][:, :, :], R['cs'][:, :, :], AF.Exp, scale=-C0)
                    act(R['E2'][:, :, :], R['cs'][:, :, :], AF.Exp, scale=C0)
                    ttn(R['tA'][:, :, :], asig[:, :, :], P4('ka'), ALU.mult, e='pool')
                    ttn(R['tA'][:, :, :], R['tA'][:, :, :], P41('ka'), ALU.add, e='pool')
                    yield
                    ttn(R['k2'][:, :, :], R['k'][:, :, :], R['tA'][:, :, :], ALU.mult)
                    ttn(R['bv'][:, :, :], R['kkn'][:, :, :], asig[:, :, :], ALU.mult, e='pool')
                    ttn(R['tB'][:, :, :], R['cs'][:, :, :], sgw[:, :, :], ALU.subtract, e='pool')
                    act(R['tB'][:, :, :], R['tB'][:, :, :], AF.Exp, scale=-C0)
                    ttn(R['tA'][:, :, :], R['r'][:, :, :], P4('rk'), ALU.mult, e='pool')
                    ttn(R['tA'][:, :, :], R['tA'][:, :, :], R['k2'][:, :, :], ALU.mult)
                    bk = psb()
                    for fb in range(4):
                        mm(sub(bk, A(bk)[:, fb * 128:(fb + 1) * 128]), blk64[:], R['tA'][:, fb, :])
                    ttn(bonus[:, :, :], sub(bk, A(bk).rearrange("p (f t) -> p f t", t=128)), R['v'][:, :, :], ALU.mult)
                    yield
                    E3 = R['tB']
                    for c in range(2):
                        for hh in range(2):
                            ps_ = slice(hh * 64, (hh + 1) * 64)
                            ts_ = slice(c * 64, (c + 1) * 64)
                            os_ = slice(hh * 64, (hh + 1) * 64)
                            stt(opzT[ps_, :, c, 0, os_], R['kkn'][ps_, :, ts_], -1.0, E3[ps_, :, ts_], ALU.mult, ALU.mult)
                            ttn(opzT[ps_, :, c, 1, os_], R['bv'][ps_, :, ts_], R['E2'][ps_, :, ts_], ALU.mult)
                            ttn(opzT[ps_, :, c, 2, os_], R['k2'][ps_, :, ts_], R['E2'][ps_, :, ts_], ALU.mult, e='pool')
                            cp(opzT[ps_, :, c, 3, os_], R['v'][ps_, :, ts_], e='pool')
                        yield
                    ttn(rstT[:, :, :], R['r'][:, :, :], R['E1'][:, :, :], ALU.mult)
                    cp(gC[:, :, 0:1], R['E1'][:, :, 63:64], e='pool')
                    cp(gC[:, :, 1:2], R['E1'][:, :, 127:128], e='pool')
                    yield

                def q4(bank):
                    return sub(bank, A(bank).rearrange("p (f t) -> p f t", t=128))
                bc4 = lambda m: m[:, :].unsqueeze(1).broadcast_to([128, 4, 128])
                TinvOf = {}
                def g_alg(c):
                    Az = [opzT[:, fb, c, 0, :] for fb in range(4)]; Bz = [opzT[:, fb, c, 1, :] for fb in range(4)]
                    Kz = [opzT[:, fb, c, 2, :] for fb in range(4)]; Vz = [opzT[:, fb, c, 3, :] for fb in range(4)]
                    Rs = [rstT[:, fb, c * 64:(c + 1) * 64] for fb in range(4)]
                    for kk_i, (nm, src) in enumerate((('BzT', Bz), ('KzT', Kz), ('VzT', Vz))):
                        bk = psb()
                        for fb in range(4):
                            mm(sub(bk, A(bk)[:, fb * 128:(fb + 1) * 128]), src[fb], identb[:])
                        cp(Gc[c][nm][:, :, :], q4(bk), e='act' if kk_i != 1 else 'dve')
                        yield
                    for nm, l_, r_, msk in (('Q0', Bz, Az, msu), ('QT0', Az, Bz, msl), ('Aak', Kz, Az, msu)):
                        bk = psb()
                        for fb in range(4):
                            mm(sub(bk, A(bk)[:, fb * 128:(fb + 1) * 128]), l_[fb], r_[fb])
                        ttn(Gc[c][nm][:, :, :], q4(bk), bc4(msk), ALU.mult)
                        yield
                    bk = psb()
                    for j, l_ in enumerate((Bz, Kz)):
                        for fb in range(4):
                            o0 = j * 256 + fb * 64
                            mm(sub(bk, A(bk)[:, o0:o0 + 64]), l_[fb], Rs[fb])
                    ttn(ArbArks[c][:, :, :, :].rearrange("p j f t -> p (j f) t"), sub(bk, A(bk).rearrange("p (g t) -> p g t", t=64)),
                        mst[:, :].unsqueeze(1).broadcast_to([128, 8, 64]), ALU.mult)
                    yield
                    ttn(Gc[c]['P0'][:, :, :], Gc[c]['Q0'][:, :, :], identb[:, :].unsqueeze(1).broadcast_to([128, 4, 128]), ALU.add, e='pool')
                    ttn(Gc[c]['PT0'][:, :, :], Gc[c]['QT0'][:, :, :], identb[:, :].unsqueeze(1).broadcast_to([128, 4, 128]), ALU.add, e='pool')
                    yield
                    Qb = [Gc[c]['Q0'], Gc[c]['Q1']]; QTb = [Gc[c]['QT0'], Gc[c]['QT1']]
                    Pb = [Gc[c]['P0'], Gc[c]['P1']]; PTb = [Gc[c]['PT0'], Gc[c]['PT1']]
                    for s_ in range(6):
                        Qs, QTs = Qb[s_ % 2], QTb[s_ % 2]
                        Qn, QTn = Qb[(s_ + 1) % 2], QTb[(s_ + 1) % 2]
                        Pp, PTp = Pb[(s_ - 1) % 2], PTb[(s_ - 1) % 2]
                        Pc, PTc = Pb[s_ % 2], PTb[s_ % 2]
                        todo = []
                        if s_ <= 4:
                            bk = psb()
                            for fb in range(4):
                                mm(sub(bk, A(bk)[:, fb * 128:(fb + 1) * 128]), QTs[:, fb, :], Qs[:, fb, :])
                            todo.append(lambda bk=bk: cp(Qn[:, :, :], q4(bk), e='act'))
                        if s_ <= 3:
                            bk = psb()
                            for fb in range(4):
                                mm(sub(bk, A(bk)[:, fb * 128:(fb + 1) * 128]), Qs[:, fb, :], QTs[:, fb, :])
                            todo.append(lambda bk=bk: cp(QTn[:, :, :], q4(bk), e='act'))
                        if s_ >= 1:
                            bk = psb()
                            for fb in range(4):
                                mm(sub(bk, A(bk)[:, fb * 128:(fb + 1) * 128]), PTp[:, fb, :], Qs[:, fb, :])
                            todo.append(lambda bk=bk: ttn(Pc[:, :, :], q4(bk), Pp[:, :, :], ALU.add))
                        if 1 <= s_ <= 4:
                            bk = psb()
                            for fb in range(4):
                                mm(sub(bk, A(bk)[:, fb * 128:(fb + 1) * 128]), Qs[:, fb, :], PTp[:, fb, :])
                            todo.append(lambda bk=bk: ttn(PTc[:, :, :], q4(bk), PTp[:, :, :], ALU.add))
                        for f_ in todo:
                            f_()
                        yield
                    cur = 1
                    TinvOf[c] = Gc[c][f'P{cur}']
                    yield
                def g_chain(c):
                    Az = [opzT[:, fb, c, 0, :] for fb in range(4)]; Bz = [opzT[:, fb, c, 1, :] for fb in range(4)]
                    Kz = [opzT[:, fb, c, 2, :] for fb in range(4)]; Vz = [opzT[:, fb, c, 3, :] for fb in range(4)]
                    Rs = [rstT[:, fb, c * 64:(c + 1) * 64] for fb in range(4)]
                    ttn(STg[:, :, :], ST32[:, :, :], gC[:, :, c:c + 1].broadcast_to([128, 4, 128]), ALU.mult, e='pool')
                    bk = psb()
                    for fb in range(4):
                        o_ = sub(bk, A(bk)[:, fb * 128:(fb + 1) * 128])
                        mm(o_, Az[fb], STb[:, fb, :], start=True, stop=False); mm(o_, Gc[c]['Aak'][:, fb, :], Gc[c]['VzT'][:, fb, :], start=False, stop=True)
                    cp(GS['W0T'][:, :, :], q4(bk), e='act')
                    yield
                    bk = psb()
                    for fb in range(4):
                        mm(sub(bk, A(bk)[:, fb * 128:(fb + 1) * 128]), TinvOf[c][:, fb, :], GS['W0T'][:, fb, :])
                    cp(GS['UT'][:, :, :], q4(bk), e='act')
                    yield
                    bkS = psb()
                    for fb in range(4):
                        o_ = sub(bkS, A(bkS)[:, fb * 128:(fb + 1) * 128])
                        mm(o_, Gc[c]['BzT'][:, fb, :], GS['UT'][:, fb, :], start=True, stop=False)
                        mm(o_, Gc[c]['KzT'][:, fb, :], Gc[c]['VzT'][:, fb, :], start=False, stop=True)
                    bkY = psb()
                    for fb in range(4):
                        o_ = sub(bkY, A(bkY)[:, fb * 64:(fb + 1) * 64])
                        mm(o_, STb[:, fb, :], Rs[fb], start=True, stop=False)
                        mm(o_, GS['UT'][:, fb, :], ArbArks[c][:, 0, fb, :], start=False, stop=False)
                        mm(o_, Gc[c]['VzT'][:, fb, :], ArbArks[c][:, 1, fb, :], start=False, stop=True)
                    for fb in range(4):
                        stt(STb[:, fb, :], sub(bkS, A(bkS)[:, fb * 128:(fb + 1) * 128]), gC[:, fb, c:c + 1], STg[:, fb, :], ALU.mult, ALU.add)
                    for fb in range(4):
                        stt(ST32[:, fb, :], sub(bkS, A(bkS)[:, fb * 128:(fb + 1) * 128]), gC[:, fb, c:c + 1], STg[:, fb, :], ALU.mult, ALU.add)
                    cp(y_a[:, :, c * 64:(c + 1) * 64], sub(bkY, A(bkY)[:, 0:256].rearrange("p (f t) -> p f t", t=64)), e='act')
                    yield
                    yield

                def g_post4(y_a=y_a, bonus=bonus, gg=gg, yT_b=yT_b):
                    P4 = lambda nm: pv[:, row[nm]:row[nm] + 4].unsqueeze(2).broadcast_to([128, 4, 128])
                    v4 = lambda bank: sub(bank, A(bank).rearrange("p (f t) -> p f t", t=128))
                    bk = psb()
                    for fb in range(4):
                        mm(sub(bk, A(bk)[:, fb * 128:(fb + 1) * 128]), blk64[:], y_a[:, fb, :])
                    act(pt4[:, :, :], v4(bk), AF.Copy, scale=1.0 / 64)
                    yield
                    ttn(y_a[:, :, :], y_a[:, :, :], pt4[:, :, :], ALU.subtract)
                    ttn(pt4[:, :, :], y_a[:, :, :], y_a[:, :, :], ALU.mult, e='pool')
                    yield
                    bk = psb()
                    for fb in range(4):
                        mm(sub(bk, A(bk)[:, fb * 128:(fb + 1) * 128]), blk64[:], pt4[:, fb, :])
                    act(pt4[:, :, :], v4(bk), AF.Sqrt, bias=64e-5, scale=1.0 / 64)
                    yield
                    recip(pt4[:, :, :], pt4[:, :, :])
                    ttn(y_a[:, :, :], y_a[:, :, :], pt4[:, :, :], ALU.mult)
                    yield
                    ttn(y_a[:, :, :], y_a[:, :, :], P4('lnw'), ALU.mult)
                    ttn(y_a[:, :, :], y_a[:, :, :], P4('lnb'), ALU.add, e='pool')
                    yield
                    ttn(y_a[:, :, :], y_a[:, :, :], bonus[:, :, :], ALU.add)
                    ttn(yT_b[:, 0:4, :], y_a[:, :, :], gg[:, :, :], ALU.mult)
                    yield

                def g_mlstm_pre():
                    cw4 = lambda t_: pv[:, row['cw'] + t_ * 4:row['cw'] + t_ * 4 + 4].unsqueeze(2).broadcast_to([128, 4, 128])
                    cb4 = pv[:, row['cb']:row['cb'] + 4].unsqueeze(2).broadcast_to([128, 4, 128])
                    ttn(cacc[:, :, :], qk_raw[:, :, 0:128], cw4(0), ALU.mult, e='pool')
                    ttn(cacc[:, :, :], cacc[:, :, :], cb4, ALU.add)
                    for t_ in range(1, 4):
                        ttn(STg[:, :, :], qk_raw[:, :, t_:t_ + 128], cw4(t_), ALU.mult, e='pool')
                        ttn(cacc[:, :, :], cacc[:, :, :], STg[:, :, :], ALU.add)
                        yield
                    act(QKf[:, :, :], cacc[:, :, :], AF.Silu)
                    yield
                    cp(qk_raw[:, :, 0:3], qk_raw[:, :, 128:131], e='pool')
                    act(th8[:], g8[:], AF.Tanh, scale=1.0 / 15.0)
                    tsc(g8[:, 0:4], th8[:, 0:4], 15.0, None, ALU.mult)
                    act(g8[:, 4:8], th8[:, 4:8], AF.Exp, scale=-15.0)
                    act(g8[:, 4:8], g8[:, 4:8], AF.Ln, bias=1.0)
                    yield
                    if i == 0:
                        mset(g8[0:112, 0:4], -1.0e4, e='dve'); mset(g8[0:112, 4:8], 0.0, e='dve')
                    pn = psq()
                    pn4 = sub(pn, A(pn)[:, 0:4]); pn8 = sub(pn, A(pn)[:, 4:8])
                    mm(pn4, tric[:], g8[:, 4:8]); mm(pn8, blk64[:], g8[:, 4:8])
                    cp(nbg[:], sub(pn, A(pn)[:, 0:8]), e='act')
                    yield
                    ttn(dbias[:], g8[:, 0:4], nbg[:, 0:4], ALU.add)
                    ttn(wgt[:], dbias[:], nbg[:, 4:8], ALU.subtract)
                    act(wgt[:], wgt[:], AF.Exp, bias=math.log(0.125))
                    yield
                    for kb in range(2):
                        p_ = psq()
                        S.op('pe', lambda E, kb=kb, p_=p_: E.transpose(out=A(p_), in_=QKf[:, 2 + kb, :], identity=ident[:]), r=[QKf, ident], w=[p_])
                        for hh in range(2):
                            h = kb * 2 + hh
                            tsc(Kw[:, h, :], sub(p_, A(p_)[:, hh * 64:(hh + 1) * 64]), wgt[:, h:h + 1], None, ALU.mult)
                    yield

                def g_mlstm_rest():
                    def g_head(h):
                        hb = (h % 2) * 64
                        hs = slice(hb, hb + 64)
                        qb = h // 2
                        L_, D_, E_, P_ = lfb[h % 2], Dm[h % 2], eB[h % 2], Pm[h % 2]
                        tsc(L_[:], ones[:], g8[:, 4 + h:5 + h], None, ALU.mult, e='pool')
                        pbr = psq(); mm(pbr, L_[:], tric[:])
                        act(D_[:], pbr, AF.Exp, bias=dbias[:, h:h + 1], scale=-1.0)
                        act(E_[:], pbr, AF.Exp, scale=-1.0)
                        yield
                        psc = psq(); mm(psc, QKf[hs, 2 + qb, :], QKf[hs, qb, :])
                        ttn(D_[:], D_[:], maskc[:], ALU.mult, e='pool')
                        ttn(P_[:], D_[:], psc, ALU.mult)
                        yield
                        ttn(Qz[h][hs, 0, 0:64], QKf[hs, qb, 0:64], E_[hs, 0:64], ALU.mult)
                        ttn(Qz[h][hs, 1, 64:128], QKf[hs, qb, 64:128], E_[hs, 64:128], ALU.mult)
                        pU = psb()
                        u0 = sub(pU, A(pU)[hs, 0:129]); u1 = sub(pU, A(pU)[hs, 256:385])
                        mm(u0, Kw[0:64, h, :], V1[0:64, h, :])
                        stt(CTb[h][hs, :], CTa[h][hs, :], E_[hs, 63:64], u0, ALU.mult, ALU.add)
                        yield
                        pO = psb(); o_ = sub(pO, A(pO)[:, 0:129])
                        mm(o_, P_[:], V1[:, h, :], start=True, stop=False)
                        mm(o_, Qz[h][hs, 0, :], CTa[h][hs, :], start=False, stop=False)
                        mm(o_, Qz[h][hs, 1, :], CTb[h][hs, :], start=False, stop=True)
                        mm(u1, Kw[64:128, h, :], V1[64:128, h, :])
                        stt(CTa[h][hs, :], CTb[h][hs, :], E_[hs, 127:128], u1, ALU.mult, ALU.add)
                        if i > 0:
                            H_ = hraw[h % 2]
                            act(sm[:, 3 * h:3 * h + 1], sub(pO, A(pO)[:, 128:129]), AF.Abs)
                            tsc(sm[:, 3 * h:3 * h + 1], sm[:, 3 * h:3 * h + 1], 1.0, None, ALU.max)
                            recip(sm[:, 3 * h:3 * h + 1], sm[:, 3 * h:3 * h + 1])
                            tsc(H_[:], sub(pO, A(pO)[:, 0:128]), sm[:, 3 * h:3 * h + 1], None, ALU.mult)
                            act(P_[:], H_[:], AF.Square, accum=sm[:, 3 * h + 1:3 * h + 2])
                            yield
                            act(sm[:, 3 * h + 2:3 * h + 3], sm[:, 3 * h + 1:3 * h + 2], AF.Sqrt, bias=1e-6, scale=1.0 / 128)
                            recip(sm[:, 3 * h + 2:3 * h + 3], sm[:, 3 * h + 2:3 * h + 3])
                            yield
                            stt(H_[:], H_[:], sm[:, 3 * h + 2:3 * h + 3], mnw_bc[:, h * 128:(h + 1) * 128], ALU.mult, ALU.mult)
                            ttn(y_b[:, h * 128:(h + 1) * 128], H_[:], sigo[:, h * 128:(h + 1) * 128], ALU.mult)
                        yield
                    yield from inter([g_head(0), g_head(1)])
                    yield from inter([g_head(2), g_head(3)])
                    if i == 0:
                        return
                    for h in range(4):
                        p_ = psq()
                        S.op('pe', lambda E, h=h, p_=p_: E.transpose(out=A(p_), in_=y_b[:, h * 128:(h + 1) * 128], identity=ident[:]), r=[y_b, ident], w=[p_])
                        cp(yT_b[:, 4 + h, :], p_, e='act')
                    yield

                def g_rwkv_prep():
                    yield from g_prep_all()
                    cp(rkv_raw[:, :, 0:1], rkv_raw[:, :, 128:129], e='pool')
                    yield

                def g_rwkv_rest():
                    yield from inter([g_alg(0), g_alg(1)])
                    yield from g_chain(0)
                    yield from g_chain(1)

                def g_post_all(i=i, yT_b=yT_b, post=g_post4()):
                    yield from post
                    dma(yT_d[i * 128:(i + 1) * 128, :], yT_b[:, :, :].rearrange("p b t -> p (b t)"))
                    yield
                for _ in inter([g_rwkv_prep(), g_mlstm_pre()] + pending):
                    pass
                pending = []
                streams = [g_rwkv_rest(), g_mlstm_rest()]
                if i + 1 < NT:
                    streams.append(g_front(i + 1))
                for _ in inter(streams, weights=[2, 1, 1][:len(streams)]):
                    pass
                if i > 0:
                    pending = [g_post_all()]
            for _ in inter(pending):
                pass

            S.barrier()
            chk('p1')
            es2.close()
            es1.close()

            with ExitStack() as es5:
                t5 = lambda name, shape, dt=F32: T(es5, name, shape, dt)
                NB1 = 4
                wout_b = t5("wout_b", [128, 8, D], BF16); wr_f = t5("wr_f", [128, 8, 36]); wffn_bc = t5("wffn_bc", [128, D])
                wst = [t5(f"wst{i}", [128, D]) for i in range(2)]
                xt1 = [t5(f"xt1_{i}", [128, D]) for i in range(NB1)]; yt1 = [t5(f"yt1_{i}", [128, 8, 128], BF16) for i in range(NB1)]
                h1s = [t5(f"h1s{i}", [128, D]) for i in range(NB1)]; big2 = [t5(f"big2_{i}", [128, D]) for i in range(NB1)]
                xn2_bs = [t5(f"xn2_b{i}", [128, D], BF16) for i in range(NB1)]; xn2T = [t5(f"xn2T{i}", [128, 8, 128]) for i in range(NB1)]
                scr = [dict(lgt=t5(f"lgt{i}", [128, 36]), rsm=t5(f"rsm{i}", [128, 32]), oh=[t5(f"oh{k}_{i}", [128, 32]) for k in range(2)],
                            cnt=t5(f"cnt{i}", [128, 32]), el=t5(f"el{i}", [128, 8]), mx8=t5(f"mx8_{i}", [128, 8]), ix8=t5(f"ix8_{i}", [128, 8], U32),
                            sm=t5(f"smb{i}", [128, 16])) for i in range(NB1)]
                carry = t5("carry", [1, 32])
                mset(carry, 0.0)
                dma(wffn_bc[:], nffn_d[0].partition_broadcast(128))
                dma(wr_f[:, :, 0:4], rgw_d[0].rearrange("(c p) e -> p c e", p=128))
                dma(wr_f[:, :, 4:36], rew_d[0].rearrange("(c p) e -> p c e", p=128))
                wout_v = wout_d[0].rearrange("(c p) f -> p c f", p=128)
                for c in range(8):
                    dma(wst[c % 2][:, :], wout_v[:, c, :])
                    cp(wout_b[:, c, :], wst[c % 2][:, :], e=('dve', 'act')[c % 2])

                def loads1b(i):
                    dma(xt1[i % NB1][:, :], x_d[(i - 1) * 128:i * 128, :])
                    dma(yt1[i % NB1][:, :, :].rearrange("p b t -> p (b t)"), yT_d[i * 128:(i + 1) * 128, :])
                def g_A(i):
                    b = i % NB1
                    xt = xt1[b]; yT_b = yt1[b]; h1 = h1s[b]; big = big2[b]; xn2_b = xn2_bs[b]; xT2 = xn2T[b]
                    Z = scr[b]; lgt = Z['lgt']; rsm = Z['rsm']; oh = Z['oh']; cnt = Z['cnt']; el = Z['el']; mx8 = Z['mx8']; ix8 = Z['ix8']; sm = Z['sm']
                    for n in range(2):
                        pm_ = psb()
                        for blk in range(8):
                            mm(pm_, yT_b[:, blk, :], wout_b[:, blk, n * 512:(n + 1) * 512], start=(blk == 0), stop=(blk == 7))
                        ttn(h1[:, n * 512:(n + 1) * 512], xt[:, n * 512:(n + 1) * 512], pm_, ALU.add)
                    dma(h1_d[i * 128:(i + 1) * 128, :], h1[:, :], q='pool')
                    yield
                    act(big[:], h1[:], AF.Square, accum=sm[:, 14:15])
                    act(sm[:, 15:16], sm[:, 14:15], AF.Sqrt, bias=1e-6, scale=1.0 / D)
                    recip(sm[:, 15:16], sm[:, 15:16])
                    stt(big[:], h1[:], sm[:, 15:16], wffn_bc[:], ALU.mult, ALU.mult)
                    cp(xn2_b[:], big[:], e='pool')
                    yield
                    for half in range(2):
                        pb = psb()
                        for j in range(4):
                            c = half * 4 + j
                            S.op('pe', lambda E, c=c, j=j, pb=pb, big=big: E.transpose(out=A(pb)[:, j * 128:(j + 1) * 128], in_=big[:, c * 128:(c + 1) * 128], identity=ident[:]),
                                 r=[big, ident], w=[pb])
                        cp(xT2[:, half * 4:half * 4 + 4, :], sub(pb, A(pb).rearrange("p (j t) -> p j t", t=128)), e='act')
                        yield
                    pl = psq(); pl36 = sub(pl, A(pl)[:, 0:36])
                    for c in range(8):
                        mm(pl36, xT2[:, c, :], wr_f[:, c, :], start=(c == 0), stop=(c == 7))
                    ttn(lgt[:], pl36, rb_bc[:], ALU.add)
                    yield
                    S.op('dve', lambda E: E.tensor_reduce(out=sm[:, 4:5], in_=lgt[:, 0:4], axis=AX.X, op=ALU.max, negate=True), r=[lgt], w=[sm])
                    act(rsm[:, 0:4], lgt[:, 0:4], AF.Exp, bias=sm[:, 4:5], accum=sm[:, 5:6])
                    recip(sm[:, 5:6], sm[:, 5:6])
                    yield
                    tsc(sm[:, 4:5], sm[:, 4:5], -1.0, None, ALU.mult)
                    tsc(rsm[:, 4:8], lgt[:, 0:4], sm[:, 4:5], None, ALU.is_equal)
                    yield
                    tsc(el[:], lgt[:, 4:12], rsm[:, 4:5], None, ALU.mult)
                    for g in range(1, 4):
                        stt(el[:], lgt[:, 4 + g * 8:12 + g * 8], rsm[:, 4 + g:5 + g], el[:], ALU.mult, ALU.add)
                    ttn(rsm[:, 8:12], rsm[:, 4:8], giota[:], ALU.mult)
                    S.op('dve', lambda E: E.tensor_reduce(out=sm[:, 6:7], in_=rsm[:, 8:12], axis=AX.X, op=ALU.add), r=[rsm], w=[sm])
                    yield
                    S.op('dve', lambda E: E.max(out=mx8[:], in_=el[:]), r=[el], w=[mx8])
                    S.op('dve', lambda E: E.max_index(out=ix8[:], in_max=mx8[:], in_values=el[:]), r=[mx8, el], w=[ix8])
                    yield
                    cp(sm[:, 8:10], ix8[:, 0:2])
                    tsc(sm[:, 8:10], sm[:, 8:10], sm[:, 6:7], None, ALU.add)
                    yield
                    ttn(sm[:, 10:11], mx8[:, 1:2], mx8[:, 0:1], ALU.subtract)
                    act(sm[:, 10:11], sm[:, 10:11], AF.Exp)
                    tsc(sm[:, 11:12], sm[:, 10:11], 1.0, None, ALU.add)
                    recip(sm[:, 11:12], sm[:, 11:12])
                    yield
                    ttn(gates_all[:, i, 0:1], sm[:, 5:6], sm[:, 11:12], ALU.mult)
                    ttn(gates_all[:, i, 1:2], gates_all[:, i, 0:1], sm[:, 10:11], ALU.mult)
                    for k in range(2):
                        tsc(oh[k][:], iota_f[:], sm[:, 8 + k:9 + k], None, ALU.is_equal)
                    ttn(cnt[:], oh[0][:], oh[1][:], ALU.add)
                    yield
                    yield

                def g_B(i):
                    b = i % NB1
                    xt = xt1[b]; yT_b = yt1[b]; h1 = h1s[b]; big = big2[b]; xn2_b = xn2_bs[b]; xT2 = xn2T[b]
                    Z = scr[b]; lgt = Z['lgt']; rsm = Z['rsm']; oh = Z['oh']; cnt = Z['cnt']; el = Z['el']; mx8 = Z['mx8']; ix8 = Z['ix8']; sm = Z['sm']
                    pp = psq(); pp32 = sub(pp, A(pp)[:, 0:32])
                    mm(pp32, msu[:], cnt[:], start=True, stop=False)
                    mm(pp32, ones[0:1, :], carry[0:1, :], start=False, stop=True)
                    for k in range(2):
                        ttn(rsm[:], oh[k][:], pp32, ALU.mult)
                        S.op('dve', lambda E, k=k: E.tensor_reduce(out=sm[:, 12 + k:13 + k], in_=rsm[:], axis=AX.X, op=ALU.add), r=[rsm], w=[sm])
                    tsc(sm[:, 12:14], sm[:, 12:14], float(CAP - 1), None, ALU.min)
                    stt(sm[:, 12:14], sm[:, 8:10], float(CAP), sm[:, 12:14], ALU.mult, ALU.add)
                    cp(slots_all[:, i, :], sm[:, 12:14])
                    yield
                    pc = psq(); pc32 = sub(pc, A(pc)[0:1, 0:32])
                    mm(pc32, ones[:, 0:1], cnt[:])
                    ttn(carry[0:1, :], carry[0:1, :], pc32, ALU.add)
                    yield
                    for k in range(2):
                        S.dma('pool', lambda E, k=k, i=i: E.indirect_dma_start(
                            out=xs_d[:, :], out_offset=bass.IndirectOffsetOnAxis(ap=slots_all[:, i, k:k + 1], axis=0),
                            in_=xn2_b[:, :], in_offset=None), r=[xn2_b, slots_all], w=['xs_scr'])
                    yield

                def g_tile(i):
                    loads1b(i)
                    yield
                    yield from g_A(i)
                    yield from g_B(i)
                active = []
                nxt_tile = 1
                rnd = 0
                while active or nxt_tile < NT:
                    if nxt_tile < NT and len(active) < NB1 and rnd % 4 == 0:
                        active.append(g_tile(nxt_tile)); nxt_tile += 1
                    for g in list(active):
                        try:
                            next(g)
                        except StopIteration:
                            active.remove(g)
                    rnd += 1
                S.barrier()
                chk('p1b')

            with ExitStack() as es3:
                t3 = lambda name, shape, dt=F32: T(es3, name, shape, dt)
                NSUB = CAP // 128
                wstg = [t3(f"ewstg{i}", [128, 2, D]) for i in range(6)]
                wgu_b = [t3(f"wgu_b{i}", [128, 8, D], BF16) for i in range(2)]
                wdn_b = [t3(f"wdn_b{i}", [128, 4, D], BF16) for i in range(2)]
                xsl = [t3(f"xsl{i}", [128, NSUB, D], BF16) for i in range(2)]
                xT = [t3(f"xT{i}", [128, 8, CAP], BF16) for i in range(2)]
                hT = t3("hT", [128, 4, CAP], BF16); gsl = t3("gsl", [128, CAP])
                ysl = [t3(f"ysl{i}", [128, D]) for i in range(2)]
                def wpiece(e, k):
                    g_ = e * 6 + k
                    s_ = wstg[g_ % 6]
                    if k < 4:
                        src = wgu_d[0, e].rearrange("(c p) f -> p c f", p=128)[:, 2 * k:2 * k + 2, :]
                        dst = wgu_b[e % 2][:, 2 * k:2 * k + 2, :]
                    else:
                        src = wdn_d[0, e].rearrange("(c p) f -> p c f", p=128)[:, 2 * (k - 4):2 * (k - 4) + 2, :]
                        dst = wdn_b[e % 2][:, 2 * (k - 4):2 * (k - 4) + 2, :]
                    return (lambda: dma(s_[:, :, :], src)), (lambda: cp(dst, s_[:, :, :], e=('dve', 'act')[g_ % 2]))

                def xload(e):
                    dma(xsl[e % 2][:, :, :], xs_d[e * CAP:(e + 1) * CAP, :].rearrange("(m p) f -> p m f", p=128), q='pool')

                for k in range(6):
                    d_, c_ = wpiece(0, k)
                    d_(); c_()
                xload(0)
                for e in range(NEXP):
                    Wg = wgu_b[e % 2]; Wd = wdn_b[e % 2]
                    X = xsl[e % 2]; XT = xT[e % 2]
                    if e + 1 < NEXP:
                        xload(e + 1)
                    steps = []

                    def st_tr(m, X=X, XT=XT):
                        for half in range(2):
                            pb = psb()
                            pbv = A(pb).bitcast(BF16)
                            for j in range(4):
                                c = half * 4 + j
                                S.op('pe', lambda E, c=c, j=j, m=m, pbv=pbv, X=X: E.transpose(out=pbv[:, j * 128:(j + 1) * 128], in_=X[:, m, c * 128:(c + 1) * 128], identity=identb[:]),
                                     r=[X, identb], w=[pb])
                            cp(XT[:, half * 4:half * 4 + 4, m * 128:(m + 1) * 128], sub(pb, pbv[:, 0:512].rearrange("p (j t) -> p j t", t=128)), e='act' if half else 'dve')

                    def st_gu(j, Wg=Wg, XT=XT):
                        pg = psb(); pu = psb()
                        for c in range(8):
                            mm(sub(pg, A(pg)[:, 0:CAP]), Wg[:, c, j * 128:(j + 1) * 128], XT[:, c, :], start=(c == 0), stop=(c == 7))
                        for c in range(8):
                            mm(sub(pu, A(pu)[:, 0:CAP]), Wg[:, c, 512 + j * 128:512 + (j + 1) * 128], XT[:, c, :], start=(c == 0), stop=(c == 7))
                        act(gsl[:, :], sub(pg, A(pg)[:, 0:CAP]), AF.Silu)
                        ttn(hT[:, j, :], gsl[:, :], sub(pu, A(pu)[:, 0:CAP]), ALU.mult)

                    def st_dn(m, Wd=Wd, e=e):
                        Y = ysl[m % 2]
                        for n in range(2):
                            py = psb()
                            for c in range(4):
                                mm(py, hT[:, c, m * 128:(m + 1) * 128], Wd[:, c, n * 512:(n + 1) * 512], start=(c == 0), stop=(c == 3))
                            cp(Y[:, n * 512:(n + 1) * 512], py, e='act' if n else 'dve')
                        dma(ys_d[e * CAP + m * 128:e * CAP + (m + 1) * 128, :], Y[:, :], q='pool')

                    for m in range(NSUB):
                        steps.append(lambda m=m: st_tr(m))
                    for j in range(4):
                        steps.append(lambda j=j: st_gu(j))
                    for m in range(NSUB):
                        steps.append(lambda m=m: st_dn(m))
                    assert len(steps) >= 6
                    casts = []
                    if e + 1 < NEXP:
                        for k in range(6):
                            d_, c_ = wpiece(e + 1, k)
                            d_()
                            casts.append(c_)
                    for si, stp in enumerate(steps):
                        stp()
                        if si < len(casts):
                            casts[si]()
                    for c_ in casts[len(steps):]:
                        c_()
                S.barrier()
                chk('p2')

            with ExitStack() as es4:
                t4 = lambda name, shape, dt=F32: T(es4, name, shape, dt)
                y0 = [t4(f"y0_{i}", [128, D]) for i in range(2)]; y1 = [t4(f"y1_{i}", [128, D]) for i in range(2)]
                hh = [t4(f"hh{i}", [128, D]) for i in range(2)]; jk = t4("jk", [128, D]); s4 = t4("s4", [128, 4])
                ob = [t4(f"ob{i}", [128, D]) for i in range(2)]
                wfin_bc = t4("wfin_bc", [128, D])
                dma(wfin_bc[:], nfin_d.partition_broadcast(128))
                def loads3(i):
                    b = i % 2
                    for k, yk in ((0, y0[b]), (1, y1[b])):
                        S.dma('pool', lambda E, k=k, i=i, yk=yk: E.indirect_dma_start(
                            out=yk[:, :], out_offset=None, in_=ys_d[:, :],
                            in_offset=bass.IndirectOffsetOnAxis(ap=slots_all[:, i, k:k + 1], axis=0)), r=['ys_scr', slots_all], w=[yk])
                    dma(hh[b][:, :], h1_d[i * 128:(i + 1) * 128, :])
                if NT > 1:
                    loads3(1)
                for i in range(1, NT):
                    b = i % 2
                    stt(hh[b][:], y0[b][:], gates_all[:, i, 0:1], hh[b][:], ALU.mult, ALU.add)
                    stt(hh[b][:], y1[b][:], gates_all[:, i, 1:2], hh[b][:], ALU.mult, ALU.add)
                    act(jk[:], hh[b][:], AF.Square, accum=s4[:, 0:1])
                    act(s4[:, 1:2], s4[:, 0:1], AF.Sqrt, bias=1e-6, scale=1.0 / D)
                    recip(s4[:, 1:2], s4[:, 1:2])
                    stt(ob[b][:], hh[b][:], s4[:, 1:2], wfin_bc[:], ALU.mult, ALU.mult)
                    if i + 1 < NT:
                        loads3(i + 1)
                    dma(out_d[(i - 1) * 128:i * 128, :], ob[b][:, :], is_out=True)
                S.finish()
        except Stop:
            S.finish()
        print("instr counts", S.total, "nsem", S.nsem)
    nc._dbg_map = dbg_map
    return nc


_NAMES = ['meta_tokens', 'norm_mix_w', 'norm_ffn_w', 'norm_final_w', 'w_in', 'w_out', 'rwkv_mu_rkv', 'rwkv_mu_wag',
          'rwkv_w0', 'rwkv_w_lora_a', 'rwkv_w_lora_b', 'rwkv_a0', 'rwkv_a_lora_a', 'rwkv_a_lora_b', 'rwkv_g_lora_a',
          'rwkv_g_lora_b', 'rwkv_k_k', 'rwkv_k_a', 'rwkv_r_k', 'rwkv_lnx_w', 'rwkv_lnx_b', 'mlstm_conv_w', 'mlstm_conv_b',
          'mlstm_gate_b', 'mlstm_norm_w', 'router_group_w', 'router_group_b', 'router_expert_w', 'router_expert_b',
          'expert_w_gate_up', 'expert_w_down']


def run(inputs, CAP=512, stop_after=None):
    x = np.asarray(inputs['x'], dtype=np.float32)
    B, L, _ = x.shape
    NT = L // 128 + 1
    nc = build(NT, CAP, stop_after=stop_after)
    shared = {n: np.ascontiguousarray(np.asarray(inputs[n], dtype=np.float32)) for n in _NAMES}
    in_maps = []
    for b in range(B):
        m = dict(shared)
        m['x'] = np.ascontiguousarray(x[b])
        in_maps.append(m)
    res = run_bass_kernel_spmd(nc, in_maps, core_ids=list(range(B)))
    if stop_after:
        d = np.asarray(res.results[0]['dbg'])
        return {k: d[0:v[2], v[0]:v[0] + v[1]] for k, v in nc._dbg_map.items()}
    return np.stack([np.asarray(r['out']).reshape(L, D) for r in res.results], axis=0).astype(np.float32)


def kernel(**inputs):
    return run(inputs, CAP=384)
```

```python
import math
import numpy as np
from contextlib import ExitStack
import concourse.bass as bass
import concourse.mybir as mybir
from concourse.bass_utils import run_bass_kernel_spmd

F32 = mybir.dt.float32
BF16 = mybir.dt.bfloat16
I32 = mybir.dt.int32
U32 = mybir.dt.uint32
AF = mybir.ActivationFunctionType
ALU = mybir.AluOpType
AX = mybir.AxisListType

D = 1024
DBG_TILE = 0
DIN = 3080
NEXP = 32
C0 = math.exp(-0.5)


class V:
    def __init__(self, ap, key):
        self.ap = ap
        self.key = key


def A(x):
    if isinstance(x, V):
        return x.ap
    if type(x).__name__.endswith('TensorHandle'):
        return x.ap()
    return x


def K(x):
    return x.key if isinstance(x, V) else x.name


class Sched:
    EPOCH = 20000

    def __init__(self, nc, es, n_dma_sems=32):
        self.nc = nc
        self.es = es
        self.eng = {'pe': nc.tensor, 'act': nc.scalar, 'dve': nc.vector,
                    'pool': nc.gpsimd, 'sp': nc.sync}
        self.sem = {}
        self.cnt = {}
        self.nsem = 0
        self.total = {k: 0 for k in self.eng}
        for k in self.eng:
            self._new_sem(k)
        self.waited = {k: {} for k in self.eng}
        self.res = {}
        self.dma_sems = [es.enter_context(nc.semaphore(f"dq{i}")) for i in range(n_dma_sems)]
        self.dma_cnt = [0] * n_dma_sems
        self.dma_rr = 0
        self.out_tokens = []

    def _new_sem(self, k):
        self.nsem += 1
        self.sem[k] = self.es.enter_context(self.nc.semaphore(f"s_{k}_{self.nsem}"))
        self.cnt[k] = 0

    def _need(self, reads, writes):
        need = []
        for key in reads:
            st = self.res.get(key)
            if st is not None and st['w'] is not None:
                need.append((st['w'], 'raw'))
        for key in writes:
            st = self.res.get(key)
            if st is not None:
                if st['w'] is not None:
                    need.append((st['w'], 'waw'))
                need.extend((t, 'war') for t in st['r'].values())
        return need

    import os as _os
    SAME_ENGINE_GAP = int(_os.environ.get('SE_GAP', 16))
    SKIP_WAX = int(_os.environ.get('SE_SKIPWAX', 1))
    SKIP_ENG = _os.environ.get('SE_ENG', 'dve,act,pool').split(',')

    def _emit_waits(self, e, need):
        for item in need:
            tok, kind = item if isinstance(item[0], tuple) else (item, 'raw')
            sem, val, src = tok[0], tok[1], tok[2]
            if src == 'pe' and e == 'pe':
                continue
            if src == e and src != 'dma':
                if e in self.SKIP_ENG:
                    if kind != 'raw' and self.SKIP_WAX:
                        continue
                    if kind == 'raw' and self.total[e] - tok[3] >= self.SAME_ENGINE_GAP:
                        continue
            w = self.waited[e]
            if w.get(id(sem), 0) >= val:
                continue
            self.eng[e].wait_ge(sem, val)
            w[id(sem)] = val

    def _record(self, tok, reads, writes):
        for key in reads:
            st = self.res.setdefault(key, {'w': None, 'r': {}})
            st['r'][id(tok[0])] = tok
        for key in writes:
            self.res[key] = {'w': tok, 'r': {}}

    def op(self, e, fn, r=(), w=()):
        r = [K(k) if not isinstance(k, (str, tuple)) else k for k in r]
        w = [K(k) if not isinstance(k, (str, tuple)) else k for k in w]
        w = w + [k for k in r if isinstance(k, str) and k.startswith('pb') and k not in w]
        if self.cnt[e] >= self.EPOCH:
            self._new_sem(e)
        self._emit_waits(e, self._need(r, w))
        inst = fn(self.eng[e])
        self.cnt[e] += 1
        self.total[e] += 1
        inst.then_inc(self.sem[e], 1)
        tok = (self.sem[e], self.cnt[e], e, self.total[e])
        self._record(tok, r, w)
        return tok

    def dma(self, q, fn, r=(), w=(), is_out=False):
        r = [K(k) if not isinstance(k, (str, tuple)) else k for k in r]
        w = [K(k) if not isinstance(k, (str, tuple)) else k for k in w]
        i = self.dma_rr
        self.dma_rr = (self.dma_rr + 1) % len(self.dma_sems)
        sem = self.dma_sems[i]
        need = self._need(r, w)
        if self.dma_cnt[i] > 0:
            need.append(((sem, 16 * self.dma_cnt[i], 'dma', 0), 'raw'))
        self._emit_waits(q, need)
        inst = fn(self.eng[q])
        self.dma_cnt[i] += 1
        inst.then_inc(sem, 16)
        tok = (sem, 16 * self.dma_cnt[i], 'dma', 0)
        self._record(tok, r, w)
        if is_out:
            self.out_tokens.append(tok)
        return tok

    def barrier(self):
        toks = [(self.sem[k], self.cnt[k], k, -10**9) for k in self.eng if self.cnt[k] > 0]
        toks += [(s, 16 * c, 'dma', 0) for s, c in zip(self.dma_sems, self.dma_cnt) if c > 0]
        for e in self.eng:
            self._emit_waits(e, [t for t in toks if t[2] != e])

    def finish(self):
        self._emit_waits('sp', self.out_tokens)
        self.barrier()


class Stop(Exception):
    pass


def inter(gens, weights=None):
    gens = list(gens)
    wts = {id(g): (weights[k] if weights else 1) for k, g in enumerate(gens)}
    while gens:
        for g in list(gens):
            for _ in range(wts[id(g)]):
                try:
                    next(g)
                except StopIteration:
                    gens.remove(g)
                    break
        yield


def build(NT, CAP, stop_after=None, dbgn=8192):
    nc = bass.Bass("TRN2", target_bir_lowering=False)
    NX = NT - 1
    dt_in = lambda name, shape: nc.dram_tensor(name, shape, F32, kind="ExternalInput").ap()
    x_d = dt_in("x", [NX * 128, D])
    meta_d = dt_in("meta_tokens", [16, D])
    nmix_d = dt_in("norm_mix_w", [1, D]); nffn_d = dt_in("norm_ffn_w", [1, D]); nfin_d = dt_in("norm_final_w", [D])
    win_d = dt_in("w_in", [1, D, DIN]); wout_d = dt_in("w_out", [1, D, D])
    murkv_d = dt_in("rwkv_mu_rkv", [1, 3, 512]); muwag_d = dt_in("rwkv_mu_wag", [1, 3, D])
    w0_d = dt_in("rwkv_w0", [1, 512]); wla_d = dt_in("rwkv_w_lora_a", [1, D, 64]); wlb_d = dt_in("rwkv_w_lora_b", [1, 64, 512])
    a0_d = dt_in("rwkv_a0", [1, 512]); ala_d = dt_in("rwkv_a_lora_a", [1, D, 64]); alb_d = dt_in("rwkv_a_lora_b", [1, 64, 512])
    gla_d = dt_in("rwkv_g_lora_a", [1, D, 160]); glb_d = dt_in("rwkv_g_lora_b", [1, 160, 512])
    kk_d = dt_in("rwkv_k_k", [1, 512]); ka_d = dt_in("rwkv_k_a", [1, 512]); rk_d = dt_in("rwkv_r_k", [1, 512])
    lnw_d = dt_in("rwkv_lnx_w", [1, 512]); lnb_d = dt_in("rwkv_lnx_b", [1, 512])
    cw_d = dt_in("mlstm_conv_w", [1, 4, 512]); cb_d = dt_in("mlstm_conv_b", [1, 512])
    gb_d = dt_in("mlstm_gate_b", [1, 8]); mnw_d = dt_in("mlstm_norm_w", [1, 512])
    rgw_d = dt_in("router_group_w", [1, D, 4]); rgb_d = dt_in("router_group_b", [1, 4])
    rew_d = dt_in("router_expert_w", [1, D, 32]); reb_d = dt_in("router_expert_b", [1, 32])
    wgu_d = dt_in("expert_w_gate_up", [1, NEXP, D, D]); wdn_d = dt_in("expert_w_down", [1, NEXP, 512, D])
    out_d = nc.dram_tensor("out", [NX * 128, D], F32, kind="ExternalOutput").ap()
    h1_d = nc.dram_tensor("h1_scr", [NT * 128, D], F32, kind="Internal").ap()
    xs_d = nc.dram_tensor("xs_scr", [NEXP * CAP, D], BF16, kind="Internal").ap()
    ys_d = nc.dram_tensor("ys_scr", [NEXP * CAP, D], F32, kind="Internal").ap()
    yT_d = nc.dram_tensor("yT_scr", [NT * 128, D], BF16, kind="Internal").ap()
    dbg_d = nc.dram_tensor("dbg", [128, dbgn], F32, kind="ExternalOutput").ap() if stop_after else None
    dbg_pos = [0]
    dbg_map = {}

    with ExitStack() as es0:
        S = Sched(nc, es0)

        def dump(name, ap, np_=128):
            if dbg_d is None:
                return
            n = ap.shape[-1] if len(ap.shape) == 2 else int(np.prod(ap.shape[1:]))
            dbg_map[name] = (dbg_pos[0], n, np_)
            S.dma('sp', lambda E: E.dma_start(out=dbg_d[0:np_, dbg_pos[0]:dbg_pos[0] + n], in_=ap, allow_slow_non_contiguous=True), r=[ap], w=['dbg'], is_out=True)
            dbg_pos[0] += n

        def chk(name):
            if stop_after == name:
                raise Stop()
        try:

            def mm(out, lhsT, rhs, start=True, stop=True):
                S.op('pe', lambda E: E.matmul(A(out), lhsT=A(lhsT), rhs=A(rhs), start=start, stop=stop),
                     r=[lhsT, rhs], w=[out])

            def act(out, in_, func, bias=None, scale=None, accum=None, e='act'):
                kw = {}
                rd = [in_]
                if bias is not None:
                    kw['bias'] = A(bias) if not isinstance(bias, float) else bias
                    if not isinstance(bias, float):
                        rd.append(bias)
                if scale is not None:
                    kw['scale'] = A(scale) if not isinstance(scale, float) else scale
                    if not isinstance(scale, float):
                        rd.append(scale)
                wr = [out]
                if accum is not None:
                    kw['accum_out'] = A(accum)
                    wr.append(accum)
                S.op('act', lambda E: E.activation(out=A(out), in_=A(in_), func=func, **kw), r=rd, w=wr)

            def tsc(out, in0, s1, s2, op0, op1=None, e='dve'):
                rd = [in0]
                a1 = s1
                a2 = s2
                if not isinstance(s1, (float, int)):
                    rd.append(s1); a1 = A(s1)
                if s2 is not None and not isinstance(s2, (float, int)):
                    rd.append(s2); a2 = A(s2)
                kw = {} if op1 is None else {'op1': op1}
                S.op(e, lambda E: E.tensor_scalar(out=A(out), in0=A(in0), scalar1=a1, scalar2=a2, op0=op0, **kw),
                     r=rd, w=[out])

            def ttn(out, a, b, op, e='dve'):
                S.op(e, lambda E: E.tensor_tensor(out=A(out), in0=A(a), in1=A(b), op=op), r=[a, b], w=[out])

            def stt(out, in0, sc, in1, op0, op1):
                rd = [in0, in1]
                a = sc
                if not isinstance(sc, (float, int)):
                    rd.append(sc); a = A(sc)
                S.op('dve', lambda E: E.scalar_tensor_tensor(out=A(out), in0=A(in0), scalar=a, in1=A(in1), op0=op0, op1=op1),
                     r=rd, w=[out])

            def cp(out, in_, e='dve'):
                if e == 'act':
                    S.op('act', lambda E: E.activation(out=A(out), in_=A(in_), func=AF.Copy), r=[in_], w=[out])
                else:
                    S.op(e, lambda E: E.tensor_copy(out=A(out), in_=A(in_)), r=[in_], w=[out])

            def mset(t, val, e='pool'):
                S.op(e, lambda E: E.memset(A(t), val), w=[t])

            def recip(out, in_):
                S.op('dve', lambda E: E.reciprocal(out=A(out), in_=A(in_)), r=[in_], w=[out])

            def dma(out, in_, q='sp', is_out=False):
                S.dma(q, lambda E: E.dma_start(out=A(out), in_=A(in_)), r=[in_], w=[out], is_out=is_out)

            banks = [es0.enter_context(nc.psum_tensor(f"pb{i}", [128, 512], F32)) for i in range(8)]
            st = {'q': 0, 'b': 0}

            def psq():
                i = st['q']; st['q'] = (i + 1) % 16
                b_, q_ = i % 4, (i // 4) % 4
                return V(banks[b_][:, q_ * 128:(q_ + 1) * 128], f"pb{b_}")

            def psb():
                i = st['b']; st['b'] = (i + 1) % 4
                return V(banks[4 + i][:, :], f"pbB{i}")

            def sub(v, ap):
                return V(ap, v.key) if isinstance(v, V) else ap

            T = lambda stack, name, shape, dt=F32: stack.enter_context(nc.sbuf_tensor(name, shape, dt))

            ones = T(es0, "ones", [128, 128]); ident = T(es0, "ident", [128, 128]); identb = T(es0, "identb", [128, 128], BF16)
            msu = T(es0, "msu", [128, 128]); msl = T(es0, "msl", [128, 128]); mst = T(es0, "mst", [128, 64])
            blk64 = T(es0, "blk64", [128, 128]); tric = T(es0, "tric", [128, 128]); maskc = T(es0, "maskc", [128, 128])
            mset(ones, 1.0)
            asel = lambda out, pat, cmp, base, cm, in_=None: S.op('pool', lambda E: E.affine_select(
                out=A(out), in_=A(in_ if in_ is not None else ones[:]), pattern=pat, compare_op=cmp, fill=0.0, base=base, channel_multiplier=cm),
                r=[in_ if in_ is not None else ones], w=[out])
            asel(ident[:], [[-1, 128]], ALU.is_equal, 0, 1)
            cp(identb[:], ident[:], e='pool')
            asel(msu[:], [[1, 128]], ALU.is_gt, 0, -1)
            asel(msl[:], [[-1, 128]], ALU.is_gt, 0, 1)
            asel(mst[0:64, :], [[1, 64]], ALU.is_ge, 0, -1, in_=ones[0:64, 0:64])
            asel(mst[64:128, :], [[1, 64]], ALU.is_ge, 0, -1, in_=ones[64:128, 0:64])
            mset(blk64, 0.0); mset(blk64[0:64, 0:64], 1.0); mset(blk64[64:128, 64:128], 1.0)
            asel(tric[:], [[1, 128]], ALU.is_ge, 0, -1)
            ttn(tric[:], tric[:], blk64[:], ALU.mult, e='pool')
            tsc(maskc[:], tric[:], 0.125, None, ALU.mult, e='pool')

            pstg = T(es0, "pstg", [128, 128]); pv = T(es0, "pv", [128, 128]); pv1 = T(es0, "pv1", [128, 128])
            mset(pstg, 0.0)
            row = {}
            rcur = [0]

            def ldrows(name, ap2d, n):
                row[name] = rcur[0]
                dma(pstg[rcur[0]:rcur[0] + n, :], ap2d)
                rcur[0] += n
            ldrows('nmix', nmix_d[0].rearrange("(c p) -> c p", p=128), 8)
            ldrows('muwag', muwag_d[0].rearrange("j (c p) -> (j c) p", p=128), 24)
            ldrows('murkv', murkv_d[0].rearrange("j (c p) -> (j c) p", p=128), 12)
            for nm, ap in (('w0', w0_d), ('a0', a0_d), ('kk', kk_d), ('ka', ka_d), ('rk', rk_d), ('lnw', lnw_d), ('lnb', lnb_d), ('cb', cb_d)):
                ldrows(nm, ap[0].rearrange("(c p) -> c p", p=128), 4)
            ldrows('cw', cw_d[0].rearrange("j (c p) -> (j c) p", p=128), 16)
            tp = psq()
            S.op('pe', lambda E: E.transpose(out=A(tp), in_=pstg[:], identity=ident[:]), r=[pstg, ident], w=[tp])
            cp(pv[:], tp, e='act')
            tsc(pv1[:], pv[:], -1.0, 1.0, ALU.mult, ALU.add)
            PV = lambda nm, j=0: pv[:, row[nm] + j:row[nm] + j + 1]
            PV1 = lambda nm, j=0: pv1[:, row[nm] + j:row[nm] + j + 1]

            mnw_bc = T(es0, "mnw_bc", [128, 512])
            gb_bc = T(es0, "gb_bc", [128, 8]); rb_bc = T(es0, "rb_bc", [128, 36]); iota_i = T(es0, "iota_i", [128, 32], I32)
            iota_f = T(es0, "iota_f", [128, 32]); giota = T(es0, "giota", [128, 4])

            dma(mnw_bc[:], mnw_d[0].partition_broadcast(128)); dma(gb_bc[:], gb_d[0].partition_broadcast(128))
            dma(rb_bc[:, 0:4], rgb_d[0].partition_broadcast(128)); dma(rb_bc[:, 4:36], reb_d[0].partition_broadcast(128))
            S.op('pool', lambda E: E.iota(iota_i[:], pattern=[[1, 32]], base=0, channel_multiplier=0), w=[iota_i])
            cp(iota_f[:], iota_i[:], e='pool')
            tsc(giota[:], iota_f[:, 0:4], 8.0, None, ALU.mult, e='pool')

            gates_all = T(es0, "gates_all", [128, NT, 2]); slots_all = T(es0, "slots_all", [128, NT, 2], I32)
            es1 = es0.enter_context(ExitStack())
            win_b = T(es1, "win_b", [128, 8, DIN], BF16)
            wla_b = T(es1, "wla_b", [128, 8, 288], BF16); wlamu_b = T(es1, "wlamu_b", [128, 8, 288], BF16)
            lb_b = T(es1, "lb_b", [128, 512], BF16); glb0_b = T(es1, "glb0_b", [128, 512], BF16); glb1_b = T(es1, "glb1_b", [32, 512], BF16)
            with ExitStack() as esl:
                stg = [T(esl, f"wstg{i}", [128, DIN]) for i in range(2)]
                win_v = win_d[0].rearrange("(c p) f -> p c f", p=128)
                ceng = ['dve', 'pool', 'act']
                k = 0
                for c in range(8):
                    s_ = stg[k % 2]
                    dma(s_[:, :], win_v[:, c, :])
                    cp(win_b[:, c, :], s_[:, :], e=ceng[k % 3]); k += 1
                s_ = stg[k % 2]; k += 1
                sv = s_[:, 0:8 * 288].rearrange("p (c j) -> p c j", j=288)
                dma(sv[:, :, 0:64], wla_d[0].rearrange("(c p) j -> p c j", p=128))
                dma(sv[:, :, 64:128], ala_d[0].rearrange("(c p) j -> p c j", p=128))
                dma(sv[:, :, 128:288], gla_d[0].rearrange("(c p) j -> p c j", p=128))
                cp(wla_b[:, :, :], sv, e='dve')
                for c in range(8):
                    for j, (lo, hi) in enumerate(((0, 64), (64, 128), (128, 288))):
                        tsc(wlamu_b[:, c, lo:hi], sv[:, c, lo:hi], PV('muwag', j * 8 + c), None, ALU.mult, e='dve' if c % 2 else 'pool')
                s_ = stg[k % 2]; k += 1
                dma(s_[0:64, 0:512], wlb_d[0]); dma(s_[64:128, 0:512], alb_d[0])
                dma(s_[:, 512:1024], glb_d[0][0:128, :]); dma(s_[0:32, 1024:1536], glb_d[0][128:160, :])
                cp(lb_b[:, :], s_[:, 0:512]); cp(glb0_b[:, :], s_[:, 512:1024]); cp(glb1_b[:, :], s_[0:32, 1024:1536])
                S.barrier()
            dump('pv', pv[:, :])
            chk('setup')

            es2 = es1.enter_context(ExitStack())
            t2 = lambda name, shape, dt=F32: T(es2, name, shape, dt)
            x_tm = [t2("x_tm0", [128, D])] * 2
            big = t2("big", [128, D])
            xnT_f = t2("xnT_f", [128, 8, 129]); xnT_b = t2("xnT_b", [128, 8, 128], BF16); xxT_b = t2("xxT_b", [128, 8, 128], BF16)
            rkv_raw = t2("rkv_raw", [128, 12, 129]); qk_raw = t2("qk_raw", [128, 4, 131])
            V1s = [t2(f"V1_{i}", [128, 4, 129]) for i in range(2)]; sigos = [t2(f"sigo{i}", [128, 512]) for i in range(2)]
            laT = t2("laT", [128, 128], BF16); lg0 = t2("lg0", [128, 128], BF16); lg1 = t2("lg1", [32, 128], BF16)
            sgw = t2("sgw", [128, 4, 128]); asig = t2("asig", [128, 4, 128]); ggs = [t2(f"gg{i}", [128, 4, 128]) for i in range(2)]
            bonuss = [t2(f"bonus{i}", [128, 4, 128]) for i in range(2)]; y_as = [t2(f"y_a{i}", [128, 4, 128]) for i in range(2)]
            pt4 = t2("pt4", [128, 4, 128])
            yT_bs = [t2(f"yT_b{i}", [128, 8, 128], BF16) for i in range(2)]
            ssq = t2("ssq", [128, 4]); rstd = t2("rstd", [128, 4])
            rb = {nm: t2(f"rb_{nm}", [128, 4, 128]) for nm in ('r', 'k', 'v', 'kkn', 'k2', 'bv', 'tA', 'tB', 'cs', 'E1', 'E2')}
            STg = t2("STg", [128, 4, 128])
            opzT = t2("opzT", [128, 4, 2, 4, 128], BF16)
            rstT = t2("rstT", [128, 4, 128], BF16)
            gC = t2("gC", [128, 4, 2])
            ArbArk = t2("alg_ArbArk", [128, 2, 4, 64], BF16)
            Gc = [{nm: t2(f"alg{c}_{nm}", [128, 4, 128], BF16) for nm in
                   ('BzT', 'KzT', 'VzT', 'Aak', 'Q0', 'Q1', 'QT0', 'QT1', 'P0', 'P1', 'PT0', 'PT1')} for c in range(2)]
            GS = {nm: t2(f"alg_{nm}", [128, 4, 128], BF16) for nm in ('W0T', 'UT')}
            ArbArks = [ArbArk, t2("alg_ArbArk1", [128, 2, 4, 64], BF16)]
            ST32 = t2("ST32", [128, 4, 128])
            STb = t2("STb", [128, 4, 128], BF16)
            QKf = t2("QKf", [128, 4, 128]); cacc = QKf
            g8s = [t2(f"g8_{i}", [128, 8]) for i in range(2)]; th8 = t2("th8", [128, 8]); nbg = t2("nbg", [128, 8]); wgt = t2("wgt", [128, 4]); dbias = t2("dbias", [128, 4])
            lfb = [t2(f"lfb{i}", [128, 128]) for i in range(2)]; Dm = [t2(f"Dm{i}", [128, 128]) for i in range(2)]
            eB = [t2(f"eB{i}", [128, 128]) for i in range(2)]; Pm = [t2(f"Pm{i}", [128, 128]) for i in range(2)]
            Qz = [t2(f"Qz{h}", [128, 2, 128]) for h in range(4)]
            Kw = t2("Kw", [128, 4, 64])
            CTa = [t2(f"CTa{h}", [128, 129]) for h in range(4)]; CTb = [t2(f"CTb{h}", [128, 129]) for h in range(4)]
            hraw = [t2(f"hraw{i}", [128, 128]) for i in range(2)]; y_b = t2("y_b", [128, 512])
            sm = t2("sm", [128, 16])

            mset(xnT_f[:, :, 0:1], 0.0); mset(rkv_raw[:, :, 0:1], 0.0); mset(qk_raw[:, :, 0:3], 0.0)
            mset(V1s[0][:, :, 128:129], 1.0); mset(V1s[1][:, :, 128:129], 1.0)
            mset(ST32, 0.0); mset(STb, 0.0)
            mset(opzT, 0.0)
            for h in range(4):
                mset(Qz[h], 0.0); mset(CTa[h], 0.0); mset(CTb[h], 0.0)

            def g_front(i):
                xt = x_tm[i % 2]; V1 = V1s[i % 2]; sigo = sigos[i % 2]; g8 = g8s[i % 2]; gg = ggs[i % 2]
                if i == 0:
                    mset(xt, 0.0)
                    dma(xt[112:128, :], meta_d)
                else:
                    dma(xt[:, :], x_d[(i - 1) * 128:i * 128, :])
                act(big[:], xt[:], AF.Square, accum=ssq[:, 0:1])
                act(rstd[:, 0:1], ssq[:, 0:1], AF.Ln, bias=1e-6, scale=1.0 / D)
                act(rstd[:, 0:1], rstd[:, 0:1], AF.Exp, scale=-0.5)
                tsc(big[:], xt[:], rstd[:, 0:1], None, ALU.mult)
                yield
                for half in range(2):
                    pb = psb()
                    for j in range(4):
                        c = half * 4 + j
                        S.op('pe', lambda E, c=c, j=j, pb=pb: E.transpose(out=A(pb)[:, j * 128:(j + 1) * 128], in_=big[:, c * 128:(c + 1) * 128], identity=ident[:]),
                             r=[big, ident], w=[pb])
                    for j in range(4):
                        c = half * 4 + j
                        if j % 2 == 0:
                            act(xnT_f[:, c, 1:129], sub(pb, A(pb)[:, j * 128:(j + 1) * 128]), AF.Copy, scale=PV('nmix', c))
                        else:
                            tsc(xnT_f[:, c, 1:129], sub(pb, A(pb)[:, j * 128:(j + 1) * 128]), PV('nmix', c), None, ALU.mult)
                    yield
                cp(xnT_b[:, :, :], xnT_f[:, :, 1:129], e='pool')
                ttn(xxT_b[:, :, :], xnT_f[:, :, 0:128], xnT_f[:, :, 1:129], ALU.subtract)
                cp(xnT_f[:, :, 0:1], xnT_f[:, :, 128:129], e='pool')
                yield

                for blk in range(16):
                    p_ = psq()
                    for c in range(8):
                        mm(p_, win_b[:, c, blk * 128:(blk + 1) * 128], xnT_b[:, c, :], start=(c == 0), stop=(c == 7))
                    if blk < 12:
                        cp(rkv_raw[:, blk, 1:129], p_, e='act')
                    else:
                        cp(qk_raw[:, blk - 12, 3:131], p_, e='act')
                    yield
                pv_ = psb()
                for c in range(8):
                    mm(pv_, xnT_b[:, c, :], win_b[:, c, 2048:2560], start=(c == 0), stop=(c == 7))
                cp(V1[:, :, 0:128], sub(pv_, A(pv_).rearrange("p (h v) -> p h v", v=128)), e='act')
                yield
                po_ = psb()
                for c in range(8):
                    mm(po_, xnT_b[:, c, :], win_b[:, c, 2560:3072], start=(c == 0), stop=(c == 7))
                act(sigo[:], po_, AF.Sigmoid)
                yield
                pg_ = psq()
                pg8 = sub(pg_, A(pg_)[:, 0:8])
                for c in range(8):
                    mm(pg8, xnT_b[:, c, :], win_b[:, c, 3072:3080], start=(c == 0), stop=(c == 7))
                ttn(g8[:], pg8, gb_bc[:], ALU.add)
                yield
                la_ps = []
                for (lo, hi) in ((0, 128), (128, 256), (256, 288)):
                    p_ = psq()
                    po = sub(p_, A(p_)[0:hi - lo, :])
                    for c in range(8):
                        mm(po, wla_b[:, c, lo:hi], xnT_b[:, c, :], start=(c == 0), stop=False)
                    for c in range(8):
                        mm(po, wlamu_b[:, c, lo:hi], xxT_b[:, c, :], start=False, stop=(c == 7))
                    la_ps.append(p_)
                act(laT[0:64, :], sub(la_ps[0], A(la_ps[0])[0:64, :]), AF.Tanh)
                cp(laT[64:128, :], sub(la_ps[0], A(la_ps[0])[64:128, :]), e='dve')
                act(lg0[:, :], la_ps[1], AF.Sigmoid)
                act(lg1[:, :], sub(la_ps[2], A(la_ps[2])[0:32, :]), AF.Sigmoid)
                yield
                for fb in range(4):
                    fs = slice(fb * 128, (fb + 1) * 128)
                    p_ = psq(); mm(p_, lb_b[0:64, fs], laT[0:64, :])
                    act(sgw[:, fb, :], p_, AF.Sigmoid, bias=PV('w0', fb))
                    p_ = psq(); mm(p_, lb_b[64:128, fs], laT[64:128, :])
                    act(asig[:, fb, :], p_, AF.Sigmoid, bias=PV('a0', fb))
                    p_ = psq(); mm(p_, glb0_b[:, fs], lg0[:, :], start=True, stop=False); mm(p_, glb1_b[0:32, fs], lg1[0:32, :], start=False, stop=True)
                    cp(gg[:, fb, :], p_, e='dve')
                    yield

                yield

            pending = []
            for i in range(NT):
                xt = x_tm[i % 2]; V1 = V1s[i % 2]; sigo = sigos[i % 2]; g8 = g8s[i % 2]; gg = ggs[i % 2]
                yT_b = yT_bs[i % 2]; bonus = bonuss[i % 2]; y_a = y_as[i % 2]
                if i == 0:
                    for _ in g_front(0):
                        pass
                def g_prep_all():
                    R = rb
                    P4 = lambda nm, j=0: pv[:, row[nm] + j:row[nm] + j + 4].unsqueeze(2).broadcast_to([128, 4, 128])
                    P41 = lambda nm, j=0: pv1[:, row[nm] + j:row[nm] + j + 4].unsqueeze(2).broadcast_to([128, 4, 128])
                    for nm, bi, tmp in (('k', 1, 'tA'), ('r', 0, 'tB'), ('v', 2, 'E2')):
                        ttn(R[tmp][:, :, :], rkv_raw[:, bi * 4:bi * 4 + 4, 0:128], P4('murkv', bi * 4), ALU.mult, e='pool')
                        ttn(R[nm][:, :, :], rkv_raw[:, bi * 4:bi * 4 + 4, 1:129], P41('murkv', bi * 4), ALU.mult)
                        ttn(R[nm][:, :, :], R[nm][:, :, :], R[tmp][:, :, :], ALU.add)
                    yield
                    ttn(R['kkn'][:, :, :], R['k'][:, :, :], P4('kk'), ALU.mult)
                    ttn(R['tA'][:, :, :], R['kkn'][:, :, :], R['kkn'][:, :, :], ALU.mult, e='pool')
                    bk = psb()
                    for fb in range(4):
                        mm(sub(bk, A(bk)[:, fb * 128:(fb + 1) * 128]), blk64[:], R['tA'][:, fb, :])
                    act(R['tB'][:, :, :], sub(bk, A(bk).rearrange("p (f t) -> p f t", t=128)), AF.Ln, bias=1e-16)
                    for fb in range(4):
                        for c in range(2):
                            cs_ = slice(c * 64, (c + 1) * 64)
                            S.op('dve', lambda E, fb=fb, cs_=cs_: E.tensor_tensor_scan(out=R['cs'][:, fb, cs_], data0=ones[:, 0:64], data1=sgw[:, fb, cs_], initial=0.0, op0=ALU.mult, op1=ALU.add),
                                 r=[ones, sgw], w=[R['cs']])
                    yield
                    act(R['tB'][:, :, :], R['tB'][:, :, :], AF.Exp, scale=-0.5)
                    ttn(R['kkn'][:, :, :], R['kkn'][:, :, :], R['tB'][:, :, :], ALU.mult)
                    act(R['E1'][:, :, :], R['cs'][:, :, :], AF.Exp, scale=-C0)
                    act(R['E2'][:, :, :], R['cs'][:, :, :], AF.Exp, scale=C0)
                    ttn(R['tA'][:, :, :], asig[:, :, :], P4('ka'), ALU.mult, e='pool')
                    ttn(R['tA'][:, :, :], R['tA'][:, :, :], P41('ka'), ALU.add, e='pool')
                    yield
                    ttn(R['k2'][:, :, :], R['k'][:, :, :], R['tA'][:, :, :], ALU.mult)
                    ttn(R['bv'][:, :, :], R['kkn'][:, :, :], asig[:, :, :], ALU.mult, e='pool')
                    ttn(R['tB'][:, :, :], R['cs'][:, :, :], sgw[:, :, :], ALU.subtract, e='pool')
                    act(R['tB'][:, :, :], R['tB'][:, :, :], AF.Exp, scale=-C0)
                    ttn(R['tA'][:, :, :], R['r'][:, :, :], P4('rk'), ALU.mult, e='pool')
                    ttn(R['tA'][:, :, :], R['tA'][:, :, :], R['k2'][:, :, :], ALU.mult)
                    bk = psb()
                    for fb in range(4):
                        mm(sub(bk, A(bk)[:, fb * 128:(fb + 1) * 128]), blk64[:], R['tA'][:, fb, :])
                    ttn(bonus[:, :, :], sub(bk, A(bk).rearrange("p (f t) -> p f t", t=128)), R['v'][:, :, :], ALU.mult)
                    yield
                    E3 = R['tB']
                    for c in range(2):
                        for hh in range(2):
                            ps_ = slice(hh * 64, (hh + 1) * 64)
                            ts_ = slice(c * 64, (c + 1) * 64)
                            os_ = slice(hh * 64, (hh + 1) * 64)
                            stt(opzT[ps_, :, c, 0, os_], R['kkn'][ps_, :, ts_], -1.0, E3[ps_, :, ts_], ALU.mult, ALU.mult)
                            ttn(opzT[ps_, :, c, 1, os_], R['bv'][ps_, :, ts_], R['E2'][ps_, :, ts_], ALU.mult)
                            ttn(opzT[ps_, :, c, 2, os_], R['k2'][ps_, :, ts_], R['E2'][ps_, :, ts_], ALU.mult, e='pool')
                            cp(opzT[ps_, :, c, 3, os_], R['v'][ps_, :, ts_], e='pool')
                        yield
                    ttn(rstT[:, :, :], R['r'][:, :, :], R['E1'][:, :, :], ALU.mult)
                    cp(gC[:, :, 0:1], R['E1'][:, :, 63:64], e='pool')
                    cp(gC[:, :, 1:2], R['E1'][:, :, 127:128], e='pool')
                    yield

                def q4(bank):
                    return sub(bank, A(bank).rearrange("p (f t) -> p f t", t=128))
                bc4 = lambda m: m[:, :].unsqueeze(1).broadcast_to([128, 4, 128])
                TinvOf = {}
                def g_alg(c):
                    Az = [opzT[:, fb, c, 0, :] for fb in range(4)]; Bz = [opzT[:, fb, c, 1, :] for fb in range(4)]
                    Kz = [opzT[:, fb, c, 2, :] for fb in range(4)]; Vz = [opzT[:, fb, c, 3, :] for fb in range(4)]
                    Rs = [rstT[:, fb, c * 64:(c + 1) * 64] for fb in range(4)]
                    for kk_i, (nm, src) in enumerate((('BzT', Bz), ('KzT', Kz), ('VzT', Vz))):
                        bk = psb()
                        for fb in range(4):
                            mm(sub(bk, A(bk)[:, fb * 128:(fb + 1) * 128]), src[fb], identb[:])
                        cp(Gc[c][nm][:, :, :], q4(bk), e='act' if kk_i != 1 else 'dve')
                        yield
                    for nm, l_, r_, msk in (('Q0', Bz, Az, msu), ('QT0', Az, Bz, msl), ('Aak', Kz, Az, msu)):
                        bk = psb()
                        for fb in range(4):
                            mm(sub(bk, A(bk)[:, fb * 128:(fb + 1) * 128]), l_[fb], r_[fb])
                        ttn(Gc[c][nm][:, :, :], q4(bk), bc4(msk), ALU.mult)
                        yield
                    bk = psb()
                    for j, l_ in enumerate((Bz, Kz)):
                        for fb in range(4):
                            o0 = j * 256 + fb * 64
                            mm(sub(bk, A(bk)[:, o0:o0 + 64]), l_[fb], Rs[fb])
                    ttn(ArbArks[c][:, :, :, :].rearrange("p j f t -> p (j f) t"), sub(bk, A(bk).rearrange("p (g t) -> p g t", t=64)),
                        mst[:, :].unsqueeze(1).broadcast_to([128, 8, 64]), ALU.mult)
                    yield
                    ttn(Gc[c]['P0'][:, :, :], Gc[c]['Q0'][:, :, :], identb[:, :].unsqueeze(1).broadcast_to([128, 4, 128]), ALU.add, e='pool')
                    ttn(Gc[c]['PT0'][:, :, :], Gc[c]['QT0'][:, :, :], identb[:, :].unsqueeze(1).broadcast_to([128, 4, 128]), ALU.add, e='pool')
                    yield
                    Qb = [Gc[c]['Q0'], Gc[c]['Q1']]; QTb = [Gc[c]['QT0'], Gc[c]['QT1']]
                    Pb = [Gc[c]['P0'], Gc[c]['P1']]; PTb = [Gc[c]['PT0'], Gc[c]['PT1']]
                    for s_ in range(6):
                        Qs, QTs = Qb[s_ % 2], QTb[s_ % 2]
                        Qn, QTn = Qb[(s_ + 1) % 2], QTb[(s_ + 1) % 2]
                        Pp, PTp = Pb[(s_ - 1) % 2], PTb[(s_ - 1) % 2]
                        Pc, PTc = Pb[s_ % 2], PTb[s_ % 2]
                        todo = []
                        if s_ <= 4:
                            bk = psb()
                            for fb in range(4):
                                mm(sub(bk, A(bk)[:, fb * 128:(fb + 1) * 128]), QTs[:, fb, :], Qs[:, fb, :])
                            todo.append(lambda bk=bk: cp(Qn[:, :, :], q4(bk), e='act'))
                        if s_ <= 3:
                            bk = psb()
                            for fb in range(4):
                                mm(sub(bk, A(bk)[:, fb * 128:(fb + 1) * 128]), Qs[:, fb, :], QTs[:, fb, :])
                            todo.append(lambda bk=bk: cp(QTn[:, :, :], q4(bk), e='act'))
                        if s_ >= 1:
                            bk = psb()
                            for fb in range(4):
                                mm(sub(bk, A(bk)[:, fb * 128:(fb + 1) * 128]), PTp[:, fb, :], Qs[:, fb, :])
                            todo.append(lambda bk=bk: ttn(Pc[:, :, :], q4(bk), Pp[:, :, :], ALU.add))
                        if 1 <= s_ <= 4:
                            bk = psb()
                            for fb in range(4):
                                mm(sub(bk, A(bk)[:, fb * 128:(fb + 1) * 128]), Qs[:, fb, :], PTp[:, fb, :])
                            todo.append(lambda bk=bk: ttn(PTc[:, :, :], q4(bk), PTp[:, :, :], ALU.add))
                        for f_ in todo:
                            f_()
                        yield
                    cur = 1
                    TinvOf[c] = Gc[c][f'P{cur}']
                    yield
                def g_chain(c):
                    Az = [opzT[:, fb, c, 0, :] for fb in range(4)]; Bz = [opzT[:, fb, c, 1, :] for fb in range(4)]
                    Kz = [opzT[:, fb, c, 2, :] for fb in range(4)]; Vz = [opzT[:, fb, c, 3, :] for fb in range(4)]
                    Rs = [rstT[:, fb, c * 64:(c + 1) * 64] for fb in range(4)]
                    ttn(STg[:, :, :], ST32[:, :, :], gC[:, :, c:c + 1].broadcast_to([128, 4, 128]), ALU.mult, e='pool')
                    bk = psb()
                    for fb in range(4):
                        o_ = sub(bk, A(bk)[:, fb * 128:(fb + 1) * 128])
                        mm(o_, Az[fb], STb[:, fb, :], start=True, stop=False); mm(o_, Gc[c]['Aak'][:, fb, :], Gc[c]['VzT'][:, fb, :], start=False, stop=True)
                    cp(GS['W0T'][:, :, :], q4(bk), e='act')
                    yield
                    bk = psb()
                    for fb in range(4):
                        mm(sub(bk, A(bk)[:, fb * 128:(fb + 1) * 128]), TinvOf[c][:, fb, :], GS['W0T'][:, fb, :])
                    cp(GS['UT'][:, :, :], q4(bk), e='act')
                    yield
                    bkS = psb()
                    for fb in range(4):
                        o_ = sub(bkS, A(bkS)[:, fb * 128:(fb + 1) * 128])
                        mm(o_, Gc[c]['BzT'][:, fb, :], GS['UT'][:, fb, :], start=True, stop=False)
                        mm(o_, Gc[c]['KzT'][:, fb, :], Gc[c]['VzT'][:, fb, :], start=False, stop=True)
                    bkY = psb()
                    for fb in range(4):
                        o_ = sub(bkY, A(bkY)[:, fb * 64:(fb + 1) * 64])
                        mm(o_, STb[:, fb, :], Rs[fb], start=True, stop=False)
                        mm(o_, GS['UT'][:, fb, :], ArbArks[c][:, 0, fb, :], start=False, stop=False)
                        mm(o_, Gc[c]['VzT'][:, fb, :], ArbArks[c][:, 1, fb, :], start=False, stop=True)
                    for fb in range(4):
                        stt(STb[:, fb, :], sub(bkS, A(bkS)[:, fb * 128:(fb + 1) * 128]), gC[:, fb, c:c + 1], STg[:, fb, :], ALU.mult, ALU.add)
                    for fb in range(4):
                        stt(ST32[:, fb, :], sub(bkS, A(bkS)[:, fb * 128:(fb + 1) * 128]), gC[:, fb, c:c + 1], STg[:, fb, :], ALU.mult, ALU.add)
                    cp(y_a[:, :, c * 64:(c + 1) * 64], sub(bkY, A(bkY)[:, 0:256].rearrange("p (f t) -> p f t", t=64)), e='act')
                    yield
                    yield

                def g_post4(y_a=y_a, bonus=bonus, gg=gg, yT_b=yT_b):
                    P4 = lambda nm: pv[:, row[nm]:row[nm] + 4].unsqueeze(2).broadcast_to([128, 4, 128])
                    v4 = lambda bank: sub(bank, A(bank).rearrange("p (f t) -> p f t", t=128))
                    bk = psb()
                    for fb in range(4):
                        mm(sub(bk, A(bk)[:, fb * 128:(fb + 1) * 128]), blk64[:], y_a[:, fb, :])
                    act(pt4[:, :, :], v4(bk), AF.Copy, scale=1.0 / 64)
                    yield
                    ttn(y_a[:, :, :], y_a[:, :, :], pt4[:, :, :], ALU.subtract)
                    ttn(pt4[:, :, :], y_a[:, :, :], y_a[:, :, :], ALU.mult, e='pool')
                    yield
                    bk = psb()
                    for fb in range(4):
                        mm(sub(bk, A(bk)[:, fb * 128:(fb + 1) * 128]), blk64[:], pt4[:, fb, :])
                    act(pt4[:, :, :], v4(bk), AF.Ln, bias=64e-5, scale=1.0 / 64)
                    act(pt4[:, :, :], pt4[:, :, :], AF.Exp, scale=-0.5)
                    yield
                    ttn(y_a[:, :, :], y_a[:, :, :], pt4[:, :, :], ALU.mult)
                    yield
                    ttn(y_a[:, :, :], y_a[:, :, :], P4('lnw'), ALU.mult)
                    ttn(y_a[:, :, :], y_a[:, :, :], P4('lnb'), ALU.add, e='pool')
                    yield
                    ttn(y_a[:, :, :], y_a[:, :, :], bonus[:, :, :], ALU.add)
                    ttn(yT_b[:, 0:4, :], y_a[:, :, :], gg[:, :, :], ALU.mult)
                    yield

                def g_mlstm_pre():
                    cw4 = lambda t_: pv[:, row['cw'] + t_ * 4:row['cw'] + t_ * 4 + 4].unsqueeze(2).broadcast_to([128, 4, 128])
                    cb4 = pv[:, row['cb']:row['cb'] + 4].unsqueeze(2).broadcast_to([128, 4, 128])
                    ttn(cacc[:, :, :], qk_raw[:, :, 0:128], cw4(0), ALU.mult, e='pool')
                    ttn(cacc[:, :, :], cacc[:, :, :], cb4, ALU.add)
                    for t_ in range(1, 4):
                        ttn(STg[:, :, :], qk_raw[:, :, t_:t_ + 128], cw4(t_), ALU.mult, e='pool')
                        ttn(cacc[:, :, :], cacc[:, :, :], STg[:, :, :], ALU.add)
                        yield
                    act(QKf[:, :, :], cacc[:, :, :], AF.Silu)
                    yield
                    cp(qk_raw[:, :, 0:3], qk_raw[:, :, 128:131], e='pool')
                    act(th8[:], g8[:], AF.Tanh, scale=1.0 / 15.0)
                    tsc(g8[:, 0:4], th8[:, 0:4], 15.0, None, ALU.mult)
                    act(g8[:, 4:8], th8[:, 4:8], AF.Exp, scale=-15.0)
                    act(g8[:, 4:8], g8[:, 4:8], AF.Ln, bias=1.0)
                    yield
                    if i == 0:
                        mset(g8[0:112, 0:4], -1.0e4, e='dve'); mset(g8[0:112, 4:8], 0.0, e='dve')
                    pn = psq()
                    pn4 = sub(pn, A(pn)[:, 0:4]); pn8 = sub(pn, A(pn)[:, 4:8])
                    mm(pn4, tric[:], g8[:, 4:8]); mm(pn8, blk64[:], g8[:, 4:8])
                    cp(nbg[:], sub(pn, A(pn)[:, 0:8]), e='act')
                    yield
                    ttn(dbias[:], g8[:, 0:4], nbg[:, 0:4], ALU.add)
                    ttn(wgt[:], dbias[:], nbg[:, 4:8], ALU.subtract)
                    act(wgt[:], wgt[:], AF.Exp, bias=math.log(0.125))
                    yield
                    for kb in range(2):
                        p_ = psq()
                        S.op('pe', lambda E, kb=kb, p_=p_: E.transpose(out=A(p_), in_=QKf[:, 2 + kb, :], identity=ident[:]), r=[QKf, ident], w=[p_])
                        for hh in range(2):
                            h = kb * 2 + hh
                            tsc(Kw[:, h, :], sub(p_, A(p_)[:, hh * 64:(hh + 1) * 64]), wgt[:, h:h + 1], None, ALU.mult)
                    yield

                def g_mlstm_rest():
                    def g_head(h):
                        hb = (h % 2) * 64
                        hs = slice(hb, hb + 64)
                        qb = h // 2
                        L_, D_, E_, P_ = lfb[h % 2], Dm[h % 2], eB[h % 2], Pm[h % 2]
                        tsc(L_[:], ones[:], g8[:, 4 + h:5 + h], None, ALU.mult, e='pool')
                        pbr = psq(); mm(pbr, L_[:], tric[:])
                        act(D_[:], pbr, AF.Exp, bias=dbias[:, h:h + 1], scale=-1.0)
                        act(E_[:], pbr, AF.Exp, scale=-1.0)
                        yield
                        psc = psq(); mm(psc, QKf[hs, 2 + qb, :], QKf[hs, qb, :])
                        ttn(D_[:], D_[:], maskc[:], ALU.mult, e='pool')
                        ttn(P_[:], D_[:], psc, ALU.mult)
                        yield
                        ttn(Qz[h][hs, 0, 0:64], QKf[hs, qb, 0:64], E_[hs, 0:64], ALU.mult)
                        ttn(Qz[h][hs, 1, 64:128], QKf[hs, qb, 64:128], E_[hs, 64:128], ALU.mult)
                        pU = psb()
                        u0 = sub(pU, A(pU)[hs, 0:129]); u1 = sub(pU, A(pU)[hs, 256:385])
                        mm(u0, Kw[0:64, h, :], V1[0:64, h, :])
                        stt(CTb[h][hs, :], CTa[h][hs, :], E_[hs, 63:64], u0, ALU.mult, ALU.add)
                        yield
                        pO = psb(); o_ = sub(pO, A(pO)[:, 0:129])
                        mm(o_, P_[:], V1[:, h, :], start=True, stop=False)
                        mm(o_, Qz[h][hs, 0, :], CTa[h][hs, :], start=False, stop=False)
                        mm(o_, Qz[h][hs, 1, :], CTb[h][hs, :], start=False, stop=True)
                        mm(u1, Kw[64:128, h, :], V1[64:128, h, :])
                        stt(CTa[h][hs, :], CTb[h][hs, :], E_[hs, 127:128], u1, ALU.mult, ALU.add)
                        if i > 0:
                            H_ = hraw[h % 2]
                            act(sm[:, 3 * h:3 * h + 1], sub(pO, A(pO)[:, 128:129]), AF.Abs)
                            tsc(sm[:, 3 * h:3 * h + 1], sm[:, 3 * h:3 * h + 1], 1.0, None, ALU.max)
                            recip(sm[:, 3 * h:3 * h + 1], sm[:, 3 * h:3 * h + 1])
                            tsc(H_[:], sub(pO, A(pO)[:, 0:128]), sm[:, 3 * h:3 * h + 1], None, ALU.mult)
                            act(P_[:], H_[:], AF.Square, accum=sm[:, 3 * h + 1:3 * h + 2])
                            yield
                            act(sm[:, 3 * h + 2:3 * h + 3], sm[:, 3 * h + 1:3 * h + 2], AF.Ln, bias=1e-6, scale=1.0 / 128)
                            act(sm[:, 3 * h + 2:3 * h + 3], sm[:, 3 * h + 2:3 * h + 3], AF.Exp, scale=-0.5)
                            yield
                            stt(H_[:], H_[:], sm[:, 3 * h + 2:3 * h + 3], mnw_bc[:, h * 128:(h + 1) * 128], ALU.mult, ALU.mult)
                            ttn(y_b[:, h * 128:(h + 1) * 128], H_[:], sigo[:, h * 128:(h + 1) * 128], ALU.mult)
                        yield
                    yield from inter([g_head(0), g_head(1)])
                    yield from inter([g_head(2), g_head(3)])
                    if i == 0:
                        return
                    for h in range(4):
                        p_ = psq()
                        S.op('pe', lambda E, h=h, p_=p_: E.transpose(out=A(p_), in_=y_b[:, h * 128:(h + 1) * 128], identity=ident[:]), r=[y_b, ident], w=[p_])
                        cp(yT_b[:, 4 + h, :], p_, e='act')
                    yield

                def g_rwkv_prep():
                    yield from g_prep_all()
                    cp(rkv_raw[:, :, 0:1], rkv_raw[:, :, 128:129], e='pool')
                    yield

                def g_rwkv_rest():
                    yield from inter([g_alg(0), g_alg(1)])
                    yield from g_chain(0)
                    yield from g_chain(1)

                def g_post_all(i=i, yT_b=yT_b, post=g_post4()):
                    yield from post
                    dma(yT_d[i * 128:(i + 1) * 128, :], yT_b[:, :, :].rearrange("p b t -> p (b t)"))
                    yield
                for _ in inter([g_rwkv_prep(), g_mlstm_pre()] + pending):
                    pass
                pending = []
                streams = [g_rwkv_rest(), g_mlstm_rest()]
                if i + 1 < NT:
                    streams.append(g_front(i + 1))
                for _ in inter(streams, weights=[2, 1, 1][:len(streams)]):
                    pass
                if i > 0:
                    pending = [g_post_all()]
            for _ in inter(pending):
                pass

            S.barrier()
            chk('p1')
            es2.close()
            es1.close()

            with ExitStack() as es5:
                t5 = lambda name, shape, dt=F32: T(es5, name, shape, dt)
                NB1 = 4
                wout_b = t5("wout_b", [128, 8, D], BF16); wr_f = t5("wr_f", [128, 8, 36]); wffn_bc = t5("wffn_bc", [128, D])
                wst = [t5(f"wst{i}", [128, D]) for i in range(2)]
                xt1 = [t5(f"xt1_{i}", [128, D]) for i in range(NB1)]; yt1 = [t5(f"yt1_{i}", [128, 8, 128], BF16) for i in range(NB1)]
                h1s = [t5(f"h1s{i}", [128, D]) for i in range(NB1)]; big2 = [t5(f"big2_{i}", [128, D]) for i in range(NB1)]
                xn2_bs = [t5(f"xn2_b{i}", [128, D], BF16) for i in range(NB1)]; xn2T = [t5(f"xn2T{i}", [128, 8, 128]) for i in range(NB1)]
                scr = [dict(lgt=t5(f"lgt{i}", [128, 36]), rsm=t5(f"rsm{i}", [128, 32]), oh=[t5(f"oh{k}_{i}", [128, 32]) for k in range(2)],
                            cnt=t5(f"cnt{i}", [128, 32]), el=t5(f"el{i}", [128, 8]), mx8=t5(f"mx8_{i}", [128, 8]), ix8=t5(f"ix8_{i}", [128, 8], U32),
                            sm=t5(f"smb{i}", [128, 16])) for i in range(NB1)]
                carry = t5("carry", [1, 32])
                mset(carry, 0.0)
                dma(wffn_bc[:], nffn_d[0].partition_broadcast(128))
                dma(wr_f[:, :, 0:4], rgw_d[0].rearrange("(c p) e -> p c e", p=128))
                dma(wr_f[:, :, 4:36], rew_d[0].rearrange("(c p) e -> p c e", p=128))
                wout_v = wout_d[0].rearrange("(c p) f -> p c f", p=128)
                for c in range(8):
                    dma(wst[c % 2][:, :], wout_v[:, c, :])
                    cp(wout_b[:, c, :], wst[c % 2][:, :], e=('dve', 'act')[c % 2])

                def loads1b(i):
                    dma(xt1[i % NB1][:, :], x_d[(i - 1) * 128:i * 128, :])
                    dma(yt1[i % NB1][:, :, :].rearrange("p b t -> p (b t)"), yT_d[i * 128:(i + 1) * 128, :])
                def g_A(i):
                    b = i % NB1
                    xt = xt1[b]; yT_b = yt1[b]; h1 = h1s[b]; big = big2[b]; xn2_b = xn2_bs[b]; xT2 = xn2T[b]
                    Z = scr[b]; lgt = Z['lgt']; rsm = Z['rsm']; oh = Z['oh']; cnt = Z['cnt']; el = Z['el']; mx8 = Z['mx8']; ix8 = Z['ix8']; sm = Z['sm']
                    for n in range(2):
                        pm_ = psb()
                        for blk in range(8):
                            mm(pm_, yT_b[:, blk, :], wout_b[:, blk, n * 512:(n + 1) * 512], start=(blk == 0), stop=(blk == 7))
                        ttn(h1[:, n * 512:(n + 1) * 512], xt[:, n * 512:(n + 1) * 512], pm_, ALU.add)
                    dma(h1_d[i * 128:(i + 1) * 128, :], h1[:, :], q='pool')
                    yield
                    act(big[:], h1[:], AF.Square, accum=sm[:, 14:15])
                    act(sm[:, 15:16], sm[:, 14:15], AF.Sqrt, bias=1e-6, scale=1.0 / D)
                    recip(sm[:, 15:16], sm[:, 15:16])
                    stt(big[:], h1[:], sm[:, 15:16], wffn_bc[:], ALU.mult, ALU.mult)
                    cp(xn2_b[:], big[:], e='pool')
                    yield
                    for half in range(2):
                        pb = psb()
                        for j in range(4):
                            c = half * 4 + j
                            S.op('pe', lambda E, c=c, j=j, pb=pb, big=big: E.transpose(out=A(pb)[:, j * 128:(j + 1) * 128], in_=big[:, c * 128:(c + 1) * 128], identity=ident[:]),
                                 r=[big, ident], w=[pb])
                        cp(xT2[:, half * 4:half * 4 + 4, :], sub(pb, A(pb).rearrange("p (j t) -> p j t", t=128)), e='act')
                        yield
                    pl = psq(); pl36 = sub(pl, A(pl)[:, 0:36])
                    for c in range(8):
                        mm(pl36, xT2[:, c, :], wr_f[:, c, :], start=(c == 0), stop=(c == 7))
                    ttn(lgt[:], pl36, rb_bc[:], ALU.add)
                    yield
                    S.op('dve', lambda E: E.tensor_reduce(out=sm[:, 4:5], in_=lgt[:, 0:4], axis=AX.X, op=ALU.max, negate=True), r=[lgt], w=[sm])
                    act(rsm[:, 0:4], lgt[:, 0:4], AF.Exp, bias=sm[:, 4:5], accum=sm[:, 5:6])
                    recip(sm[:, 5:6], sm[:, 5:6])
                    yield
                    tsc(sm[:, 4:5], sm[:, 4:5], -1.0, None, ALU.mult)
                    tsc(rsm[:, 4:8], lgt[:, 0:4], sm[:, 4:5], None, ALU.is_equal)
                    yield
                    tsc(el[:], lgt[:, 4:12], rsm[:, 4:5], None, ALU.mult)
                    for g in range(1, 4):
                        stt(el[:], lgt[:, 4 + g * 8:12 + g * 8], rsm[:, 4 + g:5 + g], el[:], ALU.mult, ALU.add)
                    ttn(rsm[:, 8:12], rsm[:, 4:8], giota[:], ALU.mult)
                    S.op('dve', lambda E: E.tensor_reduce(out=sm[:, 6:7], in_=rsm[:, 8:12], axis=AX.X, op=ALU.add), r=[rsm], w=[sm])
                    yield
                    S.op('dve', lambda E: E.max(out=mx8[:], in_=el[:]), r=[el], w=[mx8])
                    S.op('dve', lambda E: E.max_index(out=ix8[:], in_max=mx8[:], in_values=el[:]), r=[mx8, el], w=[ix8])
                    yield
                    cp(sm[:, 8:10], ix8[:, 0:2])
                    tsc(sm[:, 8:10], sm[:, 8:10], sm[:, 6:7], None, ALU.add)
                    yield
                    ttn(sm[:, 10:11], mx8[:, 1:2], mx8[:, 0:1], ALU.subtract)
                    act(sm[:, 10:11], sm[:, 10:11], AF.Exp)
                    tsc(sm[:, 11:12], sm[:, 10:11], 1.0, None, ALU.add)
                    recip(sm[:, 11:12], sm[:, 11:12])
                    yield
                    ttn(gates_all[:, i, 0:1], sm[:, 5:6], sm[:, 11:12], ALU.mult)
                    ttn(gates_all[:, i, 1:2], gates_all[:, i, 0:1], sm[:, 10:11], ALU.mult)
                    for k in range(2):
                        tsc(oh[k][:], iota_f[:], sm[:, 8 + k:9 + k], None, ALU.is_equal)
                    ttn(cnt[:], oh[0][:], oh[1][:], ALU.add)
                    yield
                    yield

                def g_B(i):
                    b = i % NB1
                    xt = xt1[b]; yT_b = yt1[b]; h1 = h1s[b]; big = big2[b]; xn2_b = xn2_bs[b]; xT2 = xn2T[b]
                    Z = scr[b]; lgt = Z['lgt']; rsm = Z['rsm']; oh = Z['oh']; cnt = Z['cnt']; el = Z['el']; mx8 = Z['mx8']; ix8 = Z['ix8']; sm = Z['sm']
                    pp = psq(); pp32 = sub(pp, A(pp)[:, 0:32])
                    mm(pp32, msu[:], cnt[:], start=True, stop=False)
                    mm(pp32, ones[0:1, :], carry[0:1, :], start=False, stop=True)
                    for k in range(2):
                        ttn(rsm[:], oh[k][:], pp32, ALU.mult)
                        S.op('dve', lambda E, k=k: E.tensor_reduce(out=sm[:, 12 + k:13 + k], in_=rsm[:], axis=AX.X, op=ALU.add), r=[rsm], w=[sm])
                    tsc(sm[:, 12:14], sm[:, 12:14], float(CAP - 1), None, ALU.min)
                    stt(sm[:, 12:14], sm[:, 8:10], float(CAP), sm[:, 12:14], ALU.mult, ALU.add)
                    cp(slots_all[:, i, :], sm[:, 12:14])
                    yield
                    pc = psq(); pc32 = sub(pc, A(pc)[0:1, 0:32])
                    mm(pc32, ones[:, 0:1], cnt[:])
                    ttn(carry[0:1, :], carry[0:1, :], pc32, ALU.add)
                    yield
                    for k in range(2):
                        S.dma('pool', lambda E, k=k, i=i: E.indirect_dma_start(
                            out=xs_d[:, :], out_offset=bass.IndirectOffsetOnAxis(ap=slots_all[:, i, k:k + 1], axis=0),
                            in_=xn2_b[:, :], in_offset=None), r=[xn2_b, slots_all], w=['xs_scr'])
                    yield

                def g_tile(i):
                    loads1b(i)
                    yield
                    yield from g_A(i)
                    yield from g_B(i)
                active = []
                nxt_tile = 1
                rnd = 0
                while active or nxt_tile < NT:
                    if nxt_tile < NT and len(active) < NB1 and rnd % 4 == 0:
                        active.append(g_tile(nxt_tile)); nxt_tile += 1
                    for g in list(active):
                        try:
                            next(g)
                        except StopIteration:
                            active.remove(g)
                    rnd += 1
                S.barrier()
                chk('p1b')

            with ExitStack() as es3:
                t3 = lambda name, shape, dt=F32: T(es3, name, shape, dt)
                NSUB = CAP // 128
                wstg = [t3(f"ewstg{i}", [128, 2, D]) for i in range(6)]
                wgu_b = [t3(f"wgu_b{i}", [128, 8, D], BF16) for i in range(2)]
                wdn_b = [t3(f"wdn_b{i}", [128, 4, D], BF16) for i in range(2)]
                xsl = [t3(f"xsl{i}", [128, NSUB, D], BF16) for i in range(2)]
                xT = [t3(f"xT{i}", [128, 8, CAP], BF16) for i in range(2)]
                hT = t3("hT", [128, 4, CAP], BF16); gsl = t3("gsl", [128, CAP])
                ysl = [t3(f"ysl{i}", [128, D]) for i in range(2)]
                def wpiece(e, k):
                    g_ = e * 6 + k
                    s_ = wstg[g_ % 6]
                    if k < 4:
                        src = wgu_d[0, e].rearrange("(c p) f -> p c f", p=128)[:, 2 * k:2 * k + 2, :]
                        dst = wgu_b[e % 2][:, 2 * k:2 * k + 2, :]
                    else:
                        src = wdn_d[0, e].rearrange("(c p) f -> p c f", p=128)[:, 2 * (k - 4):2 * (k - 4) + 2, :]
                        dst = wdn_b[e % 2][:, 2 * (k - 4):2 * (k - 4) + 2, :]
                    return (lambda: dma(s_[:, :, :], src)), (lambda: cp(dst, s_[:, :, :], e=('dve', 'act')[g_ % 2]))

                def xload(e):
                    dma(xsl[e % 2][:, :, :], xs_d[e * CAP:(e + 1) * CAP, :].rearrange("(m p) f -> p m f", p=128), q='pool')

                for k in range(6):
                    d_, c_ = wpiece(0, k)
                    d_(); c_()
                xload(0)
                for e in range(NEXP):
                    Wg = wgu_b[e % 2]; Wd = wdn_b[e % 2]
                    X = xsl[e % 2]; XT = xT[e % 2]
                    if e + 1 < NEXP:
                        xload(e + 1)
                    steps = []

                    def st_tr(m, X=X, XT=XT):
                        for half in range(2):
                            pb = psb()
                            pbv = A(pb).bitcast(BF16)
                            for j in range(4):
                                c = half * 4 + j
                                S.op('pe', lambda E, c=c, j=j, m=m, pbv=pbv, X=X: E.transpose(out=pbv[:, j * 128:(j + 1) * 128], in_=X[:, m, c * 128:(c + 1) * 128], identity=identb[:]),
                                     r=[X, identb], w=[pb])
                            cp(XT[:, half * 4:half * 4 + 4, m * 128:(m + 1) * 128], sub(pb, pbv[:, 0:512].rearrange("p (j t) -> p j t", t=128)), e='act' if half else 'dve')

                    def st_gu(j, Wg=Wg, XT=XT):
                        pg = psb(); pu = psb()
                        for c in range(8):
                            mm(sub(pg, A(pg)[:, 0:CAP]), Wg[:, c, j * 128:(j + 1) * 128], XT[:, c, :], start=(c == 0), stop=(c == 7))
                        for c in range(8):
                            mm(sub(pu, A(pu)[:, 0:CAP]), Wg[:, c, 512 + j * 128:512 + (j + 1) * 128], XT[:, c, :], start=(c == 0), stop=(c == 7))
                        act(gsl[:, :], sub(pg, A(pg)[:, 0:CAP]), AF.Silu)
                        ttn(hT[:, j, :], gsl[:, :], sub(pu, A(pu)[:, 0:CAP]), ALU.mult)

                    def st_dn(m, Wd=Wd, e=e):
                        Y = ysl[m % 2]
                        for n in range(2):
                            py = psb()
                            for c in range(4):
                                mm(py, hT[:, c, m * 128:(m + 1) * 128], Wd[:, c, n * 512:(n + 1) * 512], start=(c == 0), stop=(c == 3))
                            cp(Y[:, n * 512:(n + 1) * 512], py, e='act' if n else 'dve')
                        dma(ys_d[e * CAP + m * 128:e * CAP + (m + 1) * 128, :], Y[:, :], q='pool')

                    for m in range(NSUB):
                        steps.append(lambda m=m: st_tr(m))
                    for j in range(4):
                        steps.append(lambda j=j: st_gu(j))
                    for m in range(NSUB):
                        steps.append(lambda m=m: st_dn(m))
                    assert len(steps) >= 6
                    casts = []
                    if e + 1 < NEXP:
                        for k in range(6):
                            d_, c_ = wpiece(e + 1, k)
                            d_()
                            casts.append(c_)
                    for si, stp in enumerate(steps):
                        stp()
                        if si < len(casts):
                            casts[si]()
                    for c_ in casts[len(steps):]:
                        c_()
                S.barrier()
                chk('p2')

            with ExitStack() as es4:
                t4 = lambda name, shape, dt=F32: T(es4, name, shape, dt)
                y0 = [t4(f"y0_{i}", [128, D]) for i in range(2)]; y1 = [t4(f"y1_{i}", [128, D]) for i in range(2)]
                hh = [t4(f"hh{i}", [128, D]) for i in range(2)]; jk = t4("jk", [128, D]); s4 = t4("s4", [128, 4])
                ob = [t4(f"ob{i}", [128, D]) for i in range(2)]
                wfin_bc = t4("wfin_bc", [128, D])
                dma(wfin_bc[:], nfin_d.partition_broadcast(128))
                def loads3(i):
                    b = i % 2
                    for k, yk in ((0, y0[b]), (1, y1[b])):
                        S.dma('pool', lambda E, k=k, i=i, yk=yk: E.indirect_dma_start(
                            out=yk[:, :], out_offset=None, in_=ys_d[:, :],
                            in_offset=bass.IndirectOffsetOnAxis(ap=slots_all[:, i, k:k + 1], axis=0)), r=['ys_scr', slots_all], w=[yk])
                    dma(hh[b][:, :], h1_d[i * 128:(i + 1) * 128, :])
                if NT > 1:
                    loads3(1)
                for i in range(1, NT):
                    b = i % 2
                    stt(hh[b][:], y0[b][:], gates_all[:, i, 0:1], hh[b][:], ALU.mult, ALU.add)
                    stt(hh[b][:], y1[b][:], gates_all[:, i, 1:2], hh[b][:], ALU.mult, ALU.add)
                    act(jk[:], hh[b][:], AF.Square, accum=s4[:, 0:1])
                    act(s4[:, 1:2], s4[:, 0:1], AF.Sqrt, bias=1e-6, scale=1.0 / D)
                    recip(s4[:, 1:2], s4[:, 1:2])
                    stt(ob[b][:], hh[b][:], s4[:, 1:2], wfin_bc[:], ALU.mult, ALU.mult)
                    if i + 1 < NT:
                        loads3(i + 1)
                    dma(out_d[(i - 1) * 128:i * 128, :], ob[b][:, :], is_out=True)
                S.finish()
        except Stop:
            S.finish()
        print("instr counts", S.total, "nsem", S.nsem)
    nc._dbg_map = dbg_map
    return nc


_NAMES = ['meta_tokens', 'norm_mix_w', 'norm_ffn_w', 'norm_final_w', 'w_in', 'w_out', 'rwkv_mu_rkv', 'rwkv_mu_wag',
          'rwkv_w0', 'rwkv_w_lora_a', 'rwkv_w_lora_b', 'rwkv_a0', 'rwkv_a_lora_a', 'rwkv_a_lora_b', 'rwkv_g_lora_a',
          'rwkv_g_lora_b', 'rwkv_k_k', 'rwkv_k_a', 'rwkv_r_k', 'rwkv_lnx_w', 'rwkv_lnx_b', 'mlstm_conv_w', 'mlstm_conv_b',
          'mlstm_gate_b', 'mlstm_norm_w', 'router_group_w', 'router_group_b', 'router_expert_w', 'router_expert_b',
          'expert_w_gate_up', 'expert_w_down']


def run(inputs, CAP=512, stop_after=None):
    x = np.asarray(inputs['x'], dtype=np.float32)
    B, L, _ = x.shape
    NT = L // 128 + 1
    nc = build(NT, CAP, stop_after=stop_after)
    shared = {n: np.ascontiguousarray(np.asarray(inputs[n], dtype=np.float32)) for n in _NAMES}
    in_maps = []
    for b in range(B):
        m = dict(shared)
        m['x'] = np.ascontiguousarray(x[b])
        in_maps.append(m)
    res = run_bass_kernel_spmd(nc, in_maps, core_ids=list(range(B)))
    if stop_after:
        d = np.asarray(res.results[0]['dbg'])
        return {k: d[0:v[2], v[0]:v[0] + v[1]] for k, v in nc._dbg_map.items()}
    return np.stack([np.asarray(r['out']).reshape(L, D) for r in res.results], axis=0).astype(np.float32)


def kernel(**inputs):
    return run(inputs, CAP=384)
```
